# Optimizing a Trainium2 kernel written in Bass

```python
import math
import jax, jax.numpy as jnp
from jax import lax
import numpy as np

D_MODEL = 1024
BATCH = 8
SEQ = 4096
DEPTH = 1

HG_HEADS = 4
HG_DK = 128
HG_DV = 128
HG_WIDTH = HG_HEADS * HG_DK
HG_CHUNK = 64
DA_HEADS = 4
DA_HEAD = 64
DA_VDIM = 2 * DA_HEAD
DA_QK_WIDTH = DA_HEADS * 2 * DA_HEAD
DA_WIDTH = DA_HEADS * DA_VDIM
Q_BLOCK = 128
ROPE_THETA = 10000.0
N_GROUPS = 4
EXPERTS_PER_GROUP = 8
N_EXPERTS = N_GROUPS * EXPERTS_PER_GROUP
TOP_K = 2
D_FF_EXPERT = 512
MOE_BLOCK = 128
EPS = 1e-6
IN_COLS = 4 * HG_WIDTH + 2 * DA_QK_WIDTH + DA_WIDTH + 2 * D_MODEL

kernel_name = "hybrid_hgrn2_diffattn_hiermoe"


def rms_norm(x, gain):
    xf = x.astype(jnp.float32)
    y = xf * lax.rsqrt(jnp.mean(xf * xf, axis=-1, keepdims=True) + EPS)
    return (y * gain.astype(jnp.float32)).astype(x.dtype)


def rope(x, pos):
    half = x.shape[-1] // 2
    inv = ROPE_THETA ** (-jnp.arange(half, dtype=jnp.float32) / half)
    ang = pos.astype(jnp.float32)[:, None] * inv[None, :]
    cos = jnp.cos(ang)[None, :, None, :]
    sin = jnp.sin(ang)[None, :, None, :]
    x1, x2 = x[..., :half].astype(jnp.float32), x[..., half:].astype(jnp.float32)
    return jnp.concatenate([x1 * cos - x2 * sin, x1 * sin + x2 * cos], axis=-1).astype(x.dtype)


def hgrn2_chunked(q, k, v, log_f):
    B, S, H, DK = q.shape
    DV = v.shape[-1]
    C = HG_CHUNK
    NC = S // C

    def to_chunks(t):
        return t.astype(jnp.float32).reshape(B, NC, C, H, t.shape[-1]).transpose(1, 0, 3, 2, 4)

    qc, kc, vc, gc = to_chunks(q), to_chunks(k), to_chunks(v), to_chunks(log_f)
    causal = jnp.tril(jnp.ones((C, C), dtype=bool))

    def step(state, inp):
        qi, ki, vi, gi = inp
        b = jnp.cumsum(gi, axis=-2)
        b_ref = b[..., C // 2 - 1:C // 2, :]
        q_rel = qi * jnp.exp(b - b_ref)
        k_rel = ki * jnp.exp(b_ref - b)
        scores = jnp.einsum('bhtk,bhsk->bhts', q_rel, k_rel)
        scores = jnp.where(causal, scores, 0.0)
        o = (jnp.einsum('bhts,bhsv->bhtv', scores, vi)
             + jnp.einsum('bhtk,bhkv->bhtv', qi * jnp.exp(b), state))
        b_end = b[..., -1:, :]
        k_end = ki * jnp.exp(b_end - b)
        new_state = (jnp.exp(b_end[..., 0, :])[..., None] * state
                     + jnp.einsum('bhsk,bhsv->bhkv', k_end, vi))
        return new_state, o

    state0 = jnp.zeros((B, H, DK, DV), jnp.float32)
    _, o = lax.scan(step, state0, (qc, kc, vc, gc))
    return o.transpose(1, 0, 3, 2, 4).reshape(B, S, H, DV)


def diff_attention(q1, q2, k1, k2, v, lam):
    B, S, H, D = q1.shape
    NQ = S // Q_BLOCK
    scale = D ** -0.5
    key_pos = jnp.arange(S)

    def block(i):
        start = i * Q_BLOCK
        qb1 = lax.dynamic_slice_in_dim(q1, start, Q_BLOCK, axis=1)
        qb2 = lax.dynamic_slice_in_dim(q2, start, Q_BLOCK, axis=1)
        qpos = start + jnp.arange(Q_BLOCK)
        mask = key_pos[None, :] <= qpos[:, None]

        def probs(qb, kk):
            s = jnp.einsum('bqhd,bshd->bhqs', qb, kk).astype(jnp.float32) * scale
            return jax.nn.softmax(jnp.where(mask, s, -jnp.inf), axis=-1)

        a = probs(qb1, k1) - lam * probs(qb2, k2)
        return jnp.einsum('bhqs,bshv->bqhv', a.astype(v.dtype), v)

    o = lax.map(block, jnp.arange(NQ))
    return o.transpose(1, 0, 2, 3, 4).reshape(B, S, H, v.shape[-1])


def hier_moe(xn, w_rg, b_rg, w_re, b_re, w1, w3, w2):
    B, S, D = xn.shape
    T = B * S
    xt = xn.reshape(T, D)
    g_logits = (xt @ w_rg).astype(jnp.float32) + b_rg.astype(jnp.float32)
    g_sel = jnp.argmax(g_logits, axis=-1)
    g_w = jnp.max(jax.nn.softmax(g_logits, axis=-1), axis=-1, keepdims=True)
    e_logits = ((xt @ w_re).astype(jnp.float32) + b_re.astype(jnp.float32)).reshape(
        T, N_GROUPS, EXPERTS_PER_GROUP)
    e_in_group = jnp.sum(e_logits * jax.nn.one_hot(g_sel, N_GROUPS, dtype=jnp.float32)[:, :, None], axis=1)
    top_v, top_i = lax.top_k(e_in_group, TOP_K)
    e_w = jax.nn.softmax(top_v, axis=-1) * g_w

    A = T * TOP_K
    expert_id = (g_sel[:, None] * EXPERTS_PER_GROUP + top_i).reshape(A).astype(jnp.int32)
    token_id = jnp.repeat(jnp.arange(T, dtype=jnp.int32), TOP_K)
    weight = e_w.reshape(A)

    order = jnp.argsort(expert_id)
    se = expert_id[order]
    counts = jnp.zeros((N_EXPERTS,), jnp.int32).at[expert_id].add(1)
    padded = (counts + MOE_BLOCK - 1) // MOE_BLOCK * MOE_BLOCK
    pad_end = jnp.cumsum(padded)
    pad_start = pad_end - padded
    start = jnp.cumsum(counts) - counts
    dest = pad_start[se] + jnp.arange(A, dtype=jnp.int32) - start[se]
    P = A + N_EXPERTS * MOE_BLOCK
    NB = P // MOE_BLOCK
    buf_tok = jnp.full((P,), T, jnp.int32).at[dest].set(token_id[order])
    buf_w = jnp.zeros((P,), jnp.float32).at[dest].set(weight[order])
    block_start = jnp.arange(NB, dtype=jnp.int32) * MOE_BLOCK
    block_expert = jnp.minimum(
        jnp.sum(pad_end[None, :] <= block_start[:, None], axis=1), N_EXPERTS - 1).astype(jnp.int32)
    x_pad = jnp.concatenate([xt, jnp.zeros((1, D), xt.dtype)], axis=0)

    def run_block(args):
        tok, w, e = args
        xb = x_pad[tok]
        hid = jax.nn.silu(xb @ w1[e]) * (xb @ w3[e])
        return (hid @ w2[e]) * w[:, None].astype(xb.dtype)

    y = lax.map(run_block, (buf_tok.reshape(NB, MOE_BLOCK), buf_w.reshape(NB, MOE_BLOCK), block_expert))
    out = jnp.zeros((T + 1, D), xt.dtype).at[buf_tok].add(y.reshape(P, D))
    return out[:T].reshape(B, S, D)


def setup_inputs(seed: int = 0) -> dict:
    key = jax.random.key(seed)
    ks = jax.random.split(key, 20)

    def nrm(k, shape, scale):
        return jax.random.normal(k, shape, jnp.float32) * scale

    def gain(k, shape):
        return 1.0 + nrm(k, shape, 0.02)

    return {
        "x": nrm(ks[0], (BATCH, SEQ, D_MODEL), 1.0),
        "norm_mix": gain(ks[1], (DEPTH, D_MODEL)),
        "w_in": nrm(ks[2], (DEPTH, D_MODEL, IN_COLS), D_MODEL ** -0.5),
        "hg_lb": nrm(ks[3], (DEPTH + 1, HG_WIDTH), 0.1),
        "hg_out_norm": gain(ks[4], (DEPTH, HG_DV)),
        "da_q_norm": gain(ks[5], (DEPTH, DA_HEAD)),
        "da_k_norm": gain(ks[6], (DEPTH, DA_HEAD)),
        "da_lambda": nrm(ks[7], (DEPTH, 4, DA_HEAD), 0.1),
        "da_out_norm": gain(ks[8], (DEPTH, DA_VDIM)),
        "w_branch_hg": nrm(ks[9], (DEPTH, HG_WIDTH, D_MODEL), HG_WIDTH ** -0.5),
        "w_branch_da": nrm(ks[10], (DEPTH, DA_WIDTH, D_MODEL), DA_WIDTH ** -0.5),
        "w_out": nrm(ks[11], (DEPTH, D_MODEL, D_MODEL), D_MODEL ** -0.5),
        "norm_moe": gain(ks[12], (DEPTH, D_MODEL)),
        "w_router_group": nrm(ks[13], (DEPTH, D_MODEL, N_GROUPS), D_MODEL ** -0.5),
        "b_router_group": nrm(ks[14], (DEPTH, N_GROUPS), 0.01),
        "w_router_expert": nrm(ks[15], (DEPTH, D_MODEL, N_EXPERTS), D_MODEL ** -0.5),
        "b_router_expert": nrm(ks[16], (DEPTH, N_EXPERTS), 0.01),
        "w1": nrm(ks[17], (DEPTH, N_EXPERTS, D_MODEL, D_FF_EXPERT), D_MODEL ** -0.5),
        "w3": nrm(ks[18], (DEPTH, N_EXPERTS, D_MODEL, D_FF_EXPERT), D_MODEL ** -0.5),
        "w2": nrm(ks[19], (DEPTH, N_EXPERTS, D_FF_EXPERT, D_MODEL), D_FF_EXPERT ** -0.5),
    }


def reference(x, norm_mix, w_in, hg_lb, hg_out_norm, da_q_norm, da_k_norm, da_lambda,
              da_out_norm, w_branch_hg, w_branch_da, w_out, norm_moe, w_router_group,
              b_router_group, w_router_expert, b_router_expert, w1, w3, w2):
    B, S, _ = x.shape
    pos = jnp.arange(S)
    widths = [HG_WIDTH] * 4 + [DA_QK_WIDTH, DA_QK_WIDTH, DA_WIDTH, D_MODEL, D_MODEL]
    splits = [int(v) for v in np.cumsum(widths)[:-1]]
    lb_all = jnp.cumsum(jax.nn.softmax(hg_lb.astype(jnp.float32), axis=0), axis=0)

    for l in range(DEPTH):
        h = rms_norm(x, norm_mix[l])
        proj = h @ w_in[l]
        hq, hf, hi, hog, dq, dk, dv, gate_hg, gate_da = jnp.split(proj, splits, axis=-1)

        lb = lb_all[l]
        f = lb + (1.0 - lb) * jax.nn.sigmoid(hf.astype(jnp.float32))
        heads_hg = lambda t: t.reshape(B, S, HG_HEADS, t.shape[-1] // HG_HEADS)
        o_hg = hgrn2_chunked(heads_hg(hq), heads_hg(1.0 - f), heads_hg(hi), heads_hg(jnp.log(f)))
        o_hg = rms_norm(o_hg, hg_out_norm[l]) * jax.nn.silu(heads_hg(hog).astype(jnp.float32))
        y_hg = o_hg.reshape(B, S, HG_WIDTH).astype(x.dtype) @ w_branch_hg[l]

        qn = rope(rms_norm(dq.reshape(B, S, 2 * DA_HEADS, DA_HEAD), da_q_norm[l]), pos)
        kn = rope(rms_norm(dk.reshape(B, S, 2 * DA_HEADS, DA_HEAD), da_k_norm[l]), pos)
        qn = qn.reshape(B, S, DA_HEADS, 2, DA_HEAD)
        kn = kn.reshape(B, S, DA_HEADS, 2, DA_HEAD)
        v = dv.reshape(B, S, DA_HEADS, DA_VDIM)
        lam_p = da_lambda[l].astype(jnp.float32)
        lam_init = 0.8 - 0.6 * math.exp(-0.3 * l)
        lam = jnp.exp(jnp.sum(lam_p[0] * lam_p[1])) - jnp.exp(jnp.sum(lam_p[2] * lam_p[3])) + lam_init
        o_da = diff_attention(qn[..., 0, :], qn[..., 1, :], kn[..., 0, :], kn[..., 1, :], v, lam)
        o_da = rms_norm(o_da, da_out_norm[l]) * (1.0 - lam_init)
        y_da = o_da.reshape(B, S, DA_WIDTH).astype(x.dtype) @ w_branch_da[l]

        mixed = jax.nn.sigmoid(gate_hg) * y_hg + jax.nn.sigmoid(gate_da) * y_da
        x = x + (mixed @ w_out[l]).astype(x.dtype)

        x = x + hier_moe(rms_norm(x, norm_moe[l]), w_router_group[l], b_router_group[l],
                         w_router_expert[l], b_router_expert[l], w1[l], w3[l], w2[l]).astype(x.dtype)
    return x
```

```python
import math
from contextlib import ExitStack

import numpy as np
import concourse.bass as bass
import concourse.mybir as mybir
from concourse.bass_utils import run_bass_kernel_spmd

F32 = mybir.dt.float32
BF16 = mybir.dt.bfloat16
I32 = mybir.dt.int32
AF = mybir.ActivationFunctionType
ALU = mybir.AluOpType
AX = mybir.AxisListType

D = 1024
IN_COLS = 5632
NE = 32
DFF = 512
EPS = 1e-6
ENGS = ("pe", "act", "dve", "pool", "sp")
SEM_LIMIT = 30000


class Tok:
    __slots__ = ("name", "writers", "rc", "rd")

    def __init__(self, name):
        self.name = name
        self.writers = []
        self.rc = {}
        self.rd = []

    def reset(self):
        self.writers = []
        self.rc = {}
        self.rd = []


class Op:
    __slots__ = ("eng", "fn", "dma", "deps", "signal", "sem", "val")


class Sched:
    def __init__(self, nc, es, n_dma=32, n_eng=5, n_sw=12):
        self.nc = nc
        self.e = {"pe": nc.tensor, "act": nc.scalar, "dve": nc.vector, "pool": nc.gpsimd, "sp": nc.sync}
        self.ops = []
        self.emitted = 0
        self.toks = []
        self.sems = {}
        for k in ("pe", "act", "dve", "pool"):
            for j in range(n_eng):
                self.sems[("e", k, j)] = es.enter_context(nc.semaphore(f"se_{k}{j}"))
        self.dsem_n = n_dma
        for j in range(n_dma):
            self.sems[("d", j)] = es.enter_context(nc.semaphore(f"sd_{j}"))
        self.eidx = {k: 0 for k in ENGS}
        self.ecount = {k: 0 for k in ENGS}
        self.n_eng = n_eng
        self.dtarget = [0] * (n_dma + n_sw)
        self.dnext = 0
        self.n_sw = n_sw
        self.swnext = 0
        for j in range(n_dma, n_dma + n_sw):
            self.sems[("d", j)] = es.enter_context(nc.semaphore(f"sw_{j}"))
        self.waited = {k: {} for k in ENGS}
        self.nwaits = 0

    def tok(self, name="t"):
        t = Tok(name)
        self.toks.append(t)
        return t

    def op(self, eng, fn, r=(), w=(), wd=(), dma=False):
        idx = len(self.ops)
        o = Op()
        o.eng, o.fn, o.dma, o.signal, o.sem, o.val = eng, fn, dma, False, None, 0
        deps = {}
        ops = self.ops

        def add(pidx, raw):
            p = ops[pidx]
            if p.eng == eng and not p.dma and not dma and eng == "pe":
                return
            deps[pidx] = True

        for t in r:
            for pw in t.writers:
                add(pw, True)
        for t in list(w) + list(wd):
            for pw in t.writers:
                add(pw, False)
            for pr in t.rc.values():
                add(pr, False)
            for pr in t.rd:
                add(pr, False)
        for t in r:
            if dma:
                t.rd.append(idx)
            else:
                t.rc[eng] = idx
        for t in w:
            t.writers = [idx]
            t.rc = {}
            t.rd = []
        for t in wd:
            t.writers.append(idx)
        o.deps = sorted(deps)
        ops.append(o)
        return idx

    def _wait(self, eng, key, val):
        if val <= 0 or key is None:
            return
        if self.waited[eng].get(key, 0) >= val:
            return
        self.e[eng].wait_ge(self.sems[key], val)
        self.waited[eng][key] = val
        self.nwaits += 1

    def flush(self):
        ops = self.ops
        for i in range(self.emitted, len(ops)):
            for d in ops[i].deps:
                ops[d].signal = True
        for i in range(self.emitted, len(ops)):
            o = ops[i]
            e = self.e[o.eng]
            for d in o.deps:
                self._wait(o.eng, ops[d].sem, ops[d].val)
            if o.dma:
                if o.eng == "pool":
                    k = self.dsem_n + self.swnext
                    self.swnext = (self.swnext + 1) % self.n_sw
                else:
                    k = self.dnext
                    self.dnext = (k + 1) % self.dsem_n
                key = ("d", k)
                self._wait(o.eng, key, self.dtarget[k])
                s = self.sems[key]
                cnt = [0]

                def inc(ins, s=s, cnt=cnt):
                    ins.then_inc(s, 16)
                    cnt[0] += 1
                    return ins

                o.fn(e, inc)
                self.dtarget[k] += 16 * cnt[0]
                o.sem, o.val = key, self.dtarget[k]
            else:
                ins = o.fn(e)
                if o.signal:
                    c = self.ecount[o.eng] + 1
                    if c > SEM_LIMIT:
                        self.eidx[o.eng] += 1
                        assert self.eidx[o.eng] < self.n_eng, "out of engine semaphores"
                        c = 1
                    self.ecount[o.eng] = c
                    o.sem, o.val = ("e", o.eng, self.eidx[o.eng]), c
                    ins.then_inc(self.sems[o.sem], 1)
            o.fn = None
        self.emitted = len(ops)

    def barrier(self):
        last = {}
        for i in range(self.emitted, len(self.ops)):
            o = self.ops[i]
            if not o.dma:
                last[o.eng] = i
        for i in last.values():
            self.ops[i].signal = True
        self.flush()
        for eng in ENGS:
            for i in last.values():
                self._wait(eng, self.ops[i].sem, self.ops[i].val)
            for k in range(self.dsem_n + self.n_sw):
                self._wait(eng, ("d", k), self.dtarget[k])
        for t in self.toks:
            t.reset()


def cap_for(T):
    return 128 * int(math.ceil((T / 16.0) * 1.5 / 128.0))


def build(T, stop_after=99, dbg=()):
    NT = T // 128
    NB = T // 512
    CAP = cap_for(T)
    CT = CAP // 128
    NSLOT = NE * CAP
    nc = bass.Bass("TRN2", target_bir_lowering=False)

    def din(name, shape, dt=F32):
        return nc.dram_tensor(name, list(shape), dt, kind="ExternalInput").ap()

    x = din("x", [T, D])
    w_in = din("w_in", [D, IN_COLS])
    gmix = din("gmix", [128, 8])
    hglb = din("hglb", [128, 8])
    hgon = din("hgon", [128, 1])
    qkn = din("qkn", [128, 2])
    lamb = din("lamb", [128, 256])
    daon = din("daon", [128, 1])
    wbh = din("wbh", [512, D])
    wbd = din("wbd", [512, D])
    w_out = din("w_out", [D, D])
    nmoe = din("nmoe", [128, D])
    wr = din("wr", [D, 36])
    br = din("br", [128, 36])
    w1 = din("w1", [NE, D, DFF])
    w3 = din("w3", [NE, D, DFF])
    w2 = din("w2", [NE, DFF, D])
    cmat = din("cmat", [128, 7, 128])
    rmask = din("rmask", [128, 512])
    ecap = din("ecap", [128, 32])
    ropec = din("ropec", [128, T])
    ropes = din("ropes", [128, T])
    out = nc.dram_tensor("out", [T, D], F32, kind="ExternalOutput").ap()
    x2_d = nc.dram_tensor("x2_d", [T, D], F32).ap()
    xn_d = nc.dram_tensor("xn_d", [T, D], BF16).ap()
    xg_d = nc.dram_tensor("xg_d", [NSLOT, D], BF16).ap()
    y_d = nc.dram_tensor("y_d", [NSLOT, D], F32).ap()
    hT_d = nc.dram_tensor("hT_d", [128, 8, T], BF16).ap()
    ohg_d = nc.dram_tensor("dbg_ohg" if "ohg" in dbg else "ohg_d", [128, 4, T], BF16, kind="ExternalOutput" if "ohg" in dbg else "Internal").ap()
    oda_d = nc.dram_tensor("dbg_oda" if "oda" in dbg else "oda_d", [128, 4, T], BF16, kind="ExternalOutput" if "oda" in dbg else "Internal").ap()
    dbg_t = {}
    if "ht" in dbg:
        dbg_t["ht"] = nc.dram_tensor("dbg_ht", [128, 8, T], F32, kind="ExternalOutput").ap()
    if "x2" in dbg:
        dbg_t["x2"] = nc.dram_tensor("dbg_x2", [T, D], F32, kind="ExternalOutput").ap()
        dbg_t["lg"] = nc.dram_tensor("dbg_lg", [128, NT, 36], F32, kind="ExternalOutput").ap()
    if "rt" in dbg:
        dbg_t["rt"] = nc.dram_tensor("dbg_rt", [128, 4, NT], F32, kind="ExternalOutput").ap()

    w_in_v = w_in.rearrange("(k p) c -> p k c", p=128)

    with ExitStack() as es:
        S = Sched(nc, es)

        def sb(name, shape, dt, stack=es):
            return stack.enter_context(nc.sbuf_tensor(name, list(shape), dt))

        ps = [es.enter_context(nc.psum_tensor(f"ps{i}", [128, 512], F32)) for i in range(8)]
        pst = [S.tok(f"ps{i}") for i in range(8)]

        def psb(i):
            return ps[i][:].bitcast(BF16)

        cm_f = sb("cm_f", [128, 7, 128], F32)
        cm_b = sb("cm_b", [128, 7, 128], BF16)
        rmask_s = sb("rmask_s", [128, 512], F32)
        gmix_s = sb("gmix_s", [128, 8], F32)
        hglb_s = sb("hglb_s", [128, 8], F32)
        lbv = sb("lbv", [128, 12], F32)
        hgon_s = sb("hgon_s", [128, 1], F32)
        qkn_s = sb("qkn_s", [128, 2], F32)
        lamb_s = sb("lamb_s", [128, 256], F32)
        lam_s = sb("lam_s", [128, 8], F32)
        daon_s = sb("daon_s", [128, 2], F32)
        dmy = sb("dmy", [128, 2], F32)
        t_c = S.tok("consts")

        def ld_consts(e, inc):
            inc(e.dma_start(out=cm_f[:], in_=cmat))
            inc(e.dma_start(out=rmask_s[:], in_=rmask))
            inc(e.dma_start(out=gmix_s[:], in_=gmix))
            inc(e.dma_start(out=hglb_s[:], in_=hglb))
            inc(e.dma_start(out=hgon_s[:], in_=hgon))
            inc(e.dma_start(out=qkn_s[:], in_=qkn))
            inc(e.dma_start(out=lamb_s[:], in_=lamb))
            inc(e.dma_start(out=daon_s[:, 0:1], in_=daon))

        S.op("sp", ld_consts, w=[t_c], dma=True)
        S.op("dve", lambda e: e.tensor_copy(out=cm_b[:], in_=cm_f[:]), r=[t_c], w=[t_c])
        S.op("dve", lambda e: e.tensor_sub(out=lbv[:, 8:12], in0=hglb_s[:, 0:4], in1=hglb_s[:, 4:8]), r=[t_c], w=[t_c])
        S.op("act", lambda e: e.activation(out=lbv[:, 8:12], in_=lbv[:, 8:12], func=AF.Exp), r=[t_c], w=[t_c])
        S.op("dve", lambda e: e.tensor_scalar_add(out=lbv[:, 8:12], in0=lbv[:, 8:12], scalar1=1.0), r=[t_c], w=[t_c])
        S.op("dve", lambda e: e.reciprocal(out=lbv[:, 0:4], in_=lbv[:, 8:12]), r=[t_c], w=[t_c])
        S.op("dve", lambda e: e.tensor_scalar_mul(out=lbv[:, 4:8], in0=lbv[:, 0:4], scalar1=-1.0), r=[t_c], w=[t_c])
        S.op("dve", lambda e: e.tensor_mul(out=lamb_s[:, 0:64], in0=lamb_s[:, 0:64], in1=lamb_s[:, 64:128]), r=[t_c], w=[t_c])
        S.op("dve", lambda e: e.tensor_mul(out=lamb_s[:, 128:192], in0=lamb_s[:, 128:192], in1=lamb_s[:, 192:256]), r=[t_c], w=[t_c])
        S.op("dve", lambda e: e.reduce_sum(out=lam_s[:, 0:1], in_=lamb_s[:, 0:64], axis=AX.X), r=[t_c], w=[t_c])
        S.op("dve", lambda e: e.reduce_sum(out=lam_s[:, 1:2], in_=lamb_s[:, 128:192], axis=AX.X), r=[t_c], w=[t_c])
        S.op("act", lambda e: e.activation(out=lam_s[:, 0:2], in_=lam_s[:, 0:2], func=AF.Exp), r=[t_c], w=[t_c])
        S.op("dve", lambda e: e.tensor_sub(out=lam_s[:, 2:3], in0=lam_s[:, 1:2], in1=lam_s[:, 0:1]), r=[t_c], w=[t_c])
        S.op("dve", lambda e: e.tensor_scalar_add(out=lam_s[:, 2:3], in0=lam_s[:, 2:3], scalar1=-0.2), r=[t_c], w=[t_c])
        S.op("dve", lambda e: e.tensor_scalar_mul(out=daon_s[:, 1:2], in0=daon_s[:, 0:1], scalar1=0.8), r=[t_c], w=[t_c])
        S.op("dve", lambda e: e.memset(lam_s[:, 4:5], EPS), w=[t_c])
        S.op("dve", lambda e: e.memset(lam_s[:, 5:6], 1.0), w=[t_c])
        S.op("dve", lambda e: e.memset(dmy[:], 0.0), w=[t_c])
        S.barrier()

        EPSB = lam_s[:, 4:5]
        ONEB = lam_s[:, 5:6]
        IDF = cm_f[:, 0, :]
        IDB = cm_b[:, 0, :]
        CMASK = cm_b[:, 1, :]
        TRI01 = cm_b[:, 2, :]
        LSTR = cm_b[:, 3, :]
        ONESB = cm_b[:, 4, :]
        BD64 = cm_b[:, 5, :]
        PERM = cm_b[:, 6, :]

        slot_i = sb("slot_i", [128, 2, NT], I32)
        wgt = sb("wgt", [128, 2, NT], F32)
        t_route = S.tok("route")
        t_x2d = [S.tok("x2d") for _ in range(NT)]
        t_xnd = [S.tok("xnd") for _ in range(NT)]
        t_xg = S.tok("xg")
        t_yd = S.tok("yd")
        t_hTd = S.tok("hTd")
        t_ohgd = [[S.tok("ohgd") for _ in range(NB)] for _ in range(4)]
        t_odad = [[S.tok("odad") for _ in range(NB)] for _ in range(4)]
        zt = sb("zt", [128, D], BF16)
        t_zt = S.tok("zt")
        S.op("dve", lambda e: e.memset(zt[:], 0.0), w=[t_zt])
        es_h = ExitStack()
        hT = sb("hT", [128, 8, T], BF16, es_h)
        t_hT = [S.tok(f"hT{i}") for i in range(NB)]

        with ExitStack() as p1:
            xt = [sb(f"p1x{i}", [128, D], F32, p1) for i in range(2)]
            xs = [sb(f"p1xs{i}", [128, D], BF16, p1) for i in range(2)]
            junk = sb("p1junk", [128, D], F32, p1)
            st = [sb(f"p1st{i}", [128, 2], F32, p1) for i in range(2)]
            t_xt = [S.tok("xt") for _ in range(2)]
            t_xs = [S.tok("xs") for _ in range(2)]
            t_st = [S.tok("st") for _ in range(2)]
            t_junk = S.tok("junk")
            for i in range(NT):
                b = i % 2
                S.op("sp", lambda e, inc, i=i, b=b: inc(e.dma_start(out=xt[b][:], in_=x[i * 128:(i + 1) * 128, :])),
                     w=[t_xt[b]], dma=True)
                S.op("act", lambda e, b=b: e.activation(out=junk[:], in_=xt[b][:], func=AF.Square), r=[t_xt[b]], w=[t_junk])
                S.op("dve", lambda e, b=b: e.reduce_sum(out=st[b][:, 0:1], in_=junk[:], axis=AX.X), r=[t_junk], w=[t_st[b]])
                S.op("act", lambda e, b=b: e.activation(out=st[b][:, 1:2], in_=st[b][:, 0:1], func=AF.Ln, scale=1.0 / D, bias=EPSB),
                     r=[t_st[b]], w=[t_st[b]])
                S.op("act", lambda e, b=b: e.activation(out=st[b][:, 1:2], in_=st[b][:, 1:2], func=AF.Exp, scale=-0.5),
                     r=[t_st[b]], w=[t_st[b]])
                S.op("dve", lambda e, b=b: e.tensor_scalar(out=xs[b][:], in0=xt[b][:], scalar1=st[b][:, 1:2], scalar2=None,
                                                           op0=ALU.mult), r=[t_st[b], t_xt[b]], w=[t_xs[b]])
                pb = 0 + (i % 2)
                for k in range(8):
                    S.op("pe", lambda e, k=k, b=b, pb=pb: e.transpose(out=psb(pb)[:, k * 128:(k + 1) * 128],
                                                                       in_=xs[b][:, k * 128:(k + 1) * 128], identity=IDB),
                         r=[t_xs[b]], w=[pst[pb]])
                S.op("dve", lambda e, i=i, pb=pb: e.tensor_tensor(
                    out=hT[:, :, i * 128:(i + 1) * 128],
                    in0=psb(pb).rearrange("p (k t) -> p k t", k=8),
                    in1=gmix_s[:].unsqueeze(2).to_broadcast([128, 8, 128]), op=ALU.mult),
                    r=[pst[pb]], w=[t_hT[i // 4]])
            S.op("sp", lambda e, inc: inc(e.dma_start(out=hT_d, in_=hT[:])), r=t_hT, w=[t_hTd], dma=True)
            for ex_ in range(NE):
                S.op("sp", lambda e, inc, ex_=ex_: inc(e.dma_start(
                    out=xg_d[ex_ * CAP:(ex_ + 1) * CAP, :].rearrange("(c p) d -> p c d", p=128),
                    in_=zt[:].unsqueeze(1).to_broadcast([128, CT, D]))), r=[t_zt], wd=[t_xg], dma=True)
            S.barrier()
        if "ht" in dbg:
            with ExitStack() as pd:
                tmp = sb("dbgtmp", [128, 8, T], F32, pd)
                tt = S.tok("dbgtmp")
                S.op("dve", lambda e: e.tensor_copy(out=tmp[:], in_=hT[:]), r=t_hT, w=[tt])
                S.op("sp", lambda e, inc: inc(e.dma_start(out=dbg_t["ht"], in_=tmp[:])), r=[tt], dma=True)
                S.barrier()
        if stop_after <= 1:
            S.barrier()
            es_h.close()
            return nc

        def mm(out_, lhsT, rhs, start, stop):
            return lambda e: e.matmul(out_, lhsT, rhs, start=start, stop=stop)

        def act_accum(e, **kw):
            e.activation(**kw)
            return e.activation(out=dmy[:, 0:1], in_=dmy[:, 1:2], func=AF.Copy)

        dump_names = []
        if "dump" in dbg:
            dbg_t["dump"] = nc.dram_tensor("dbg_dump", [24, 128, 512], F32, kind="ExternalOutput").ap()

        def dump(name, ap, toks):
            if "dump" not in dbg or len(dump_names) >= 24:
                return
            k = len(dump_names)
            dump_names.append(name)
            S.op("sp", lambda e, inc, k=k, ap=ap: inc(e.dma_start(out=dbg_t["dump"][k][:, 0:ap.shape[1]], in_=ap)), r=toks, dma=True)

        with ExitStack() as p2:
            wst = sb("p2wst", [128, 8, 512], F32, p2)
            wq = sb("p2wq", [128, 8, 512], BF16, p2)
            t_wst, t_wq = S.tok("wst"), S.tok("wq")
            state = sb("p2state", [128, 128], F32, p2)
            stbf = [sb(f"p2stbf{i}", [128, 128], BF16, p2) for i in range(2)]
            t_state = S.tok("state")
            t_stbf = [S.tok("stbf") for _ in range(2)]
            NBUF = 2
            def mk(name, dt, n=NBUF, shape=(128, 512)):
                return [sb(f"p2{name}{i}", list(shape), dt, p2) for i in range(n)], [S.tok(name) for _ in range(n)]
            v_tm, t_v = mk("v", BF16)
            q_f, t_q = mk("q", F32)
            ef, t_ef = mk("ef", F32)
            e2f, t_e2 = mk("e2", F32)
            kk, t_kk = mk("kk", F32)
            gg, t_g = mk("g", F32)
            bcs, t_b = mk("b", F32)
            bm, t_bm = mk("bm", F32)
            E1, t_E1 = mk("E1", F32)
            E2, t_E2 = mk("E2", F32)
            E3, t_E3 = mk("E3", F32)
            qrel, t_qrel = mk("qrel", BF16)
            qb, t_qb = mk("qb", BF16)
            krel, t_krel = mk("krel", BF16)
            sog, t_sog = mk("sog", F32)
            o_f, t_of = mk("of", F32)
            sqb, t_sqb = mk("sqb", BF16)
            ob2, t_ob2 = mk("ob2", BF16)
            rst, t_rst = mk("rst", F32)
            kt, t_kt = mk("kt", BF16, 2, (128, 128))
            stm, t_stm = mk("stm", BF16, 2, (128, 128))
            sidx = 0
            for h in range(4):
                def ldw(e, inc, h=h):
                    for j in range(4):
                        inc(e.dma_start(out=wst[:, :, j * 128:(j + 1) * 128],
                                        in_=w_in_v[:, :, j * 512 + h * 128: j * 512 + (h + 1) * 128]))
                S.op("sp", ldw, w=[t_wst], dma=True)
                S.op("dve", lambda e: e.tensor_copy(out=wq[:], in_=wst[:]), r=[t_wst], w=[t_wq])
                S.op("dve", lambda e: e.memset(state[:], 0.0), w=[t_state])
                S.op("dve", lambda e, sidx=sidx: e.memset(stbf[sidx % 2][:], 0.0), w=[t_stbf[sidx % 2]])
                for b in range(NB):
                    u = b % NBUF
                    tb = slice(b * 512, (b + 1) * 512)
                    for j, bank in ((0, 0), (1, 1), (3, 2)):
                        for k in range(8):
                            S.op("pe", mm(ps[bank][:, :], wq[:, k, j * 128:(j + 1) * 128], hT[:, k, tb], k == 0, k == 7),
                                 r=[t_wq, t_hT[b]], w=[pst[bank]])
                    for ti in range(4):
                        for k in range(8):
                            S.op("pe", mm(ps[3][:, ti * 128:(ti + 1) * 128], hT[:, k, b * 512 + ti * 128: b * 512 + (ti + 1) * 128],
                                          wq[:, k, 256:384], k == 0, k == 7), r=[t_wq, t_hT[b]], w=[pst[3]])
                    S.op("act", lambda e, u=u: e.activation(out=v_tm[u][:], in_=ps[3][:, :], func=AF.Copy), r=[pst[3]], w=[t_v[u]])
                    S.op("act", lambda e, u=u: e.activation(out=q_f[u][:], in_=ps[0][:, :], func=AF.Copy), r=[pst[0]], w=[t_q[u]])
                    S.op("act", lambda e, u=u: e.activation(out=ef[u][:], in_=ps[1][:, :], func=AF.Exp, scale=-1.0), r=[pst[1]], w=[t_ef[u]])
                    S.op("act", lambda e, u=u: e.activation(out=e2f[u][:], in_=ps[2][:, :], func=AF.Exp, scale=-1.0), r=[pst[2]], w=[t_e2[u]])
                    S.op("act", lambda e, u=u: e.activation(out=ef[u][:], in_=ef[u][:], func=AF.Ln, bias=ONEB), r=[t_ef[u]], w=[t_ef[u]])
                    S.op("act", lambda e, u=u: e.activation(out=ef[u][:], in_=ef[u][:], func=AF.Exp, scale=-1.0), r=[t_ef[u]], w=[t_ef[u]])
                    S.op("dve", lambda e, u=u, h=h: e.tensor_scalar(out=kk[u][:], in0=ef[u][:], scalar1=lbv[:, 4 + h:5 + h], scalar2=lbv[:, h:h + 1],
                                                                   op0=ALU.mult, op1=ALU.add), r=[t_ef[u]], w=[t_kk[u]])
                    S.op("act", lambda e, u=u: e.activation(out=gg[u][:], in_=kk[u][:], func=AF.Ln, scale=-1.0, bias=ONEB), r=[t_kk[u]], w=[t_g[u]])
                    S.op("dve", lambda e, u=u: e.tensor_tensor_scan(out=bcs[u][:], data0=rmask_s[:], data1=gg[u][:], initial=0.0,
                                                                    op0=ALU.mult, op1=ALU.add), r=[t_g[u]], w=[t_b[u]])
                    S.op("dve", lambda e, u=u: e.tensor_tensor(
                        out=bm[u][:].rearrange("p (c t) -> p c t", t=64),
                        in0=bcs[u][:].rearrange("p (c t) -> p c t", t=64),
                        in1=bcs[u][:].rearrange("p (c t) -> p c t", t=64)[:, :, 31:32].to_broadcast([128, 8, 64]),
                        op=ALU.subtract), r=[t_b[u]], w=[t_bm[u]])
                    S.op("act", lambda e, u=u: e.activation(out=E1[u][:], in_=bm[u][:], func=AF.Exp), r=[t_bm[u]], w=[t_E1[u]])
                    S.op("act", lambda e, u=u: e.activation(out=E2[u][:], in_=bm[u][:], func=AF.Exp, scale=-1.0), r=[t_bm[u]], w=[t_E2[u]])
                    S.op("act", lambda e, u=u: e.activation(out=E3[u][:], in_=bcs[u][:], func=AF.Exp), r=[t_b[u]], w=[t_E3[u]])
                    S.op("dve", lambda e, u=u: e.tensor_tensor(out=qrel[u][:], in0=q_f[u][:], in1=E1[u][:], op=ALU.mult),
                         r=[t_q[u], t_E1[u]], w=[t_qrel[u]])
                    S.op("dve", lambda e, u=u: e.tensor_tensor(out=qb[u][:], in0=q_f[u][:], in1=E3[u][:], op=ALU.mult),
                         r=[t_q[u], t_E3[u]], w=[t_qb[u]])
                    S.op("dve", lambda e, u=u: e.tensor_tensor(out=krel[u][:], in0=kk[u][:], in1=E2[u][:], op=ALU.mult),
                         r=[t_kk[u], t_E2[u]], w=[t_krel[u]])
                    S.op("act", lambda e, u=u: e.activation(out=e2f[u][:], in_=e2f[u][:], func=AF.Ln, bias=ONEB), r=[t_e2[u]], w=[t_e2[u]])
                    S.op("act", lambda e, u=u: e.activation(out=e2f[u][:], in_=e2f[u][:], func=AF.Exp, scale=-1.0), r=[t_e2[u]], w=[t_e2[u]])
                    S.op("dve", lambda e, u=u: e.tensor_tensor(out=sog[u][:], in0=ps[2][:, :], in1=e2f[u][:], op=ALU.mult),
                         r=[pst[2], t_e2[u]], w=[t_sog[u]])
                    if h == 0 and b == 0:
                        dump("q_f", q_f[u][:], [t_q[u]]); dump("ef", ef[u][:], [t_ef[u]]); dump("kk", kk[u][:], [t_kk[u]])
                        dump("gg", gg[u][:], [t_g[u]]); dump("bcs", bcs[u][:], [t_b[u]]); dump("bm", bm[u][:], [t_bm[u]])
                        dump("E1", E1[u][:], [t_E1[u]]); dump("E2", E2[u][:], [t_E2[u]]); dump("E3", E3[u][:], [t_E3[u]])
                        dump("sog", sog[u][:], [t_sog[u]])
                    for ti in range(4):
                        tsl = slice(ti * 128, (ti + 1) * 128)
                        w_ = ti % 2
                        S.op("pe", lambda e, u=u, tsl=tsl: e.transpose(out=psb(4)[:, 0:128], in_=krel[u][:, tsl], identity=IDB),
                             r=[t_krel[u]], w=[pst[4]])
                        S.op("act", lambda e, w_=w_: e.activation(out=kt[w_][:], in_=psb(4)[:, 0:128], func=AF.Copy), r=[pst[4]], w=[t_kt[w_]])
                        S.op("pe", mm(ps[5][:, 0:128], krel[u][:, tsl], qrel[u][:, tsl], True, True), r=[t_krel[u], t_qrel[u]], w=[pst[5]])
                        S.op("dve", lambda e, w_=w_: e.tensor_tensor(out=stm[w_][:], in0=ps[5][:, 0:128], in1=CMASK, op=ALU.mult),
                             r=[pst[5]], w=[t_stm[w_]])
                        S.op("pe", mm(ps[6][:, 0:128], v_tm[u][:, tsl], stm[w_][:], True, False), r=[t_v[u], t_stm[w_]], w=[pst[6]])
                        for half in range(2):
                            c = ti * 2 + half
                            csl = slice(ti * 128 + half * 64, ti * 128 + half * 64 + 64)
                            pr = slice(half * 64, half * 64 + 64)
                            cur = sidx % 2
                            S.op("pe", mm(ps[6][:, half * 64:half * 64 + 64], stbf[cur][:], qb[u][:, csl], False, half == 1),
                                 r=[t_stbf[cur], t_qb[u]], w=[pst[6]])
                            S.op("pe", mm(ps[7][:, half * 128:half * 128 + 128], kt[w_][pr, :], v_tm[u][pr, tsl], True, True),
                                 r=[t_kt[w_], t_v[u]], w=[pst[7]])
                            e5 = E3[u][:, c * 64 + 63:c * 64 + 64]
                            c1 = E1[u][:, c * 64 + 63:c * 64 + 64]
                            S.op("dve", lambda e, e5=e5: e.tensor_scalar(out=state[:], in0=state[:], scalar1=e5, scalar2=None, op0=ALU.mult),
                                 r=[t_state, t_E3[u]], w=[t_state])
                            S.op("dve", lambda e, c1=c1, half=half: e.scalar_tensor_tensor(
                                out=state[:], in0=ps[7][:, half * 128:half * 128 + 128], scalar=c1, in1=state[:], op0=ALU.mult, op1=ALU.add),
                                r=[t_state, t_E1[u], pst[7]], w=[t_state])
                            sidx += 1
                            nxt = sidx % 2
                            S.op("act", lambda e, nxt=nxt: e.activation(out=stbf[nxt][:], in_=state[:], func=AF.Copy), r=[t_state], w=[t_stbf[nxt]])
                        S.op("dve", lambda e, u=u, tsl=tsl: e.tensor_copy(out=o_f[u][:, tsl], in_=ps[6][:, 0:128]), r=[pst[6]], w=[t_of[u]])
                    if h == 0 and b == 0:
                        dump("o_f", o_f[u][:], [t_of[u]]); dump("state", state[:], [t_state])
                    S.op("act", lambda e, u=u: e.activation(out=sqb[u][:], in_=o_f[u][:], func=AF.Square), r=[t_of[u]], w=[t_sqb[u]])
                    S.op("pe", mm(ps[5][:, :], ONESB, sqb[u][:], True, True), r=[t_sqb[u]], w=[pst[5]])
                    S.op("act", lambda e, u=u: e.activation(out=rst[u][:], in_=ps[5][:, :], func=AF.Ln, scale=1.0 / 128, bias=EPSB), r=[pst[5]], w=[t_rst[u]])
                    S.op("act", lambda e, u=u: e.activation(out=rst[u][:], in_=rst[u][:], func=AF.Exp, scale=-0.5), r=[t_rst[u]], w=[t_rst[u]])
                    if h == 0 and b == 0:
                        dump("rst", rst[u][:], [t_rst[u]])
                    S.op("dve", lambda e, u=u: e.tensor_tensor(out=o_f[u][:], in0=o_f[u][:], in1=rst[u][:], op=ALU.mult), r=[t_of[u], t_rst[u]], w=[t_of[u]])
                    S.op("dve", lambda e, u=u: e.scalar_tensor_tensor(out=ob2[u][:], in0=o_f[u][:], scalar=hgon_s[:, 0:1], in1=sog[u][:],
                                                                      op0=ALU.mult, op1=ALU.mult),
                         r=[t_of[u], t_sog[u]], w=[t_ob2[u]])
                    S.op("sp", lambda e, inc, u=u, h=h, tb=tb: inc(e.dma_start(out=ohg_d[:, h, tb], in_=ob2[u][:])), r=[t_ob2[u]], w=[t_ohgd[h][b]], dma=True)
            S.barrier()
        if "ht2" in dbg:
            dbg_t["ht2"] = nc.dram_tensor("dbg_ht2", [128, 8, T], F32, kind="ExternalOutput").ap()
            with ExitStack() as pd:
                tmp = sb("dbgtmp3", [128, 8, T], F32, pd)
                tt = S.tok("dbgtmp3")
                S.op("dve", lambda e: e.tensor_copy(out=tmp[:], in_=hT[:]), r=t_hT, w=[tt])
                S.op("sp", lambda e, inc: inc(e.dma_start(out=dbg_t["ht2"], in_=tmp[:])), r=[tt], dma=True)
                S.barrier()
        if stop_after <= 2:
            S.barrier()
            es_h.close()
            return nc

        with ExitStack() as p4:
            ropec_s = sb("p4rc", [128, T], F32, p4)
            ropes_s = sb("p4rs", [128, T], F32, p4)
            t_rope = S.tok("rope")
            S.op("sp", lambda e, inc: (inc(e.dma_start(out=ropec_s[:], in_=ropec)), inc(e.dma_start(out=ropes_s[:], in_=ropes))),
                 w=[t_rope], dma=True)
            wst = sb("p4wst", [128, 8, 384], F32, p4)
            wa = sb("p4wa", [128, 8, 384], BF16, p4)
            t_wst, t_wa = S.tok("wst4"), S.tok("wa4")
            qT_s = sb("p4qT", [128, T], BF16, p4)
            kT_s = sb("p4kT", [128, T], BF16, p4)
            vt = sb("p4v", [128, NT, 128], BF16, p4)
            t_qT = [S.tok("qT") for _ in range(NB)]
            t_kT = [S.tok("kT") for _ in range(NB)]
            t_vt = [S.tok("vt") for _ in range(NB)]
            def mk4(name, dt, n=2, shape=(128, 512)):
                return [sb(f"p4{name}{i}", list(shape), dt, p4) for i in range(n)], [S.tok(name) for _ in range(n)]
            sq4, t_sq4 = mk4("sq", BF16)
            qf4, t_qf4 = mk4("qf", F32)
            rs4, t_rs4 = mk4("rs", F32)
            qn4, t_qn4 = mk4("qn", BF16)
            t14, t_t14 = mk4("t1", F32)
            t24, t_t24 = mk4("t2", F32)
            Pm, t_P = mk4("P", BF16, 4)
            r0, t_r0 = mk4("r0", F32, 1)
            r1, t_r1 = mk4("r1", F32, 1)
            o1, t_o1 = mk4("o1", F32, 1)
            o2, t_o2 = mk4("o2", F32, 1)
            ob4, t_ob4 = mk4("ob4", BF16, 2)
            cnt4 = 0
            for h in range(4):
                def ldw4(e, inc, h=h):
                    for j in range(3):
                        inc(e.dma_start(out=wst[:, :, j * 128:(j + 1) * 128],
                                        in_=w_in_v[:, :, 2048 + j * 512 + h * 128: 2048 + j * 512 + (h + 1) * 128]))
                S.op("sp", ldw4, w=[t_wst], dma=True)
                S.op("dve", lambda e: e.tensor_copy(out=wa[:], in_=wst[:]), r=[t_wst], w=[t_wa])
                for b in range(NB):
                    tb = slice(b * 512, (b + 1) * 512)
                    for j, dst, t_dst in ((0, qT_s, t_qT), (1, kT_s, t_kT)):
                        u = cnt4 % 2
                        cnt4 += 1
                        for k in range(8):
                            S.op("pe", mm(ps[j][:, :], wa[:, k, j * 128:(j + 1) * 128], hT[:, k, tb], k == 0, k == 7),
                                 r=[t_wa, t_hT[b]], w=[pst[j]])
                        S.op("act", lambda e, u=u, j=j: e.activation(out=sq4[u][:], in_=ps[j][:, :], func=AF.Square), r=[pst[j]], w=[t_sq4[u]])
                        S.op("act", lambda e, u=u, j=j: e.activation(out=qf4[u][:], in_=ps[j][:, :], func=AF.Copy), r=[pst[j]], w=[t_qf4[u]])
                        S.op("pe", mm(ps[2][:, :], BD64, sq4[u][:], True, True), r=[t_sq4[u]], w=[pst[2]])
                        S.op("act", lambda e, u=u: e.activation(out=rs4[u][:], in_=ps[2][:, :], func=AF.Ln, scale=1.0 / 64, bias=EPSB), r=[pst[2]], w=[t_rs4[u]])
                        S.op("act", lambda e, u=u: e.activation(out=rs4[u][:], in_=rs4[u][:], func=AF.Exp, scale=-0.5), r=[t_rs4[u]], w=[t_rs4[u]])
                        S.op("dve", lambda e, u=u, j=j: e.scalar_tensor_tensor(out=qn4[u][:], in0=qf4[u][:], scalar=qkn_s[:, j:j + 1], in1=rs4[u][:],
                                                                              op0=ALU.mult, op1=ALU.mult), r=[t_qf4[u], t_rs4[u]], w=[t_qn4[u]])
                        S.op("pe", mm(ps[3][:, :], PERM, qn4[u][:], True, True), r=[t_qn4[u]], w=[pst[3]])
                        S.op("dve", lambda e, u=u, tb=tb: e.tensor_tensor(out=t14[u][:], in0=qn4[u][:], in1=ropec_s[:, tb], op=ALU.mult),
                             r=[t_qn4[u], t_rope], w=[t_t14[u]])
                        S.op("dve", lambda e, u=u, tb=tb: e.tensor_tensor(out=t24[u][:], in0=ps[3][:, :], in1=ropes_s[:, tb], op=ALU.mult),
                             r=[pst[3], t_rope], w=[t_t24[u]])
                        S.op("dve", lambda e, u=u, tb=tb, dst=dst: e.tensor_tensor(out=dst[:, tb], in0=t14[u][:], in1=t24[u][:], op=ALU.add),
                             r=[t_t14[u], t_t24[u]], w=[t_dst[b]])
                    for ti in range(4):
                        for k in range(8):
                            S.op("pe", mm(ps[4][:, ti * 128:(ti + 1) * 128], hT[:, k, b * 512 + ti * 128: b * 512 + (ti + 1) * 128],
                                          wa[:, k, 256:384], k == 0, k == 7), r=[t_wa, t_hT[b]], w=[pst[4]])
                    S.op("act", lambda e, b=b: e.activation(out=vt[:, b * 4:(b + 1) * 4, :], in_=ps[4][:, :].rearrange("p (a c) -> p a c", a=4),
                                                            func=AF.Copy), r=[pst[4]], w=[t_vt[b]])
                S.barrier()
                steps = [(j, i) for j in range(NB) for i in range(4 * j + 4)]

                def qk(s):
                    j, i = steps[s]
                    r_ = i - 4 * j
                    col0 = 128 * r_ if r_ > 0 else 0
                    for m in range(2):
                        bank = m * 2 + (s % 2)
                        S.op("pe", mm(ps[bank][:, col0:512], kT_s[m * 64:(m + 1) * 64, i * 128:(i + 1) * 128],
                                      qT_s[m * 64:(m + 1) * 64, j * 512 + col0:(j + 1) * 512], True, True),
                             r=[t_kT[i // 4], t_qT[j]], w=[pst[bank]])

                qk(0)
                for s in range(len(steps)):
                    j, i = steps[s]
                    r_ = i - 4 * j
                    col0 = 128 * r_ if r_ > 0 else 0
                    last = (i == 4 * j + 3)
                    for m in range(2):
                        bank = m * 2 + (s % 2)
                        pb_ = m * 2 + (s % 2)
                        S.op("act", lambda e, bank=bank, pb_=pb_, col0=col0: e.activation(out=Pm[pb_][:, col0:512], in_=ps[bank][:, col0:512],
                                                                                           func=AF.Exp, scale=0.125),
                             r=[pst[bank]], w=[t_P[pb_]])
                    if s + 1 < len(steps):
                        qk(s + 1)
                    for m in range(2):
                        pb_ = m * 2 + (s % 2)
                        if r_ >= 0:
                            S.op("dve", lambda e, pb_=pb_, col0=col0: e.tensor_tensor(out=Pm[pb_][:, col0:col0 + 128], in0=Pm[pb_][:, col0:col0 + 128],
                                                                                       in1=TRI01, op=ALU.mult), r=[t_P[pb_]], w=[t_P[pb_]])
                        S.op("pe", mm(ps[4 + m][:, col0:512], vt[:, i, :], Pm[pb_][:, col0:512], i == 0, last), r=[t_vt[i // 4], t_P[pb_]], w=[pst[4 + m]])
                        S.op("pe", mm(ps[6 + m][:, col0:512], ONESB, Pm[pb_][:, col0:512], i == 0, last), r=[t_P[pb_]], w=[pst[6 + m]])
                    if last:
                        jb = slice(j * 512, (j + 1) * 512)
                        S.op("dve", lambda e: e.tensor_copy(out=o1[0][:], in_=ps[4][:, :]), r=[pst[4]], w=[t_o1[0]])
                        S.op("dve", lambda e: e.tensor_copy(out=o2[0][:], in_=ps[5][:, :]), r=[pst[5]], w=[t_o2[0]])
                        S.op("act", lambda e: e.activation(out=r0[0][:], in_=ps[6][:, :], func=AF.Ln), r=[pst[6]], w=[t_r0[0]])
                        S.op("act", lambda e: e.activation(out=r1[0][:], in_=ps[7][:, :], func=AF.Ln), r=[pst[7]], w=[t_r1[0]])
                        S.op("act", lambda e: e.activation(out=r0[0][:], in_=r0[0][:], func=AF.Exp, scale=-1.0), r=[t_r0[0]], w=[t_r0[0]])
                        S.op("act", lambda e: e.activation(out=r1[0][:], in_=r1[0][:], func=AF.Exp, scale=-1.0), r=[t_r1[0]], w=[t_r1[0]])
                        S.op("dve", lambda e: e.tensor_tensor(out=o1[0][:], in0=o1[0][:], in1=r0[0][:], op=ALU.mult), r=[t_o1[0], t_r0[0]], w=[t_o1[0]])
                        S.op("dve", lambda e: e.tensor_tensor(out=o2[0][:], in0=o2[0][:], in1=r1[0][:], op=ALU.mult), r=[t_o2[0], t_r1[0]], w=[t_o2[0]])
                        S.op("dve", lambda e: e.scalar_tensor_tensor(out=o1[0][:], in0=o2[0][:], scalar=lam_s[:, 2:3], in1=o1[0][:], op0=ALU.mult, op1=ALU.add),
                             r=[t_o1[0], t_o2[0]], w=[t_o1[0]])
                        S.op("act", lambda e: e.activation(out=sq4[0][:], in_=o1[0][:], func=AF.Square), r=[t_o1[0]], w=[t_sq4[0]])
                        S.op("pe", mm(ps[6][:, :], ONESB, sq4[0][:], True, True), r=[t_sq4[0]], w=[pst[6]])
                        S.op("act", lambda e: e.activation(out=rs4[0][:], in_=ps[6][:, :], func=AF.Ln, scale=1.0 / 128, bias=EPSB), r=[pst[6]], w=[t_rs4[0]])
                        S.op("act", lambda e: e.activation(out=rs4[0][:], in_=rs4[0][:], func=AF.Exp, scale=-0.5), r=[t_rs4[0]], w=[t_rs4[0]])
                        ou = j % 2
                        S.op("dve", lambda e, ou=ou: e.scalar_tensor_tensor(out=ob4[ou][:], in0=o1[0][:], scalar=daon_s[:, 1:2], in1=rs4[0][:],
                                                                            op0=ALU.mult, op1=ALU.mult), r=[t_o1[0], t_rs4[0]], w=[t_ob4[ou]])
                        S.op("sp", lambda e, inc, ou=ou, h=h, jb=jb: inc(e.dma_start(out=oda_d[:, h, jb], in_=ob4[ou][:])), r=[t_ob4[ou]], w=[t_odad[h][j]], dma=True)
                S.barrier()
        es_h.close()
        if stop_after <= 4:
            S.barrier()
            return nc

        BCREG = nc.gpsimd.to_reg(NSLOT - 1)
        logit = sb("logit", [128, NT, 36], F32)
        t_logit = S.tok("logit")
        with ExitStack() as p5:
            wbh_s = sb("p5wbh", [128, 4, D], BF16, p5)
            wbd_s = sb("p5wbd", [128, 4, D], BF16, p5)
            wg_s = sb("p5wg", [128, 8, 2048], BF16, p5)
            wo_s = sb("p5wo", [128, 8, D], BF16, p5)
            nm_s = sb("p5nm", [128, D], F32, p5)
            wr_s = sb("p5wr", [128, 8, 36], F32, p5)
            br_s = sb("p5br", [128, 36], F32, p5)
            t_w5 = S.tok("w5")
            wbh_v = wbh.rearrange("(k p) c -> p k c", p=128)
            wbd_v = wbd.rearrange("(k p) c -> p k c", p=128)
            wo_v = w_out.rearrange("(k p) c -> p k c", p=128)
            stg = [sb(f"p5stg{i}", [128, 8, 512], F32, p5) for i in range(2)]
            t_stg = [S.tok("stg") for _ in range(2)]
            t_wbh = [S.tok("wbh") for _ in range(2)]
            t_wbd = [S.tok("wbd") for _ in range(2)]
            t_wo = [S.tok("wo") for _ in range(2)]
            t_wg = [S.tok("wg") for _ in range(4)]
            sgc = [0]
            def ldcast(src_ap, dst_ap, kk_, tk):
                g = sgc[0] % 2
                sgc[0] += 1
                S.op("sp", lambda e, inc, g=g: inc(e.dma_start(out=stg[g][:, 0:kk_, :], in_=src_ap)), w=[t_stg[g]], dma=True)
                if g == 0:
                    S.op("dve", lambda e, g=g: e.tensor_copy(out=dst_ap, in_=stg[g][:, 0:kk_, :]), r=[t_stg[g]], w=[tk])
                else:
                    S.op("act", lambda e, g=g: e.activation(out=dst_ap, in_=stg[g][:, 0:kk_, :], func=AF.Copy), r=[t_stg[g]], w=[tk])
            def ld_wg(n):
                ldcast(w_in_v[:, :, 3584 + n * 512: 3584 + (n + 1) * 512], wg_s[:, :, n * 512:(n + 1) * 512], 8, t_wg[n])
            for n in range(2):
                ldcast(wbh_v[:, :, n * 512:(n + 1) * 512], wbh_s[:, :, n * 512:(n + 1) * 512], 4, t_wbh[n])
                ldcast(wbd_v[:, :, n * 512:(n + 1) * 512], wbd_s[:, :, n * 512:(n + 1) * 512], 4, t_wbd[n])
                ld_wg(n)
                ld_wg(2 + n)
            for n in range(2):
                ldcast(wo_v[:, :, n * 512:(n + 1) * 512], wo_s[:, :, n * 512:(n + 1) * 512], 8, t_wo[n])
            S.op("sp", lambda e, inc: (inc(e.dma_start(out=nm_s[:], in_=nmoe)), inc(e.dma_start(out=wr_s[:], in_=wr.rearrange("(k p) c -> p k c", p=128))),
                                       inc(e.dma_start(out=br_s[:], in_=br))), w=[t_w5], dma=True)
            hTb = [sb(f"p5hT{i}", [128, 8, 512], BF16, p5) for i in range(2)]
            ohb = [sb(f"p5oh{i}", [128, 4, 512], BF16, p5) for i in range(2)]
            odb = [sb(f"p5od{i}", [128, 4, 512], BF16, p5) for i in range(2)]
            t_hTb = [S.tok("hTb") for _ in range(2)]
            t_ohb = [S.tok("ohb") for _ in range(2)]
            t_odb = [S.tok("odb") for _ in range(2)]
            mixT = [sb(f"p5mix{i}", [128, 8, 512], BF16, p5) for i in range(2)]
            t_mix = [S.tok("mix") for _ in range(2)]
            def mk5(name, dt, n=2, shape=(128, 512)):
                return [sb(f"p5{name}{i}", list(shape), dt, p5) for i in range(n)], [S.tok(name) for _ in range(n)]
            s1b, t_s1 = mk5("s1", F32)
            s2b, t_s2 = mk5("s2", F32)
            m1b, t_m1 = mk5("m1", F32, 1)
            m2b, t_m2 = mk5("m2", F32, 1)
            xt5, t_xt5 = mk5("xt", F32, 2, (128, D))
            x2b, t_x2 = mk5("x2", F32, 1, (128, D))
            xnf, t_xnf = mk5("xnf", F32, 1, (128, D))
            xnb, t_xnb = mk5("xnb", BF16, 2, (128, D))
            xnT, t_xnT = mk5("xnT", F32, 1, (128, D))
            sq5, t_sq5 = mk5("sq", F32, 1, (128, D))
            st5, t_st5 = mk5("st", F32, 2, (128, 2))
            cc = 0
            for b in range(NB):
                tb = slice(b * 512, (b + 1) * 512)
                mb = b % 2
                S.op("sp", lambda e, inc, mb=mb, tb=tb: inc(e.dma_start(out=hTb[mb][:], in_=hT_d[:, :, tb])), r=[t_hTd], w=[t_hTb[mb]], dma=True)
                S.op("sp", lambda e, inc, mb=mb, tb=tb: inc(e.dma_start(out=ohb[mb][:], in_=ohg_d[:, :, tb])), r=[t_ohgd[k][b] for k in range(4)], w=[t_ohb[mb]], dma=True)
                S.op("sp", lambda e, inc, mb=mb, tb=tb: inc(e.dma_start(out=odb[mb][:], in_=oda_d[:, :, tb])), r=[t_odad[k][b] for k in range(4)], w=[t_odb[mb]], dma=True)
                for c in range(8):
                    u = cc % 2
                    cc += 1
                    cs = slice(c * 128, (c + 1) * 128)
                    pb0 = 4 * u
                    for k in range(4):
                        S.op("pe", mm(ps[pb0][:, :], wbh_s[:, k, cs], ohb[mb][:, k, :], k == 0, k == 3), r=[t_wbh[c // 4], t_ohb[mb]], w=[pst[pb0]])
                    for k in range(4):
                        S.op("pe", mm(ps[pb0 + 1][:, :], wbd_s[:, k, cs], odb[mb][:, k, :], k == 0, k == 3), r=[t_wbd[c // 4], t_odb[mb]], w=[pst[pb0 + 1]])
                    for k in range(8):
                        S.op("pe", mm(ps[pb0 + 2][:, :], wg_s[:, k, c * 128:(c + 1) * 128], hTb[mb][:, k, :], k == 0, k == 7), r=[t_wg[c // 4], t_hTb[mb]], w=[pst[pb0 + 2]])
                    for k in range(8):
                        S.op("pe", mm(ps[pb0 + 3][:, :], wg_s[:, k, 1024 + c * 128:1024 + (c + 1) * 128], hTb[mb][:, k, :], k == 0, k == 7),
                             r=[t_wg[2 + c // 4], t_hTb[mb]], w=[pst[pb0 + 3]])
                    S.op("act", lambda e, u=u, pb0=pb0: e.activation(out=s1b[u][:], in_=ps[pb0 + 2][:, :], func=AF.Sigmoid), r=[pst[pb0 + 2]], w=[t_s1[u]])
                    S.op("act", lambda e, u=u, pb0=pb0: e.activation(out=s2b[u][:], in_=ps[pb0 + 3][:, :], func=AF.Sigmoid), r=[pst[pb0 + 3]], w=[t_s2[u]])
                    S.op("dve", lambda e, u=u, pb0=pb0: e.tensor_tensor(out=m1b[0][:], in0=ps[pb0][:, :], in1=s1b[u][:], op=ALU.mult), r=[pst[pb0], t_s1[u]], w=[t_m1[0]])
                    S.op("dve", lambda e, u=u, pb0=pb0: e.tensor_tensor(out=m2b[0][:], in0=ps[pb0 + 1][:, :], in1=s2b[u][:], op=ALU.mult), r=[pst[pb0 + 1], t_s2[u]], w=[t_m2[0]])
                    S.op("dve", lambda e, mb=mb, c=c: e.tensor_tensor(out=mixT[mb][:, c, :], in0=m1b[0][:], in1=m2b[0][:], op=ALU.add),
                         r=[t_m1[0], t_m2[0]], wd=[t_mix[mb]])
                for ti in range(4):
                    i = b * 4 + ti
                    u = i % 2
                    if i == 0:
                        S.op("sp", lambda e, inc: inc(e.dma_start(out=xt5[0][:], in_=x[0:128, :])), w=[t_xt5[0]], dma=True)
                    if i + 1 < NT:
                        S.op("sp", lambda e, inc, i=i: inc(e.dma_start(out=xt5[(i + 1) % 2][:], in_=x[(i + 1) * 128:(i + 2) * 128, :])), w=[t_xt5[(i + 1) % 2]], dma=True)
                    for n in range(2):
                        for k in range(8):
                            S.op("pe", mm(ps[n][:, :], mixT[mb][:, k, ti * 128:(ti + 1) * 128], wo_s[:, k, n * 512:(n + 1) * 512], k == 0, k == 7),
                                 r=[t_mix[mb], t_wo[n]], w=[pst[n]])
                        S.op("dve", lambda e, u=u, n=n: e.tensor_tensor(out=x2b[0][:, n * 512:(n + 1) * 512], in0=ps[n][:, :], in1=xt5[u][:, n * 512:(n + 1) * 512],
                                                                       op=ALU.add), r=[pst[n], t_xt5[u]], wd=[t_x2[0]])
                    S.op("sp", lambda e, inc, i=i: inc(e.dma_start(out=x2_d[i * 128:(i + 1) * 128, :], in_=x2b[0][:])), r=[t_x2[0]], w=[t_x2d[i]], dma=True)
                    if "x2" in dbg:
                        S.op("sp", lambda e, inc, i=i: inc(e.dma_start(out=dbg_t["x2"][i * 128:(i + 1) * 128, :], in_=x2b[0][:])), r=[t_x2[0]], dma=True)
                    S.op("act", lambda e: e.activation(out=sq5[0][:], in_=x2b[0][:], func=AF.Square), r=[t_x2[0]], w=[t_sq5[0]])
                    S.op("dve", lambda e, u=u: e.reduce_sum(out=st5[u][:, 0:1], in_=sq5[0][:], axis=AX.X), r=[t_sq5[0]], w=[t_st5[u]])
                    S.op("act", lambda e, u=u: e.activation(out=st5[u][:, 1:2], in_=st5[u][:, 0:1], func=AF.Ln, scale=1.0 / D, bias=EPSB), r=[t_st5[u]], w=[t_st5[u]])
                    S.op("act", lambda e, u=u: e.activation(out=st5[u][:, 1:2], in_=st5[u][:, 1:2], func=AF.Exp, scale=-0.5), r=[t_st5[u]], w=[t_st5[u]])
                    S.op("dve", lambda e, u=u: e.scalar_tensor_tensor(out=xnf[0][:], in0=x2b[0][:], scalar=st5[u][:, 1:2], in1=nm_s[:], op0=ALU.mult, op1=ALU.mult),
                         r=[t_x2[0], t_st5[u], t_w5], w=[t_xnf[0]])
                    S.op("act", lambda e, u=u: e.activation(out=xnb[u][:].rearrange("p (k j) -> p k j", k=8), in_=xnf[0][:].rearrange("p (j k) -> p k j", k=8),
                                                            func=AF.Copy), r=[t_xnf[0]], w=[t_xnb[u]])
                    S.op("sp", lambda e, inc, i=i, u=u: inc(e.dma_start(out=xn_d[i * 128:(i + 1) * 128, :], in_=xnb[u][:])), r=[t_xnb[u]], w=[t_xnd[i]], dma=True)
                    for k in range(8):
                        bank = 2 + k // 4
                        S.op("pe", lambda e, k=k, bank=bank: e.transpose(out=ps[bank][:, (k % 4) * 128:(k % 4 + 1) * 128],
                                                                       in_=xnf[0][:, k * 128:(k + 1) * 128], identity=IDF),
                             r=[t_xnf[0]], w=[pst[bank]])
                    S.op("act", lambda e: e.activation(out=xnT[0][:, 0:512], in_=ps[2][:, :], func=AF.Copy), r=[pst[2]], wd=[t_xnT[0]])
                    S.op("dve", lambda e: e.tensor_copy(out=xnT[0][:, 512:1024], in_=ps[3][:, :]), r=[pst[3]], wd=[t_xnT[0]])
                    for k in range(8):
                        S.op("pe", mm(ps[2][:, 0:36], xnT[0][:, k * 128:(k + 1) * 128], wr_s[:, k, :], k == 0, k == 7), r=[t_xnT[0], t_w5], w=[pst[2]])
                    S.op("dve", lambda e, i=i: e.tensor_tensor(out=logit[:, i, :], in0=ps[2][:, 0:36], in1=br_s[:], op=ALU.add), r=[pst[2], t_w5], wd=[t_logit])
            if "x2" in dbg:
                S.op("sp", lambda e, inc: inc(e.dma_start(out=dbg_t["lg"], in_=logit[:])), r=[t_logit], dma=True)
            S.barrier()
        with ExitStack() as p5:
            ecap_s = sb("p5ecap", [128, 32], F32, p5)
            xnb = [sb(f"p5cxnb{i}", [128, D], BF16, p5) for i in range(4)]
            t_xnb = [S.tok("cxnb") for _ in range(4)]
            t_w5 = S.tok("w5b")
            S.op("sp", lambda e, inc: inc(e.dma_start(out=ecap_s[:], in_=ecap)), w=[t_w5], dma=True)
            def rb(name, shape, dt=F32):
                return sb("r_" + name, shape, dt, p5)
            t_r = S.tok("router")
            G = logit[:, :, 0:4]
            E4 = logit[:, :, 4:36].rearrange("p n (g j) -> p n g j", g=4)
            gmax = rb("gmax", [128, NT]); goh = rb("goh", [128, NT, 4]); gsh = rb("gsh", [128, NT, 4]); gsum = rb("gsum", [128, NT])
            gw = rb("gw", [128, NT]); sel = rb("sel", [128, NT, 4, 8]); eg = rb("eg", [128, NT, 8]); m1 = rb("m1", [128, NT])
            oh1 = rb("oh1", [128, NT, 8]); eg2 = rb("eg2", [128, NT, 8]); m2 = rb("m2", [128, NT]); oh2 = rb("oh2", [128, NT, 8])
            dd = rb("dd", [128, NT]); ex = rb("ex", [128, NT]); den = rb("den", [128, NT])
            A1 = rb("A1", [128, NT, 4, 8]); A2 = rb("A2", [128, NT, 4, 8]); Ab = rb("Ab", [128, NT, 32], BF16)
            rk = rb("rk", [128, NT, 32]); tmp5 = rb("tmp5", [128, NT, 32]); sl = rb("sl", [128, 2, NT])
            def R_(eng, fn):
                S.op(eng, fn, r=[t_r, t_logit], w=[t_r])
            R_("dve", lambda e: e.tensor_reduce(out=gmax[:], in_=G, axis=AX.X, op=ALU.max))
            R_("dve", lambda e: e.tensor_tensor(out=goh[:], in0=G, in1=gmax[:].unsqueeze(2).to_broadcast([128, NT, 4]), op=ALU.is_equal))
            R_("dve", lambda e: e.tensor_tensor(out=gsh[:], in0=G, in1=gmax[:].unsqueeze(2).to_broadcast([128, NT, 4]), op=ALU.subtract))
            R_("act", lambda e: e.activation(out=gsh[:], in_=gsh[:], func=AF.Exp))
            R_("dve", lambda e: e.tensor_reduce(out=gsum[:], in_=gsh[:], axis=AX.X, op=ALU.add))
            R_("dve", lambda e: e.reciprocal(out=gw[:], in_=gsum[:]))
            R_("dve", lambda e: e.tensor_tensor(out=sel[:], in0=E4, in1=goh[:].unsqueeze(3).to_broadcast([128, NT, 4, 8]), op=ALU.mult))
            R_("dve", lambda e: e.tensor_reduce(out=eg[:], in_=sel[:].rearrange("p n g j -> p n j g"), axis=AX.X, op=ALU.add))
            R_("dve", lambda e: e.tensor_reduce(out=m1[:], in_=eg[:], axis=AX.X, op=ALU.max))
            R_("dve", lambda e: e.tensor_tensor(out=oh1[:], in0=eg[:], in1=m1[:].unsqueeze(2).to_broadcast([128, NT, 8]), op=ALU.is_equal))
            R_("dve", lambda e: e.scalar_tensor_tensor(out=eg2[:], in0=oh1[:], scalar=-1e30, in1=eg[:], op0=ALU.mult, op1=ALU.add))
            R_("dve", lambda e: e.tensor_reduce(out=m2[:], in_=eg2[:], axis=AX.X, op=ALU.max))
            R_("dve", lambda e: e.tensor_tensor(out=oh2[:], in0=eg2[:], in1=m2[:].unsqueeze(2).to_broadcast([128, NT, 8]), op=ALU.is_equal))
            R_("dve", lambda e: e.tensor_sub(out=dd[:], in0=m2[:], in1=m1[:]))
            R_("act", lambda e: e.activation(out=ex[:], in_=dd[:], func=AF.Exp))
            R_("dve", lambda e: e.tensor_scalar_add(out=den[:], in0=ex[:], scalar1=1.0))
            R_("dve", lambda e: e.reciprocal(out=den[:], in_=den[:]))
            S.op("dve", lambda e: e.tensor_mul(out=wgt[:, 0, :], in0=den[:], in1=gw[:]), r=[t_r], w=[t_r, t_route])
            S.op("dve", lambda e: e.tensor_mul(out=wgt[:, 1, :], in0=wgt[:, 0, :], in1=ex[:]), r=[t_r, t_route], w=[t_r, t_route])
            R_("dve", lambda e: e.tensor_tensor(out=A1[:], in0=goh[:].unsqueeze(3).to_broadcast([128, NT, 4, 8]),
                                                in1=oh1[:].unsqueeze(2).to_broadcast([128, NT, 4, 8]), op=ALU.mult))
            R_("dve", lambda e: e.tensor_tensor(out=A2[:], in0=goh[:].unsqueeze(3).to_broadcast([128, NT, 4, 8]),
                                                in1=oh2[:].unsqueeze(2).to_broadcast([128, NT, 4, 8]), op=ALU.mult))
            R_("dve", lambda e: e.tensor_tensor(out=Ab[:], in0=A1[:].rearrange("p n g j -> p n (g j)"), in1=A2[:].rearrange("p n g j -> p n (g j)"), op=ALU.add))
            for i in range(NT):
                bank = i // 16
                oc = slice((i % 16) * 32, (i % 16) * 32 + 32)
                S.op("pe", mm(ps[bank][:, oc], LSTR, Ab[:, i, :], True, i == 0), r=[t_r], w=[pst[bank]])
                for i2 in range(i):
                    S.op("pe", mm(ps[bank][:, oc], ONESB, Ab[:, i2, :], False, i2 == i - 1), r=[t_r], w=[pst[bank]])
            nb_ = (NT + 15) // 16
            for bank in range(nb_):
                n0 = bank * 16
                n1 = min(NT, n0 + 16)
                S.op("dve", lambda e, bank=bank, n0=n0, n1=n1: e.tensor_tensor(
                    out=rk[:, n0:n1, :], in0=ps[bank][:, 0:(n1 - n0) * 32].rearrange("p (n e) -> p n e", e=32),
                    in1=ecap_s[:].unsqueeze(1).to_broadcast([128, n1 - n0, 32]), op=ALU.add), r=[pst[bank], t_r, t_w5], w=[t_r])
            for a_, Aa in ((0, A1), (1, A2)):
                R_("dve", lambda e, Aa=Aa: e.tensor_tensor(out=tmp5[:], in0=rk[:], in1=Aa[:].rearrange("p n g j -> p n (g j)"), op=ALU.mult))
                R_("dve", lambda e, a_=a_: e.tensor_reduce(out=sl[:, a_, :], in_=tmp5[:], axis=AX.X, op=ALU.add))
            S.op("dve", lambda e: e.tensor_copy(out=slot_i[:], in_=sl[:]), r=[t_r], w=[t_route])
            S.barrier()
            if "rt" in dbg:
                S.op("sp", lambda e, inc: (inc(e.dma_start(out=dbg_t["rt"][:, 0:2, :], in_=sl[:])), inc(e.dma_start(out=dbg_t["rt"][:, 2:4, :], in_=wgt[:]))),
                     r=[t_r, t_route], dma=True)
                S.barrier()
            for i in range(NT):
                u = i % 4
                S.op("sp", lambda e, inc, i=i, u=u: inc(e.dma_start(out=xnb[u][:], in_=xn_d[i * 128:(i + 1) * 128, :])), r=[t_xnd[i]], w=[t_xnb[u]], dma=True)
                for a_ in range(2):
                    S.op("pool", lambda e, inc, i=i, u=u, a_=a_: inc(e.indirect_dma_start(
                        out=xg_d[:, :], out_offset=bass.IndirectOffsetOnAxis(ap=slot_i[:, a_, i:i + 1], axis=0),
                        in_=xnb[u][:], in_offset=None, bounds_check=BCREG, oob_is_err=False)),
                        r=[t_xnb[u], t_route], wd=[t_xg], dma=True)
            S.barrier()

        with ExitStack() as p6:
            stg6 = [sb(f"p6stg{i}", [128, 8, 512], F32, p6) for i in range(3)]
            t_stg6 = [S.tok("stg6") for _ in range(3)]
            w1b = [sb(f"p6w1{i}", [128, 8, 512], BF16, p6) for i in range(2)]
            w3b = [sb(f"p6w3{i}", [128, 8, 512], BF16, p6) for i in range(2)]
            w2b = [sb(f"p6w2{i}", [128, 4, D], BF16, p6) for i in range(2)]
            t_w1 = [S.tok("w1") for _ in range(2)]
            t_w3 = [S.tok("w3") for _ in range(2)]
            t_w2 = [S.tok("w2") for _ in range(2)]
            xg_s = [sb(f"p6xg{i}", [128, CT, D], BF16, p6) for i in range(2)]
            t_xgs = [S.tok("xgs") for _ in range(2)]
            xgT = [sb(f"p6xgT{i}", [128, 8, CAP], BF16, p6) for i in range(2)]
            t_xgT = [S.tok("xgT") for _ in range(2)]
            sil = [sb(f"p6sil{i}", [128, CAP], F32, p6) for i in range(2)]
            t_sil = [S.tok("sil") for _ in range(2)]
            hid = [sb(f"p6hid{i}", [128, 4, CAP], BF16, p6) for i in range(2)]
            t_hid = [S.tok("hid") for _ in range(2)]
            ysb = [sb(f"p6y{i}", [128, D], F32, p6) for i in range(2)]
            t_ysb = [S.tok("ysb") for _ in range(2)]
            w1_v = w1.rearrange("e (p k) c -> e p k c", k=8)
            w3_v = w3.rearrange("e (p k) c -> e p k c", k=8)
            w2_v = w2.rearrange("e (p k) c -> e p k c", k=4)
            stg2v = stg6[2][:].rearrange("p k c -> p (k c)").rearrange("p (k c) -> p k c", k=4)

            def load_expert(ex_):
                u = ex_ % 2
                S.op("sp", lambda e, inc: inc(e.dma_start(out=stg6[0][:], in_=w1_v[ex_])), w=[t_stg6[0]], dma=True)
                S.op("sp", lambda e, inc: inc(e.dma_start(out=stg6[1][:], in_=w3_v[ex_])), w=[t_stg6[1]], dma=True)
                S.op("sp", lambda e, inc: inc(e.dma_start(out=stg2v, in_=w2_v[ex_])), w=[t_stg6[2]], dma=True)
                S.op("sp", lambda e, inc: inc(e.dma_start(out=xg_s[u][:], in_=xg_d[ex_ * CAP:(ex_ + 1) * CAP, :].rearrange("(c p) d -> p c d", p=128))),
                     r=[t_xg], w=[t_xgs[u]], dma=True)

            def cast_expert(ex_):
                u = ex_ % 2
                S.op("dve", lambda e: e.tensor_copy(out=w1b[u][:].rearrange("p k (kk m) -> p k kk m", kk=4),
                                                    in_=stg6[0][:].rearrange("p k (m kk) -> p k kk m", kk=4)), r=[t_stg6[0]], w=[t_w1[u]])
                S.op("act", lambda e: e.activation(out=w3b[u][:].rearrange("p k (kk m) -> p k kk m", kk=4),
                                                   in_=stg6[1][:].rearrange("p k (m kk) -> p k kk m", kk=4), func=AF.Copy), r=[t_stg6[1]], w=[t_w3[u]])
                S.op("dve", lambda e: e.tensor_copy(out=w2b[u][:, 0:2, :], in_=stg2v[:, 0:2, :]), r=[t_stg6[2]], wd=[t_w2[u]])
                S.op("act", lambda e: e.activation(out=w2b[u][:, 2:4, :], in_=stg2v[:, 2:4, :], func=AF.Copy), r=[t_stg6[2]], wd=[t_w2[u]])

            load_expert(0)
            cast_expert(0)
            yc = 0
            for ex_ in range(NE):
                u = ex_ % 2
                if ex_ + 1 < NE:
                    load_expert(ex_ + 1)
                for c in range(CT):
                    for k in range(8):
                        bank = 6 + (k // 4) % 2
                        S.op("pe", lambda e, u=u, c=c, k=k, bank=bank: e.transpose(out=psb(bank)[:, (k % 4) * 128:(k % 4 + 1) * 128],
                                                                                   in_=xg_s[u][:, c, k * 128:(k + 1) * 128], identity=IDB),
                             r=[t_xgs[u]], w=[pst[bank]])
                        if k % 4 == 3:
                            kb = k - 3
                            if (k // 4) % 2 == 0:
                                S.op("dve", lambda e, u=u, c=c, kb=kb, bank=bank: e.tensor_copy(
                                    out=xgT[u][:, kb:kb + 4, c * 128:(c + 1) * 128], in_=psb(bank)[:, 0:512].rearrange("p (k t) -> p k t", k=4)),
                                    r=[pst[bank]], wd=[t_xgT[u]])
                            else:
                                S.op("act", lambda e, u=u, c=c, kb=kb, bank=bank: e.activation(
                                    out=xgT[u][:, kb:kb + 4, c * 128:(c + 1) * 128], in_=psb(bank)[:, 0:512].rearrange("p (k t) -> p k t", k=4), func=AF.Copy),
                                    r=[pst[bank]], wd=[t_xgT[u]])
                for fc in range(4):
                    pb0 = 2 * (fc % 2)
                    fs = slice(fc * 128, (fc + 1) * 128)
                    for k in range(8):
                        S.op("pe", mm(ps[pb0][:, 0:CAP], w1b[u][:, k, fs], xgT[u][:, k, :], k == 0, k == 7), r=[t_w1[u], t_xgT[u]], w=[pst[pb0]])
                    for k in range(8):
                        S.op("pe", mm(ps[pb0 + 1][:, 0:CAP], w3b[u][:, k, fs], xgT[u][:, k, :], k == 0, k == 7), r=[t_w3[u], t_xgT[u]], w=[pst[pb0 + 1]])
                    v_ = fc % 2
                    S.op("act", lambda e, v_=v_, pb0=pb0: e.activation(out=sil[v_][:], in_=ps[pb0][:, 0:CAP], func=AF.Silu), r=[pst[pb0]], w=[t_sil[v_]])
                    S.op("dve", lambda e, v_=v_, pb0=pb0, u=u, fc=fc: e.tensor_tensor(out=hid[u][:, fc, :], in0=ps[pb0 + 1][:, 0:CAP], in1=sil[v_][:], op=ALU.mult),
                         r=[pst[pb0 + 1], t_sil[v_]], wd=[t_hid[u]])
                for c in range(CT):
                    yu = yc % 2
                    yc += 1
                    for n in range(2):
                        bank = 4 + n
                        for k in range(4):
                            S.op("pe", mm(ps[bank][:, :], hid[u][:, k, c * 128:(c + 1) * 128], w2b[u][:, k, n * 512:(n + 1) * 512], k == 0, k == 3),
                                 r=[t_hid[u], t_w2[u]], w=[pst[bank]])
                        if n == 0:
                            S.op("act", lambda e, yu=yu, bank=bank: e.activation(out=ysb[yu][:, 0:512], in_=ps[bank][:, :], func=AF.Copy), r=[pst[bank]], wd=[t_ysb[yu]])
                        else:
                            S.op("dve", lambda e, yu=yu, bank=bank: e.tensor_copy(out=ysb[yu][:, 512:1024], in_=ps[bank][:, :]), r=[pst[bank]], wd=[t_ysb[yu]])
                    r0_ = ex_ * CAP + c * 128
                    S.op("sp", lambda e, inc, yu=yu, r0_=r0_: inc(e.dma_start(out=y_d[r0_:r0_ + 128, :], in_=ysb[yu][:])), r=[t_ysb[yu]], wd=[t_yd], dma=True)
                if ex_ + 1 < NE:
                    cast_expert(ex_ + 1)
            S.barrier()

        with ExitStack() as p7:
            NB7 = 4
            x2s = [sb(f"p7x{i}", [128, D], F32, p7) for i in range(NB7)]
            ya = [sb(f"p7ya{i}", [128, D], F32, p7) for i in range(NB7)]
            yb = [sb(f"p7yb{i}", [128, D], F32, p7) for i in range(NB7)]
            t_x2s = [S.tok("x2s") for _ in range(NB7)]
            t_ya = [S.tok("ya") for _ in range(NB7)]
            t_yb = [S.tok("yb") for _ in range(NB7)]
            for i in range(NT):
                u = i % NB7
                S.op("sp", lambda e, inc, i=i, u=u: inc(e.dma_start(out=x2s[u][:], in_=x2_d[i * 128:(i + 1) * 128, :])), r=[t_x2d[i]], w=[t_x2s[u]], dma=True)
                for a_, (yy, t_yy) in enumerate(((ya, t_ya), (yb, t_yb))):
                    S.op("pool", lambda e, inc, i=i, u=u, a_=a_, yy=yy: inc(e.indirect_dma_start(
                        out=yy[u][:], out_offset=None, in_=y_d[:, :],
                        in_offset=bass.IndirectOffsetOnAxis(ap=slot_i[:, a_, i:i + 1], axis=0), bounds_check=BCREG, oob_is_err=False)),
                        r=[t_yd, t_route], w=[t_yy[u]], dma=True)
                S.op("dve", lambda e, i=i, u=u: e.scalar_tensor_tensor(out=x2s[u][:], in0=ya[u][:], scalar=wgt[:, 0, i:i + 1], in1=x2s[u][:], op0=ALU.mult, op1=ALU.add),
                     r=[t_ya[u], t_x2s[u], t_route], w=[t_x2s[u]])
                S.op("dve", lambda e, i=i, u=u: e.scalar_tensor_tensor(out=x2s[u][:], in0=yb[u][:], scalar=wgt[:, 1, i:i + 1], in1=x2s[u][:], op0=ALU.mult, op1=ALU.add),
                     r=[t_yb[u], t_x2s[u], t_route], w=[t_x2s[u]])
                S.op("sp", lambda e, inc, i=i, u=u: inc(e.dma_start(out=out[i * 128:(i + 1) * 128, :], in_=x2s[u][:])), r=[t_x2s[u]], dma=True)
            S.barrier()
        S.barrier()
    return nc


def host_consts(T):
    CAP = cap_for(T)
    p = np.arange(128)
    cm = np.zeros((128, 7, 128), np.float32)
    cm[:, 0, :] = np.eye(128, dtype=np.float32)
    cm[:, 1, :] = ((p[:, None] // 64 == p[None, :] // 64) & (p[:, None] <= p[None, :])).astype(np.float32)
    cm[:, 2, :] = (p[:, None] <= p[None, :]).astype(np.float32)
    cm[:, 3, :] = (p[:, None] < p[None, :]).astype(np.float32)
    cm[:, 4, :] = 1.0
    cm[:, 5, :] = (p[:, None] // 64 == p[None, :] // 64).astype(np.float32)
    m = p
    src = np.where((m % 64) < 32, m + 32, m - 32)
    perm = np.zeros((128, 128), np.float32)
    perm[src, m] = 1.0
    cm[:, 6, :] = perm
    rmask = np.ones((128, 512), np.float32)
    rmask[:, ::64] = 0.0
    ecap = np.broadcast_to((np.arange(32, dtype=np.float32) * CAP)[None, :], (128, 32)).copy()
    half = 32
    inv = (np.float32(10000.0) ** (-np.arange(half, dtype=np.float32) / np.float32(half))).astype(np.float32)
    ang = np.arange(T, dtype=np.float32)[:, None] * inv[None, :]
    cos = np.cos(ang).astype(np.float32).T
    sin = np.sin(ang).astype(np.float32).T
    ropec = np.concatenate([cos, cos, cos, cos], axis=0)
    ropes = np.concatenate([-sin, sin, -sin, sin], axis=0)
    return dict(cmat=cm, rmask=rmask, ecap=ecap, ropec=np.ascontiguousarray(ropec), ropes=np.ascontiguousarray(ropes))


def host_layout(inp, T):
    f = lambda a: np.ascontiguousarray(np.asarray(a, dtype=np.float32))
    d = {}
    d["w_in"] = f(inp["w_in"][0])
    d["gmix"] = f(np.asarray(inp["norm_mix"][0]).reshape(8, 128).T)
    d["hglb"] = f(np.asarray(inp["hg_lb"]).reshape(2, 4, 128).transpose(2, 0, 1).reshape(128, 8))
    d["hgon"] = f(np.asarray(inp["hg_out_norm"][0]).reshape(128, 1))
    d["qkn"] = f(np.stack([np.tile(np.asarray(inp["da_q_norm"][0]), 2), np.tile(np.asarray(inp["da_k_norm"][0]), 2)], axis=1))
    d["lamb"] = f(np.broadcast_to(np.asarray(inp["da_lambda"][0]).reshape(1, 256), (128, 256)))
    d["daon"] = f(np.asarray(inp["da_out_norm"][0]).reshape(128, 1))
    d["wbh"] = f(inp["w_branch_hg"][0])
    d["wbd"] = f(inp["w_branch_da"][0])
    d["w_out"] = f(inp["w_out"][0])
    d["nmoe"] = f(np.broadcast_to(np.asarray(inp["norm_moe"][0]).reshape(1, D), (128, D)))
    d["wr"] = f(np.concatenate([np.asarray(inp["w_router_group"][0]), np.asarray(inp["w_router_expert"][0])], axis=1))
    d["br"] = f(np.broadcast_to(np.concatenate([np.asarray(inp["b_router_group"][0]),
                                                np.asarray(inp["b_router_expert"][0])]).reshape(1, 36), (128, 36)))
    d["w1"] = f(inp["w1"][0])
    d["w3"] = f(inp["w3"][0])
    d["w2"] = f(inp["w2"][0])
    d.update(host_consts(T))
    return d


_NC_CACHE = {}


def kernel(**inputs):
    xfull = np.asarray(inputs["x"], dtype=np.float32)
    B, T, _ = xfull.shape
    shared = host_layout(inputs, T)
    if T not in _NC_CACHE:
        _NC_CACHE[T] = build(T)
    nc = _NC_CACHE[T]
    in_maps = []
    for c in range(B):
        m = dict(shared)
        m["x"] = np.ascontiguousarray(xfull[c])
        in_maps.append(m)
    res = run_bass_kernel_spmd(nc, in_maps, core_ids=list(range(B)))
    return np.stack([np.asarray(r["out"], dtype=np.float32) for r in res.results], axis=0)
```

```python
import math
from contextlib import ExitStack

import numpy as np
import concourse.bass as bass
import concourse.mybir as mybir
from concourse.bass_utils import run_bass_kernel_spmd

F32 = mybir.dt.float32
BF16 = mybir.dt.bfloat16
I32 = mybir.dt.int32
AF = mybir.ActivationFunctionType
ALU = mybir.AluOpType
AX = mybir.AxisListType

D = 1024
IN_COLS = 5632
NE = 32
DFF = 512
EPS = 1e-6
ENGS = ("pe", "act", "dve", "pool", "sp")
SEM_LIMIT = 30000


class Tok:
    __slots__ = ("name", "writers", "rc", "rd")

    def __init__(self, name):
        self.name = name
        self.writers = []
        self.rc = {}
        self.rd = []

    def reset(self):
        self.writers = []
        self.rc = {}
        self.rd = []


class Op:
    __slots__ = ("eng", "fn", "dma", "deps", "signal", "sem", "val")


class Sched:
    def __init__(self, nc, es, n_dma=32, n_eng=5, n_sw=12):
        self.nc = nc
        self.e = {"pe": nc.tensor, "act": nc.scalar, "dve": nc.vector, "pool": nc.gpsimd, "sp": nc.sync}
        self.ops = []
        self.emitted = 0
        self.toks = []
        self.sems = {}
        for k in ("pe", "act", "dve", "pool"):
            for j in range(n_eng):
                self.sems[("e", k, j)] = es.enter_context(nc.semaphore(f"se_{k}{j}"))
        self.dsem_n = n_dma
        for j in range(n_dma):
            self.sems[("d", j)] = es.enter_context(nc.semaphore(f"sd_{j}"))
        self.eidx = {k: 0 for k in ENGS}
        self.ecount = {k: 0 for k in ENGS}
        self.n_eng = n_eng
        self.dtarget = [0] * (n_dma + n_sw)
        self.dnext = 0
        self.n_sw = n_sw
        self.swnext = 0
        for j in range(n_dma, n_dma + n_sw):
            self.sems[("d", j)] = es.enter_context(nc.semaphore(f"sw_{j}"))
        self.waited = {k: {} for k in ENGS}
        self.nwaits = 0

    def tok(self, name="t"):
        t = Tok(name)
        self.toks.append(t)
        return t

    def op(self, eng, fn, r=(), w=(), wd=(), dma=False):
        idx = len(self.ops)
        o = Op()
        o.eng, o.fn, o.dma, o.signal, o.sem, o.val = eng, fn, dma, False, None, 0
        deps = {}
        ops = self.ops

        def add(pidx, raw):
            p = ops[pidx]
            if p.eng == eng and not p.dma and not dma and eng == "pe":
                return
            deps[pidx] = True

        for t in r:
            for pw in t.writers:
                add(pw, True)
        for t in list(w) + list(wd):
            for pw in t.writers:
                add(pw, False)
            for pr in t.rc.values():
                add(pr, False)
            for pr in t.rd:
                add(pr, False)
        for t in r:
            if dma:
                t.rd.append(idx)
            else:
                t.rc[eng] = idx
        for t in w:
            t.writers = [idx]
            t.rc = {}
            t.rd = []
        for t in wd:
            t.writers.append(idx)
        o.deps = sorted(deps)
        ops.append(o)
        return idx

    def _wait(self, eng, key, val):
        if val <= 0 or key is None:
            return
        if self.waited[eng].get(key, 0) >= val:
            return
        self.e[eng].wait_ge(self.sems[key], val)
        self.waited[eng][key] = val
        self.nwaits += 1

    def flush(self):
        ops = self.ops
        for i in range(self.emitted, len(ops)):
            for d in ops[i].deps:
                ops[d].signal = True
        for i in range(self.emitted, len(ops)):
            o = ops[i]
            e = self.e[o.eng]
            for d in o.deps:
                self._wait(o.eng, ops[d].sem, ops[d].val)
            if o.dma:
                if o.eng == "pool":
                    k = self.dsem_n + self.swnext
                    self.swnext = (self.swnext + 1) % self.n_sw
                else:
                    k = self.dnext
                    self.dnext = (k + 1) % self.dsem_n
                key = ("d", k)
                self._wait(o.eng, key, self.dtarget[k])
                s = self.sems[key]
                cnt = [0]

                def inc(ins, s=s, cnt=cnt):
                    ins.then_inc(s, 16)
                    cnt[0] += 1
                    return ins

                o.fn(e, inc)
                self.dtarget[k] += 16 * cnt[0]
                o.sem, o.val = key, self.dtarget[k]
            else:
                ins = o.fn(e)
                if o.signal:
                    c = self.ecount[o.eng] + 1
                    if c > SEM_LIMIT:
                        self.eidx[o.eng] += 1
                        assert self.eidx[o.eng] < self.n_eng, "out of engine semaphores"
                        c = 1
                    self.ecount[o.eng] = c
                    o.sem, o.val = ("e", o.eng, self.eidx[o.eng]), c
                    ins.then_inc(self.sems[o.sem], 1)
            o.fn = None
        self.emitted = len(ops)

    def barrier(self):
        last = {}
        for i in range(self.emitted, len(self.ops)):
            o = self.ops[i]
            if not o.dma:
                last[o.eng] = i
        for i in last.values():
            self.ops[i].signal = True
        self.flush()
        for eng in ENGS:
            for i in last.values():
                self._wait(eng, self.ops[i].sem, self.ops[i].val)
            for k in range(self.dsem_n + self.n_sw):
                self._wait(eng, ("d", k), self.dtarget[k])
        for t in self.toks:
            t.reset()


def cap_for(T):
    return 128 * int(math.ceil((T / 16.0) * 1.5 / 128.0))


def build(T, stop_after=99, dbg=()):
    NT = T // 128
    NB = T // 512
    CAP = cap_for(T)
    CT = CAP // 128
    NSLOT = NE * CAP
    nc = bass.Bass("TRN2", target_bir_lowering=False)

    def din(name, shape, dt=F32):
        return nc.dram_tensor(name, list(shape), dt, kind="ExternalInput").ap()

    x = din("x", [T, D])
    w_in = din("w_in", [D, IN_COLS])
    gmix = din("gmix", [128, 8])
    hglb = din("hglb", [128, 8])
    hgon = din("hgon", [128, 1])
    qkn = din("qkn", [128, 2])
    lamb = din("lamb", [128, 256])
    daon = din("daon", [128, 1])
    wbh = din("wbh", [512, D])
    wbd = din("wbd", [512, D])
    w_out = din("w_out", [D, D])
    nmoe = din("nmoe", [128, D])
    wr = din("wr", [D, 36])
    br = din("br", [128, 36])
    w1 = din("w1", [NE, D, DFF])
    w3 = din("w3", [NE, D, DFF])
    w2 = din("w2", [NE, DFF, D])
    cmat = din("cmat", [128, 7, 128])
    rmask = din("rmask", [128, 512])
    ecap = din("ecap", [128, 32])
    ropec = din("ropec", [128, T])
    ropes = din("ropes", [128, T])
    out = nc.dram_tensor("out", [T, D], F32, kind="ExternalOutput").ap()
    x2_d = nc.dram_tensor("x2_d", [T, D], F32).ap()
    xn_d = nc.dram_tensor("xn_d", [T, D], BF16).ap()
    xg_d = nc.dram_tensor("xg_d", [NSLOT, D], BF16).ap()
    y_d = nc.dram_tensor("y_d", [NSLOT, D], F32).ap()
    hT_d = nc.dram_tensor("hT_d", [128, 8, T], BF16).ap()
    ohg_d = nc.dram_tensor("dbg_ohg" if "ohg" in dbg else "ohg_d", [128, 4, T], BF16, kind="ExternalOutput" if "ohg" in dbg else "Internal").ap()
    oda_d = nc.dram_tensor("dbg_oda" if "oda" in dbg else "oda_d", [128, 4, T], BF16, kind="ExternalOutput" if "oda" in dbg else "Internal").ap()
    dbg_t = {}
    if "ht" in dbg:
        dbg_t["ht"] = nc.dram_tensor("dbg_ht", [128, 8, T], F32, kind="ExternalOutput").ap()
    if "x2" in dbg:
        dbg_t["x2"] = nc.dram_tensor("dbg_x2", [T, D], F32, kind="ExternalOutput").ap()
        dbg_t["lg"] = nc.dram_tensor("dbg_lg", [128, NT, 36], F32, kind="ExternalOutput").ap()
    if "rt" in dbg:
        dbg_t["rt"] = nc.dram_tensor("dbg_rt", [128, 4, NT], F32, kind="ExternalOutput").ap()

    w_in_v = w_in.rearrange("(k p) c -> p k c", p=128)

    with ExitStack() as es:
        S = Sched(nc, es)

        def sb(name, shape, dt, stack=es):
            return stack.enter_context(nc.sbuf_tensor(name, list(shape), dt))

        ps = [es.enter_context(nc.psum_tensor(f"ps{i}", [128, 512], F32)) for i in range(8)]
        pst = [S.tok(f"ps{i}") for i in range(8)]

        def psb(i):
            return ps[i][:].bitcast(BF16)

        cm_f = sb("cm_f", [128, 7, 128], F32)
        cm_b = sb("cm_b", [128, 7, 128], BF16)
        rmask_s = sb("rmask_s", [128, 512], F32)
        gmix_s = sb("gmix_s", [128, 8], F32)
        hglb_s = sb("hglb_s", [128, 8], F32)
        lbv = sb("lbv", [128, 12], F32)
        hgon_s = sb("hgon_s", [128, 1], F32)
        qkn_s = sb("qkn_s", [128, 2], F32)
        lamb_s = sb("lamb_s", [128, 256], F32)
        lam_s = sb("lam_s", [128, 8], F32)
        daon_s = sb("daon_s", [128, 2], F32)
        dmy = sb("dmy", [128, 2], F32)
        t_c = S.tok("consts")

        def ld_consts(e, inc):
            inc(e.dma_start(out=cm_f[:], in_=cmat))
            inc(e.dma_start(out=rmask_s[:], in_=rmask))
            inc(e.dma_start(out=gmix_s[:], in_=gmix))
            inc(e.dma_start(out=hglb_s[:], in_=hglb))
            inc(e.dma_start(out=hgon_s[:], in_=hgon))
            inc(e.dma_start(out=qkn_s[:], in_=qkn))
            inc(e.dma_start(out=lamb_s[:], in_=lamb))
            inc(e.dma_start(out=daon_s[:, 0:1], in_=daon))

        S.op("sp", ld_consts, w=[t_c], dma=True)
        S.op("dve", lambda e: e.tensor_copy(out=cm_b[:], in_=cm_f[:]), r=[t_c], w=[t_c])
        S.op("dve", lambda e: e.tensor_sub(out=lbv[:, 8:12], in0=hglb_s[:, 0:4], in1=hglb_s[:, 4:8]), r=[t_c], w=[t_c])
        S.op("act", lambda e: e.activation(out=lbv[:, 8:12], in_=lbv[:, 8:12], func=AF.Exp), r=[t_c], w=[t_c])
        S.op("dve", lambda e: e.tensor_scalar_add(out=lbv[:, 8:12], in0=lbv[:, 8:12], scalar1=1.0), r=[t_c], w=[t_c])
        S.op("dve", lambda e: e.reciprocal(out=lbv[:, 0:4], in_=lbv[:, 8:12]), r=[t_c], w=[t_c])
        S.op("dve", lambda e: e.tensor_scalar_mul(out=lbv[:, 4:8], in0=lbv[:, 0:4], scalar1=-1.0), r=[t_c], w=[t_c])
        S.op("dve", lambda e: e.tensor_mul(out=lamb_s[:, 0:64], in0=lamb_s[:, 0:64], in1=lamb_s[:, 64:128]), r=[t_c], w=[t_c])
        S.op("dve", lambda e: e.tensor_mul(out=lamb_s[:, 128:192], in0=lamb_s[:, 128:192], in1=lamb_s[:, 192:256]), r=[t_c], w=[t_c])
        S.op("dve", lambda e: e.reduce_sum(out=lam_s[:, 0:1], in_=lamb_s[:, 0:64], axis=AX.X), r=[t_c], w=[t_c])
        S.op("dve", lambda e: e.reduce_sum(out=lam_s[:, 1:2], in_=lamb_s[:, 128:192], axis=AX.X), r=[t_c], w=[t_c])
        S.op("act", lambda e: e.activation(out=lam_s[:, 0:2], in_=lam_s[:, 0:2], func=AF.Exp), r=[t_c], w=[t_c])
        S.op("dve", lambda e: e.tensor_sub(out=lam_s[:, 2:3], in0=lam_s[:, 1:2], in1=lam_s[:, 0:1]), r=[t_c], w=[t_c])
        S.op("dve", lambda e: e.tensor_scalar_add(out=lam_s[:, 2:3], in0=lam_s[:, 2:3], scalar1=-0.2), r=[t_c], w=[t_c])
        S.op("dve", lambda e: e.tensor_scalar_mul(out=daon_s[:, 1:2], in0=daon_s[:, 0:1], scalar1=0.8), r=[t_c], w=[t_c])
        S.op("dve", lambda e: e.memset(lam_s[:, 4:5], EPS), w=[t_c])
        S.op("dve", lambda e: e.memset(lam_s[:, 5:6], 1.0), w=[t_c])
        S.op("dve", lambda e: e.memset(dmy[:], 0.0), w=[t_c])
        S.barrier()

        EPSB = lam_s[:, 4:5]
        ONEB = lam_s[:, 5:6]
        IDF = cm_f[:, 0, :]
        IDB = cm_b[:, 0, :]
        CMASK = cm_b[:, 1, :]
        TRI01 = cm_b[:, 2, :]
        LSTR = cm_b[:, 3, :]
        ONESB = cm_b[:, 4, :]
        BD64 = cm_b[:, 5, :]
        PERM = cm_b[:, 6, :]

        slot_i = sb("slot_i", [128, 2, NT], I32)
        wgt = sb("wgt", [128, 2, NT], F32)
        t_route = S.tok("route")
        t_x2d = [S.tok("x2d") for _ in range(NT)]
        t_xnd = [S.tok("xnd") for _ in range(NT)]
        t_xg = S.tok("xg")
        t_yd = S.tok("yd")
        t_hTd = S.tok("hTd")
        t_ohgd = [[S.tok("ohgd") for _ in range(NB)] for _ in range(4)]
        t_odad = [[S.tok("odad") for _ in range(NB)] for _ in range(4)]
        zt = sb("zt", [128, D], BF16)
        t_zt = S.tok("zt")
        S.op("dve", lambda e: e.memset(zt[:], 0.0), w=[t_zt])
        es_h = ExitStack()
        hT = sb("hT", [128, 8, T], BF16, es_h)
        t_hT = [S.tok(f"hT{i}") for i in range(NB)]

        with ExitStack() as p1:
            xt = [sb(f"p1x{i}", [128, D], F32, p1) for i in range(2)]
            xs = [sb(f"p1xs{i}", [128, D], BF16, p1) for i in range(2)]
            junk = sb("p1junk", [128, D], F32, p1)
            st = [sb(f"p1st{i}", [128, 2], F32, p1) for i in range(2)]
            t_xt = [S.tok("xt") for _ in range(2)]
            t_xs = [S.tok("xs") for _ in range(2)]
            t_st = [S.tok("st") for _ in range(2)]
            t_junk = S.tok("junk")
            for i in range(NT):
                b = i % 2
                S.op("sp", lambda e, inc, i=i, b=b: inc(e.dma_start(out=xt[b][:], in_=x[i * 128:(i + 1) * 128, :])),
                     w=[t_xt[b]], dma=True)
                S.op("act", lambda e, b=b: e.activation(out=junk[:], in_=xt[b][:], func=AF.Square), r=[t_xt[b]], w=[t_junk])
                S.op("dve", lambda e, b=b: e.reduce_sum(out=st[b][:, 0:1], in_=junk[:], axis=AX.X), r=[t_junk], w=[t_st[b]])
                S.op("act", lambda e, b=b: e.activation(out=st[b][:, 1:2], in_=st[b][:, 0:1], func=AF.Ln, scale=1.0 / D, bias=EPSB),
                     r=[t_st[b]], w=[t_st[b]])
                S.op("act", lambda e, b=b: e.activation(out=st[b][:, 1:2], in_=st[b][:, 1:2], func=AF.Exp, scale=-0.5),
                     r=[t_st[b]], w=[t_st[b]])
                S.op("dve", lambda e, b=b: e.tensor_scalar(out=xs[b][:], in0=xt[b][:], scalar1=st[b][:, 1:2], scalar2=None,
                                                           op0=ALU.mult), r=[t_st[b], t_xt[b]], w=[t_xs[b]])
                pb = 0 + (i % 2)
                for k in range(8):
                    S.op("pe", lambda e, k=k, b=b, pb=pb: e.transpose(out=psb(pb)[:, k * 128:(k + 1) * 128],
                                                                       in_=xs[b][:, k * 128:(k + 1) * 128], identity=IDB),
                         r=[t_xs[b]], w=[pst[pb]])
                S.op("dve", lambda e, i=i, pb=pb: e.tensor_tensor(
                    out=hT[:, :, i * 128:(i + 1) * 128],
                    in0=psb(pb).rearrange("p (k t) -> p k t", k=8),
                    in1=gmix_s[:].unsqueeze(2).to_broadcast([128, 8, 128]), op=ALU.mult),
                    r=[pst[pb]], w=[t_hT[i // 4]])
            S.op("sp", lambda e, inc: inc(e.dma_start(out=hT_d, in_=hT[:])), r=t_hT, w=[t_hTd], dma=True)
            S.barrier()
        if "ht" in dbg:
            with ExitStack() as pd:
                tmp = sb("dbgtmp", [128, 8, T], F32, pd)
                tt = S.tok("dbgtmp")
                S.op("dve", lambda e: e.tensor_copy(out=tmp[:], in_=hT[:]), r=t_hT, w=[tt])
                S.op("sp", lambda e, inc: inc(e.dma_start(out=dbg_t["ht"], in_=tmp[:])), r=[tt], dma=True)
                S.barrier()
        if stop_after <= 1:
            S.barrier()
            es_h.close()
            return nc

        def mm(out_, lhsT, rhs, start, stop):
            return lambda e: e.matmul(out_, lhsT, rhs, start=start, stop=stop)

        def act_accum(e, **kw):
            e.activation(**kw)
            return e.activation(out=dmy[:, 0:1], in_=dmy[:, 1:2], func=AF.Copy)

        dump_names = []
        if "dump" in dbg:
            dbg_t["dump"] = nc.dram_tensor("dbg_dump", [24, 128, 512], F32, kind="ExternalOutput").ap()

        def dump(name, ap, toks):
            if "dump" not in dbg or len(dump_names) >= 24:
                return
            k = len(dump_names)
            dump_names.append(name)
            S.op("sp", lambda e, inc, k=k, ap=ap: inc(e.dma_start(out=dbg_t["dump"][k][:, 0:ap.shape[1]], in_=ap)), r=toks, dma=True)

        with ExitStack() as p2:
            wst = sb("p2wst", [128, 8, 512], F32, p2)
            t_wst = S.tok("wst")
            wq = [sb(f"p2wq{i}", [128, 8, 512], BF16, p2) for i in range(2)]
            t_wq = [S.tok("wq") for _ in range(2)]
            state = [sb(f"p2state{i}", [128, 128], F32, p2) for i in range(2)]
            t_state = [S.tok("state") for _ in range(2)]
            stbf = [[sb(f"p2stbf{i}_{j}", [128, 128], BF16, p2) for j in range(2)] for i in range(2)]
            t_stbf = [[S.tok("stbf") for _ in range(2)] for _ in range(2)]
            def mk(name, dt, n=2, shape=(128, 512)):
                return [sb(f"p2{name}{i}", list(shape), dt, p2) for i in range(n)], [S.tok(name) for _ in range(n)]
            v_tm, t_v = mk("v", BF16)
            q_f, t_q = mk("q", F32)
            ef, t_ef = mk("ef", F32)
            e2f, t_e2 = mk("e2", F32)
            kk, t_kk = mk("kk", F32)
            gg, t_g = mk("g", F32)
            bcs, t_b = mk("b", F32)
            bm, t_bm = mk("bm", F32)
            E1, t_E1 = mk("E1", F32)
            E2, t_E2 = mk("E2", F32)
            E3, t_E3 = mk("E3", F32)
            qrel, t_qrel = mk("qrel", BF16)
            qb, t_qb = mk("qb", BF16)
            krel, t_krel = mk("krel", BF16)
            sog, t_sog = mk("sog", F32)
            o_f, t_of = mk("of", F32)
            sqb, t_sqb = mk("sqb", BF16)
            ob2, t_ob2 = mk("ob2", BF16)
            rst, t_rst = mk("rst", F32)
            kt, t_kt = mk("kt", BF16, 4, (128, 128))
            stm, t_stm = mk("stm", BF16, 4, (128, 128))
            sidx = [0, 0]
            RB = [(4, 5, 6, 7), (0, 1, 2, 3)]

            def prep(h, b, u):
                tb = slice(b * 512, (b + 1) * 512)
                for j, bank in ((0, 0), (1, 1), (3, 2)):
                    for k in range(8):
                        S.op("pe", mm(ps[bank][:, :], wq[u][:, k, j * 128:(j + 1) * 128], hT[:, k, tb], k == 0, k == 7),
                             r=[t_wq[u], t_hT[b]], w=[pst[bank]])
                for ti in range(4):
                    for k in range(8):
                        S.op("pe", mm(ps[3][:, ti * 128:(ti + 1) * 128], hT[:, k, b * 512 + ti * 128: b * 512 + (ti + 1) * 128],
                                      wq[u][:, k, 256:384], k == 0, k == 7), r=[t_wq[u], t_hT[b]], w=[pst[3]])
                S.op("act", lambda e: e.activation(out=v_tm[u][:], in_=ps[3][:, :], func=AF.Copy), r=[pst[3]], w=[t_v[u]])
                S.op("act", lambda e: e.activation(out=q_f[u][:], in_=ps[0][:, :], func=AF.Copy), r=[pst[0]], w=[t_q[u]])
                S.op("act", lambda e: e.activation(out=ef[u][:], in_=ps[1][:, :], func=AF.Exp, scale=-1.0), r=[pst[1]], w=[t_ef[u]])
                S.op("act", lambda e: e.activation(out=e2f[u][:], in_=ps[2][:, :], func=AF.Exp, scale=-1.0), r=[pst[2]], w=[t_e2[u]])
                S.op("act", lambda e: e.activation(out=ef[u][:], in_=ef[u][:], func=AF.Ln, bias=ONEB), r=[t_ef[u]], w=[t_ef[u]])
                S.op("act", lambda e: e.activation(out=ef[u][:], in_=ef[u][:], func=AF.Exp, scale=-1.0), r=[t_ef[u]], w=[t_ef[u]])
                S.op("dve", lambda e: e.tensor_scalar(out=kk[u][:], in0=ef[u][:], scalar1=lbv[:, 4 + h:5 + h], scalar2=lbv[:, h:h + 1],
                                                      op0=ALU.mult, op1=ALU.add), r=[t_ef[u]], w=[t_kk[u]])
                S.op("act", lambda e: e.activation(out=gg[u][:], in_=kk[u][:], func=AF.Ln, scale=-1.0, bias=ONEB), r=[t_kk[u]], w=[t_g[u]])
                S.op("dve", lambda e: e.tensor_tensor_scan(out=bcs[u][:], data0=rmask_s[:], data1=gg[u][:], initial=0.0,
                                                           op0=ALU.mult, op1=ALU.add), r=[t_g[u]], w=[t_b[u]])
                S.op("dve", lambda e: e.tensor_tensor(
                    out=bm[u][:].rearrange("p (c t) -> p c t", t=64),
                    in0=bcs[u][:].rearrange("p (c t) -> p c t", t=64),
                    in1=bcs[u][:].rearrange("p (c t) -> p c t", t=64)[:, :, 31:32].to_broadcast([128, 8, 64]),
                    op=ALU.subtract), r=[t_b[u]], w=[t_bm[u]])
                S.op("act", lambda e: e.activation(out=E1[u][:], in_=bm[u][:], func=AF.Exp), r=[t_bm[u]], w=[t_E1[u]])
                S.op("act", lambda e: e.activation(out=E2[u][:], in_=bm[u][:], func=AF.Exp, scale=-1.0), r=[t_bm[u]], w=[t_E2[u]])
                S.op("act", lambda e: e.activation(out=E3[u][:], in_=bcs[u][:], func=AF.Exp), r=[t_b[u]], w=[t_E3[u]])
                S.op("dve", lambda e: e.tensor_tensor(out=qrel[u][:], in0=q_f[u][:], in1=E1[u][:], op=ALU.mult),
                     r=[t_q[u], t_E1[u]], w=[t_qrel[u]])
                S.op("dve", lambda e: e.tensor_tensor(out=qb[u][:], in0=q_f[u][:], in1=E3[u][:], op=ALU.mult),
                     r=[t_q[u], t_E3[u]], w=[t_qb[u]])
                S.op("dve", lambda e: e.tensor_tensor(out=krel[u][:], in0=kk[u][:], in1=E2[u][:], op=ALU.mult),
                     r=[t_kk[u], t_E2[u]], w=[t_krel[u]])
                S.op("act", lambda e: e.activation(out=e2f[u][:], in_=e2f[u][:], func=AF.Ln, bias=ONEB), r=[t_e2[u]], w=[t_e2[u]])
                S.op("act", lambda e: e.activation(out=e2f[u][:], in_=e2f[u][:], func=AF.Exp, scale=-1.0), r=[t_e2[u]], w=[t_e2[u]])
                S.op("dve", lambda e: e.tensor_tensor(out=sog[u][:], in0=ps[2][:, :], in1=e2f[u][:], op=ALU.mult),
                     r=[pst[2], t_e2[u]], w=[t_sog[u]])

            def tile_pre(u, ti):
                btr, bsT, boT, bdS = RB[u]
                tsl = slice(ti * 128, (ti + 1) * 128)
                w_ = u * 2 + ti % 2
                S.op("pe", lambda e: e.transpose(out=psb(btr)[:, 0:128], in_=krel[u][:, tsl], identity=IDB), r=[t_krel[u]], w=[pst[btr]])
                S.op("act", lambda e: e.activation(out=kt[w_][:], in_=psb(btr)[:, 0:128], func=AF.Copy), r=[pst[btr]], w=[t_kt[w_]])
                S.op("pe", mm(ps[bsT][:, 0:128], krel[u][:, tsl], qrel[u][:, tsl], True, True), r=[t_krel[u], t_qrel[u]], w=[pst[bsT]])
                S.op("dve", lambda e: e.tensor_tensor(out=stm[w_][:], in0=ps[bsT][:, 0:128], in1=CMASK, op=ALU.mult), r=[pst[bsT]], w=[t_stm[w_]])
                S.op("pe", mm(ps[boT][:, 0:128], v_tm[u][:, tsl], stm[w_][:], True, False), r=[t_v[u], t_stm[w_]], w=[pst[boT]])

            def chunk(u, ti, half):
                btr, bsT, boT, bdS = RB[u]
                tsl = slice(ti * 128, (ti + 1) * 128)
                w_ = u * 2 + ti % 2
                c = ti * 2 + half
                csl = slice(ti * 128 + half * 64, ti * 128 + half * 64 + 64)
                pr = slice(half * 64, half * 64 + 64)
                cur = sidx[u] % 2
                S.op("pe", mm(ps[boT][:, half * 64:half * 64 + 64], stbf[u][cur][:], qb[u][:, csl], False, half == 1),
                     r=[t_stbf[u][cur], t_qb[u]], w=[pst[boT]])
                S.op("pe", mm(ps[bdS][:, half * 128:half * 128 + 128], kt[w_][pr, :], v_tm[u][pr, tsl], True, True),
                     r=[t_kt[w_], t_v[u]], w=[pst[bdS]])
                e5 = E3[u][:, c * 64 + 63:c * 64 + 64]
                c1 = E1[u][:, c * 64 + 63:c * 64 + 64]
                S.op("dve", lambda e: e.tensor_scalar(out=state[u][:], in0=state[u][:], scalar1=e5, scalar2=None, op0=ALU.mult),
                     r=[t_state[u], t_E3[u]], w=[t_state[u]])
                S.op("dve", lambda e: e.scalar_tensor_tensor(
                    out=state[u][:], in0=ps[bdS][:, half * 128:half * 128 + 128], scalar=c1, in1=state[u][:], op0=ALU.mult, op1=ALU.add),
                    r=[t_state[u], t_E1[u], pst[bdS]], w=[t_state[u]])
                sidx[u] += 1
                nxt = sidx[u] % 2
                S.op("act", lambda e: e.activation(out=stbf[u][nxt][:], in_=state[u][:], func=AF.Copy), r=[t_state[u]], w=[t_stbf[u][nxt]])

            def tile_post(u, ti):
                btr, bsT, boT, bdS = RB[u]
                tsl = slice(ti * 128, (ti + 1) * 128)
                S.op("dve", lambda e: e.tensor_copy(out=o_f[u][:, tsl], in_=ps[boT][:, 0:128]), r=[pst[boT]], w=[t_of[u]])

            def post(h, b, u):
                btr, bsT, boT, bdS = RB[u]
                tb = slice(b * 512, (b + 1) * 512)
                S.op("act", lambda e: e.activation(out=sqb[u][:], in_=o_f[u][:], func=AF.Square), r=[t_of[u]], w=[t_sqb[u]])
                S.op("pe", mm(ps[bsT][:, :], ONESB, sqb[u][:], True, True), r=[t_sqb[u]], w=[pst[bsT]])
                S.op("act", lambda e: e.activation(out=rst[u][:], in_=ps[bsT][:, :], func=AF.Ln, scale=1.0 / 128, bias=EPSB), r=[pst[bsT]], w=[t_rst[u]])
                S.op("act", lambda e: e.activation(out=rst[u][:], in_=rst[u][:], func=AF.Exp, scale=-0.5), r=[t_rst[u]], w=[t_rst[u]])
                S.op("dve", lambda e: e.tensor_tensor(out=o_f[u][:], in0=o_f[u][:], in1=rst[u][:], op=ALU.mult), r=[t_of[u], t_rst[u]], w=[t_of[u]])
                S.op("dve", lambda e: e.scalar_tensor_tensor(out=ob2[u][:], in0=o_f[u][:], scalar=hgon_s[:, 0:1], in1=sog[u][:],
                                                             op0=ALU.mult, op1=ALU.mult), r=[t_of[u], t_sog[u]], w=[t_ob2[u]])
                S.op("sp", lambda e, inc: inc(e.dma_start(out=ohg_d[:, h, tb], in_=ob2[u][:])), r=[t_ob2[u]], w=[t_ohgd[h][b]], dma=True)

            for hp in range(2):
                for u in range(2):
                    h = 2 * hp + u
                    def ldw(e, inc, h=h):
                        for j in range(4):
                            inc(e.dma_start(out=wst[:, :, j * 128:(j + 1) * 128],
                                            in_=w_in_v[:, :, j * 512 + h * 128: j * 512 + (h + 1) * 128]))
                    S.op("sp", ldw, w=[t_wst], dma=True)
                    if h == 0:
                        for ex_ in range(NE):
                            S.op("sp", lambda e, inc, ex_=ex_: inc(e.dma_start(
                                out=xg_d[ex_ * CAP:(ex_ + 1) * CAP, :].rearrange("(c p) d -> p c d", p=128),
                                in_=zt[:].unsqueeze(1).to_broadcast([128, CT, D]))), r=[t_zt], wd=[t_xg], dma=True)
                    S.op("dve", lambda e, u=u: e.tensor_copy(out=wq[u][:], in_=wst[:]), r=[t_wst], w=[t_wq[u]])
                    S.op("dve", lambda e, u=u: e.memset(state[u][:], 0.0), w=[t_state[u]])
                    S.op("dve", lambda e, u=u, c0=sidx[u] % 2: e.memset(stbf[u][c0][:], 0.0), w=[t_stbf[u][sidx[u] % 2]])
                for b in range(NB):
                    for u in range(2):
                        prep(2 * hp + u, b, u)
                    for ti in range(4):
                        for u in range(2):
                            tile_pre(u, ti)
                        for half in range(2):
                            for u in range(2):
                                chunk(u, ti, half)
                        for u in range(2):
                            tile_post(u, ti)
                    for u in range(2):
                        post(2 * hp + u, b, u)
            S.barrier()
        if "ht2" in dbg:
            dbg_t["ht2"] = nc.dram_tensor("dbg_ht2", [128, 8, T], F32, kind="ExternalOutput").ap()
            with ExitStack() as pd:
                tmp = sb("dbgtmp3", [128, 8, T], F32, pd)
                tt = S.tok("dbgtmp3")
                S.op("dve", lambda e: e.tensor_copy(out=tmp[:], in_=hT[:]), r=t_hT, w=[tt])
                S.op("sp", lambda e, inc: inc(e.dma_start(out=dbg_t["ht2"], in_=tmp[:])), r=[tt], dma=True)
                S.barrier()
        if stop_after <= 2:
            S.barrier()
            es_h.close()
            return nc

        with ExitStack() as p4:
            ropec_s = sb("p4rc", [128, T], F32, p4)
            ropes_s = sb("p4rs", [128, T], F32, p4)
            t_rope = S.tok("rope")
            S.op("sp", lambda e, inc: (inc(e.dma_start(out=ropec_s[:], in_=ropec)), inc(e.dma_start(out=ropes_s[:], in_=ropes))),
                 w=[t_rope], dma=True)
            wst = sb("p4wst", [128, 8, 384], F32, p4)
            wa = sb("p4wa", [128, 8, 384], BF16, p4)
            t_wst, t_wa = S.tok("wst4"), S.tok("wa4")
            qT_s = sb("p4qT", [128, T], BF16, p4)
            kT_s = sb("p4kT", [128, T], BF16, p4)
            vt = sb("p4v", [128, NT, 128], BF16, p4)
            t_qT = [S.tok("qT") for _ in range(NB)]
            t_kT = [S.tok("kT") for _ in range(NB)]
            t_vt = [S.tok("vt") for _ in range(NB)]
            def mk4(name, dt, n=2, shape=(128, 512)):
                return [sb(f"p4{name}{i}", list(shape), dt, p4) for i in range(n)], [S.tok(name) for _ in range(n)]
            sq4, t_sq4 = mk4("sq", BF16)
            qf4, t_qf4 = mk4("qf", F32)
            rs4, t_rs4 = mk4("rs", F32)
            qn4, t_qn4 = mk4("qn", BF16)
            t14, t_t14 = mk4("t1", F32)
            t24, t_t24 = mk4("t2", F32)
            Pm, t_P = mk4("P", BF16, 4)
            r0, t_r0 = mk4("r0", F32, 1)
            r1, t_r1 = mk4("r1", F32, 1)
            o1, t_o1 = mk4("o1", F32, 1)
            o2, t_o2 = mk4("o2", F32, 1)
            ob4, t_ob4 = mk4("ob4", BF16, 2)
            cnt4 = 0
            for h in range(4):
                def ldw4(e, inc, h=h):
                    for j in range(3):
                        inc(e.dma_start(out=wst[:, :, j * 128:(j + 1) * 128],
                                        in_=w_in_v[:, :, 2048 + j * 512 + h * 128: 2048 + j * 512 + (h + 1) * 128]))
                S.op("sp", ldw4, w=[t_wst], dma=True)
                S.op("dve", lambda e: e.tensor_copy(out=wa[:], in_=wst[:]), r=[t_wst], w=[t_wa])
                for b in range(NB):
                    tb = slice(b * 512, (b + 1) * 512)
                    for j, dst, t_dst in ((0, qT_s, t_qT), (1, kT_s, t_kT)):
                        u = cnt4 % 2
                        cnt4 += 1
                        for k in range(8):
                            S.op("pe", mm(ps[j][:, :], wa[:, k, j * 128:(j + 1) * 128], hT[:, k, tb], k == 0, k == 7),
                                 r=[t_wa, t_hT[b]], w=[pst[j]])
                        S.op("act", lambda e, u=u, j=j: e.activation(out=sq4[u][:], in_=ps[j][:, :], func=AF.Square), r=[pst[j]], w=[t_sq4[u]])
                        S.op("act", lambda e, u=u, j=j: e.activation(out=qf4[u][:], in_=ps[j][:, :], func=AF.Copy), r=[pst[j]], w=[t_qf4[u]])
                        S.op("pe", mm(ps[2][:, :], BD64, sq4[u][:], True, True), r=[t_sq4[u]], w=[pst[2]])
                        S.op("act", lambda e, u=u: e.activation(out=rs4[u][:], in_=ps[2][:, :], func=AF.Ln, scale=1.0 / 64, bias=EPSB), r=[pst[2]], w=[t_rs4[u]])
                        S.op("act", lambda e, u=u: e.activation(out=rs4[u][:], in_=rs4[u][:], func=AF.Exp, scale=-0.5), r=[t_rs4[u]], w=[t_rs4[u]])
                        S.op("dve", lambda e, u=u, j=j: e.scalar_tensor_tensor(out=qn4[u][:], in0=qf4[u][:], scalar=qkn_s[:, j:j + 1], in1=rs4[u][:],
                                                                              op0=ALU.mult, op1=ALU.mult), r=[t_qf4[u], t_rs4[u]], w=[t_qn4[u]])
                        S.op("pe", mm(ps[3][:, :], PERM, qn4[u][:], True, True), r=[t_qn4[u]], w=[pst[3]])
                        S.op("dve", lambda e, u=u, tb=tb: e.tensor_tensor(out=t14[u][:], in0=qn4[u][:], in1=ropec_s[:, tb], op=ALU.mult),
                             r=[t_qn4[u], t_rope], w=[t_t14[u]])
                        S.op("dve", lambda e, u=u, tb=tb: e.tensor_tensor(out=t24[u][:], in0=ps[3][:, :], in1=ropes_s[:, tb], op=ALU.mult),
                             r=[pst[3], t_rope], w=[t_t24[u]])
                        S.op("dve", lambda e, u=u, tb=tb, dst=dst: e.tensor_tensor(out=dst[:, tb], in0=t14[u][:], in1=t24[u][:], op=ALU.add),
                             r=[t_t14[u], t_t24[u]], w=[t_dst[b]])
                    for ti in range(4):
                        for k in range(8):
                            S.op("pe", mm(ps[4][:, ti * 128:(ti + 1) * 128], hT[:, k, b * 512 + ti * 128: b * 512 + (ti + 1) * 128],
                                          wa[:, k, 256:384], k == 0, k == 7), r=[t_wa, t_hT[b]], w=[pst[4]])
                    S.op("act", lambda e, b=b: e.activation(out=vt[:, b * 4:(b + 1) * 4, :], in_=ps[4][:, :].rearrange("p (a c) -> p a c", a=4),
                                                            func=AF.Copy), r=[pst[4]], w=[t_vt[b]])
                S.barrier()
                steps = [(j, i) for j in range(NB) for i in range(4 * j + 4)]

                def qk(s):
                    j, i = steps[s]
                    r_ = i - 4 * j
                    col0 = 128 * r_ if r_ > 0 else 0
                    for m in range(2):
                        bank = m * 2 + (s % 2)
                        S.op("pe", mm(ps[bank][:, col0:512], kT_s[m * 64:(m + 1) * 64, i * 128:(i + 1) * 128],
                                      qT_s[m * 64:(m + 1) * 64, j * 512 + col0:(j + 1) * 512], True, True),
                             r=[t_kT[i // 4], t_qT[j]], w=[pst[bank]])

                qk(0)
                for s in range(len(steps)):
                    j, i = steps[s]
                    r_ = i - 4 * j
                    col0 = 128 * r_ if r_ > 0 else 0
                    last = (i == 4 * j + 3)
                    for m in range(2):
                        bank = m * 2 + (s % 2)
                        pb_ = m * 2 + (s % 2)
                        S.op("act", lambda e, bank=bank, pb_=pb_, col0=col0: e.activation(out=Pm[pb_][:, col0:512], in_=ps[bank][:, col0:512],
                                                                                           func=AF.Exp, scale=0.125),
                             r=[pst[bank]], w=[t_P[pb_]])
                    if s + 1 < len(steps):
                        qk(s + 1)
                    for m in range(2):
                        pb_ = m * 2 + (s % 2)
                        if r_ >= 0:
                            S.op("dve", lambda e, pb_=pb_, col0=col0: e.tensor_tensor(out=Pm[pb_][:, col0:col0 + 128], in0=Pm[pb_][:, col0:col0 + 128],
                                                                                       in1=TRI01, op=ALU.mult), r=[t_P[pb_]], w=[t_P[pb_]])
                        S.op("pe", mm(ps[4 + m][:, col0:512], vt[:, i, :], Pm[pb_][:, col0:512], i == 0, last), r=[t_vt[i // 4], t_P[pb_]], w=[pst[4 + m]])
                        S.op("pe", mm(ps[6 + m][:, col0:512], ONESB, Pm[pb_][:, col0:512], i == 0, last), r=[t_P[pb_]], w=[pst[6 + m]])
                    if last:
                        jb = slice(j * 512, (j + 1) * 512)
                        S.op("dve", lambda e: e.tensor_copy(out=o1[0][:], in_=ps[4][:, :]), r=[pst[4]], w=[t_o1[0]])
                        S.op("dve", lambda e: e.tensor_copy(out=o2[0][:], in_=ps[5][:, :]), r=[pst[5]], w=[t_o2[0]])
                        S.op("act", lambda e: e.activation(out=r0[0][:], in_=ps[6][:, :], func=AF.Ln), r=[pst[6]], w=[t_r0[0]])
                        S.op("act", lambda e: e.activation(out=r1[0][:], in_=ps[7][:, :], func=AF.Ln), r=[pst[7]], w=[t_r1[0]])
                        S.op("act", lambda e: e.activation(out=r0[0][:], in_=r0[0][:], func=AF.Exp, scale=-1.0), r=[t_r0[0]], w=[t_r0[0]])
                        S.op("act", lambda e: e.activation(out=r1[0][:], in_=r1[0][:], func=AF.Exp, scale=-1.0), r=[t_r1[0]], w=[t_r1[0]])
                        S.op("dve", lambda e: e.tensor_tensor(out=o1[0][:], in0=o1[0][:], in1=r0[0][:], op=ALU.mult), r=[t_o1[0], t_r0[0]], w=[t_o1[0]])
                        S.op("dve", lambda e: e.tensor_tensor(out=o2[0][:], in0=o2[0][:], in1=r1[0][:], op=ALU.mult), r=[t_o2[0], t_r1[0]], w=[t_o2[0]])
                        S.op("dve", lambda e: e.scalar_tensor_tensor(out=o1[0][:], in0=o2[0][:], scalar=lam_s[:, 2:3], in1=o1[0][:], op0=ALU.mult, op1=ALU.add),
                             r=[t_o1[0], t_o2[0]], w=[t_o1[0]])
                        S.op("act", lambda e: e.activation(out=sq4[0][:], in_=o1[0][:], func=AF.Square), r=[t_o1[0]], w=[t_sq4[0]])
                        S.op("pe", mm(ps[6][:, :], ONESB, sq4[0][:], True, True), r=[t_sq4[0]], w=[pst[6]])
                        S.op("act", lambda e: e.activation(out=rs4[0][:], in_=ps[6][:, :], func=AF.Ln, scale=1.0 / 128, bias=EPSB), r=[pst[6]], w=[t_rs4[0]])
                        S.op("act", lambda e: e.activation(out=rs4[0][:], in_=rs4[0][:], func=AF.Exp, scale=-0.5), r=[t_rs4[0]], w=[t_rs4[0]])
                        ou = j % 2
                        S.op("dve", lambda e, ou=ou: e.scalar_tensor_tensor(out=ob4[ou][:], in0=o1[0][:], scalar=daon_s[:, 1:2], in1=rs4[0][:],
                                                                            op0=ALU.mult, op1=ALU.mult), r=[t_o1[0], t_rs4[0]], w=[t_ob4[ou]])
                        S.op("sp", lambda e, inc, ou=ou, h=h, jb=jb: inc(e.dma_start(out=oda_d[:, h, jb], in_=ob4[ou][:])), r=[t_ob4[ou]], w=[t_odad[h][j]], dma=True)
                S.barrier()
        es_h.close()
        if stop_after <= 4:
            S.barrier()
            return nc

        BCREG = nc.gpsimd.to_reg(NSLOT - 1)
        logit = sb("logit", [128, NT, 36], F32)
        t_logit = S.tok("logit")
        with ExitStack() as p5:
            wbh_s = sb("p5wbh", [128, 4, D], BF16, p5)
            wbd_s = sb("p5wbd", [128, 4, D], BF16, p5)
            wg_s = sb("p5wg", [128, 8, 2048], BF16, p5)
            wo_s = sb("p5wo", [128, 8, D], BF16, p5)
            nm_s = sb("p5nm", [128, D], F32, p5)
            wr_s = sb("p5wr", [128, 8, 36], F32, p5)
            br_s = sb("p5br", [128, 36], F32, p5)
            t_w5 = S.tok("w5")
            wbh_v = wbh.rearrange("(k p) c -> p k c", p=128)
            wbd_v = wbd.rearrange("(k p) c -> p k c", p=128)
            wo_v = w_out.rearrange("(k p) c -> p k c", p=128)
            stg = [sb(f"p5stg{i}", [128, 8, 512], F32, p5) for i in range(2)]
            t_stg = [S.tok("stg") for _ in range(2)]
            t_wbh = [S.tok("wbh") for _ in range(2)]
            t_wbd = [S.tok("wbd") for _ in range(2)]
            t_wo = [S.tok("wo") for _ in range(2)]
            t_wg = [S.tok("wg") for _ in range(4)]
            sgc = [0]
            def ldcast(src_ap, dst_ap, kk_, tk):
                g = sgc[0] % 2
                sgc[0] += 1
                S.op("sp", lambda e, inc, g=g: inc(e.dma_start(out=stg[g][:, 0:kk_, :], in_=src_ap)), w=[t_stg[g]], dma=True)
                if g == 0:
                    S.op("dve", lambda e, g=g: e.tensor_copy(out=dst_ap, in_=stg[g][:, 0:kk_, :]), r=[t_stg[g]], w=[tk])
                else:
                    S.op("act", lambda e, g=g: e.activation(out=dst_ap, in_=stg[g][:, 0:kk_, :], func=AF.Copy), r=[t_stg[g]], w=[tk])
            def ld_wg(n):
                ldcast(w_in_v[:, :, 3584 + n * 512: 3584 + (n + 1) * 512], wg_s[:, :, n * 512:(n + 1) * 512], 8, t_wg[n])
            for n in range(2):
                ldcast(wbh_v[:, :, n * 512:(n + 1) * 512], wbh_s[:, :, n * 512:(n + 1) * 512], 4, t_wbh[n])
                ldcast(wbd_v[:, :, n * 512:(n + 1) * 512], wbd_s[:, :, n * 512:(n + 1) * 512], 4, t_wbd[n])
                ld_wg(n)
                ld_wg(2 + n)
            for n in range(2):
                ldcast(wo_v[:, :, n * 512:(n + 1) * 512], wo_s[:, :, n * 512:(n + 1) * 512], 8, t_wo[n])
            S.op("sp", lambda e, inc: (inc(e.dma_start(out=nm_s[:], in_=nmoe)), inc(e.dma_start(out=wr_s[:], in_=wr.rearrange("(k p) c -> p k c", p=128))),
                                       inc(e.dma_start(out=br_s[:], in_=br))), w=[t_w5], dma=True)
            hTb = [sb(f"p5hT{i}", [128, 8, 512], BF16, p5) for i in range(2)]
            ohb = [sb(f"p5oh{i}", [128, 4, 512], BF16, p5) for i in range(2)]
            odb = [sb(f"p5od{i}", [128, 4, 512], BF16, p5) for i in range(2)]
            t_hTb = [S.tok("hTb") for _ in range(2)]
            t_ohb = [S.tok("ohb") for _ in range(2)]
            t_odb = [S.tok("odb") for _ in range(2)]
            mixT = [sb(f"p5mix{i}", [128, 8, 512], BF16, p5) for i in range(2)]
            t_mix = [S.tok("mix") for _ in range(2)]
            def mk5(name, dt, n=2, shape=(128, 512)):
                return [sb(f"p5{name}{i}", list(shape), dt, p5) for i in range(n)], [S.tok(name) for _ in range(n)]
            s1b, t_s1 = mk5("s1", F32)
            s2b, t_s2 = mk5("s2", F32)
            m1b, t_m1 = mk5("m1", F32, 1)
            m2b, t_m2 = mk5("m2", F32, 1)
            xt5, t_xt5 = mk5("xt", F32, 2, (128, D))
            x2b, t_x2 = mk5("x2", F32, 1, (128, D))
            xnf, t_xnf = mk5("xnf", F32, 1, (128, D))
            xnb, t_xnb = mk5("xnb", BF16, 2, (128, D))
            xnT, t_xnT = mk5("xnT", F32, 1, (128, D))
            sq5, t_sq5 = mk5("sq", F32, 1, (128, D))
            st5, t_st5 = mk5("st", F32, 2, (128, 2))
            cc = 0
            for b in range(NB):
                tb = slice(b * 512, (b + 1) * 512)
                mb = b % 2
                S.op("sp", lambda e, inc, mb=mb, tb=tb: inc(e.dma_start(out=hTb[mb][:], in_=hT_d[:, :, tb])), r=[t_hTd], w=[t_hTb[mb]], dma=True)
                S.op("sp", lambda e, inc, mb=mb, tb=tb: inc(e.dma_start(out=ohb[mb][:], in_=ohg_d[:, :, tb])), r=[t_ohgd[k][b] for k in range(4)], w=[t_ohb[mb]], dma=True)
                S.op("sp", lambda e, inc, mb=mb, tb=tb: inc(e.dma_start(out=odb[mb][:], in_=oda_d[:, :, tb])), r=[t_odad[k][b] for k in range(4)], w=[t_odb[mb]], dma=True)
                for c in range(8):
                    u = cc % 2
                    cc += 1
                    cs = slice(c * 128, (c + 1) * 128)
                    pb0 = 4 * u
                    for k in range(4):
                        S.op("pe", mm(ps[pb0][:, :], wbh_s[:, k, cs], ohb[mb][:, k, :], k == 0, k == 3), r=[t_wbh[c // 4], t_ohb[mb]], w=[pst[pb0]])
                    for k in range(4):
                        S.op("pe", mm(ps[pb0 + 1][:, :], wbd_s[:, k, cs], odb[mb][:, k, :], k == 0, k == 3), r=[t_wbd[c // 4], t_odb[mb]], w=[pst[pb0 + 1]])
                    for k in range(8):
                        S.op("pe", mm(ps[pb0 + 2][:, :], wg_s[:, k, c * 128:(c + 1) * 128], hTb[mb][:, k, :], k == 0, k == 7), r=[t_wg[c // 4], t_hTb[mb]], w=[pst[pb0 + 2]])
                    for k in range(8):
                        S.op("pe", mm(ps[pb0 + 3][:, :], wg_s[:, k, 1024 + c * 128:1024 + (c + 1) * 128], hTb[mb][:, k, :], k == 0, k == 7),
                             r=[t_wg[2 + c // 4], t_hTb[mb]], w=[pst[pb0 + 3]])
                    S.op("act", lambda e, u=u, pb0=pb0: e.activation(out=s1b[u][:], in_=ps[pb0 + 2][:, :], func=AF.Sigmoid), r=[pst[pb0 + 2]], w=[t_s1[u]])
                    S.op("act", lambda e, u=u, pb0=pb0: e.activation(out=s2b[u][:], in_=ps[pb0 + 3][:, :], func=AF.Sigmoid), r=[pst[pb0 + 3]], w=[t_s2[u]])
                    S.op("dve", lambda e, u=u, pb0=pb0: e.tensor_tensor(out=m1b[0][:], in0=ps[pb0][:, :], in1=s1b[u][:], op=ALU.mult), r=[pst[pb0], t_s1[u]], w=[t_m1[0]])
                    S.op("dve", lambda e, u=u, pb0=pb0: e.tensor_tensor(out=m2b[0][:], in0=ps[pb0 + 1][:, :], in1=s2b[u][:], op=ALU.mult), r=[pst[pb0 + 1], t_s2[u]], w=[t_m2[0]])
                    S.op("dve", lambda e, mb=mb, c=c: e.tensor_tensor(out=mixT[mb][:, c, :], in0=m1b[0][:], in1=m2b[0][:], op=ALU.add),
                         r=[t_m1[0], t_m2[0]], wd=[t_mix[mb]])
                for ti in range(4):
                    i = b * 4 + ti
                    u = i % 2
                    if i == 0:
                        S.op("sp", lambda e, inc: inc(e.dma_start(out=xt5[0][:], in_=x[0:128, :])), w=[t_xt5[0]], dma=True)
                    if i + 1 < NT:
                        S.op("sp", lambda e, inc, i=i: inc(e.dma_start(out=xt5[(i + 1) % 2][:], in_=x[(i + 1) * 128:(i + 2) * 128, :])), w=[t_xt5[(i + 1) % 2]], dma=True)
                    for n in range(2):
                        for k in range(8):
                            S.op("pe", mm(ps[n][:, :], mixT[mb][:, k, ti * 128:(ti + 1) * 128], wo_s[:, k, n * 512:(n + 1) * 512], k == 0, k == 7),
                                 r=[t_mix[mb], t_wo[n]], w=[pst[n]])
                        S.op("dve", lambda e, u=u, n=n: e.tensor_tensor(out=x2b[0][:, n * 512:(n + 1) * 512], in0=ps[n][:, :], in1=xt5[u][:, n * 512:(n + 1) * 512],
                                                                       op=ALU.add), r=[pst[n], t_xt5[u]], wd=[t_x2[0]])
                    S.op("sp", lambda e, inc, i=i: inc(e.dma_start(out=x2_d[i * 128:(i + 1) * 128, :], in_=x2b[0][:])), r=[t_x2[0]], w=[t_x2d[i]], dma=True)
                    if "x2" in dbg:
                        S.op("sp", lambda e, inc, i=i: inc(e.dma_start(out=dbg_t["x2"][i * 128:(i + 1) * 128, :], in_=x2b[0][:])), r=[t_x2[0]], dma=True)
                    S.op("act", lambda e: e.activation(out=sq5[0][:], in_=x2b[0][:], func=AF.Square), r=[t_x2[0]], w=[t_sq5[0]])
                    S.op("dve", lambda e, u=u: e.reduce_sum(out=st5[u][:, 0:1], in_=sq5[0][:], axis=AX.X), r=[t_sq5[0]], w=[t_st5[u]])
                    S.op("act", lambda e, u=u: e.activation(out=st5[u][:, 1:2], in_=st5[u][:, 0:1], func=AF.Ln, scale=1.0 / D, bias=EPSB), r=[t_st5[u]], w=[t_st5[u]])
                    S.op("act", lambda e, u=u: e.activation(out=st5[u][:, 1:2], in_=st5[u][:, 1:2], func=AF.Exp, scale=-0.5), r=[t_st5[u]], w=[t_st5[u]])
                    S.op("dve", lambda e, u=u: e.scalar_tensor_tensor(out=xnf[0][:], in0=x2b[0][:], scalar=st5[u][:, 1:2], in1=nm_s[:], op0=ALU.mult, op1=ALU.mult),
                         r=[t_x2[0], t_st5[u], t_w5], w=[t_xnf[0]])
                    S.op("act", lambda e, u=u: e.activation(out=xnb[u][:].rearrange("p (k j) -> p k j", k=8), in_=xnf[0][:].rearrange("p (j k) -> p k j", k=8),
                                                            func=AF.Copy), r=[t_xnf[0]], w=[t_xnb[u]])
                    S.op("sp", lambda e, inc, i=i, u=u: inc(e.dma_start(out=xn_d[i * 128:(i + 1) * 128, :], in_=xnb[u][:])), r=[t_xnb[u]], w=[t_xnd[i]], dma=True)
                    for k in range(8):
                        bank = 2 + k // 4
                        S.op("pe", lambda e, k=k, bank=bank: e.transpose(out=ps[bank][:, (k % 4) * 128:(k % 4 + 1) * 128],
                                                                       in_=xnf[0][:, k * 128:(k + 1) * 128], identity=IDF),
                             r=[t_xnf[0]], w=[pst[bank]])
                    S.op("act", lambda e: e.activation(out=xnT[0][:, 0:512], in_=ps[2][:, :], func=AF.Copy), r=[pst[2]], wd=[t_xnT[0]])
                    S.op("dve", lambda e: e.tensor_copy(out=xnT[0][:, 512:1024], in_=ps[3][:, :]), r=[pst[3]], wd=[t_xnT[0]])
                    for k in range(8):
                        S.op("pe", mm(ps[2][:, 0:36], xnT[0][:, k * 128:(k + 1) * 128], wr_s[:, k, :], k == 0, k == 7), r=[t_xnT[0], t_w5], w=[pst[2]])
                    S.op("dve", lambda e, i=i: e.tensor_tensor(out=logit[:, i, :], in0=ps[2][:, 0:36], in1=br_s[:], op=ALU.add), r=[pst[2], t_w5], wd=[t_logit])
            if "x2" in dbg:
                S.op("sp", lambda e, inc: inc(e.dma_start(out=dbg_t["lg"], in_=logit[:])), r=[t_logit], dma=True)
            S.barrier()
        with ExitStack() as p5:
            ecap_s = sb("p5ecap", [128, 32], F32, p5)
            xnb = [sb(f"p5cxnb{i}", [128, D], BF16, p5) for i in range(4)]
            t_xnb = [S.tok("cxnb") for _ in range(4)]
            t_w5 = S.tok("w5b")
            S.op("sp", lambda e, inc: inc(e.dma_start(out=ecap_s[:], in_=ecap)), w=[t_w5], dma=True)
            def rb(name, shape, dt=F32):
                return sb("r_" + name, shape, dt, p5)
            t_r = S.tok("router")
            G = logit[:, :, 0:4]
            E4 = logit[:, :, 4:36].rearrange("p n (g j) -> p n g j", g=4)
            gmax = rb("gmax", [128, NT]); goh = rb("goh", [128, NT, 4]); gsh = rb("gsh", [128, NT, 4]); gsum = rb("gsum", [128, NT])
            gw = rb("gw", [128, NT]); sel = rb("sel", [128, NT, 4, 8]); eg = rb("eg", [128, NT, 8]); m1 = rb("m1", [128, NT])
            oh1 = rb("oh1", [128, NT, 8]); eg2 = rb("eg2", [128, NT, 8]); m2 = rb("m2", [128, NT]); oh2 = rb("oh2", [128, NT, 8])
            dd = rb("dd", [128, NT]); ex = rb("ex", [128, NT]); den = rb("den", [128, NT])
            A1 = rb("A1", [128, NT, 4, 8]); A2 = rb("A2", [128, NT, 4, 8]); Ab = rb("Ab", [128, NT, 32], BF16)
            rk = rb("rk", [128, NT, 32]); tmp5 = rb("tmp5", [128, NT, 32]); sl = rb("sl", [128, 2, NT])
            def R_(eng, fn):
                S.op(eng, fn, r=[t_r, t_logit], w=[t_r])
            R_("dve", lambda e: e.tensor_reduce(out=gmax[:], in_=G, axis=AX.X, op=ALU.max))
            R_("dve", lambda e: e.tensor_tensor(out=goh[:], in0=G, in1=gmax[:].unsqueeze(2).to_broadcast([128, NT, 4]), op=ALU.is_equal))
            R_("dve", lambda e: e.tensor_tensor(out=gsh[:], in0=G, in1=gmax[:].unsqueeze(2).to_broadcast([128, NT, 4]), op=ALU.subtract))
            R_("act", lambda e: e.activation(out=gsh[:], in_=gsh[:], func=AF.Exp))
            R_("dve", lambda e: e.tensor_reduce(out=gsum[:], in_=gsh[:], axis=AX.X, op=ALU.add))
            R_("dve", lambda e: e.reciprocal(out=gw[:], in_=gsum[:]))
            R_("dve", lambda e: e.tensor_tensor(out=sel[:], in0=E4, in1=goh[:].unsqueeze(3).to_broadcast([128, NT, 4, 8]), op=ALU.mult))
            R_("dve", lambda e: e.tensor_reduce(out=eg[:], in_=sel[:].rearrange("p n g j -> p n j g"), axis=AX.X, op=ALU.add))
            R_("dve", lambda e: e.tensor_reduce(out=m1[:], in_=eg[:], axis=AX.X, op=ALU.max))
            R_("dve", lambda e: e.tensor_tensor(out=oh1[:], in0=eg[:], in1=m1[:].unsqueeze(2).to_broadcast([128, NT, 8]), op=ALU.is_equal))
            R_("dve", lambda e: e.scalar_tensor_tensor(out=eg2[:], in0=oh1[:], scalar=-1e30, in1=eg[:], op0=ALU.mult, op1=ALU.add))
            R_("dve", lambda e: e.tensor_reduce(out=m2[:], in_=eg2[:], axis=AX.X, op=ALU.max))
            R_("dve", lambda e: e.tensor_tensor(out=oh2[:], in0=eg2[:], in1=m2[:].unsqueeze(2).to_broadcast([128, NT, 8]), op=ALU.is_equal))
            R_("dve", lambda e: e.tensor_sub(out=dd[:], in0=m2[:], in1=m1[:]))
            R_("act", lambda e: e.activation(out=ex[:], in_=dd[:], func=AF.Exp))
            R_("dve", lambda e: e.tensor_scalar_add(out=den[:], in0=ex[:], scalar1=1.0))
            R_("dve", lambda e: e.reciprocal(out=den[:], in_=den[:]))
            S.op("dve", lambda e: e.tensor_mul(out=wgt[:, 0, :], in0=den[:], in1=gw[:]), r=[t_r], w=[t_r, t_route])
            S.op("dve", lambda e: e.tensor_mul(out=wgt[:, 1, :], in0=wgt[:, 0, :], in1=ex[:]), r=[t_r, t_route], w=[t_r, t_route])
            R_("dve", lambda e: e.tensor_tensor(out=A1[:], in0=goh[:].unsqueeze(3).to_broadcast([128, NT, 4, 8]),
                                                in1=oh1[:].unsqueeze(2).to_broadcast([128, NT, 4, 8]), op=ALU.mult))
            R_("dve", lambda e: e.tensor_tensor(out=A2[:], in0=goh[:].unsqueeze(3).to_broadcast([128, NT, 4, 8]),
                                                in1=oh2[:].unsqueeze(2).to_broadcast([128, NT, 4, 8]), op=ALU.mult))
            R_("dve", lambda e: e.tensor_tensor(out=Ab[:], in0=A1[:].rearrange("p n g j -> p n (g j)"), in1=A2[:].rearrange("p n g j -> p n (g j)"), op=ALU.add))
            for i in range(NT):
                bank = i // 16
                oc = slice((i % 16) * 32, (i % 16) * 32 + 32)
                S.op("pe", mm(ps[bank][:, oc], LSTR, Ab[:, i, :], True, i == 0), r=[t_r], w=[pst[bank]])
                for i2 in range(i):
                    S.op("pe", mm(ps[bank][:, oc], ONESB, Ab[:, i2, :], False, i2 == i - 1), r=[t_r], w=[pst[bank]])
            nb_ = (NT + 15) // 16
            for bank in range(nb_):
                n0 = bank * 16
                n1 = min(NT, n0 + 16)
                S.op("dve", lambda e, bank=bank, n0=n0, n1=n1: e.tensor_tensor(
                    out=rk[:, n0:n1, :], in0=ps[bank][:, 0:(n1 - n0) * 32].rearrange("p (n e) -> p n e", e=32),
                    in1=ecap_s[:].unsqueeze(1).to_broadcast([128, n1 - n0, 32]), op=ALU.add), r=[pst[bank], t_r, t_w5], w=[t_r])
            for a_, Aa in ((0, A1), (1, A2)):
                R_("dve", lambda e, Aa=Aa: e.tensor_tensor(out=tmp5[:], in0=rk[:], in1=Aa[:].rearrange("p n g j -> p n (g j)"), op=ALU.mult))
                R_("dve", lambda e, a_=a_: e.tensor_reduce(out=sl[:, a_, :], in_=tmp5[:], axis=AX.X, op=ALU.add))
            S.op("dve", lambda e: e.tensor_copy(out=slot_i[:], in_=sl[:]), r=[t_r], w=[t_route])
            S.barrier()
            if "rt" in dbg:
                S.op("sp", lambda e, inc: (inc(e.dma_start(out=dbg_t["rt"][:, 0:2, :], in_=sl[:])), inc(e.dma_start(out=dbg_t["rt"][:, 2:4, :], in_=wgt[:]))),
                     r=[t_r, t_route], dma=True)
                S.barrier()
            for i in range(NT):
                u = i % 4
                S.op("sp", lambda e, inc, i=i, u=u: inc(e.dma_start(out=xnb[u][:], in_=xn_d[i * 128:(i + 1) * 128, :])), r=[t_xnd[i]], w=[t_xnb[u]], dma=True)
                for a_ in range(2):
                    S.op("pool", lambda e, inc, i=i, u=u, a_=a_: inc(e.indirect_dma_start(
                        out=xg_d[:, :], out_offset=bass.IndirectOffsetOnAxis(ap=slot_i[:, a_, i:i + 1], axis=0),
                        in_=xnb[u][:], in_offset=None, bounds_check=BCREG, oob_is_err=False)),
                        r=[t_xnb[u], t_route], wd=[t_xg], dma=True)
            S.barrier()

        with ExitStack() as p6:
            stg6 = [sb(f"p6stg{i}", [128, 8, 512], F32, p6) for i in range(3)]
            t_stg6 = [S.tok("stg6") for _ in range(3)]
            w1b = [sb(f"p6w1{i}", [128, 8, 512], BF16, p6) for i in range(2)]
            w3b = [sb(f"p6w3{i}", [128, 8, 512], BF16, p6) for i in range(2)]
            w2b = [sb(f"p6w2{i}", [128, 4, D], BF16, p6) for i in range(2)]
            t_w1 = [S.tok("w1") for _ in range(2)]
            t_w3 = [S.tok("w3") for _ in range(2)]
            t_w2 = [S.tok("w2") for _ in range(2)]
            xg_s = [sb(f"p6xg{i}", [128, CT, D], BF16, p6) for i in range(2)]
            t_xgs = [S.tok("xgs") for _ in range(2)]
            xgT = [sb(f"p6xgT{i}", [128, 8, CAP], BF16, p6) for i in range(2)]
            t_xgT = [S.tok("xgT") for _ in range(2)]
            sil = [sb(f"p6sil{i}", [128, CAP], F32, p6) for i in range(2)]
            t_sil = [S.tok("sil") for _ in range(2)]
            hid = [sb(f"p6hid{i}", [128, 4, CAP], BF16, p6) for i in range(2)]
            t_hid = [S.tok("hid") for _ in range(2)]
            ysb = [sb(f"p6y{i}", [128, D], F32, p6) for i in range(2)]
            t_ysb = [S.tok("ysb") for _ in range(2)]
            w1_v = w1.rearrange("e (p k) c -> e p k c", k=8)
            w3_v = w3.rearrange("e (p k) c -> e p k c", k=8)
            w2_v = w2.rearrange("e (p k) c -> e p k c", k=4)
            stg2v = stg6[2][:].rearrange("p k c -> p (k c)").rearrange("p (k c) -> p k c", k=4)

            def load_expert(ex_):
                u = ex_ % 2
                S.op("sp", lambda e, inc: inc(e.dma_start(out=stg6[0][:], in_=w1_v[ex_])), w=[t_stg6[0]], dma=True)
                S.op("sp", lambda e, inc: inc(e.dma_start(out=stg6[1][:], in_=w3_v[ex_])), w=[t_stg6[1]], dma=True)
                S.op("sp", lambda e, inc: inc(e.dma_start(out=stg2v, in_=w2_v[ex_])), w=[t_stg6[2]], dma=True)
                S.op("sp", lambda e, inc: inc(e.dma_start(out=xg_s[u][:], in_=xg_d[ex_ * CAP:(ex_ + 1) * CAP, :].rearrange("(c p) d -> p c d", p=128))),
                     r=[t_xg], w=[t_xgs[u]], dma=True)

            def cast_expert(ex_):
                u = ex_ % 2
                S.op("dve", lambda e: e.tensor_copy(out=w1b[u][:].rearrange("p k (kk m) -> p k kk m", kk=4),
                                                    in_=stg6[0][:].rearrange("p k (m kk) -> p k kk m", kk=4)), r=[t_stg6[0]], w=[t_w1[u]])
                S.op("act", lambda e: e.activation(out=w3b[u][:].rearrange("p k (kk m) -> p k kk m", kk=4),
                                                   in_=stg6[1][:].rearrange("p k (m kk) -> p k kk m", kk=4), func=AF.Copy), r=[t_stg6[1]], w=[t_w3[u]])
                S.op("dve", lambda e: e.tensor_copy(out=w2b[u][:, 0:2, :], in_=stg2v[:, 0:2, :]), r=[t_stg6[2]], wd=[t_w2[u]])
                S.op("act", lambda e: e.activation(out=w2b[u][:, 2:4, :], in_=stg2v[:, 2:4, :], func=AF.Copy), r=[t_stg6[2]], wd=[t_w2[u]])

            load_expert(0)
            cast_expert(0)
            yc = 0
            for ex_ in range(NE):
                u = ex_ % 2
                if ex_ + 1 < NE:
                    load_expert(ex_ + 1)
                for c in range(CT):
                    for k in range(8):
                        bank = 6 + (k // 4) % 2
                        S.op("pe", lambda e, u=u, c=c, k=k, bank=bank: e.transpose(out=psb(bank)[:, (k % 4) * 128:(k % 4 + 1) * 128],
                                                                                   in_=xg_s[u][:, c, k * 128:(k + 1) * 128], identity=IDB),
                             r=[t_xgs[u]], w=[pst[bank]])
                        if k % 4 == 3:
                            kb = k - 3
                            if (k // 4) % 2 == 0:
                                S.op("dve", lambda e, u=u, c=c, kb=kb, bank=bank: e.tensor_copy(
                                    out=xgT[u][:, kb:kb + 4, c * 128:(c + 1) * 128], in_=psb(bank)[:, 0:512].rearrange("p (k t) -> p k t", k=4)),
                                    r=[pst[bank]], wd=[t_xgT[u]])
                            else:
                                S.op("act", lambda e, u=u, c=c, kb=kb, bank=bank: e.activation(
                                    out=xgT[u][:, kb:kb + 4, c * 128:(c + 1) * 128], in_=psb(bank)[:, 0:512].rearrange("p (k t) -> p k t", k=4), func=AF.Copy),
                                    r=[pst[bank]], wd=[t_xgT[u]])
                for fc in range(4):
                    pb0 = 2 * (fc % 2)
                    fs = slice(fc * 128, (fc + 1) * 128)
                    for k in range(8):
                        S.op("pe", mm(ps[pb0][:, 0:CAP], w1b[u][:, k, fs], xgT[u][:, k, :], k == 0, k == 7), r=[t_w1[u], t_xgT[u]], w=[pst[pb0]])
                    for k in range(8):
                        S.op("pe", mm(ps[pb0 + 1][:, 0:CAP], w3b[u][:, k, fs], xgT[u][:, k, :], k == 0, k == 7), r=[t_w3[u], t_xgT[u]], w=[pst[pb0 + 1]])
                    v_ = fc % 2
                    S.op("act", lambda e, v_=v_, pb0=pb0: e.activation(out=sil[v_][:], in_=ps[pb0][:, 0:CAP], func=AF.Silu), r=[pst[pb0]], w=[t_sil[v_]])
                    S.op("dve", lambda e, v_=v_, pb0=pb0, u=u, fc=fc: e.tensor_tensor(out=hid[u][:, fc, :], in0=ps[pb0 + 1][:, 0:CAP], in1=sil[v_][:], op=ALU.mult),
                         r=[pst[pb0 + 1], t_sil[v_]], wd=[t_hid[u]])
                for c in range(CT):
                    yu = yc % 2
                    yc += 1
                    for n in range(2):
                        bank = 4 + n
                        for k in range(4):
                            S.op("pe", mm(ps[bank][:, :], hid[u][:, k, c * 128:(c + 1) * 128], w2b[u][:, k, n * 512:(n + 1) * 512], k == 0, k == 3),
                                 r=[t_hid[u], t_w2[u]], w=[pst[bank]])
                        if n == 0:
                            S.op("act", lambda e, yu=yu, bank=bank: e.activation(out=ysb[yu][:, 0:512], in_=ps[bank][:, :], func=AF.Copy), r=[pst[bank]], wd=[t_ysb[yu]])
                        else:
                            S.op("dve", lambda e, yu=yu, bank=bank: e.tensor_copy(out=ysb[yu][:, 512:1024], in_=ps[bank][:, :]), r=[pst[bank]], wd=[t_ysb[yu]])
                    r0_ = ex_ * CAP + c * 128
                    S.op("sp", lambda e, inc, yu=yu, r0_=r0_: inc(e.dma_start(out=y_d[r0_:r0_ + 128, :], in_=ysb[yu][:])), r=[t_ysb[yu]], wd=[t_yd], dma=True)
                if ex_ + 1 < NE:
                    cast_expert(ex_ + 1)
            S.barrier()

        with ExitStack() as p7:
            NB7 = 4
            x2s = [sb(f"p7x{i}", [128, D], F32, p7) for i in range(NB7)]
            ya = [sb(f"p7ya{i}", [128, D], F32, p7) for i in range(NB7)]
            yb = [sb(f"p7yb{i}", [128, D], F32, p7) for i in range(NB7)]
            t_x2s = [S.tok("x2s") for _ in range(NB7)]
            t_ya = [S.tok("ya") for _ in range(NB7)]
            t_yb = [S.tok("yb") for _ in range(NB7)]
            for i in range(NT):
                u = i % NB7
                S.op("sp", lambda e, inc, i=i, u=u: inc(e.dma_start(out=x2s[u][:], in_=x2_d[i * 128:(i + 1) * 128, :])), r=[t_x2d[i]], w=[t_x2s[u]], dma=True)
                for a_, (yy, t_yy) in enumerate(((ya, t_ya), (yb, t_yb))):
                    S.op("pool", lambda e, inc, i=i, u=u, a_=a_, yy=yy: inc(e.indirect_dma_start(
                        out=yy[u][:], out_offset=None, in_=y_d[:, :],
                        in_offset=bass.IndirectOffsetOnAxis(ap=slot_i[:, a_, i:i + 1], axis=0), bounds_check=BCREG, oob_is_err=False)),
                        r=[t_yd, t_route], w=[t_yy[u]], dma=True)
                S.op("dve", lambda e, i=i, u=u: e.scalar_tensor_tensor(out=x2s[u][:], in0=ya[u][:], scalar=wgt[:, 0, i:i + 1], in1=x2s[u][:], op0=ALU.mult, op1=ALU.add),
                     r=[t_ya[u], t_x2s[u], t_route], w=[t_x2s[u]])
                S.op("dve", lambda e, i=i, u=u: e.scalar_tensor_tensor(out=x2s[u][:], in0=yb[u][:], scalar=wgt[:, 1, i:i + 1], in1=x2s[u][:], op0=ALU.mult, op1=ALU.add),
                     r=[t_yb[u], t_x2s[u], t_route], w=[t_x2s[u]])
                S.op("sp", lambda e, inc, i=i, u=u: inc(e.dma_start(out=out[i * 128:(i + 1) * 128, :], in_=x2s[u][:])), r=[t_x2s[u]], dma=True)
            S.barrier()
        S.barrier()
    return nc


def host_consts(T):
    CAP = cap_for(T)
    p = np.arange(128)
    cm = np.zeros((128, 7, 128), np.float32)
    cm[:, 0, :] = np.eye(128, dtype=np.float32)
    cm[:, 1, :] = ((p[:, None] // 64 == p[None, :] // 64) & (p[:, None] <= p[None, :])).astype(np.float32)
    cm[:, 2, :] = (p[:, None] <= p[None, :]).astype(np.float32)
    cm[:, 3, :] = (p[:, None] < p[None, :]).astype(np.float32)
    cm[:, 4, :] = 1.0
    cm[:, 5, :] = (p[:, None] // 64 == p[None, :] // 64).astype(np.float32)
    m = p
    src = np.where((m % 64) < 32, m + 32, m - 32)
    perm = np.zeros((128, 128), np.float32)
    perm[src, m] = 1.0
    cm[:, 6, :] = perm
    rmask = np.ones((128, 512), np.float32)
    rmask[:, ::64] = 0.0
    ecap = np.broadcast_to((np.arange(32, dtype=np.float32) * CAP)[None, :], (128, 32)).copy()
    half = 32
    inv = (np.float32(10000.0) ** (-np.arange(half, dtype=np.float32) / np.float32(half))).astype(np.float32)
    ang = np.arange(T, dtype=np.float32)[:, None] * inv[None, :]
    cos = np.cos(ang).astype(np.float32).T
    sin = np.sin(ang).astype(np.float32).T
    ropec = np.concatenate([cos, cos, cos, cos], axis=0)
    ropes = np.concatenate([-sin, sin, -sin, sin], axis=0)
    return dict(cmat=cm, rmask=rmask, ecap=ecap, ropec=np.ascontiguousarray(ropec), ropes=np.ascontiguousarray(ropes))


def host_layout(inp, T):
    f = lambda a: np.ascontiguousarray(np.asarray(a, dtype=np.float32))
    d = {}
    d["w_in"] = f(inp["w_in"][0])
    d["gmix"] = f(np.asarray(inp["norm_mix"][0]).reshape(8, 128).T)
    d["hglb"] = f(np.asarray(inp["hg_lb"]).reshape(2, 4, 128).transpose(2, 0, 1).reshape(128, 8))
    d["hgon"] = f(np.asarray(inp["hg_out_norm"][0]).reshape(128, 1))
    d["qkn"] = f(np.stack([np.tile(np.asarray(inp["da_q_norm"][0]), 2), np.tile(np.asarray(inp["da_k_norm"][0]), 2)], axis=1))
    d["lamb"] = f(np.broadcast_to(np.asarray(inp["da_lambda"][0]).reshape(1, 256), (128, 256)))
    d["daon"] = f(np.asarray(inp["da_out_norm"][0]).reshape(128, 1))
    d["wbh"] = f(inp["w_branch_hg"][0])
    d["wbd"] = f(inp["w_branch_da"][0])
    d["w_out"] = f(inp["w_out"][0])
    d["nmoe"] = f(np.broadcast_to(np.asarray(inp["norm_moe"][0]).reshape(1, D), (128, D)))
    d["wr"] = f(np.concatenate([np.asarray(inp["w_router_group"][0]), np.asarray(inp["w_router_expert"][0])], axis=1))
    d["br"] = f(np.broadcast_to(np.concatenate([np.asarray(inp["b_router_group"][0]),
                                                np.asarray(inp["b_router_expert"][0])]).reshape(1, 36), (128, 36)))
    d["w1"] = f(inp["w1"][0])
    d["w3"] = f(inp["w3"][0])
    d["w2"] = f(inp["w2"][0])
    d.update(host_consts(T))
    return d


_NC_CACHE = {}


def kernel(**inputs):
    xfull = np.asarray(inputs["x"], dtype=np.float32)
    B, T, _ = xfull.shape
    shared = host_layout(inputs, T)
    if T not in _NC_CACHE:
        _NC_CACHE[T] = build(T)
    nc = _NC_CACHE[T]
    in_maps = []
    for c in range(B):
        m = dict(shared)
        m["x"] = np.ascontiguousarray(xfull[c])
        in_maps.append(m)
    res = run_bass_kernel_spmd(nc, in_maps, core_ids=list(range(B)))
    return np.stack([np.asarray(r["out"], dtype=np.float32) for r in res.results], axis=0)
```

```python
import math
from contextlib import ExitStack

import numpy as np
import concourse.bass as bass
import concourse.mybir as mybir
from concourse.bass_utils import run_bass_kernel_spmd

F32 = mybir.dt.float32
BF16 = mybir.dt.bfloat16
I32 = mybir.dt.int32
AF = mybir.ActivationFunctionType
ALU = mybir.AluOpType
AX = mybir.AxisListType

D = 1024
IN_COLS = 5632
NE = 32
DFF = 512
EPS = 1e-6
ENGS = ("pe", "act", "dve", "pool", "sp")
SEM_LIMIT = 30000


class Tok:
    __slots__ = ("name", "writers", "rc", "rd")

    def __init__(self, name):
        self.name = name
        self.writers = []
        self.rc = {}
        self.rd = []

    def reset(self):
        self.writers = []
        self.rc = {}
        self.rd = []


class Op:
    __slots__ = ("eng", "fn", "dma", "deps", "signal", "sem", "val")


class Sched:
    def __init__(self, nc, es, n_dma=32, n_eng=5, n_sw=12):
        self.nc = nc
        self.e = {"pe": nc.tensor, "act": nc.scalar, "dve": nc.vector, "pool": nc.gpsimd, "sp": nc.sync}
        self.ops = []
        self.emitted = 0
        self.toks = []
        self.sems = {}
        for k in ("pe", "act", "dve", "pool"):
            for j in range(n_eng):
                self.sems[("e", k, j)] = es.enter_context(nc.semaphore(f"se_{k}{j}"))
        self.dsem_n = n_dma
        for j in range(n_dma):
            self.sems[("d", j)] = es.enter_context(nc.semaphore(f"sd_{j}"))
        self.eidx = {k: 0 for k in ENGS}
        self.ecount = {k: 0 for k in ENGS}
        self.n_eng = n_eng
        self.dtarget = [0] * (n_dma + n_sw)
        self.dnext = 0
        self.n_sw = n_sw
        self.swnext = 0
        for j in range(n_dma, n_dma + n_sw):
            self.sems[("d", j)] = es.enter_context(nc.semaphore(f"sw_{j}"))
        self.waited = {k: {} for k in ENGS}
        self.nwaits = 0

    def tok(self, name="t"):
        t = Tok(name)
        self.toks.append(t)
        return t

    def op(self, eng, fn, r=(), w=(), wd=(), dma=False):
        idx = len(self.ops)
        o = Op()
        o.eng, o.fn, o.dma, o.signal, o.sem, o.val = eng, fn, dma, False, None, 0
        deps = {}
        ops = self.ops

        def add(pidx, raw):
            p = ops[pidx]
            if p.eng == eng and not p.dma and not dma and eng == "pe":
                return
            deps[pidx] = True

        for t in r:
            for pw in t.writers:
                add(pw, True)
        for t in list(w) + list(wd):
            for pw in t.writers:
                add(pw, False)
            for pr in t.rc.values():
                add(pr, False)
            for pr in t.rd:
                add(pr, False)
        for t in r:
            if dma:
                t.rd.append(idx)
            else:
                t.rc[eng] = idx
        for t in w:
            t.writers = [idx]
            t.rc = {}
            t.rd = []
        for t in wd:
            t.writers.append(idx)
        o.deps = sorted(deps)
        ops.append(o)
        return idx

    def _wait(self, eng, key, val):
        if val <= 0 or key is None:
            return
        if self.waited[eng].get(key, 0) >= val:
            return
        self.e[eng].wait_ge(self.sems[key], val)
        self.waited[eng][key] = val
        self.nwaits += 1

    def flush(self):
        ops = self.ops
        for i in range(self.emitted, len(ops)):
            for d in ops[i].deps:
                ops[d].signal = True
        for i in range(self.emitted, len(ops)):
            o = ops[i]
            e = self.e[o.eng]
            for d in o.deps:
                self._wait(o.eng, ops[d].sem, ops[d].val)
            if o.dma:
                if o.eng == "pool":
                    k = self.dsem_n + self.swnext
                    self.swnext = (self.swnext + 1) % self.n_sw
                else:
                    k = self.dnext
                    self.dnext = (k + 1) % self.dsem_n
                key = ("d", k)
                self._wait(o.eng, key, self.dtarget[k])
                s = self.sems[key]
                cnt = [0]

                def inc(ins, s=s, cnt=cnt):
                    ins.then_inc(s, 16)
                    cnt[0] += 1
                    return ins

                o.fn(e, inc)
                self.dtarget[k] += 16 * cnt[0]
                o.sem, o.val = key, self.dtarget[k]
            else:
                ins = o.fn(e)
                if o.signal:
                    c = self.ecount[o.eng] + 1
                    if c > SEM_LIMIT:
                        self.eidx[o.eng] += 1
                        assert self.eidx[o.eng] < self.n_eng, "out of engine semaphores"
                        c = 1
                    self.ecount[o.eng] = c
                    o.sem, o.val = ("e", o.eng, self.eidx[o.eng]), c
                    ins.then_inc(self.sems[o.sem], 1)
            o.fn = None
        self.emitted = len(ops)

    def barrier(self):
        last = {}
        for i in range(self.emitted, len(self.ops)):
            o = self.ops[i]
            if not o.dma:
                last[o.eng] = i
        for i in last.values():
            self.ops[i].signal = True
        self.flush()
        for eng in ENGS:
            for i in last.values():
                self._wait(eng, self.ops[i].sem, self.ops[i].val)
            for k in range(self.dsem_n + self.n_sw):
                self._wait(eng, ("d", k), self.dtarget[k])
        for t in self.toks:
            t.reset()


def cap_for(T):
    return 128 * int(math.ceil((T / 16.0) * 1.5 / 128.0))


def build(T, stop_after=99, dbg=()):
    NT = T // 128
    NB = T // 512
    CAP = cap_for(T)
    CT = CAP // 128
    NSLOT = NE * CAP
    nc = bass.Bass("TRN2", target_bir_lowering=False)

    def din(name, shape, dt=F32):
        return nc.dram_tensor(name, list(shape), dt, kind="ExternalInput").ap()

    x = din("x", [T, D])
    w_in = din("w_in", [D, IN_COLS])
    gmix = din("gmix", [128, 8])
    hglb = din("hglb", [128, 8])
    hgon = din("hgon", [128, 1])
    qkn = din("qkn", [128, 2])
    lamb = din("lamb", [128, 256])
    daon = din("daon", [128, 1])
    wbh = din("wbh", [512, D])
    wbd = din("wbd", [512, D])
    w_out = din("w_out", [D, D])
    nmoe = din("nmoe", [128, D])
    wr = din("wr", [D, 36])
    br = din("br", [128, 36])
    w1 = din("w1", [NE, D, DFF])
    w3 = din("w3", [NE, D, DFF])
    w2 = din("w2", [NE, DFF, D])
    cmat = din("cmat", [128, 7, 128])
    rmask = din("rmask", [128, 512])
    ecap = din("ecap", [128, 32])
    ropec = din("ropec", [128, T])
    ropes = din("ropes", [128, T])
    out = nc.dram_tensor("out", [T, D], F32, kind="ExternalOutput").ap()
    x2_d = nc.dram_tensor("x2_d", [T, D], F32).ap()
    xn_d = nc.dram_tensor("xn_d", [T, D], BF16).ap()
    xg_d = nc.dram_tensor("xg_d", [NSLOT, D], BF16).ap()
    y_d = nc.dram_tensor("y_d", [NSLOT, D], F32).ap()
    hT_d = nc.dram_tensor("hT_d", [128, 8, T], BF16).ap()
    ohg_d = nc.dram_tensor("dbg_ohg" if "ohg" in dbg else "ohg_d", [128, 4, T], BF16, kind="ExternalOutput" if "ohg" in dbg else "Internal").ap()
    oda_d = nc.dram_tensor("dbg_oda" if "oda" in dbg else "oda_d", [128, 4, T], BF16, kind="ExternalOutput" if "oda" in dbg else "Internal").ap()
    dbg_t = {}
    if "ht" in dbg:
        dbg_t["ht"] = nc.dram_tensor("dbg_ht", [128, 8, T], F32, kind="ExternalOutput").ap()
    if "x2" in dbg:
        dbg_t["x2"] = nc.dram_tensor("dbg_x2", [T, D], F32, kind="ExternalOutput").ap()
        dbg_t["lg"] = nc.dram_tensor("dbg_lg", [128, NT, 36], F32, kind="ExternalOutput").ap()
    if "rt" in dbg:
        dbg_t["rt"] = nc.dram_tensor("dbg_rt", [128, 4, NT], F32, kind="ExternalOutput").ap()

    w_in_v = w_in.rearrange("(k p) c -> p k c", p=128)

    with ExitStack() as es:
        S = Sched(nc, es)

        def sb(name, shape, dt, stack=es):
            return stack.enter_context(nc.sbuf_tensor(name, list(shape), dt))

        ps = [es.enter_context(nc.psum_tensor(f"ps{i}", [128, 512], F32)) for i in range(8)]
        pst = [S.tok(f"ps{i}") for i in range(8)]

        def psb(i):
            return ps[i][:].bitcast(BF16)

        cm_f = sb("cm_f", [128, 7, 128], F32)
        cm_b = sb("cm_b", [128, 7, 128], BF16)
        rmask_s = sb("rmask_s", [128, 512], F32)
        gmix_s = sb("gmix_s", [128, 8], F32)
        hglb_s = sb("hglb_s", [128, 8], F32)
        lbv = sb("lbv", [128, 12], F32)
        hgon_s = sb("hgon_s", [128, 1], F32)
        qkn_s = sb("qkn_s", [128, 2], F32)
        lamb_s = sb("lamb_s", [128, 256], F32)
        lam_s = sb("lam_s", [128, 8], F32)
        daon_s = sb("daon_s", [128, 2], F32)
        dmy = sb("dmy", [128, 2], F32)
        t_c = S.tok("consts")

        def ld_consts(e, inc):
            inc(e.dma_start(out=cm_f[:], in_=cmat))
            inc(e.dma_start(out=rmask_s[:], in_=rmask))
            inc(e.dma_start(out=gmix_s[:], in_=gmix))
            inc(e.dma_start(out=hglb_s[:], in_=hglb))
            inc(e.dma_start(out=hgon_s[:], in_=hgon))
            inc(e.dma_start(out=qkn_s[:], in_=qkn))
            inc(e.dma_start(out=lamb_s[:], in_=lamb))
            inc(e.dma_start(out=daon_s[:, 0:1], in_=daon))

        S.op("sp", ld_consts, w=[t_c], dma=True)
        S.op("dve", lambda e: e.tensor_copy(out=cm_b[:], in_=cm_f[:]), r=[t_c], w=[t_c])
        S.op("dve", lambda e: e.tensor_sub(out=lbv[:, 8:12], in0=hglb_s[:, 0:4], in1=hglb_s[:, 4:8]), r=[t_c], w=[t_c])
        S.op("act", lambda e: e.activation(out=lbv[:, 8:12], in_=lbv[:, 8:12], func=AF.Exp), r=[t_c], w=[t_c])
        S.op("dve", lambda e: e.tensor_scalar_add(out=lbv[:, 8:12], in0=lbv[:, 8:12], scalar1=1.0), r=[t_c], w=[t_c])
        S.op("dve", lambda e: e.reciprocal(out=lbv[:, 0:4], in_=lbv[:, 8:12]), r=[t_c], w=[t_c])
        S.op("dve", lambda e: e.tensor_scalar_mul(out=lbv[:, 4:8], in0=lbv[:, 0:4], scalar1=-1.0), r=[t_c], w=[t_c])
        S.op("dve", lambda e: e.tensor_mul(out=lamb_s[:, 0:64], in0=lamb_s[:, 0:64], in1=lamb_s[:, 64:128]), r=[t_c], w=[t_c])
        S.op("dve", lambda e: e.tensor_mul(out=lamb_s[:, 128:192], in0=lamb_s[:, 128:192], in1=lamb_s[:, 192:256]), r=[t_c], w=[t_c])
        S.op("dve", lambda e: e.reduce_sum(out=lam_s[:, 0:1], in_=lamb_s[:, 0:64], axis=AX.X), r=[t_c], w=[t_c])
        S.op("dve", lambda e: e.reduce_sum(out=lam_s[:, 1:2], in_=lamb_s[:, 128:192], axis=AX.X), r=[t_c], w=[t_c])
        S.op("act", lambda e: e.activation(out=lam_s[:, 0:2], in_=lam_s[:, 0:2], func=AF.Exp), r=[t_c], w=[t_c])
        S.op("dve", lambda e: e.tensor_sub(out=lam_s[:, 2:3], in0=lam_s[:, 1:2], in1=lam_s[:, 0:1]), r=[t_c], w=[t_c])
        S.op("dve", lambda e: e.tensor_scalar_add(out=lam_s[:, 2:3], in0=lam_s[:, 2:3], scalar1=-0.2), r=[t_c], w=[t_c])
        S.op("dve", lambda e: e.tensor_scalar_mul(out=daon_s[:, 1:2], in0=daon_s[:, 0:1], scalar1=0.8), r=[t_c], w=[t_c])
        S.op("dve", lambda e: e.memset(lam_s[:, 4:5], EPS), w=[t_c])
        S.op("dve", lambda e: e.memset(lam_s[:, 5:6], 1.0), w=[t_c])
        S.op("dve", lambda e: e.memset(dmy[:], 0.0), w=[t_c])
        S.barrier()

        EPSB = lam_s[:, 4:5]
        ONEB = lam_s[:, 5:6]
        IDF = cm_f[:, 0, :]
        IDB = cm_b[:, 0, :]
        CMASK = cm_b[:, 1, :]
        TRI01 = cm_b[:, 2, :]
        LSTR = cm_b[:, 3, :]
        ONESB = cm_b[:, 4, :]
        BD64 = cm_b[:, 5, :]
        PERM = cm_b[:, 6, :]

        slot_i = sb("slot_i", [128, 2, NT], I32)
        wgt = sb("wgt", [128, 2, NT], F32)
        t_route = S.tok("route")
        t_x2d = [S.tok("x2d") for _ in range(NT)]
        t_xnd = [S.tok("xnd") for _ in range(NT)]
        t_xg = S.tok("xg")
        t_yd = S.tok("yd")
        t_hTd = S.tok("hTd")
        t_ohgd = [[S.tok("ohgd") for _ in range(NB)] for _ in range(4)]
        t_odad = [[S.tok("odad") for _ in range(NB)] for _ in range(4)]
        zt = sb("zt", [128, D], BF16)
        t_zt = S.tok("zt")
        S.op("dve", lambda e: e.memset(zt[:], 0.0), w=[t_zt])
        es_h = ExitStack()
        hT = sb("hT", [128, 8, T], BF16, es_h)
        t_hT = [S.tok(f"hT{i}") for i in range(NB)]

        with ExitStack() as p1:
            xt = [sb(f"p1x{i}", [128, D], F32, p1) for i in range(2)]
            xs = [sb(f"p1xs{i}", [128, D], BF16, p1) for i in range(2)]
            junk = sb("p1junk", [128, D], F32, p1)
            st = [sb(f"p1st{i}", [128, 2], F32, p1) for i in range(2)]
            t_xt = [S.tok("xt") for _ in range(2)]
            t_xs = [S.tok("xs") for _ in range(2)]
            t_st = [S.tok("st") for _ in range(2)]
            t_junk = S.tok("junk")
            for i in range(NT):
                b = i % 2
                S.op("sp", lambda e, inc, i=i, b=b: inc(e.dma_start(out=xt[b][:], in_=x[i * 128:(i + 1) * 128, :])),
                     w=[t_xt[b]], dma=True)
                S.op("act", lambda e, b=b: e.activation(out=junk[:], in_=xt[b][:], func=AF.Square), r=[t_xt[b]], w=[t_junk])
                S.op("dve", lambda e, b=b: e.reduce_sum(out=st[b][:, 0:1], in_=junk[:], axis=AX.X), r=[t_junk], w=[t_st[b]])
                S.op("act", lambda e, b=b: e.activation(out=st[b][:, 1:2], in_=st[b][:, 0:1], func=AF.Ln, scale=1.0 / D, bias=EPSB),
                     r=[t_st[b]], w=[t_st[b]])
                S.op("act", lambda e, b=b: e.activation(out=st[b][:, 1:2], in_=st[b][:, 1:2], func=AF.Exp, scale=-0.5),
                     r=[t_st[b]], w=[t_st[b]])
                S.op("dve", lambda e, b=b: e.tensor_scalar(out=xs[b][:], in0=xt[b][:], scalar1=st[b][:, 1:2], scalar2=None,
                                                           op0=ALU.mult), r=[t_st[b], t_xt[b]], w=[t_xs[b]])
                pb = 0 + (i % 2)
                for k in range(8):
                    S.op("pe", lambda e, k=k, b=b, pb=pb: e.transpose(out=psb(pb)[:, k * 128:(k + 1) * 128],
                                                                       in_=xs[b][:, k * 128:(k + 1) * 128], identity=IDB),
                         r=[t_xs[b]], w=[pst[pb]])
                S.op("dve", lambda e, i=i, pb=pb: e.tensor_tensor(
                    out=hT[:, :, i * 128:(i + 1) * 128],
                    in0=psb(pb).rearrange("p (k t) -> p k t", k=8),
                    in1=gmix_s[:].unsqueeze(2).to_broadcast([128, 8, 128]), op=ALU.mult),
                    r=[pst[pb]], w=[t_hT[i // 4]])
            S.op("sp", lambda e, inc: inc(e.dma_start(out=hT_d, in_=hT[:])), r=t_hT, w=[t_hTd], dma=True)
            S.barrier()
        if "ht" in dbg:
            with ExitStack() as pd:
                tmp = sb("dbgtmp", [128, 8, T], F32, pd)
                tt = S.tok("dbgtmp")
                S.op("dve", lambda e: e.tensor_copy(out=tmp[:], in_=hT[:]), r=t_hT, w=[tt])
                S.op("sp", lambda e, inc: inc(e.dma_start(out=dbg_t["ht"], in_=tmp[:])), r=[tt], dma=True)
                S.barrier()
        if stop_after <= 1:
            S.barrier()
            es_h.close()
            return nc

        def mm(out_, lhsT, rhs, start, stop):
            return lambda e: e.matmul(out_, lhsT, rhs, start=start, stop=stop)

        def act_accum(e, **kw):
            e.activation(**kw)
            return e.activation(out=dmy[:, 0:1], in_=dmy[:, 1:2], func=AF.Copy)

        dump_names = []
        if "dump" in dbg:
            dbg_t["dump"] = nc.dram_tensor("dbg_dump", [24, 128, 512], F32, kind="ExternalOutput").ap()

        def dump(name, ap, toks):
            if "dump" not in dbg or len(dump_names) >= 24:
                return
            k = len(dump_names)
            dump_names.append(name)
            S.op("sp", lambda e, inc, k=k, ap=ap: inc(e.dma_start(out=dbg_t["dump"][k][:, 0:ap.shape[1]], in_=ap)), r=toks, dma=True)

        with ExitStack() as p2:
            wst = sb("p2wst", [128, 8, 512], F32, p2)
            t_wst = S.tok("wst")
            wq = [sb(f"p2wq{i}", [128, 8, 512], BF16, p2) for i in range(2)]
            t_wq = [S.tok("wq") for _ in range(2)]
            state = [sb(f"p2state{i}", [128, 128], F32, p2) for i in range(2)]
            t_state = [S.tok("state") for _ in range(2)]
            stbf = [[sb(f"p2stbf{i}_{j}", [128, 128], BF16, p2) for j in range(2)] for i in range(2)]
            t_stbf = [[S.tok("stbf") for _ in range(2)] for _ in range(2)]
            def mk(name, dt, n=2, shape=(128, 512)):
                return [sb(f"p2{name}{i}", list(shape), dt, p2) for i in range(n)], [S.tok(name) for _ in range(n)]
            v_tm, t_v = mk("v", BF16)
            q_f, t_q = mk("q", F32)
            ef, t_ef = mk("ef", F32)
            e2f, t_e2 = mk("e2", F32)
            kk, t_kk = mk("kk", F32)
            gg, t_g = mk("g", F32)
            bcs, t_b = mk("b", F32)
            bm, t_bm = mk("bm", F32)
            E1, t_E1 = mk("E1", F32)
            E2, t_E2 = mk("E2", F32)
            E3, t_E3 = mk("E3", F32)
            qrel, t_qrel = mk("qrel", BF16)
            qb, t_qb = mk("qb", BF16)
            krel, t_krel = mk("krel", BF16)
            sog, t_sog = mk("sog", F32)
            o_f, t_of = mk("of", F32)
            sqb, t_sqb = mk("sqb", BF16)
            ob2, t_ob2 = mk("ob2", BF16)
            rst, t_rst = mk("rst", F32)
            kt, t_kt = mk("kt", BF16, 4, (128, 128))
            stm, t_stm = mk("stm", BF16, 4, (128, 128))
            sidx = [0, 0]
            RB = [(4, 5, 6, 7), (0, 1, 2, 3)]

            def prep(h, b, u):
                tb = slice(b * 512, (b + 1) * 512)
                for j, bank in ((0, 0), (1, 1), (3, 2)):
                    for k in range(8):
                        S.op("pe", mm(ps[bank][:, :], wq[u][:, k, j * 128:(j + 1) * 128], hT[:, k, tb], k == 0, k == 7),
                             r=[t_wq[u], t_hT[b]], w=[pst[bank]])
                for ti in range(4):
                    for k in range(8):
                        S.op("pe", mm(ps[3][:, ti * 128:(ti + 1) * 128], hT[:, k, b * 512 + ti * 128: b * 512 + (ti + 1) * 128],
                                      wq[u][:, k, 256:384], k == 0, k == 7), r=[t_wq[u], t_hT[b]], w=[pst[3]])
                S.op("act", lambda e: e.activation(out=v_tm[u][:], in_=ps[3][:, :], func=AF.Copy), r=[pst[3]], w=[t_v[u]])
                S.op("act", lambda e: e.activation(out=q_f[u][:], in_=ps[0][:, :], func=AF.Copy), r=[pst[0]], w=[t_q[u]])
                S.op("act", lambda e: e.activation(out=ef[u][:], in_=ps[1][:, :], func=AF.Exp, scale=-1.0), r=[pst[1]], w=[t_ef[u]])
                S.op("act", lambda e: e.activation(out=e2f[u][:], in_=ps[2][:, :], func=AF.Exp, scale=-1.0), r=[pst[2]], w=[t_e2[u]])
                S.op("act", lambda e: e.activation(out=ef[u][:], in_=ef[u][:], func=AF.Ln, bias=ONEB), r=[t_ef[u]], w=[t_ef[u]])
                S.op("act", lambda e: e.activation(out=ef[u][:], in_=ef[u][:], func=AF.Exp, scale=-1.0), r=[t_ef[u]], w=[t_ef[u]])
                S.op("dve", lambda e: e.tensor_scalar(out=kk[u][:], in0=ef[u][:], scalar1=lbv[:, 4 + h:5 + h], scalar2=lbv[:, h:h + 1],
                                                      op0=ALU.mult, op1=ALU.add), r=[t_ef[u]], w=[t_kk[u]])
                S.op("act", lambda e: e.activation(out=gg[u][:], in_=kk[u][:], func=AF.Ln, scale=-1.0, bias=ONEB), r=[t_kk[u]], w=[t_g[u]])
                S.op("dve", lambda e: e.tensor_tensor_scan(out=bcs[u][:], data0=rmask_s[:], data1=gg[u][:], initial=0.0,
                                                           op0=ALU.mult, op1=ALU.add), r=[t_g[u]], w=[t_b[u]])
                S.op("dve", lambda e: e.tensor_tensor(
                    out=bm[u][:].rearrange("p (c t) -> p c t", t=64),
                    in0=bcs[u][:].rearrange("p (c t) -> p c t", t=64),
                    in1=bcs[u][:].rearrange("p (c t) -> p c t", t=64)[:, :, 31:32].to_broadcast([128, 8, 64]),
                    op=ALU.subtract), r=[t_b[u]], w=[t_bm[u]])
                S.op("act", lambda e: e.activation(out=E1[u][:], in_=bm[u][:], func=AF.Exp), r=[t_bm[u]], w=[t_E1[u]])
                S.op("act", lambda e: e.activation(out=E2[u][:], in_=bm[u][:], func=AF.Exp, scale=-1.0), r=[t_bm[u]], w=[t_E2[u]])
                S.op("act", lambda e: e.activation(out=E3[u][:], in_=bcs[u][:], func=AF.Exp), r=[t_b[u]], w=[t_E3[u]])
                S.op("dve", lambda e: e.tensor_tensor(out=qrel[u][:], in0=q_f[u][:], in1=E1[u][:], op=ALU.mult),
                     r=[t_q[u], t_E1[u]], w=[t_qrel[u]])
                S.op("dve", lambda e: e.tensor_tensor(out=qb[u][:], in0=q_f[u][:], in1=E3[u][:], op=ALU.mult),
                     r=[t_q[u], t_E3[u]], w=[t_qb[u]])
                S.op("dve", lambda e: e.tensor_tensor(out=krel[u][:], in0=kk[u][:], in1=E2[u][:], op=ALU.mult),
                     r=[t_kk[u], t_E2[u]], w=[t_krel[u]])
                S.op("act", lambda e: e.activation(out=e2f[u][:], in_=e2f[u][:], func=AF.Ln, bias=ONEB), r=[t_e2[u]], w=[t_e2[u]])
                S.op("act", lambda e: e.activation(out=e2f[u][:], in_=e2f[u][:], func=AF.Exp, scale=-1.0), r=[t_e2[u]], w=[t_e2[u]])
                S.op("dve", lambda e: e.tensor_tensor(out=sog[u][:], in0=ps[2][:, :], in1=e2f[u][:], op=ALU.mult),
                     r=[pst[2], t_e2[u]], w=[t_sog[u]])

            def tile_pre(u, ti):
                btr, bsT, boT, bdS = RB[u]
                tsl = slice(ti * 128, (ti + 1) * 128)
                w_ = u * 2 + ti % 2
                S.op("pe", lambda e: e.transpose(out=psb(btr)[:, 0:128], in_=krel[u][:, tsl], identity=IDB), r=[t_krel[u]], w=[pst[btr]])
                S.op("act", lambda e: e.activation(out=kt[w_][:], in_=psb(btr)[:, 0:128], func=AF.Copy), r=[pst[btr]], w=[t_kt[w_]])
                S.op("pe", mm(ps[bsT][:, 0:128], krel[u][:, tsl], qrel[u][:, tsl], True, True), r=[t_krel[u], t_qrel[u]], w=[pst[bsT]])
                S.op("dve", lambda e: e.tensor_tensor(out=stm[w_][:], in0=ps[bsT][:, 0:128], in1=CMASK, op=ALU.mult), r=[pst[bsT]], w=[t_stm[w_]])
                S.op("pe", mm(ps[boT][:, 0:128], v_tm[u][:, tsl], stm[w_][:], True, False), r=[t_v[u], t_stm[w_]], w=[pst[boT]])

            def chunk(u, ti, half):
                btr, bsT, boT, bdS = RB[u]
                tsl = slice(ti * 128, (ti + 1) * 128)
                w_ = u * 2 + ti % 2
                c = ti * 2 + half
                csl = slice(ti * 128 + half * 64, ti * 128 + half * 64 + 64)
                pr = slice(half * 64, half * 64 + 64)
                cur = sidx[u] % 2
                S.op("pe", mm(ps[boT][:, half * 64:half * 64 + 64], stbf[u][cur][:], qb[u][:, csl], False, half == 1),
                     r=[t_stbf[u][cur], t_qb[u]], w=[pst[boT]])
                S.op("pe", mm(ps[bdS][:, half * 128:half * 128 + 128], kt[w_][pr, :], v_tm[u][pr, tsl], True, True),
                     r=[t_kt[w_], t_v[u]], w=[pst[bdS]])
                e5 = E3[u][:, c * 64 + 63:c * 64 + 64]
                c1 = E1[u][:, c * 64 + 63:c * 64 + 64]
                S.op("dve", lambda e: e.tensor_scalar(out=state[u][:], in0=state[u][:], scalar1=e5, scalar2=None, op0=ALU.mult),
                     r=[t_state[u], t_E3[u]], w=[t_state[u]])
                S.op("dve", lambda e: e.scalar_tensor_tensor(
                    out=state[u][:], in0=ps[bdS][:, half * 128:half * 128 + 128], scalar=c1, in1=state[u][:], op0=ALU.mult, op1=ALU.add),
                    r=[t_state[u], t_E1[u], pst[bdS]], w=[t_state[u]])
                sidx[u] += 1
                nxt = sidx[u] % 2
                S.op("act", lambda e: e.activation(out=stbf[u][nxt][:], in_=state[u][:], func=AF.Copy), r=[t_state[u]], w=[t_stbf[u][nxt]])

            def tile_post(u, ti):
                btr, bsT, boT, bdS = RB[u]
                tsl = slice(ti * 128, (ti + 1) * 128)
                S.op("dve", lambda e: e.tensor_copy(out=o_f[u][:, tsl], in_=ps[boT][:, 0:128]), r=[pst[boT]], w=[t_of[u]])

            def post(h, b, u):
                btr, bsT, boT, bdS = RB[u]
                tb = slice(b * 512, (b + 1) * 512)
                S.op("act", lambda e: e.activation(out=sqb[u][:], in_=o_f[u][:], func=AF.Square), r=[t_of[u]], w=[t_sqb[u]])
                S.op("pe", mm(ps[bsT][:, :], ONESB, sqb[u][:], True, True), r=[t_sqb[u]], w=[pst[bsT]])
                S.op("act", lambda e: e.activation(out=rst[u][:], in_=ps[bsT][:, :], func=AF.Ln, scale=1.0 / 128, bias=EPSB), r=[pst[bsT]], w=[t_rst[u]])
                S.op("act", lambda e: e.activation(out=rst[u][:], in_=rst[u][:], func=AF.Exp, scale=-0.5), r=[t_rst[u]], w=[t_rst[u]])
                S.op("dve", lambda e: e.tensor_tensor(out=o_f[u][:], in0=o_f[u][:], in1=rst[u][:], op=ALU.mult), r=[t_of[u], t_rst[u]], w=[t_of[u]])
                S.op("dve", lambda e: e.scalar_tensor_tensor(out=ob2[u][:], in0=o_f[u][:], scalar=hgon_s[:, 0:1], in1=sog[u][:],
                                                             op0=ALU.mult, op1=ALU.mult), r=[t_of[u], t_sog[u]], w=[t_ob2[u]])
                S.op("sp", lambda e, inc: inc(e.dma_start(out=ohg_d[:, h, tb], in_=ob2[u][:])), r=[t_ob2[u]], w=[t_ohgd[h][b]], dma=True)

            for hp in range(2):
                for u in range(2):
                    h = 2 * hp + u
                    def ldw(e, inc, h=h):
                        for j in range(4):
                            inc(e.dma_start(out=wst[:, :, j * 128:(j + 1) * 128],
                                            in_=w_in_v[:, :, j * 512 + h * 128: j * 512 + (h + 1) * 128]))
                    S.op("sp", ldw, w=[t_wst], dma=True)
                    if h == 0:
                        for ex_ in range(NE):
                            S.op("sp", lambda e, inc, ex_=ex_: inc(e.dma_start(
                                out=xg_d[ex_ * CAP:(ex_ + 1) * CAP, :].rearrange("(c p) d -> p c d", p=128),
                                in_=zt[:].unsqueeze(1).to_broadcast([128, CT, D]))), r=[t_zt], wd=[t_xg], dma=True)
                    S.op("dve", lambda e, u=u: e.tensor_copy(out=wq[u][:], in_=wst[:]), r=[t_wst], w=[t_wq[u]])
                    S.op("dve", lambda e, u=u: e.memset(state[u][:], 0.0), w=[t_state[u]])
                    S.op("dve", lambda e, u=u, c0=sidx[u] % 2: e.memset(stbf[u][c0][:], 0.0), w=[t_stbf[u][sidx[u] % 2]])
                for b in range(NB):
                    for u in range(2):
                        prep(2 * hp + u, b, u)
                    for ti in range(4):
                        for u in range(2):
                            tile_pre(u, ti)
                        for half in range(2):
                            for u in range(2):
                                chunk(u, ti, half)
                        for u in range(2):
                            tile_post(u, ti)
                    for u in range(2):
                        post(2 * hp + u, b, u)
            S.barrier()
        if "ht2" in dbg:
            dbg_t["ht2"] = nc.dram_tensor("dbg_ht2", [128, 8, T], F32, kind="ExternalOutput").ap()
            with ExitStack() as pd:
                tmp = sb("dbgtmp3", [128, 8, T], F32, pd)
                tt = S.tok("dbgtmp3")
                S.op("dve", lambda e: e.tensor_copy(out=tmp[:], in_=hT[:]), r=t_hT, w=[tt])
                S.op("sp", lambda e, inc: inc(e.dma_start(out=dbg_t["ht2"], in_=tmp[:])), r=[tt], dma=True)
                S.barrier()
        if stop_after <= 2:
            S.barrier()
            es_h.close()
            return nc

        with ExitStack() as p4:
            ropec_s = sb("p4rc", [128, T], F32, p4)
            ropes_s = sb("p4rs", [128, T], F32, p4)
            t_rope = S.tok("rope")
            S.op("sp", lambda e, inc: (inc(e.dma_start(out=ropec_s[:], in_=ropec)), inc(e.dma_start(out=ropes_s[:], in_=ropes))),
                 w=[t_rope], dma=True)
            wst = sb("p4wst", [128, 8, 384], F32, p4)
            wa = sb("p4wa", [128, 8, 384], BF16, p4)
            t_wst, t_wa = S.tok("wst4"), S.tok("wa4")
            qT_s = sb("p4qT", [128, T], BF16, p4)
            kT_s = sb("p4kT", [128, T], BF16, p4)
            vt = sb("p4v", [128, NT, 128], BF16, p4)
            t_qT = [S.tok("qT") for _ in range(NB)]
            t_kT = [S.tok("kT") for _ in range(NB)]
            t_vt = [S.tok("vt") for _ in range(NB)]
            def mk4(name, dt, n=2, shape=(128, 512)):
                return [sb(f"p4{name}{i}", list(shape), dt, p4) for i in range(n)], [S.tok(name) for _ in range(n)]
            sq4, t_sq4 = mk4("sq", BF16)
            qf4, t_qf4 = mk4("qf", F32)
            rs4, t_rs4 = mk4("rs", F32)
            qn4, t_qn4 = mk4("qn", BF16)
            t14, t_t14 = mk4("t1", F32)
            t24, t_t24 = mk4("t2", F32)
            Pm, t_P = mk4("P", BF16, 4)
            r0, t_r0 = mk4("r0", F32, 1)
            r1, t_r1 = mk4("r1", F32, 1)
            o1, t_o1 = mk4("o1", F32, 1)
            o2, t_o2 = mk4("o2", F32, 1)
            ob4, t_ob4 = mk4("ob4", BF16, 2)
            cnt4 = 0
            for h in range(4):
                def ldw4(e, inc, h=h):
                    for j in range(3):
                        inc(e.dma_start(out=wst[:, :, j * 128:(j + 1) * 128],
                                        in_=w_in_v[:, :, 2048 + j * 512 + h * 128: 2048 + j * 512 + (h + 1) * 128]))
                S.op("sp", ldw4, w=[t_wst], dma=True)
                S.op("dve", lambda e: e.tensor_copy(out=wa[:], in_=wst[:]), r=[t_wst], w=[t_wa])
                units = [(b, j) for b in range(NB) for j in range(2)]

                def stA(n):
                    b, j = units[n]
                    u = n % 2
                    tb = slice(b * 512, (b + 1) * 512)
                    for k in range(8):
                        S.op("pe", mm(ps[u][:, :], wa[:, k, j * 128:(j + 1) * 128], hT[:, k, tb], k == 0, k == 7),
                             r=[t_wa, t_hT[b]], w=[pst[u]])
                    S.op("act", lambda e: e.activation(out=sq4[u][:], in_=ps[u][:, :], func=AF.Square), r=[pst[u]], w=[t_sq4[u]])
                    S.op("act", lambda e: e.activation(out=qf4[u][:], in_=ps[u][:, :], func=AF.Copy), r=[pst[u]], w=[t_qf4[u]])
                    if j == 0:
                        for ti in range(4):
                            for k in range(8):
                                S.op("pe", mm(ps[4][:, ti * 128:(ti + 1) * 128], hT[:, k, b * 512 + ti * 128: b * 512 + (ti + 1) * 128],
                                              wa[:, k, 256:384], k == 0, k == 7), r=[t_wa, t_hT[b]], w=[pst[4]])
                        S.op("act", lambda e: e.activation(out=vt[:, b * 4:(b + 1) * 4, :], in_=ps[4][:, :].rearrange("p (a c) -> p a c", a=4),
                                                           func=AF.Copy), r=[pst[4]], w=[t_vt[b]])

                def stB(n):
                    b, j = units[n]
                    u = n % 2
                    S.op("pe", mm(ps[2][:, :], BD64, sq4[u][:], True, True), r=[t_sq4[u]], w=[pst[2]])
                    S.op("act", lambda e: e.activation(out=rs4[u][:], in_=ps[2][:, :], func=AF.Ln, scale=1.0 / 64, bias=EPSB), r=[pst[2]], w=[t_rs4[u]])
                    S.op("act", lambda e: e.activation(out=rs4[u][:], in_=rs4[u][:], func=AF.Exp, scale=-0.5), r=[t_rs4[u]], w=[t_rs4[u]])
                    S.op("dve", lambda e: e.scalar_tensor_tensor(out=qn4[u][:], in0=qf4[u][:], scalar=qkn_s[:, j:j + 1], in1=rs4[u][:],
                                                                 op0=ALU.mult, op1=ALU.mult), r=[t_qf4[u], t_rs4[u]], w=[t_qn4[u]])

                def stC(n):
                    b, j = units[n]
                    u = n % 2
                    tb = slice(b * 512, (b + 1) * 512)
                    dst, t_dst = (qT_s, t_qT) if j == 0 else (kT_s, t_kT)
                    S.op("pe", mm(ps[3][:, :], PERM, qn4[u][:], True, True), r=[t_qn4[u]], w=[pst[3]])
                    S.op("dve", lambda e: e.tensor_tensor(out=t14[u][:], in0=qn4[u][:], in1=ropec_s[:, tb], op=ALU.mult),
                         r=[t_qn4[u], t_rope], w=[t_t14[u]])
                    S.op("dve", lambda e: e.tensor_tensor(out=t24[u][:], in0=ps[3][:, :], in1=ropes_s[:, tb], op=ALU.mult),
                         r=[pst[3], t_rope], w=[t_t24[u]])
                    S.op("dve", lambda e: e.tensor_tensor(out=dst[:, tb], in0=t14[u][:], in1=t24[u][:], op=ALU.add),
                         r=[t_t14[u], t_t24[u]], w=[t_dst[b]])

                NU = len(units)
                for n in range(NU + 2):
                    if n < NU:
                        stA(n)
                    if 0 <= n - 1 < NU:
                        stB(n - 1)
                    if 0 <= n - 2 < NU:
                        stC(n - 2)
                S.barrier()
                steps = [(j, i) for j in range(NB) for i in range(4 * j + 4)]

                def qk(s):
                    j, i = steps[s]
                    r_ = i - 4 * j
                    col0 = 128 * r_ if r_ > 0 else 0
                    for m in range(2):
                        bank = m * 2 + (s % 2)
                        S.op("pe", mm(ps[bank][:, col0:512], kT_s[m * 64:(m + 1) * 64, i * 128:(i + 1) * 128],
                                      qT_s[m * 64:(m + 1) * 64, j * 512 + col0:(j + 1) * 512], True, True),
                             r=[t_kT[i // 4], t_qT[j]], w=[pst[bank]])

                qk(0)
                for s in range(len(steps)):
                    j, i = steps[s]
                    r_ = i - 4 * j
                    col0 = 128 * r_ if r_ > 0 else 0
                    last = (i == 4 * j + 3)
                    for m in range(2):
                        bank = m * 2 + (s % 2)
                        pb_ = m * 2 + (s % 2)
                        S.op("act", lambda e, bank=bank, pb_=pb_, col0=col0: e.activation(out=Pm[pb_][:, col0:512], in_=ps[bank][:, col0:512],
                                                                                           func=AF.Exp, scale=0.125),
                             r=[pst[bank]], w=[t_P[pb_]])
                    if s + 1 < len(steps):
                        qk(s + 1)
                    for m in range(2):
                        pb_ = m * 2 + (s % 2)
                        if r_ >= 0:
                            S.op("dve", lambda e, pb_=pb_, col0=col0: e.tensor_tensor(out=Pm[pb_][:, col0:col0 + 128], in0=Pm[pb_][:, col0:col0 + 128],
                                                                                       in1=TRI01, op=ALU.mult), r=[t_P[pb_]], w=[t_P[pb_]])
                        S.op("pe", mm(ps[4 + m][:, col0:512], vt[:, i, :], Pm[pb_][:, col0:512], i == 0, last), r=[t_vt[i // 4], t_P[pb_]], w=[pst[4 + m]])
                        S.op("pe", mm(ps[6 + m][:, col0:512], ONESB, Pm[pb_][:, col0:512], i == 0, last), r=[t_P[pb_]], w=[pst[6 + m]])
                    if last:
                        jb = slice(j * 512, (j + 1) * 512)
                        S.op("dve", lambda e: e.tensor_copy(out=o1[0][:], in_=ps[4][:, :]), r=[pst[4]], w=[t_o1[0]])
                        S.op("dve", lambda e: e.tensor_copy(out=o2[0][:], in_=ps[5][:, :]), r=[pst[5]], w=[t_o2[0]])
                        S.op("act", lambda e: e.activation(out=r0[0][:], in_=ps[6][:, :], func=AF.Ln), r=[pst[6]], w=[t_r0[0]])
                        S.op("act", lambda e: e.activation(out=r1[0][:], in_=ps[7][:, :], func=AF.Ln), r=[pst[7]], w=[t_r1[0]])
                        S.op("act", lambda e: e.activation(out=r0[0][:], in_=r0[0][:], func=AF.Exp, scale=-1.0), r=[t_r0[0]], w=[t_r0[0]])
                        S.op("act", lambda e: e.activation(out=r1[0][:], in_=r1[0][:], func=AF.Exp, scale=-1.0), r=[t_r1[0]], w=[t_r1[0]])
                        S.op("dve", lambda e: e.tensor_tensor(out=o1[0][:], in0=o1[0][:], in1=r0[0][:], op=ALU.mult), r=[t_o1[0], t_r0[0]], w=[t_o1[0]])
                        S.op("dve", lambda e: e.tensor_tensor(out=o2[0][:], in0=o2[0][:], in1=r1[0][:], op=ALU.mult), r=[t_o2[0], t_r1[0]], w=[t_o2[0]])
                        S.op("dve", lambda e: e.scalar_tensor_tensor(out=o1[0][:], in0=o2[0][:], scalar=lam_s[:, 2:3], in1=o1[0][:], op0=ALU.mult, op1=ALU.add),
                             r=[t_o1[0], t_o2[0]], w=[t_o1[0]])
                        S.op("act", lambda e: e.activation(out=sq4[0][:], in_=o1[0][:], func=AF.Square), r=[t_o1[0]], w=[t_sq4[0]])
                        S.op("pe", mm(ps[6][:, :], ONESB, sq4[0][:], True, True), r=[t_sq4[0]], w=[pst[6]])
                        S.op("act", lambda e: e.activation(out=rs4[0][:], in_=ps[6][:, :], func=AF.Ln, scale=1.0 / 128, bias=EPSB), r=[pst[6]], w=[t_rs4[0]])
                        S.op("act", lambda e: e.activation(out=rs4[0][:], in_=rs4[0][:], func=AF.Exp, scale=-0.5), r=[t_rs4[0]], w=[t_rs4[0]])
                        ou = j % 2
                        S.op("dve", lambda e, ou=ou: e.scalar_tensor_tensor(out=ob4[ou][:], in0=o1[0][:], scalar=daon_s[:, 1:2], in1=rs4[0][:],
                                                                            op0=ALU.mult, op1=ALU.mult), r=[t_o1[0], t_rs4[0]], w=[t_ob4[ou]])
                        S.op("sp", lambda e, inc, ou=ou, h=h, jb=jb: inc(e.dma_start(out=oda_d[:, h, jb], in_=ob4[ou][:])), r=[t_ob4[ou]], w=[t_odad[h][j]], dma=True)
                S.barrier()
        es_h.close()
        if stop_after <= 4:
            S.barrier()
            return nc

        BCREG = nc.gpsimd.to_reg(NSLOT - 1)
        logit = sb("logit", [128, NT, 36], F32)
        t_logit = S.tok("logit")
        with ExitStack() as p5:
            wbh_s = sb("p5wbh", [128, 4, D], BF16, p5)
            wbd_s = sb("p5wbd", [128, 4, D], BF16, p5)
            wg_s = sb("p5wg", [128, 8, 2048], BF16, p5)
            wo_s = sb("p5wo", [128, 8, D], BF16, p5)
            nm_s = sb("p5nm", [128, D], F32, p5)
            wr_s = sb("p5wr", [128, 8, 36], F32, p5)
            br_s = sb("p5br", [128, 36], F32, p5)
            t_w5 = S.tok("w5")
            wbh_v = wbh.rearrange("(k p) c -> p k c", p=128)
            wbd_v = wbd.rearrange("(k p) c -> p k c", p=128)
            wo_v = w_out.rearrange("(k p) c -> p k c", p=128)
            stg = [sb(f"p5stg{i}", [128, 8, 512], F32, p5) for i in range(2)]
            t_stg = [S.tok("stg") for _ in range(2)]
            t_wbh = [S.tok("wbh") for _ in range(2)]
            t_wbd = [S.tok("wbd") for _ in range(2)]
            t_wo = [S.tok("wo") for _ in range(2)]
            t_wg = [S.tok("wg") for _ in range(4)]
            sgc = [0]
            def ldcast(src_ap, dst_ap, kk_, tk):
                g = sgc[0] % 2
                sgc[0] += 1
                S.op("sp", lambda e, inc, g=g: inc(e.dma_start(out=stg[g][:, 0:kk_, :], in_=src_ap)), w=[t_stg[g]], dma=True)
                if g == 0:
                    S.op("dve", lambda e, g=g: e.tensor_copy(out=dst_ap, in_=stg[g][:, 0:kk_, :]), r=[t_stg[g]], w=[tk])
                else:
                    S.op("act", lambda e, g=g: e.activation(out=dst_ap, in_=stg[g][:, 0:kk_, :], func=AF.Copy), r=[t_stg[g]], w=[tk])
            def ld_wg(n):
                ldcast(w_in_v[:, :, 3584 + n * 512: 3584 + (n + 1) * 512], wg_s[:, :, n * 512:(n + 1) * 512], 8, t_wg[n])
            for n in range(2):
                ldcast(wbh_v[:, :, n * 512:(n + 1) * 512], wbh_s[:, :, n * 512:(n + 1) * 512], 4, t_wbh[n])
                ldcast(wbd_v[:, :, n * 512:(n + 1) * 512], wbd_s[:, :, n * 512:(n + 1) * 512], 4, t_wbd[n])
                ld_wg(n)
                ld_wg(2 + n)
            for n in range(2):
                ldcast(wo_v[:, :, n * 512:(n + 1) * 512], wo_s[:, :, n * 512:(n + 1) * 512], 8, t_wo[n])
            S.op("sp", lambda e, inc: (inc(e.dma_start(out=nm_s[:], in_=nmoe)), inc(e.dma_start(out=wr_s[:], in_=wr.rearrange("(k p) c -> p k c", p=128))),
                                       inc(e.dma_start(out=br_s[:], in_=br))), w=[t_w5], dma=True)
            hTb = [sb(f"p5hT{i}", [128, 8, 512], BF16, p5) for i in range(2)]
            ohb = [sb(f"p5oh{i}", [128, 4, 512], BF16, p5) for i in range(2)]
            odb = [sb(f"p5od{i}", [128, 4, 512], BF16, p5) for i in range(2)]
            t_hTb = [S.tok("hTb") for _ in range(2)]
            t_ohb = [S.tok("ohb") for _ in range(2)]
            t_odb = [S.tok("odb") for _ in range(2)]
            mixT = [sb(f"p5mix{i}", [128, 8, 512], BF16, p5) for i in range(2)]
            t_mix = [S.tok("mix") for _ in range(2)]
            def mk5(name, dt, n=2, shape=(128, 512)):
                return [sb(f"p5{name}{i}", list(shape), dt, p5) for i in range(n)], [S.tok(name) for _ in range(n)]
            s1b, t_s1 = mk5("s1", F32)
            s2b, t_s2 = mk5("s2", F32)
            m1b, t_m1 = mk5("m1", F32, 1)
            m2b, t_m2 = mk5("m2", F32, 1)
            xt5, t_xt5 = mk5("xt", F32, 2, (128, D))
            x2b, t_x2 = mk5("x2", F32, 1, (128, D))
            xnf, t_xnf = mk5("xnf", F32, 1, (128, D))
            xnb, t_xnb = mk5("xnb", BF16, 2, (128, D))
            xnT, t_xnT = mk5("xnT", F32, 1, (128, D))
            sq5, t_sq5 = mk5("sq", F32, 1, (128, D))
            st5, t_st5 = mk5("st", F32, 2, (128, 2))
            cc = 0
            for b in range(NB):
                tb = slice(b * 512, (b + 1) * 512)
                mb = b % 2
                S.op("sp", lambda e, inc, mb=mb, tb=tb: inc(e.dma_start(out=hTb[mb][:], in_=hT_d[:, :, tb])), r=[t_hTd], w=[t_hTb[mb]], dma=True)
                S.op("sp", lambda e, inc, mb=mb, tb=tb: inc(e.dma_start(out=ohb[mb][:], in_=ohg_d[:, :, tb])), r=[t_ohgd[k][b] for k in range(4)], w=[t_ohb[mb]], dma=True)
                S.op("sp", lambda e, inc, mb=mb, tb=tb: inc(e.dma_start(out=odb[mb][:], in_=oda_d[:, :, tb])), r=[t_odad[k][b] for k in range(4)], w=[t_odb[mb]], dma=True)
                for c in range(8):
                    u = cc % 2
                    cc += 1
                    cs = slice(c * 128, (c + 1) * 128)
                    pb0 = 4 * u
                    for k in range(4):
                        S.op("pe", mm(ps[pb0][:, :], wbh_s[:, k, cs], ohb[mb][:, k, :], k == 0, k == 3), r=[t_wbh[c // 4], t_ohb[mb]], w=[pst[pb0]])
                    for k in range(4):
                        S.op("pe", mm(ps[pb0 + 1][:, :], wbd_s[:, k, cs], odb[mb][:, k, :], k == 0, k == 3), r=[t_wbd[c // 4], t_odb[mb]], w=[pst[pb0 + 1]])
                    for k in range(8):
                        S.op("pe", mm(ps[pb0 + 2][:, :], wg_s[:, k, c * 128:(c + 1) * 128], hTb[mb][:, k, :], k == 0, k == 7), r=[t_wg[c // 4], t_hTb[mb]], w=[pst[pb0 + 2]])
                    for k in range(8):
                        S.op("pe", mm(ps[pb0 + 3][:, :], wg_s[:, k, 1024 + c * 128:1024 + (c + 1) * 128], hTb[mb][:, k, :], k == 0, k == 7),
                             r=[t_wg[2 + c // 4], t_hTb[mb]], w=[pst[pb0 + 3]])
                    S.op("act", lambda e, u=u, pb0=pb0: e.activation(out=s1b[u][:], in_=ps[pb0 + 2][:, :], func=AF.Sigmoid), r=[pst[pb0 + 2]], w=[t_s1[u]])
                    S.op("act", lambda e, u=u, pb0=pb0: e.activation(out=s2b[u][:], in_=ps[pb0 + 3][:, :], func=AF.Sigmoid), r=[pst[pb0 + 3]], w=[t_s2[u]])
                    S.op("dve", lambda e, u=u, pb0=pb0: e.tensor_tensor(out=m1b[0][:], in0=ps[pb0][:, :], in1=s1b[u][:], op=ALU.mult), r=[pst[pb0], t_s1[u]], w=[t_m1[0]])
                    S.op("dve", lambda e, u=u, pb0=pb0: e.tensor_tensor(out=m2b[0][:], in0=ps[pb0 + 1][:, :], in1=s2b[u][:], op=ALU.mult), r=[pst[pb0 + 1], t_s2[u]], w=[t_m2[0]])
                    S.op("dve", lambda e, mb=mb, c=c: e.tensor_tensor(out=mixT[mb][:, c, :], in0=m1b[0][:], in1=m2b[0][:], op=ALU.add),
                         r=[t_m1[0], t_m2[0]], wd=[t_mix[mb]])
                for ti in range(4):
                    i = b * 4 + ti
                    u = i % 2
                    if i == 0:
                        S.op("sp", lambda e, inc: inc(e.dma_start(out=xt5[0][:], in_=x[0:128, :])), w=[t_xt5[0]], dma=True)
                    if i + 1 < NT:
                        S.op("sp", lambda e, inc, i=i: inc(e.dma_start(out=xt5[(i + 1) % 2][:], in_=x[(i + 1) * 128:(i + 2) * 128, :])), w=[t_xt5[(i + 1) % 2]], dma=True)
                    for n in range(2):
                        for k in range(8):
                            S.op("pe", mm(ps[n][:, :], mixT[mb][:, k, ti * 128:(ti + 1) * 128], wo_s[:, k, n * 512:(n + 1) * 512], k == 0, k == 7),
                                 r=[t_mix[mb], t_wo[n]], w=[pst[n]])
                        S.op("dve", lambda e, u=u, n=n: e.tensor_tensor(out=x2b[0][:, n * 512:(n + 1) * 512], in0=ps[n][:, :], in1=xt5[u][:, n * 512:(n + 1) * 512],
                                                                       op=ALU.add), r=[pst[n], t_xt5[u]], wd=[t_x2[0]])
                    S.op("sp", lambda e, inc, i=i: inc(e.dma_start(out=x2_d[i * 128:(i + 1) * 128, :], in_=x2b[0][:])), r=[t_x2[0]], w=[t_x2d[i]], dma=True)
                    if "x2" in dbg:
                        S.op("sp", lambda e, inc, i=i: inc(e.dma_start(out=dbg_t["x2"][i * 128:(i + 1) * 128, :], in_=x2b[0][:])), r=[t_x2[0]], dma=True)
                    S.op("act", lambda e: e.activation(out=sq5[0][:], in_=x2b[0][:], func=AF.Square), r=[t_x2[0]], w=[t_sq5[0]])
                    S.op("dve", lambda e, u=u: e.reduce_sum(out=st5[u][:, 0:1], in_=sq5[0][:], axis=AX.X), r=[t_sq5[0]], w=[t_st5[u]])
                    S.op("act", lambda e, u=u: e.activation(out=st5[u][:, 1:2], in_=st5[u][:, 0:1], func=AF.Ln, scale=1.0 / D, bias=EPSB), r=[t_st5[u]], w=[t_st5[u]])
                    S.op("act", lambda e, u=u: e.activation(out=st5[u][:, 1:2], in_=st5[u][:, 1:2], func=AF.Exp, scale=-0.5), r=[t_st5[u]], w=[t_st5[u]])
                    S.op("dve", lambda e, u=u: e.scalar_tensor_tensor(out=xnf[0][:], in0=x2b[0][:], scalar=st5[u][:, 1:2], in1=nm_s[:], op0=ALU.mult, op1=ALU.mult),
                         r=[t_x2[0], t_st5[u], t_w5], w=[t_xnf[0]])
                    S.op("act", lambda e, u=u: e.activation(out=xnb[u][:].rearrange("p (k j) -> p k j", k=8), in_=xnf[0][:].rearrange("p (j k) -> p k j", k=8),
                                                            func=AF.Copy), r=[t_xnf[0]], w=[t_xnb[u]])
                    S.op("sp", lambda e, inc, i=i, u=u: inc(e.dma_start(out=xn_d[i * 128:(i + 1) * 128, :], in_=xnb[u][:])), r=[t_xnb[u]], w=[t_xnd[i]], dma=True)
                    for k in range(8):
                        bank = 2 + k // 4
                        S.op("pe", lambda e, k=k, bank=bank: e.transpose(out=ps[bank][:, (k % 4) * 128:(k % 4 + 1) * 128],
                                                                       in_=xnf[0][:, k * 128:(k + 1) * 128], identity=IDF),
                             r=[t_xnf[0]], w=[pst[bank]])
                    S.op("act", lambda e: e.activation(out=xnT[0][:, 0:512], in_=ps[2][:, :], func=AF.Copy), r=[pst[2]], wd=[t_xnT[0]])
                    S.op("dve", lambda e: e.tensor_copy(out=xnT[0][:, 512:1024], in_=ps[3][:, :]), r=[pst[3]], wd=[t_xnT[0]])
                    for k in range(8):
                        S.op("pe", mm(ps[2][:, 0:36], xnT[0][:, k * 128:(k + 1) * 128], wr_s[:, k, :], k == 0, k == 7), r=[t_xnT[0], t_w5], w=[pst[2]])
                    S.op("dve", lambda e, i=i: e.tensor_tensor(out=logit[:, i, :], in0=ps[2][:, 0:36], in1=br_s[:], op=ALU.add), r=[pst[2], t_w5], wd=[t_logit])
            if "x2" in dbg:
                S.op("sp", lambda e, inc: inc(e.dma_start(out=dbg_t["lg"], in_=logit[:])), r=[t_logit], dma=True)
            S.barrier()
        with ExitStack() as p5:
            ecap_s = sb("p5ecap", [128, 32], F32, p5)
            xnb = [sb(f"p5cxnb{i}", [128, D], BF16, p5) for i in range(4)]
            t_xnb = [S.tok("cxnb") for _ in range(4)]
            t_w5 = S.tok("w5b")
            S.op("sp", lambda e, inc: inc(e.dma_start(out=ecap_s[:], in_=ecap)), w=[t_w5], dma=True)
            def rb(name, shape, dt=F32):
                return sb("r_" + name, shape, dt, p5)
            t_r = S.tok("router")
            G = logit[:, :, 0:4]
            E4 = logit[:, :, 4:36].rearrange("p n (g j) -> p n g j", g=4)
            gmax = rb("gmax", [128, NT]); goh = rb("goh", [128, NT, 4]); gsh = rb("gsh", [128, NT, 4]); gsum = rb("gsum", [128, NT])
            gw = rb("gw", [128, NT]); sel = rb("sel", [128, NT, 4, 8]); eg = rb("eg", [128, NT, 8]); m1 = rb("m1", [128, NT])
            oh1 = rb("oh1", [128, NT, 8]); eg2 = rb("eg2", [128, NT, 8]); m2 = rb("m2", [128, NT]); oh2 = rb("oh2", [128, NT, 8])
            dd = rb("dd", [128, NT]); ex = rb("ex", [128, NT]); den = rb("den", [128, NT])
            A1 = rb("A1", [128, NT, 4, 8]); A2 = rb("A2", [128, NT, 4, 8]); Ab = rb("Ab", [128, NT, 32], BF16)
            rk = rb("rk", [128, NT, 32]); tmp5 = rb("tmp5", [128, NT, 32]); sl = rb("sl", [128, 2, NT])
            def R_(eng, fn):
                S.op(eng, fn, r=[t_r, t_logit], w=[t_r])
            R_("dve", lambda e: e.tensor_reduce(out=gmax[:], in_=G, axis=AX.X, op=ALU.max))
            R_("dve", lambda e: e.tensor_tensor(out=goh[:], in0=G, in1=gmax[:].unsqueeze(2).to_broadcast([128, NT, 4]), op=ALU.is_equal))
            R_("dve", lambda e: e.tensor_tensor(out=gsh[:], in0=G, in1=gmax[:].unsqueeze(2).to_broadcast([128, NT, 4]), op=ALU.subtract))
            R_("act", lambda e: e.activation(out=gsh[:], in_=gsh[:], func=AF.Exp))
            R_("dve", lambda e: e.tensor_reduce(out=gsum[:], in_=gsh[:], axis=AX.X, op=ALU.add))
            R_("dve", lambda e: e.reciprocal(out=gw[:], in_=gsum[:]))
            R_("dve", lambda e: e.tensor_tensor(out=sel[:], in0=E4, in1=goh[:].unsqueeze(3).to_broadcast([128, NT, 4, 8]), op=ALU.mult))
            R_("dve", lambda e: e.tensor_reduce(out=eg[:], in_=sel[:].rearrange("p n g j -> p n j g"), axis=AX.X, op=ALU.add))
            R_("dve", lambda e: e.tensor_reduce(out=m1[:], in_=eg[:], axis=AX.X, op=ALU.max))
            R_("dve", lambda e: e.tensor_tensor(out=oh1[:], in0=eg[:], in1=m1[:].unsqueeze(2).to_broadcast([128, NT, 8]), op=ALU.is_equal))
            R_("dve", lambda e: e.scalar_tensor_tensor(out=eg2[:], in0=oh1[:], scalar=-1e30, in1=eg[:], op0=ALU.mult, op1=ALU.add))
            R_("dve", lambda e: e.tensor_reduce(out=m2[:], in_=eg2[:], axis=AX.X, op=ALU.max))
            R_("dve", lambda e: e.tensor_tensor(out=oh2[:], in0=eg2[:], in1=m2[:].unsqueeze(2).to_broadcast([128, NT, 8]), op=ALU.is_equal))
            R_("dve", lambda e: e.tensor_sub(out=dd[:], in0=m2[:], in1=m1[:]))
            R_("act", lambda e: e.activation(out=ex[:], in_=dd[:], func=AF.Exp))
            R_("dve", lambda e: e.tensor_scalar_add(out=den[:], in0=ex[:], scalar1=1.0))
            R_("dve", lambda e: e.reciprocal(out=den[:], in_=den[:]))
            S.op("dve", lambda e: e.tensor_mul(out=wgt[:, 0, :], in0=den[:], in1=gw[:]), r=[t_r], w=[t_r, t_route])
            S.op("dve", lambda e: e.tensor_mul(out=wgt[:, 1, :], in0=wgt[:, 0, :], in1=ex[:]), r=[t_r, t_route], w=[t_r, t_route])
            R_("dve", lambda e: e.tensor_tensor(out=A1[:], in0=goh[:].unsqueeze(3).to_broadcast([128, NT, 4, 8]),
                                                in1=oh1[:].unsqueeze(2).to_broadcast([128, NT, 4, 8]), op=ALU.mult))
            R_("dve", lambda e: e.tensor_tensor(out=A2[:], in0=goh[:].unsqueeze(3).to_broadcast([128, NT, 4, 8]),
                                                in1=oh2[:].unsqueeze(2).to_broadcast([128, NT, 4, 8]), op=ALU.mult))
            R_("dve", lambda e: e.tensor_tensor(out=Ab[:], in0=A1[:].rearrange("p n g j -> p n (g j)"), in1=A2[:].rearrange("p n g j -> p n (g j)"), op=ALU.add))
            for i in range(NT):
                bank = i // 16
                oc = slice((i % 16) * 32, (i % 16) * 32 + 32)
                S.op("pe", mm(ps[bank][:, oc], LSTR, Ab[:, i, :], True, i == 0), r=[t_r], w=[pst[bank]])
                for i2 in range(i):
                    S.op("pe", mm(ps[bank][:, oc], ONESB, Ab[:, i2, :], False, i2 == i - 1), r=[t_r], w=[pst[bank]])
            nb_ = (NT + 15) // 16
            for bank in range(nb_):
                n0 = bank * 16
                n1 = min(NT, n0 + 16)
                S.op("dve", lambda e, bank=bank, n0=n0, n1=n1: e.tensor_tensor(
                    out=rk[:, n0:n1, :], in0=ps[bank][:, 0:(n1 - n0) * 32].rearrange("p (n e) -> p n e", e=32),
                    in1=ecap_s[:].unsqueeze(1).to_broadcast([128, n1 - n0, 32]), op=ALU.add), r=[pst[bank], t_r, t_w5], w=[t_r])
            for a_, Aa in ((0, A1), (1, A2)):
                R_("dve", lambda e, Aa=Aa: e.tensor_tensor(out=tmp5[:], in0=rk[:], in1=Aa[:].rearrange("p n g j -> p n (g j)"), op=ALU.mult))
                R_("dve", lambda e, a_=a_: e.tensor_reduce(out=sl[:, a_, :], in_=tmp5[:], axis=AX.X, op=ALU.add))
            S.op("dve", lambda e: e.tensor_copy(out=slot_i[:], in_=sl[:]), r=[t_r], w=[t_route])
            S.barrier()
            if "rt" in dbg:
                S.op("sp", lambda e, inc: (inc(e.dma_start(out=dbg_t["rt"][:, 0:2, :], in_=sl[:])), inc(e.dma_start(out=dbg_t["rt"][:, 2:4, :], in_=wgt[:]))),
                     r=[t_r, t_route], dma=True)
                S.barrier()
            for i in range(NT):
                u = i % 4
                S.op("sp", lambda e, inc, i=i, u=u: inc(e.dma_start(out=xnb[u][:], in_=xn_d[i * 128:(i + 1) * 128, :])), r=[t_xnd[i]], w=[t_xnb[u]], dma=True)
                for a_ in range(2):
                    S.op("pool", lambda e, inc, i=i, u=u, a_=a_: inc(e.indirect_dma_start(
                        out=xg_d[:, :], out_offset=bass.IndirectOffsetOnAxis(ap=slot_i[:, a_, i:i + 1], axis=0),
                        in_=xnb[u][:], in_offset=None, bounds_check=BCREG, oob_is_err=False)),
                        r=[t_xnb[u], t_route], wd=[t_xg], dma=True)
            S.barrier()

        with ExitStack() as p6:
            stg6 = [sb(f"p6stg{i}", [128, 8, 512], F32, p6) for i in range(3)]
            t_stg6 = [S.tok("stg6") for _ in range(3)]
            w1b = [sb(f"p6w1{i}", [128, 8, 512], BF16, p6) for i in range(2)]
            w3b = [sb(f"p6w3{i}", [128, 8, 512], BF16, p6) for i in range(2)]
            w2b = [sb(f"p6w2{i}", [128, 4, D], BF16, p6) for i in range(2)]
            t_w1 = [S.tok("w1") for _ in range(2)]
            t_w3 = [S.tok("w3") for _ in range(2)]
            t_w2 = [S.tok("w2") for _ in range(2)]
            xg_s = [sb(f"p6xg{i}", [128, CT, D], BF16, p6) for i in range(2)]
            t_xgs = [S.tok("xgs") for _ in range(2)]
            xgT = [sb(f"p6xgT{i}", [128, 8, CAP], BF16, p6) for i in range(2)]
            t_xgT = [S.tok("xgT") for _ in range(2)]
            sil = [sb(f"p6sil{i}", [128, CAP], F32, p6) for i in range(2)]
            t_sil = [S.tok("sil") for _ in range(2)]
            hid = [sb(f"p6hid{i}", [128, 4, CAP], BF16, p6) for i in range(2)]
            t_hid = [S.tok("hid") for _ in range(2)]
            ysb = [sb(f"p6y{i}", [128, D], F32, p6) for i in range(2)]
            t_ysb = [S.tok("ysb") for _ in range(2)]
            w1_v = w1.rearrange("e (p k) c -> e p k c", k=8)
            w3_v = w3.rearrange("e (p k) c -> e p k c", k=8)
            w2_v = w2.rearrange("e (p k) c -> e p k c", k=4)
            stg2v = stg6[2][:].rearrange("p k c -> p (k c)").rearrange("p (k c) -> p k c", k=4)

            def load_expert(ex_):
                u = ex_ % 2
                S.op("sp", lambda e, inc: inc(e.dma_start(out=stg6[0][:], in_=w1_v[ex_])), w=[t_stg6[0]], dma=True)
                S.op("sp", lambda e, inc: inc(e.dma_start(out=stg6[1][:], in_=w3_v[ex_])), w=[t_stg6[1]], dma=True)
                S.op("sp", lambda e, inc: inc(e.dma_start(out=stg2v, in_=w2_v[ex_])), w=[t_stg6[2]], dma=True)
                S.op("sp", lambda e, inc: inc(e.dma_start(out=xg_s[u][:], in_=xg_d[ex_ * CAP:(ex_ + 1) * CAP, :].rearrange("(c p) d -> p c d", p=128))),
                     r=[t_xg], w=[t_xgs[u]], dma=True)

            def cast_expert(ex_):
                u = ex_ % 2
                S.op("dve", lambda e: e.tensor_copy(out=w1b[u][:].rearrange("p k (kk m) -> p k kk m", kk=4),
                                                    in_=stg6[0][:].rearrange("p k (m kk) -> p k kk m", kk=4)), r=[t_stg6[0]], w=[t_w1[u]])
                S.op("act", lambda e: e.activation(out=w3b[u][:].rearrange("p k (kk m) -> p k kk m", kk=4),
                                                   in_=stg6[1][:].rearrange("p k (m kk) -> p k kk m", kk=4), func=AF.Copy), r=[t_stg6[1]], w=[t_w3[u]])
                S.op("dve", lambda e: e.tensor_copy(out=w2b[u][:, 0:2, :], in_=stg2v[:, 0:2, :]), r=[t_stg6[2]], wd=[t_w2[u]])
                S.op("act", lambda e: e.activation(out=w2b[u][:, 2:4, :], in_=stg2v[:, 2:4, :], func=AF.Copy), r=[t_stg6[2]], wd=[t_w2[u]])

            load_expert(0)
            cast_expert(0)
            yc = 0
            for ex_ in range(NE):
                u = ex_ % 2
                if ex_ + 1 < NE:
                    load_expert(ex_ + 1)
                for c in range(CT):
                    for k in range(8):
                        bank = 6 + (k // 4) % 2
                        S.op("pe", lambda e, u=u, c=c, k=k, bank=bank: e.transpose(out=psb(bank)[:, (k % 4) * 128:(k % 4 + 1) * 128],
                                                                                   in_=xg_s[u][:, c, k * 128:(k + 1) * 128], identity=IDB),
                             r=[t_xgs[u]], w=[pst[bank]])
                        if k % 4 == 3:
                            kb = k - 3
                            if (k // 4) % 2 == 0:
                                S.op("dve", lambda e, u=u, c=c, kb=kb, bank=bank: e.tensor_copy(
                                    out=xgT[u][:, kb:kb + 4, c * 128:(c + 1) * 128], in_=psb(bank)[:, 0:512].rearrange("p (k t) -> p k t", k=4)),
                                    r=[pst[bank]], wd=[t_xgT[u]])
                            else:
                                S.op("act", lambda e, u=u, c=c, kb=kb, bank=bank: e.activation(
                                    out=xgT[u][:, kb:kb + 4, c * 128:(c + 1) * 128], in_=psb(bank)[:, 0:512].rearrange("p (k t) -> p k t", k=4), func=AF.Copy),
                                    r=[pst[bank]], wd=[t_xgT[u]])
                for fc in range(4):
                    pb0 = 2 * (fc % 2)
                    fs = slice(fc * 128, (fc + 1) * 128)
                    for k in range(8):
                        S.op("pe", mm(ps[pb0][:, 0:CAP], w1b[u][:, k, fs], xgT[u][:, k, :], k == 0, k == 7), r=[t_w1[u], t_xgT[u]], w=[pst[pb0]])
                    for k in range(8):
                        S.op("pe", mm(ps[pb0 + 1][:, 0:CAP], w3b[u][:, k, fs], xgT[u][:, k, :], k == 0, k == 7), r=[t_w3[u], t_xgT[u]], w=[pst[pb0 + 1]])
                    v_ = fc % 2
                    S.op("act", lambda e, v_=v_, pb0=pb0: e.activation(out=sil[v_][:], in_=ps[pb0][:, 0:CAP], func=AF.Silu), r=[pst[pb0]], w=[t_sil[v_]])
                    S.op("dve", lambda e, v_=v_, pb0=pb0, u=u, fc=fc: e.tensor_tensor(out=hid[u][:, fc, :], in0=ps[pb0 + 1][:, 0:CAP], in1=sil[v_][:], op=ALU.mult),
                         r=[pst[pb0 + 1], t_sil[v_]], wd=[t_hid[u]])
                for c in range(CT):
                    yu = yc % 2
                    yc += 1
                    for n in range(2):
                        bank = 4 + n
                        for k in range(4):
                            S.op("pe", mm(ps[bank][:, :], hid[u][:, k, c * 128:(c + 1) * 128], w2b[u][:, k, n * 512:(n + 1) * 512], k == 0, k == 3),
                                 r=[t_hid[u], t_w2[u]], w=[pst[bank]])
                        if n == 0:
                            S.op("act", lambda e, yu=yu, bank=bank: e.activation(out=ysb[yu][:, 0:512], in_=ps[bank][:, :], func=AF.Copy), r=[pst[bank]], wd=[t_ysb[yu]])
                        else:
                            S.op("dve", lambda e, yu=yu, bank=bank: e.tensor_copy(out=ysb[yu][:, 512:1024], in_=ps[bank][:, :]), r=[pst[bank]], wd=[t_ysb[yu]])
                    r0_ = ex_ * CAP + c * 128
                    S.op("sp", lambda e, inc, yu=yu, r0_=r0_: inc(e.dma_start(out=y_d[r0_:r0_ + 128, :], in_=ysb[yu][:])), r=[t_ysb[yu]], wd=[t_yd], dma=True)
                if ex_ + 1 < NE:
                    cast_expert(ex_ + 1)
            S.barrier()

        with ExitStack() as p7:
            NB7 = 4
            x2s = [sb(f"p7x{i}", [128, D], F32, p7) for i in range(NB7)]
            ya = [sb(f"p7ya{i}", [128, D], F32, p7) for i in range(NB7)]
            yb = [sb(f"p7yb{i}", [128, D], F32, p7) for i in range(NB7)]
            t_x2s = [S.tok("x2s") for _ in range(NB7)]
            t_ya = [S.tok("ya") for _ in range(NB7)]
            t_yb = [S.tok("yb") for _ in range(NB7)]
            for i in range(NT):
                u = i % NB7
                S.op("sp", lambda e, inc, i=i, u=u: inc(e.dma_start(out=x2s[u][:], in_=x2_d[i * 128:(i + 1) * 128, :])), r=[t_x2d[i]], w=[t_x2s[u]], dma=True)
                for a_, (yy, t_yy) in enumerate(((ya, t_ya), (yb, t_yb))):
                    S.op("pool", lambda e, inc, i=i, u=u, a_=a_, yy=yy: inc(e.indirect_dma_start(
                        out=yy[u][:], out_offset=None, in_=y_d[:, :],
                        in_offset=bass.IndirectOffsetOnAxis(ap=slot_i[:, a_, i:i + 1], axis=0), bounds_check=BCREG, oob_is_err=False)),
                        r=[t_yd, t_route], w=[t_yy[u]], dma=True)
                S.op("dve", lambda e, i=i, u=u: e.scalar_tensor_tensor(out=x2s[u][:], in0=ya[u][:], scalar=wgt[:, 0, i:i + 1], in1=x2s[u][:], op0=ALU.mult, op1=ALU.add),
                     r=[t_ya[u], t_x2s[u], t_route], w=[t_x2s[u]])
                S.op("dve", lambda e, i=i, u=u: e.scalar_tensor_tensor(out=x2s[u][:], in0=yb[u][:], scalar=wgt[:, 1, i:i + 1], in1=x2s[u][:], op0=ALU.mult, op1=ALU.add),
                     r=[t_yb[u], t_x2s[u], t_route], w=[t_x2s[u]])
                S.op("sp", lambda e, inc, i=i, u=u: inc(e.dma_start(out=out[i * 128:(i + 1) * 128, :], in_=x2s[u][:])), r=[t_x2s[u]], dma=True)
            S.barrier()
        S.barrier()
    return nc


def host_consts(T):
    CAP = cap_for(T)
    p = np.arange(128)
    cm = np.zeros((128, 7, 128), np.float32)
    cm[:, 0, :] = np.eye(128, dtype=np.float32)
    cm[:, 1, :] = ((p[:, None] // 64 == p[None, :] // 64) & (p[:, None] <= p[None, :])).astype(np.float32)
    cm[:, 2, :] = (p[:, None] <= p[None, :]).astype(np.float32)
    cm[:, 3, :] = (p[:, None] < p[None, :]).astype(np.float32)
    cm[:, 4, :] = 1.0
    cm[:, 5, :] = (p[:, None] // 64 == p[None, :] // 64).astype(np.float32)
    m = p
    src = np.where((m % 64) < 32, m + 32, m - 32)
    perm = np.zeros((128, 128), np.float32)
    perm[src, m] = 1.0
    cm[:, 6, :] = perm
    rmask = np.ones((128, 512), np.float32)
    rmask[:, ::64] = 0.0
    ecap = np.broadcast_to((np.arange(32, dtype=np.float32) * CAP)[None, :], (128, 32)).copy()
    half = 32
    inv = (np.float32(10000.0) ** (-np.arange(half, dtype=np.float32) / np.float32(half))).astype(np.float32)
    ang = np.arange(T, dtype=np.float32)[:, None] * inv[None, :]
    cos = np.cos(ang).astype(np.float32).T
    sin = np.sin(ang).astype(np.float32).T
    ropec = np.concatenate([cos, cos, cos, cos], axis=0)
    ropes = np.concatenate([-sin, sin, -sin, sin], axis=0)
    return dict(cmat=cm, rmask=rmask, ecap=ecap, ropec=np.ascontiguousarray(ropec), ropes=np.ascontiguousarray(ropes))


def host_layout(inp, T):
    f = lambda a: np.ascontiguousarray(np.asarray(a, dtype=np.float32))
    d = {}
    d["w_in"] = f(inp["w_in"][0])
    d["gmix"] = f(np.asarray(inp["norm_mix"][0]).reshape(8, 128).T)
    d["hglb"] = f(np.asarray(inp["hg_lb"]).reshape(2, 4, 128).transpose(2, 0, 1).reshape(128, 8))
    d["hgon"] = f(np.asarray(inp["hg_out_norm"][0]).reshape(128, 1))
    d["qkn"] = f(np.stack([np.tile(np.asarray(inp["da_q_norm"][0]), 2), np.tile(np.asarray(inp["da_k_norm"][0]), 2)], axis=1))
    d["lamb"] = f(np.broadcast_to(np.asarray(inp["da_lambda"][0]).reshape(1, 256), (128, 256)))
    d["daon"] = f(np.asarray(inp["da_out_norm"][0]).reshape(128, 1))
    d["wbh"] = f(inp["w_branch_hg"][0])
    d["wbd"] = f(inp["w_branch_da"][0])
    d["w_out"] = f(inp["w_out"][0])
    d["nmoe"] = f(np.broadcast_to(np.asarray(inp["norm_moe"][0]).reshape(1, D), (128, D)))
    d["wr"] = f(np.concatenate([np.asarray(inp["w_router_group"][0]), np.asarray(inp["w_router_expert"][0])], axis=1))
    d["br"] = f(np.broadcast_to(np.concatenate([np.asarray(inp["b_router_group"][0]),
                                                np.asarray(inp["b_router_expert"][0])]).reshape(1, 36), (128, 36)))
    d["w1"] = f(inp["w1"][0])
    d["w3"] = f(inp["w3"][0])
    d["w2"] = f(inp["w2"][0])
    d.update(host_consts(T))
    return d


_NC_CACHE = {}


def kernel(**inputs):
    xfull = np.asarray(inputs["x"], dtype=np.float32)
    B, T, _ = xfull.shape
    shared = host_layout(inputs, T)
    if T not in _NC_CACHE:
        _NC_CACHE[T] = build(T)
    nc = _NC_CACHE[T]
    in_maps = []
    for c in range(B):
        m = dict(shared)
        m["x"] = np.ascontiguousarray(xfull[c])
        in_maps.append(m)
    res = run_bass_kernel_spmd(nc, in_maps, core_ids=list(range(B)))
    return np.stack([np.asarray(r["out"], dtype=np.float32) for r in res.results], axis=0)
```

```python
import math
from contextlib import ExitStack

import numpy as np
import concourse.bass as bass
import concourse.mybir as mybir
from concourse.bass_utils import run_bass_kernel_spmd

F32 = mybir.dt.float32
BF16 = mybir.dt.bfloat16
I32 = mybir.dt.int32
AF = mybir.ActivationFunctionType
ALU = mybir.AluOpType
AX = mybir.AxisListType

D = 1024
IN_COLS = 5632
NE = 32
DFF = 512
EPS = 1e-6
ENGS = ("pe", "act", "dve", "pool", "sp")
SEM_LIMIT = 30000


class Tok:
    __slots__ = ("name", "writers", "rc", "rd")

    def __init__(self, name):
        self.name = name
        self.writers = []
        self.rc = {}
        self.rd = []

    def reset(self):
        self.writers = []
        self.rc = {}
        self.rd = []


class Op:
    __slots__ = ("eng", "fn", "dma", "deps", "signal", "sem", "val")


class Sched:
    def __init__(self, nc, es, n_dma=32, n_eng=5, n_sw=12):
        self.nc = nc
        self.e = {"pe": nc.tensor, "act": nc.scalar, "dve": nc.vector, "pool": nc.gpsimd, "sp": nc.sync}
        self.ops = []
        self.emitted = 0
        self.toks = []
        self.sems = {}
        for k in ("pe", "act", "dve", "pool"):
            for j in range(n_eng):
                self.sems[("e", k, j)] = es.enter_context(nc.semaphore(f"se_{k}{j}"))
        self.dsem_n = n_dma
        for j in range(n_dma):
            self.sems[("d", j)] = es.enter_context(nc.semaphore(f"sd_{j}"))
        self.eidx = {k: 0 for k in ENGS}
        self.ecount = {k: 0 for k in ENGS}
        self.n_eng = n_eng
        self.dtarget = [0] * (n_dma + n_sw)
        self.dnext = 0
        self.n_sw = n_sw
        self.swnext = 0
        for j in range(n_dma, n_dma + n_sw):
            self.sems[("d", j)] = es.enter_context(nc.semaphore(f"sw_{j}"))
        self.waited = {k: {} for k in ENGS}
        self.nwaits = 0

    def tok(self, name="t"):
        t = Tok(name)
        self.toks.append(t)
        return t

    def op(self, eng, fn, r=(), w=(), wd=(), dma=False):
        idx = len(self.ops)
        o = Op()
        o.eng, o.fn, o.dma, o.signal, o.sem, o.val = eng, fn, dma, False, None, 0
        deps = {}
        ops = self.ops

        def add(pidx, raw):
            p = ops[pidx]
            if p.eng == eng and not p.dma and not dma and eng == "pe":
                return
            deps[pidx] = True

        for t in r:
            for pw in t.writers:
                add(pw, True)
        for t in list(w) + list(wd):
            for pw in t.writers:
                add(pw, False)
            for pr in t.rc.values():
                add(pr, False)
            for pr in t.rd:
                add(pr, False)
        for t in r:
            if dma:
                t.rd.append(idx)
            else:
                t.rc[eng] = idx
        for t in w:
            t.writers = [idx]
            t.rc = {}
            t.rd = []
        for t in wd:
            t.writers.append(idx)
        o.deps = sorted(deps)
        ops.append(o)
        return idx

    def _wait(self, eng, key, val):
        if val <= 0 or key is None:
            return
        if self.waited[eng].get(key, 0) >= val:
            return
        self.e[eng].wait_ge(self.sems[key], val)
        self.waited[eng][key] = val
        self.nwaits += 1

    def flush(self):
        ops = self.ops
        for i in range(self.emitted, len(ops)):
            for d in ops[i].deps:
                ops[d].signal = True
        for i in range(self.emitted, len(ops)):
            o = ops[i]
            e = self.e[o.eng]
            for d in o.deps:
                self._wait(o.eng, ops[d].sem, ops[d].val)
            if o.dma:
                if o.eng == "pool":
                    k = self.dsem_n + self.swnext
                    self.swnext = (self.swnext + 1) % self.n_sw
                else:
                    k = self.dnext
                    self.dnext = (k + 1) % self.dsem_n
                key = ("d", k)
                self._wait(o.eng, key, self.dtarget[k])
                s = self.sems[key]
                cnt = [0]

                def inc(ins, s=s, cnt=cnt):
                    ins.then_inc(s, 16)
                    cnt[0] += 1
                    return ins

                o.fn(e, inc)
                self.dtarget[k] += 16 * cnt[0]
                o.sem, o.val = key, self.dtarget[k]
            else:
                ins = o.fn(e)
                if o.signal:
                    c = self.ecount[o.eng] + 1
                    if c > SEM_LIMIT:
                        self.eidx[o.eng] += 1
                        assert self.eidx[o.eng] < self.n_eng, "out of engine semaphores"
                        c = 1
                    self.ecount[o.eng] = c
                    o.sem, o.val = ("e", o.eng, self.eidx[o.eng]), c
                    ins.then_inc(self.sems[o.sem], 1)
            o.fn = None
        self.emitted = len(ops)

    def barrier(self):
        last = {}
        for i in range(self.emitted, len(self.ops)):
            o = self.ops[i]
            if not o.dma:
                last[o.eng] = i
        for i in last.values():
            self.ops[i].signal = True
        self.flush()
        for eng in ENGS:
            for i in last.values():
                self._wait(eng, self.ops[i].sem, self.ops[i].val)
            for k in range(self.dsem_n + self.n_sw):
                self._wait(eng, ("d", k), self.dtarget[k])
        for t in self.toks:
            t.reset()


def cap_for(T):
    return 128 * int(math.ceil((T / 16.0) * 1.5 / 128.0))


def build(T, stop_after=99, dbg=()):
    NT = T // 128
    NB = T // 512
    CAP = cap_for(T)
    CT = CAP // 128
    NSLOT = NE * CAP
    nc = bass.Bass("TRN2", target_bir_lowering=False)

    def din(name, shape, dt=F32):
        return nc.dram_tensor(name, list(shape), dt, kind="ExternalInput").ap()

    x = din("x", [T, D])
    w_in = din("w_in", [D, IN_COLS])
    gmix = din("gmix", [128, 8])
    hglb = din("hglb", [128, 8])
    hgon = din("hgon", [128, 1])
    qkn = din("qkn", [128, 2])
    lamb = din("lamb", [128, 256])
    daon = din("daon", [128, 1])
    wbh = din("wbh", [512, D])
    wbd = din("wbd", [512, D])
    w_out = din("w_out", [D, D])
    nmoe = din("nmoe", [128, D])
    wr = din("wr", [D, 36])
    br = din("br", [128, 36])
    w1 = din("w1", [NE, D, DFF])
    w3 = din("w3", [NE, D, DFF])
    w2 = din("w2", [NE, DFF, D])
    cmat = din("cmat", [128, 7, 128])
    rmask = din("rmask", [128, 512])
    ecap = din("ecap", [128, 32])
    ropec = din("ropec", [128, T])
    ropes = din("ropes", [128, T])
    out = nc.dram_tensor("out", [T, D], F32, kind="ExternalOutput").ap()
    x2_d = nc.dram_tensor("x2_d", [T, D], F32).ap()
    xn_d = nc.dram_tensor("xn_d", [T, D], BF16).ap()
    xg_d = nc.dram_tensor("xg_d", [NSLOT, D], BF16).ap()
    y_d = nc.dram_tensor("y_d", [NSLOT, D], F32).ap()
    hT_d = nc.dram_tensor("hT_d", [128, 8, T], BF16).ap()
    ohg_d = nc.dram_tensor("dbg_ohg" if "ohg" in dbg else "ohg_d", [128, 4, T], BF16, kind="ExternalOutput" if "ohg" in dbg else "Internal").ap()
    oda_d = nc.dram_tensor("dbg_oda" if "oda" in dbg else "oda_d", [128, 4, T], BF16, kind="ExternalOutput" if "oda" in dbg else "Internal").ap()
    dbg_t = {}
    if "ht" in dbg:
        dbg_t["ht"] = nc.dram_tensor("dbg_ht", [128, 8, T], F32, kind="ExternalOutput").ap()
    if "x2" in dbg:
        dbg_t["x2"] = nc.dram_tensor("dbg_x2", [T, D], F32, kind="ExternalOutput").ap()
        dbg_t["lg"] = nc.dram_tensor("dbg_lg", [128, NT, 36], F32, kind="ExternalOutput").ap()
    if "rt" in dbg:
        dbg_t["rt"] = nc.dram_tensor("dbg_rt", [128, 4, NT], F32, kind="ExternalOutput").ap()

    w_in_v = w_in.rearrange("(k p) c -> p k c", p=128)

    with ExitStack() as es:
        S = Sched(nc, es)

        def sb(name, shape, dt, stack=es):
            return stack.enter_context(nc.sbuf_tensor(name, list(shape), dt))

        ps = [es.enter_context(nc.psum_tensor(f"ps{i}", [128, 512], F32)) for i in range(8)]
        pst = [S.tok(f"ps{i}") for i in range(8)]

        def psb(i):
            return ps[i][:].bitcast(BF16)

        cm_f = sb("cm_f", [128, 7, 128], F32)
        cm_b = sb("cm_b", [128, 7, 128], BF16)
        rmask_s = sb("rmask_s", [128, 512], F32)
        gmix_s = sb("gmix_s", [128, 8], F32)
        hglb_s = sb("hglb_s", [128, 8], F32)
        lbv = sb("lbv", [128, 12], F32)
        hgon_s = sb("hgon_s", [128, 1], F32)
        qkn_s = sb("qkn_s", [128, 2], F32)
        lamb_s = sb("lamb_s", [128, 256], F32)
        lam_s = sb("lam_s", [128, 8], F32)
        daon_s = sb("daon_s", [128, 2], F32)
        dmy = sb("dmy", [128, 2], F32)
        t_c = S.tok("consts")

        def ld_consts(e, inc):
            inc(e.dma_start(out=cm_f[:], in_=cmat))
            inc(e.dma_start(out=rmask_s[:], in_=rmask))
            inc(e.dma_start(out=gmix_s[:], in_=gmix))
            inc(e.dma_start(out=hglb_s[:], in_=hglb))
            inc(e.dma_start(out=hgon_s[:], in_=hgon))
            inc(e.dma_start(out=qkn_s[:], in_=qkn))
            inc(e.dma_start(out=lamb_s[:], in_=lamb))
            inc(e.dma_start(out=daon_s[:, 0:1], in_=daon))

        S.op("sp", ld_consts, w=[t_c], dma=True)
        S.op("dve", lambda e: e.tensor_copy(out=cm_b[:], in_=cm_f[:]), r=[t_c], w=[t_c])
        S.op("dve", lambda e: e.tensor_sub(out=lbv[:, 8:12], in0=hglb_s[:, 0:4], in1=hglb_s[:, 4:8]), r=[t_c], w=[t_c])
        S.op("act", lambda e: e.activation(out=lbv[:, 8:12], in_=lbv[:, 8:12], func=AF.Exp), r=[t_c], w=[t_c])
        S.op("dve", lambda e: e.tensor_scalar_add(out=lbv[:, 8:12], in0=lbv[:, 8:12], scalar1=1.0), r=[t_c], w=[t_c])
        S.op("dve", lambda e: e.reciprocal(out=lbv[:, 0:4], in_=lbv[:, 8:12]), r=[t_c], w=[t_c])
        S.op("dve", lambda e: e.tensor_scalar_mul(out=lbv[:, 4:8], in0=lbv[:, 0:4], scalar1=-1.0), r=[t_c], w=[t_c])
        S.op("dve", lambda e: e.tensor_mul(out=lamb_s[:, 0:64], in0=lamb_s[:, 0:64], in1=lamb_s[:, 64:128]), r=[t_c], w=[t_c])
        S.op("dve", lambda e: e.tensor_mul(out=lamb_s[:, 128:192], in0=lamb_s[:, 128:192], in1=lamb_s[:, 192:256]), r=[t_c], w=[t_c])
        S.op("dve", lambda e: e.reduce_sum(out=lam_s[:, 0:1], in_=lamb_s[:, 0:64], axis=AX.X), r=[t_c], w=[t_c])
        S.op("dve", lambda e: e.reduce_sum(out=lam_s[:, 1:2], in_=lamb_s[:, 128:192], axis=AX.X), r=[t_c], w=[t_c])
        S.op("act", lambda e: e.activation(out=lam_s[:, 0:2], in_=lam_s[:, 0:2], func=AF.Exp), r=[t_c], w=[t_c])
        S.op("dve", lambda e: e.tensor_sub(out=lam_s[:, 2:3], in0=lam_s[:, 1:2], in1=lam_s[:, 0:1]), r=[t_c], w=[t_c])
        S.op("dve", lambda e: e.tensor_scalar_add(out=lam_s[:, 2:3], in0=lam_s[:, 2:3], scalar1=-0.2), r=[t_c], w=[t_c])
        S.op("dve", lambda e: e.tensor_scalar_mul(out=daon_s[:, 1:2], in0=daon_s[:, 0:1], scalar1=0.8), r=[t_c], w=[t_c])
        S.op("dve", lambda e: e.memset(lam_s[:, 4:5], EPS), w=[t_c])
        S.op("dve", lambda e: e.memset(lam_s[:, 5:6], 1.0), w=[t_c])
        S.op("dve", lambda e: e.memset(dmy[:], 0.0), w=[t_c])
        S.barrier()

        EPSB = lam_s[:, 4:5]
        ONEB = lam_s[:, 5:6]
        IDF = cm_f[:, 0, :]
        IDB = cm_b[:, 0, :]
        CMASK = cm_b[:, 1, :]
        TRI01 = cm_b[:, 2, :]
        LSTR = cm_b[:, 3, :]
        ONESB = cm_b[:, 4, :]
        BD64 = cm_b[:, 5, :]
        PERM = cm_b[:, 6, :]

        slot_i = sb("slot_i", [128, 2, NT], I32)
        wgt = sb("wgt", [128, 2, NT], F32)
        t_route = S.tok("route")
        t_x2d = [S.tok("x2d") for _ in range(NT)]
        t_xnd = [S.tok("xnd") for _ in range(NT)]
        t_xg = S.tok("xg")
        t_yd = S.tok("yd")
        t_hTd = S.tok("hTd")
        t_ohgd = [[S.tok("ohgd") for _ in range(NB)] for _ in range(4)]
        t_odad = [[S.tok("odad") for _ in range(NB)] for _ in range(4)]
        zt = sb("zt", [128, D], BF16)
        t_zt = S.tok("zt")
        S.op("dve", lambda e: e.memset(zt[:], 0.0), w=[t_zt])
        es_h = ExitStack()
        hT = sb("hT", [128, 8, T], BF16, es_h)
        t_hT = [S.tok(f"hT{i}") for i in range(NB)]

        with ExitStack() as p1:
            xt = [sb(f"p1x{i}", [128, D], F32, p1) for i in range(2)]
            xs = [sb(f"p1xs{i}", [128, D], BF16, p1) for i in range(2)]
            junk = sb("p1junk", [128, D], F32, p1)
            st = [sb(f"p1st{i}", [128, 2], F32, p1) for i in range(2)]
            t_xt = [S.tok("xt") for _ in range(2)]
            t_xs = [S.tok("xs") for _ in range(2)]
            t_st = [S.tok("st") for _ in range(2)]
            t_junk = S.tok("junk")
            for i in range(NT):
                b = i % 2
                S.op("sp", lambda e, inc, i=i, b=b: inc(e.dma_start(out=xt[b][:], in_=x[i * 128:(i + 1) * 128, :])),
                     w=[t_xt[b]], dma=True)
                S.op("act", lambda e, b=b: e.activation(out=junk[:], in_=xt[b][:], func=AF.Square), r=[t_xt[b]], w=[t_junk])
                S.op("dve", lambda e, b=b: e.reduce_sum(out=st[b][:, 0:1], in_=junk[:], axis=AX.X), r=[t_junk], w=[t_st[b]])
                S.op("act", lambda e, b=b: e.activation(out=st[b][:, 1:2], in_=st[b][:, 0:1], func=AF.Ln, scale=1.0 / D, bias=EPSB),
                     r=[t_st[b]], w=[t_st[b]])
                S.op("act", lambda e, b=b: e.activation(out=st[b][:, 1:2], in_=st[b][:, 1:2], func=AF.Exp, scale=-0.5),
                     r=[t_st[b]], w=[t_st[b]])
                S.op("dve", lambda e, b=b: e.tensor_scalar(out=xs[b][:], in0=xt[b][:], scalar1=st[b][:, 1:2], scalar2=None,
                                                           op0=ALU.mult), r=[t_st[b], t_xt[b]], w=[t_xs[b]])
                pb = 0 + (i % 2)
                for k in range(8):
                    S.op("pe", lambda e, k=k, b=b, pb=pb: e.transpose(out=psb(pb)[:, k * 128:(k + 1) * 128],
                                                                       in_=xs[b][:, k * 128:(k + 1) * 128], identity=IDB),
                         r=[t_xs[b]], w=[pst[pb]])
                S.op("dve", lambda e, i=i, pb=pb: e.tensor_tensor(
                    out=hT[:, :, i * 128:(i + 1) * 128],
                    in0=psb(pb).rearrange("p (k t) -> p k t", k=8),
                    in1=gmix_s[:].unsqueeze(2).to_broadcast([128, 8, 128]), op=ALU.mult),
                    r=[pst[pb]], w=[t_hT[i // 4]])
            S.op("sp", lambda e, inc: inc(e.dma_start(out=hT_d, in_=hT[:])), r=t_hT, w=[t_hTd], dma=True)
            S.barrier()
        if "ht" in dbg:
            with ExitStack() as pd:
                tmp = sb("dbgtmp", [128, 8, T], F32, pd)
                tt = S.tok("dbgtmp")
                S.op("dve", lambda e: e.tensor_copy(out=tmp[:], in_=hT[:]), r=t_hT, w=[tt])
                S.op("sp", lambda e, inc: inc(e.dma_start(out=dbg_t["ht"], in_=tmp[:])), r=[tt], dma=True)
                S.barrier()
        if stop_after <= 1:
            S.barrier()
            es_h.close()
            return nc

        def mm(out_, lhsT, rhs, start, stop):
            return lambda e: e.matmul(out_, lhsT, rhs, start=start, stop=stop)

        def act_accum(e, **kw):
            e.activation(**kw)
            return e.activation(out=dmy[:, 0:1], in_=dmy[:, 1:2], func=AF.Copy)

        dump_names = []
        if "dump" in dbg:
            dbg_t["dump"] = nc.dram_tensor("dbg_dump", [24, 128, 512], F32, kind="ExternalOutput").ap()

        def dump(name, ap, toks):
            if "dump" not in dbg or len(dump_names) >= 24:
                return
            k = len(dump_names)
            dump_names.append(name)
            S.op("sp", lambda e, inc, k=k, ap=ap: inc(e.dma_start(out=dbg_t["dump"][k][:, 0:ap.shape[1]], in_=ap)), r=toks, dma=True)

        with ExitStack() as p2:
            wst = sb("p2wst", [128, 8, 512], F32, p2)
            t_wst = S.tok("wst")
            wq = [sb(f"p2wq{i}", [128, 8, 512], BF16, p2) for i in range(2)]
            t_wq = [S.tok("wq") for _ in range(2)]
            state = [sb(f"p2state{i}", [128, 128], F32, p2) for i in range(2)]
            t_state = [S.tok("state") for _ in range(2)]
            stbf = [[sb(f"p2stbf{i}_{j}", [128, 128], BF16, p2) for j in range(2)] for i in range(2)]
            t_stbf = [[S.tok("stbf") for _ in range(2)] for _ in range(2)]
            def mk(name, dt, n=2, shape=(128, 512)):
                return [sb(f"p2{name}{i}", list(shape), dt, p2) for i in range(n)], [S.tok(name) for _ in range(n)]
            v_tm, t_v = mk("v", BF16)
            q_f, t_q = mk("q", F32)
            ef, t_ef = mk("ef", F32)
            e2f, t_e2 = mk("e2", F32)
            kk, t_kk = mk("kk", F32)
            gg, t_g = mk("g", F32)
            bcs, t_b = mk("b", F32)
            bm, t_bm = mk("bm", F32)
            E1, t_E1 = mk("E1", F32)
            E2, t_E2 = mk("E2", F32)
            E3, t_E3 = mk("E3", F32)
            qrel, t_qrel = mk("qrel", BF16)
            qb, t_qb = mk("qb", BF16)
            krel, t_krel = mk("krel", BF16)
            sog, t_sog = mk("sog", F32)
            o_f, t_of = mk("of", F32)
            sqb, t_sqb = mk("sqb", BF16)
            ob2, t_ob2 = mk("ob2", BF16)
            rst, t_rst = mk("rst", F32)
            kt, t_kt = mk("kt", BF16, 4, (128, 128))
            stm, t_stm = mk("stm", BF16, 4, (128, 128))
            sidx = [0, 0]
            RB = [(4, 5, 6, 7), (0, 1, 2, 3)]

            def prep(h, b, u):
                tb = slice(b * 512, (b + 1) * 512)
                for j, bank in ((0, 0), (1, 1), (3, 2)):
                    for k in range(8):
                        S.op("pe", mm(ps[bank][:, :], wq[u][:, k, j * 128:(j + 1) * 128], hT[:, k, tb], k == 0, k == 7),
                             r=[t_wq[u], t_hT[b]], w=[pst[bank]])
                for ti in range(4):
                    for k in range(8):
                        S.op("pe", mm(ps[3][:, ti * 128:(ti + 1) * 128], hT[:, k, b * 512 + ti * 128: b * 512 + (ti + 1) * 128],
                                      wq[u][:, k, 256:384], k == 0, k == 7), r=[t_wq[u], t_hT[b]], w=[pst[3]])
                S.op("act", lambda e: e.activation(out=v_tm[u][:], in_=ps[3][:, :], func=AF.Copy), r=[pst[3]], w=[t_v[u]])
                S.op("act", lambda e: e.activation(out=q_f[u][:], in_=ps[0][:, :], func=AF.Copy), r=[pst[0]], w=[t_q[u]])
                S.op("act", lambda e: e.activation(out=ef[u][:], in_=ps[1][:, :], func=AF.Exp, scale=-1.0), r=[pst[1]], w=[t_ef[u]])
                S.op("act", lambda e: e.activation(out=e2f[u][:], in_=ps[2][:, :], func=AF.Exp, scale=-1.0), r=[pst[2]], w=[t_e2[u]])
                S.op("act", lambda e: e.activation(out=ef[u][:], in_=ef[u][:], func=AF.Ln, bias=ONEB), r=[t_ef[u]], w=[t_ef[u]])
                S.op("act", lambda e: e.activation(out=ef[u][:], in_=ef[u][:], func=AF.Exp, scale=-1.0), r=[t_ef[u]], w=[t_ef[u]])
                S.op("dve", lambda e: e.tensor_scalar(out=kk[u][:], in0=ef[u][:], scalar1=lbv[:, 4 + h:5 + h], scalar2=lbv[:, h:h + 1],
                                                      op0=ALU.mult, op1=ALU.add), r=[t_ef[u]], w=[t_kk[u]])
                S.op("act", lambda e: e.activation(out=gg[u][:], in_=kk[u][:], func=AF.Ln, scale=-1.0, bias=ONEB), r=[t_kk[u]], w=[t_g[u]])
                S.op("dve", lambda e: e.tensor_tensor_scan(out=bcs[u][:], data0=rmask_s[:], data1=gg[u][:], initial=0.0,
                                                           op0=ALU.mult, op1=ALU.add), r=[t_g[u]], w=[t_b[u]])
                S.op("dve", lambda e: e.tensor_tensor(
                    out=bm[u][:].rearrange("p (c t) -> p c t", t=64),
                    in0=bcs[u][:].rearrange("p (c t) -> p c t", t=64),
                    in1=bcs[u][:].rearrange("p (c t) -> p c t", t=64)[:, :, 31:32].to_broadcast([128, 8, 64]),
                    op=ALU.subtract), r=[t_b[u]], w=[t_bm[u]])
                S.op("act", lambda e: e.activation(out=E1[u][:], in_=bm[u][:], func=AF.Exp), r=[t_bm[u]], w=[t_E1[u]])
                S.op("act", lambda e: e.activation(out=E2[u][:], in_=bm[u][:], func=AF.Exp, scale=-1.0), r=[t_bm[u]], w=[t_E2[u]])
                S.op("act", lambda e: e.activation(out=E3[u][:], in_=bcs[u][:], func=AF.Exp), r=[t_b[u]], w=[t_E3[u]])
                S.op("dve", lambda e: e.tensor_tensor(out=qrel[u][:], in0=q_f[u][:], in1=E1[u][:], op=ALU.mult),
                     r=[t_q[u], t_E1[u]], w=[t_qrel[u]])
                S.op("dve", lambda e: e.tensor_tensor(out=qb[u][:], in0=q_f[u][:], in1=E3[u][:], op=ALU.mult),
                     r=[t_q[u], t_E3[u]], w=[t_qb[u]])
                S.op("dve", lambda e: e.tensor_tensor(out=krel[u][:], in0=kk[u][:], in1=E2[u][:], op=ALU.mult),
                     r=[t_kk[u], t_E2[u]], w=[t_krel[u]])
                S.op("act", lambda e: e.activation(out=e2f[u][:], in_=e2f[u][:], func=AF.Ln, bias=ONEB), r=[t_e2[u]], w=[t_e2[u]])
                S.op("act", lambda e: e.activation(out=e2f[u][:], in_=e2f[u][:], func=AF.Exp, scale=-1.0), r=[t_e2[u]], w=[t_e2[u]])
                S.op("dve", lambda e: e.tensor_tensor(out=sog[u][:], in0=ps[2][:, :], in1=e2f[u][:], op=ALU.mult),
                     r=[pst[2], t_e2[u]], w=[t_sog[u]])

            def tile_pre(u, ti):
                btr, bsT, boT, bdS = RB[u]
                tsl = slice(ti * 128, (ti + 1) * 128)
                w_ = u * 2 + ti % 2
                S.op("pe", lambda e: e.transpose(out=psb(btr)[:, 0:128], in_=krel[u][:, tsl], identity=IDB), r=[t_krel[u]], w=[pst[btr]])
                S.op("act", lambda e: e.activation(out=kt[w_][:], in_=psb(btr)[:, 0:128], func=AF.Copy), r=[pst[btr]], w=[t_kt[w_]])
                S.op("pe", mm(ps[bsT][:, 0:128], krel[u][:, tsl], qrel[u][:, tsl], True, True), r=[t_krel[u], t_qrel[u]], w=[pst[bsT]])
                S.op("dve", lambda e: e.tensor_tensor(out=stm[w_][:], in0=ps[bsT][:, 0:128], in1=CMASK, op=ALU.mult), r=[pst[bsT]], w=[t_stm[w_]])
                S.op("pe", mm(ps[boT][:, 0:128], v_tm[u][:, tsl], stm[w_][:], True, False), r=[t_v[u], t_stm[w_]], w=[pst[boT]])

            def chunk(u, ti, half):
                btr, bsT, boT, bdS = RB[u]
                tsl = slice(ti * 128, (ti + 1) * 128)
                w_ = u * 2 + ti % 2
                c = ti * 2 + half
                csl = slice(ti * 128 + half * 64, ti * 128 + half * 64 + 64)
                pr = slice(half * 64, half * 64 + 64)
                cur = sidx[u] % 2
                S.op("pe", mm(ps[boT][:, half * 64:half * 64 + 64], stbf[u][cur][:], qb[u][:, csl], False, half == 1),
                     r=[t_stbf[u][cur], t_qb[u]], w=[pst[boT]])
                S.op("pe", mm(ps[bdS][:, half * 128:half * 128 + 128], kt[w_][pr, :], v_tm[u][pr, tsl], True, True),
                     r=[t_kt[w_], t_v[u]], w=[pst[bdS]])
                e5 = E3[u][:, c * 64 + 63:c * 64 + 64]
                c1 = E1[u][:, c * 64 + 63:c * 64 + 64]
                S.op("dve", lambda e: e.tensor_scalar(out=state[u][:], in0=state[u][:], scalar1=e5, scalar2=None, op0=ALU.mult),
                     r=[t_state[u], t_E3[u]], w=[t_state[u]])
                S.op("dve", lambda e: e.scalar_tensor_tensor(
                    out=state[u][:], in0=ps[bdS][:, half * 128:half * 128 + 128], scalar=c1, in1=state[u][:], op0=ALU.mult, op1=ALU.add),
                    r=[t_state[u], t_E1[u], pst[bdS]], w=[t_state[u]])
                sidx[u] += 1
                nxt = sidx[u] % 2
                S.op("act", lambda e: e.activation(out=stbf[u][nxt][:], in_=state[u][:], func=AF.Copy), r=[t_state[u]], w=[t_stbf[u][nxt]])

            def tile_post(u, ti):
                btr, bsT, boT, bdS = RB[u]
                tsl = slice(ti * 128, (ti + 1) * 128)
                S.op("dve", lambda e: e.tensor_copy(out=o_f[u][:, tsl], in_=ps[boT][:, 0:128]), r=[pst[boT]], w=[t_of[u]])

            def post(h, b, u):
                btr, bsT, boT, bdS = RB[u]
                tb = slice(b * 512, (b + 1) * 512)
                S.op("act", lambda e: e.activation(out=sqb[u][:], in_=o_f[u][:], func=AF.Square), r=[t_of[u]], w=[t_sqb[u]])
                S.op("pe", mm(ps[bsT][:, :], ONESB, sqb[u][:], True, True), r=[t_sqb[u]], w=[pst[bsT]])
                S.op("act", lambda e: e.activation(out=rst[u][:], in_=ps[bsT][:, :], func=AF.Ln, scale=1.0 / 128, bias=EPSB), r=[pst[bsT]], w=[t_rst[u]])
                S.op("act", lambda e: e.activation(out=rst[u][:], in_=rst[u][:], func=AF.Exp, scale=-0.5), r=[t_rst[u]], w=[t_rst[u]])
                S.op("dve", lambda e: e.tensor_tensor(out=o_f[u][:], in0=o_f[u][:], in1=rst[u][:], op=ALU.mult), r=[t_of[u], t_rst[u]], w=[t_of[u]])
                S.op("dve", lambda e: e.scalar_tensor_tensor(out=ob2[u][:], in0=o_f[u][:], scalar=hgon_s[:, 0:1], in1=sog[u][:],
                                                             op0=ALU.mult, op1=ALU.mult), r=[t_of[u], t_sog[u]], w=[t_ob2[u]])
                S.op("sp", lambda e, inc: inc(e.dma_start(out=ohg_d[:, h, tb], in_=ob2[u][:])), r=[t_ob2[u]], w=[t_ohgd[h][b]], dma=True)

            for hp in range(2):
                for u in range(2):
                    h = 2 * hp + u
                    def ldw(e, inc, h=h):
                        for j in range(4):
                            inc(e.dma_start(out=wst[:, :, j * 128:(j + 1) * 128],
                                            in_=w_in_v[:, :, j * 512 + h * 128: j * 512 + (h + 1) * 128]))
                    S.op("sp", ldw, w=[t_wst], dma=True)
                    if h == 0:
                        for ex_ in range(NE):
                            S.op("sp", lambda e, inc, ex_=ex_: inc(e.dma_start(
                                out=xg_d[ex_ * CAP:(ex_ + 1) * CAP, :].rearrange("(c p) d -> p c d", p=128),
                                in_=zt[:].unsqueeze(1).to_broadcast([128, CT, D]))), r=[t_zt], wd=[t_xg], dma=True)
                    S.op("dve", lambda e, u=u: e.tensor_copy(out=wq[u][:], in_=wst[:]), r=[t_wst], w=[t_wq[u]])
                    S.op("dve", lambda e, u=u: e.memset(state[u][:], 0.0), w=[t_state[u]])
                    S.op("dve", lambda e, u=u, c0=sidx[u] % 2: e.memset(stbf[u][c0][:], 0.0), w=[t_stbf[u][sidx[u] % 2]])
                for b in range(NB):
                    for u in range(2):
                        prep(2 * hp + u, b, u)
                    for ti in range(4):
                        for u in range(2):
                            tile_pre(u, ti)
                        for half in range(2):
                            for u in range(2):
                                chunk(u, ti, half)
                        for u in range(2):
                            tile_post(u, ti)
                    for u in range(2):
                        post(2 * hp + u, b, u)
            S.barrier()
        if "ht2" in dbg:
            dbg_t["ht2"] = nc.dram_tensor("dbg_ht2", [128, 8, T], F32, kind="ExternalOutput").ap()
            with ExitStack() as pd:
                tmp = sb("dbgtmp3", [128, 8, T], F32, pd)
                tt = S.tok("dbgtmp3")
                S.op("dve", lambda e: e.tensor_copy(out=tmp[:], in_=hT[:]), r=t_hT, w=[tt])
                S.op("sp", lambda e, inc: inc(e.dma_start(out=dbg_t["ht2"], in_=tmp[:])), r=[tt], dma=True)
                S.barrier()
        if stop_after <= 2:
            S.barrier()
            es_h.close()
            return nc

        with ExitStack() as p4:
            ropec_s = sb("p4rc", [128, T], F32, p4)
            ropes_s = sb("p4rs", [128, T], F32, p4)
            t_rope = S.tok("rope")
            S.op("sp", lambda e, inc: (inc(e.dma_start(out=ropec_s[:], in_=ropec)), inc(e.dma_start(out=ropes_s[:], in_=ropes))),
                 w=[t_rope], dma=True)
            wst = sb("p4wst", [128, 8, 384], F32, p4)
            wa = sb("p4wa", [128, 8, 384], BF16, p4)
            t_wst, t_wa = S.tok("wst4"), S.tok("wa4")
            qT_s = sb("p4qT", [128, T], BF16, p4)
            kT_s = sb("p4kT", [128, T], BF16, p4)
            vt = sb("p4v", [128, NT, 128], BF16, p4)
            t_qT = [S.tok("qT") for _ in range(NB)]
            t_kT = [S.tok("kT") for _ in range(NB)]
            t_vt = [S.tok("vt") for _ in range(NB)]
            def mk4(name, dt, n=2, shape=(128, 512)):
                return [sb(f"p4{name}{i}", list(shape), dt, p4) for i in range(n)], [S.tok(name) for _ in range(n)]
            sq4, t_sq4 = mk4("sq", BF16)
            qf4, t_qf4 = mk4("qf", F32)
            rs4, t_rs4 = mk4("rs", F32)
            qn4, t_qn4 = mk4("qn", BF16)
            t14, t_t14 = mk4("t1", F32)
            t24, t_t24 = mk4("t2", F32)
            Pm, t_P = mk4("P", BF16, 4)
            r0, t_r0 = mk4("r0", F32, 1)
            r1, t_r1 = mk4("r1", F32, 1)
            o1, t_o1 = mk4("o1", F32, 1)
            o2, t_o2 = mk4("o2", F32, 1)
            ob4, t_ob4 = mk4("ob4", BF16, 2)
            cnt4 = 0
            for h in range(4):
                def ldw4(e, inc, h=h):
                    for j in range(3):
                        inc(e.dma_start(out=wst[:, :, j * 128:(j + 1) * 128],
                                        in_=w_in_v[:, :, 2048 + j * 512 + h * 128: 2048 + j * 512 + (h + 1) * 128]))
                S.op("sp", ldw4, w=[t_wst], dma=True)
                S.op("dve", lambda e: e.tensor_copy(out=wa[:], in_=wst[:]), r=[t_wst], w=[t_wa])
                units = [(b, j) for b in range(NB) for j in range(2)]

                def stA(n):
                    b, j = units[n]
                    u = n % 2
                    tb = slice(b * 512, (b + 1) * 512)
                    for k in range(8):
                        S.op("pe", mm(ps[u][:, :], wa[:, k, j * 128:(j + 1) * 128], hT[:, k, tb], k == 0, k == 7),
                             r=[t_wa, t_hT[b]], w=[pst[u]])
                    S.op("act", lambda e: e.activation(out=sq4[u][:], in_=ps[u][:, :], func=AF.Square), r=[pst[u]], w=[t_sq4[u]])
                    S.op("act", lambda e: e.activation(out=qf4[u][:], in_=ps[u][:, :], func=AF.Copy), r=[pst[u]], w=[t_qf4[u]])
                    if j == 0:
                        for ti in range(4):
                            for k in range(8):
                                S.op("pe", mm(ps[4][:, ti * 128:(ti + 1) * 128], hT[:, k, b * 512 + ti * 128: b * 512 + (ti + 1) * 128],
                                              wa[:, k, 256:384], k == 0, k == 7), r=[t_wa, t_hT[b]], w=[pst[4]])
                        S.op("act", lambda e: e.activation(out=vt[:, b * 4:(b + 1) * 4, :], in_=ps[4][:, :].rearrange("p (a c) -> p a c", a=4),
                                                           func=AF.Copy), r=[pst[4]], w=[t_vt[b]])

                def stB(n):
                    b, j = units[n]
                    u = n % 2
                    S.op("pe", mm(ps[2][:, :], BD64, sq4[u][:], True, True), r=[t_sq4[u]], w=[pst[2]])
                    S.op("act", lambda e: e.activation(out=rs4[u][:], in_=ps[2][:, :], func=AF.Ln, scale=1.0 / 64, bias=EPSB), r=[pst[2]], w=[t_rs4[u]])
                    S.op("act", lambda e: e.activation(out=rs4[u][:], in_=rs4[u][:], func=AF.Exp, scale=-0.5), r=[t_rs4[u]], w=[t_rs4[u]])
                    S.op("dve", lambda e: e.scalar_tensor_tensor(out=qn4[u][:], in0=qf4[u][:], scalar=qkn_s[:, j:j + 1], in1=rs4[u][:],
                                                                 op0=ALU.mult, op1=ALU.mult), r=[t_qf4[u], t_rs4[u]], w=[t_qn4[u]])

                def stC(n):
                    b, j = units[n]
                    u = n % 2
                    tb = slice(b * 512, (b + 1) * 512)
                    dst, t_dst = (qT_s, t_qT) if j == 0 else (kT_s, t_kT)
                    S.op("pe", mm(ps[3][:, :], PERM, qn4[u][:], True, True), r=[t_qn4[u]], w=[pst[3]])
                    S.op("dve", lambda e: e.tensor_tensor(out=t14[u][:], in0=qn4[u][:], in1=ropec_s[:, tb], op=ALU.mult),
                         r=[t_qn4[u], t_rope], w=[t_t14[u]])
                    S.op("dve", lambda e: e.tensor_tensor(out=t24[u][:], in0=ps[3][:, :], in1=ropes_s[:, tb], op=ALU.mult),
                         r=[pst[3], t_rope], w=[t_t24[u]])
                    S.op("dve", lambda e: e.tensor_tensor(out=dst[:, tb], in0=t14[u][:], in1=t24[u][:], op=ALU.add),
                         r=[t_t14[u], t_t24[u]], w=[t_dst[b]])

                NU = len(units)
                for n in range(NU + 2):
                    if n < NU:
                        stA(n)
                    if 0 <= n - 1 < NU:
                        stB(n - 1)
                    if 0 <= n - 2 < NU:
                        stC(n - 2)
                S.barrier()
                steps = [(j, i) for j in range(NB) for i in range(4 * j + 4)]

                def qk(s):
                    j, i = steps[s]
                    r_ = i - 4 * j
                    col0 = 128 * r_ if r_ > 0 else 0
                    for m in range(2):
                        bank = m * 2 + (s % 2)
                        S.op("pe", mm(ps[bank][:, col0:512], kT_s[m * 64:(m + 1) * 64, i * 128:(i + 1) * 128],
                                      qT_s[m * 64:(m + 1) * 64, j * 512 + col0:(j + 1) * 512], True, True),
                             r=[t_kT[i // 4], t_qT[j]], w=[pst[bank]])

                qk(0)
                for s in range(len(steps)):
                    j, i = steps[s]
                    r_ = i - 4 * j
                    col0 = 128 * r_ if r_ > 0 else 0
                    last = (i == 4 * j + 3)
                    for m in range(2):
                        bank = m * 2 + (s % 2)
                        pb_ = m * 2 + (s % 2)
                        S.op("act", lambda e, bank=bank, pb_=pb_, col0=col0: e.activation(out=Pm[pb_][:, col0:512], in_=ps[bank][:, col0:512],
                                                                                           func=AF.Exp, scale=0.125),
                             r=[pst[bank]], w=[t_P[pb_]])
                    if s + 1 < len(steps):
                        qk(s + 1)
                    for m in range(2):
                        pb_ = m * 2 + (s % 2)
                        if r_ >= 0:
                            S.op("dve", lambda e, pb_=pb_, col0=col0: e.tensor_tensor(out=Pm[pb_][:, col0:col0 + 128], in0=Pm[pb_][:, col0:col0 + 128],
                                                                                       in1=TRI01, op=ALU.mult), r=[t_P[pb_]], w=[t_P[pb_]])
                        S.op("pe", mm(ps[4 + m][:, col0:512], vt[:, i, :], Pm[pb_][:, col0:512], i == 0, last), r=[t_vt[i // 4], t_P[pb_]], w=[pst[4 + m]])
                        S.op("pe", mm(ps[6 + m][:, col0:512], ONESB, Pm[pb_][:, col0:512], i == 0, last), r=[t_P[pb_]], w=[pst[6 + m]])
                    if last:
                        jb = slice(j * 512, (j + 1) * 512)
                        S.op("dve", lambda e: e.tensor_copy(out=o1[0][:], in_=ps[4][:, :]), r=[pst[4]], w=[t_o1[0]])
                        S.op("dve", lambda e: e.tensor_copy(out=o2[0][:], in_=ps[5][:, :]), r=[pst[5]], w=[t_o2[0]])
                        S.op("act", lambda e: e.activation(out=r0[0][:], in_=ps[6][:, :], func=AF.Ln), r=[pst[6]], w=[t_r0[0]])
                        S.op("act", lambda e: e.activation(out=r1[0][:], in_=ps[7][:, :], func=AF.Ln), r=[pst[7]], w=[t_r1[0]])
                        S.op("act", lambda e: e.activation(out=r0[0][:], in_=r0[0][:], func=AF.Exp, scale=-1.0), r=[t_r0[0]], w=[t_r0[0]])
                        S.op("act", lambda e: e.activation(out=r1[0][:], in_=r1[0][:], func=AF.Exp, scale=-1.0), r=[t_r1[0]], w=[t_r1[0]])
                        S.op("dve", lambda e: e.tensor_tensor(out=o1[0][:], in0=o1[0][:], in1=r0[0][:], op=ALU.mult), r=[t_o1[0], t_r0[0]], w=[t_o1[0]])
                        S.op("dve", lambda e: e.tensor_tensor(out=o2[0][:], in0=o2[0][:], in1=r1[0][:], op=ALU.mult), r=[t_o2[0], t_r1[0]], w=[t_o2[0]])
                        S.op("dve", lambda e: e.scalar_tensor_tensor(out=o1[0][:], in0=o2[0][:], scalar=lam_s[:, 2:3], in1=o1[0][:], op0=ALU.mult, op1=ALU.add),
                             r=[t_o1[0], t_o2[0]], w=[t_o1[0]])
                        S.op("act", lambda e: e.activation(out=sq4[0][:], in_=o1[0][:], func=AF.Square), r=[t_o1[0]], w=[t_sq4[0]])
                        S.op("pe", mm(ps[6][:, :], ONESB, sq4[0][:], True, True), r=[t_sq4[0]], w=[pst[6]])
                        S.op("act", lambda e: e.activation(out=rs4[0][:], in_=ps[6][:, :], func=AF.Ln, scale=1.0 / 128, bias=EPSB), r=[pst[6]], w=[t_rs4[0]])
                        S.op("act", lambda e: e.activation(out=rs4[0][:], in_=rs4[0][:], func=AF.Exp, scale=-0.5), r=[t_rs4[0]], w=[t_rs4[0]])
                        ou = j % 2
                        S.op("dve", lambda e, ou=ou: e.scalar_tensor_tensor(out=ob4[ou][:], in0=o1[0][:], scalar=daon_s[:, 1:2], in1=rs4[0][:],
                                                                            op0=ALU.mult, op1=ALU.mult), r=[t_o1[0], t_rs4[0]], w=[t_ob4[ou]])
                        S.op("sp", lambda e, inc, ou=ou, h=h, jb=jb: inc(e.dma_start(out=oda_d[:, h, jb], in_=ob4[ou][:])), r=[t_ob4[ou]], w=[t_odad[h][j]], dma=True)
                S.barrier()
        es_h.close()
        if stop_after <= 4:
            S.barrier()
            return nc

        BCREG = nc.gpsimd.to_reg(NSLOT - 1)
        logit = sb("logit", [128, NT, 36], F32)
        t_logit = S.tok("logit")
        with ExitStack() as p5:
            wbh_s = sb("p5wbh", [128, 4, D], BF16, p5)
            wbd_s = sb("p5wbd", [128, 4, D], BF16, p5)
            wg_s = sb("p5wg", [128, 8, 2048], BF16, p5)
            wo_s = sb("p5wo", [128, 8, D], BF16, p5)
            nm_s = sb("p5nm", [128, D], F32, p5)
            wr_s = sb("p5wr", [128, 8, 36], F32, p5)
            br_s = sb("p5br", [128, 36], F32, p5)
            t_w5 = S.tok("w5")
            wbh_v = wbh.rearrange("(k p) c -> p k c", p=128)
            wbd_v = wbd.rearrange("(k p) c -> p k c", p=128)
            wo_v = w_out.rearrange("(k p) c -> p k c", p=128)
            stg = [sb("p5stg0", [128, 8, 512], F32, p5)] * 2
            t_stg = [S.tok("stg")] * 2
            t_wbh = [S.tok("wbh") for _ in range(2)]
            t_wbd = [S.tok("wbd") for _ in range(2)]
            t_wo = [S.tok("wo") for _ in range(2)]
            t_wg = [S.tok("wg") for _ in range(4)]
            sgc = [0]
            def ldcast(src_ap, dst_ap, kk_, tk):
                g = sgc[0] % 2
                sgc[0] += 1
                S.op("sp", lambda e, inc, g=g: inc(e.dma_start(out=stg[g][:, 0:kk_, :], in_=src_ap)), w=[t_stg[g]], dma=True)
                if g == 0:
                    S.op("dve", lambda e, g=g: e.tensor_copy(out=dst_ap, in_=stg[g][:, 0:kk_, :]), r=[t_stg[g]], w=[tk])
                else:
                    S.op("act", lambda e, g=g: e.activation(out=dst_ap, in_=stg[g][:, 0:kk_, :], func=AF.Copy), r=[t_stg[g]], w=[tk])
            def ld_wg(n):
                ldcast(w_in_v[:, :, 3584 + n * 512: 3584 + (n + 1) * 512], wg_s[:, :, n * 512:(n + 1) * 512], 8, t_wg[n])
            for n in range(2):
                ldcast(wbh_v[:, :, n * 512:(n + 1) * 512], wbh_s[:, :, n * 512:(n + 1) * 512], 4, t_wbh[n])
                ldcast(wbd_v[:, :, n * 512:(n + 1) * 512], wbd_s[:, :, n * 512:(n + 1) * 512], 4, t_wbd[n])
                ld_wg(n)
                ld_wg(2 + n)
            for n in range(2):
                ldcast(wo_v[:, :, n * 512:(n + 1) * 512], wo_s[:, :, n * 512:(n + 1) * 512], 8, t_wo[n])
            S.op("sp", lambda e, inc: (inc(e.dma_start(out=nm_s[:], in_=nmoe)), inc(e.dma_start(out=wr_s[:], in_=wr.rearrange("(k p) c -> p k c", p=128))),
                                       inc(e.dma_start(out=br_s[:], in_=br))), w=[t_w5], dma=True)
            hTb = [sb(f"p5hT{i}", [128, 8, 512], BF16, p5) for i in range(2)]
            ohb = [sb(f"p5oh{i}", [128, 4, 512], BF16, p5) for i in range(2)]
            odb = [sb(f"p5od{i}", [128, 4, 512], BF16, p5) for i in range(2)]
            t_hTb = [S.tok("hTb") for _ in range(2)]
            t_ohb = [S.tok("ohb") for _ in range(2)]
            t_odb = [S.tok("odb") for _ in range(2)]
            mixT = [sb(f"p5mix{i}", [128, 8, 512], BF16, p5) for i in range(2)]
            t_mix = [S.tok("mix") for _ in range(2)]
            def mk5(name, dt, n=2, shape=(128, 512)):
                return [sb(f"p5{name}{i}", list(shape), dt, p5) for i in range(n)], [S.tok(name) for _ in range(n)]
            s1b, t_s1 = mk5("s1", F32)
            s2b, t_s2 = mk5("s2", F32)
            m1b, t_m1 = mk5("m1", F32, 1)
            m2b, t_m2 = mk5("m2", F32, 1)
            xt5, t_xt5 = mk5("xt", F32, 2, (128, D))
            x2b, t_x2 = mk5("x2", F32, 2, (128, D))
            xnf, t_xnf = mk5("xnf", F32, 2, (128, D))
            xnb, t_xnb = mk5("xnb", BF16, 2, (128, D))
            xnT, t_xnT = mk5("xnT", F32, 2, (128, D))
            st5, t_st5 = mk5("st", F32, 2, (128, 2))
            def tile_S1(i, mb, ti):
                u = i % 2
                if i == 0:
                    S.op("sp", lambda e, inc: inc(e.dma_start(out=xt5[0][:], in_=x[0:128, :])), w=[t_xt5[0]], dma=True)
                if i + 1 < NT:
                    S.op("sp", lambda e, inc: inc(e.dma_start(out=xt5[(i + 1) % 2][:], in_=x[(i + 1) * 128:(i + 2) * 128, :])), w=[t_xt5[(i + 1) % 2]], dma=True)
                for n in range(2):
                    for k in range(8):
                        S.op("pe", mm(ps[n][:, :], mixT[mb][:, k, ti * 128:(ti + 1) * 128], wo_s[:, k, n * 512:(n + 1) * 512], k == 0, k == 7),
                             r=[t_mix[mb], t_wo[n]], w=[pst[n]])
                    S.op("dve", lambda e, n=n: e.tensor_tensor(out=x2b[u][:, n * 512:(n + 1) * 512], in0=ps[n][:, :], in1=xt5[u][:, n * 512:(n + 1) * 512],
                                                              op=ALU.add), r=[pst[n], t_xt5[u]], wd=[t_x2[u]])

            def tile_S2(i):
                u = i % 2
                S.op("sp", lambda e, inc: inc(e.dma_start(out=x2_d[i * 128:(i + 1) * 128, :], in_=x2b[u][:])), r=[t_x2[u]], w=[t_x2d[i]], dma=True)
                if "x2" in dbg:
                    S.op("sp", lambda e, inc: inc(e.dma_start(out=dbg_t["x2"][i * 128:(i + 1) * 128, :], in_=x2b[u][:])), r=[t_x2[u]], dma=True)
                S.op("act", lambda e: e.activation(out=xnT[u][:], in_=x2b[u][:], func=AF.Square), r=[t_x2[u]], w=[t_xnT[u]])
                S.op("dve", lambda e: e.reduce_sum(out=st5[u][:, 0:1], in_=xnT[u][:], axis=AX.X), r=[t_xnT[u]], w=[t_st5[u]])
                S.op("act", lambda e: e.activation(out=st5[u][:, 1:2], in_=st5[u][:, 0:1], func=AF.Ln, scale=1.0 / D, bias=EPSB), r=[t_st5[u]], w=[t_st5[u]])
                S.op("act", lambda e: e.activation(out=st5[u][:, 1:2], in_=st5[u][:, 1:2], func=AF.Exp, scale=-0.5), r=[t_st5[u]], w=[t_st5[u]])
                S.op("dve", lambda e: e.scalar_tensor_tensor(out=xnf[u][:], in0=x2b[u][:], scalar=st5[u][:, 1:2], in1=nm_s[:], op0=ALU.mult, op1=ALU.mult),
                     r=[t_x2[u], t_st5[u], t_w5], w=[t_xnf[u]])
                S.op("act", lambda e: e.activation(out=xnb[u][:].rearrange("p (k j) -> p k j", k=8), in_=xnf[u][:].rearrange("p (j k) -> p k j", k=8),
                                                   func=AF.Copy), r=[t_xnf[u]], w=[t_xnb[u]])
                S.op("sp", lambda e, inc: inc(e.dma_start(out=xn_d[i * 128:(i + 1) * 128, :], in_=xnb[u][:])), r=[t_xnb[u]], w=[t_xnd[i]], dma=True)

            def tile_S3(i):
                u = i % 2
                for k in range(8):
                    bank = 2 + k // 4
                    S.op("pe", lambda e, k=k, bank=bank: e.transpose(out=ps[bank][:, (k % 4) * 128:(k % 4 + 1) * 128],
                                                                   in_=xnf[u][:, k * 128:(k + 1) * 128], identity=IDF),
                         r=[t_xnf[u]], w=[pst[bank]])
                S.op("act", lambda e: e.activation(out=xnT[u][:, 0:512], in_=ps[2][:, :], func=AF.Copy), r=[pst[2]], wd=[t_xnT[u]])
                S.op("dve", lambda e: e.tensor_copy(out=xnT[u][:, 512:1024], in_=ps[3][:, :]), r=[pst[3]], wd=[t_xnT[u]])

            def tile_S4(i):
                u = i % 2
                for k in range(8):
                    S.op("pe", mm(ps[2][:, 0:36], xnT[u][:, k * 128:(k + 1) * 128], wr_s[:, k, :], k == 0, k == 7), r=[t_xnT[u], t_w5], w=[pst[2]])
                S.op("dve", lambda e: e.tensor_tensor(out=logit[:, i, :], in0=ps[2][:, 0:36], in1=br_s[:], op=ALU.add), r=[pst[2], t_w5], wd=[t_logit])

            cc = 0
            for b in range(NB):
                tb = slice(b * 512, (b + 1) * 512)
                mb = b % 2
                S.op("sp", lambda e, inc, mb=mb, tb=tb: inc(e.dma_start(out=hTb[mb][:], in_=hT_d[:, :, tb])), r=[t_hTd], w=[t_hTb[mb]], dma=True)
                S.op("sp", lambda e, inc, mb=mb, tb=tb: inc(e.dma_start(out=ohb[mb][:], in_=ohg_d[:, :, tb])), r=[t_ohgd[k][b] for k in range(4)], w=[t_ohb[mb]], dma=True)
                S.op("sp", lambda e, inc, mb=mb, tb=tb: inc(e.dma_start(out=odb[mb][:], in_=oda_d[:, :, tb])), r=[t_odad[k][b] for k in range(4)], w=[t_odb[mb]], dma=True)
                for c in range(8):
                    u = cc % 2
                    cc += 1
                    cs = slice(c * 128, (c + 1) * 128)
                    pb0 = 4 * u
                    for k in range(4):
                        S.op("pe", mm(ps[pb0][:, :], wbh_s[:, k, cs], ohb[mb][:, k, :], k == 0, k == 3), r=[t_wbh[c // 4], t_ohb[mb]], w=[pst[pb0]])
                    for k in range(4):
                        S.op("pe", mm(ps[pb0 + 1][:, :], wbd_s[:, k, cs], odb[mb][:, k, :], k == 0, k == 3), r=[t_wbd[c // 4], t_odb[mb]], w=[pst[pb0 + 1]])
                    for k in range(8):
                        S.op("pe", mm(ps[pb0 + 2][:, :], wg_s[:, k, c * 128:(c + 1) * 128], hTb[mb][:, k, :], k == 0, k == 7), r=[t_wg[c // 4], t_hTb[mb]], w=[pst[pb0 + 2]])
                    for k in range(8):
                        S.op("pe", mm(ps[pb0 + 3][:, :], wg_s[:, k, 1024 + c * 128:1024 + (c + 1) * 128], hTb[mb][:, k, :], k == 0, k == 7),
                             r=[t_wg[2 + c // 4], t_hTb[mb]], w=[pst[pb0 + 3]])
                    S.op("act", lambda e, u=u, pb0=pb0: e.activation(out=s1b[u][:], in_=ps[pb0 + 2][:, :], func=AF.Sigmoid), r=[pst[pb0 + 2]], w=[t_s1[u]])
                    S.op("act", lambda e, u=u, pb0=pb0: e.activation(out=s2b[u][:], in_=ps[pb0 + 3][:, :], func=AF.Sigmoid), r=[pst[pb0 + 3]], w=[t_s2[u]])
                    S.op("dve", lambda e, u=u, pb0=pb0: e.tensor_tensor(out=m1b[0][:], in0=ps[pb0][:, :], in1=s1b[u][:], op=ALU.mult), r=[pst[pb0], t_s1[u]], w=[t_m1[0]])
                    S.op("dve", lambda e, u=u, pb0=pb0: e.tensor_tensor(out=m2b[0][:], in0=ps[pb0 + 1][:, :], in1=s2b[u][:], op=ALU.mult), r=[pst[pb0 + 1], t_s2[u]], w=[t_m2[0]])
                    S.op("dve", lambda e, mb=mb, c=c: e.tensor_tensor(out=mixT[mb][:, c, :], in0=m1b[0][:], in1=m2b[0][:], op=ALU.add),
                         r=[t_m1[0], t_m2[0]], wd=[t_mix[mb]])
                for ti in range(4):
                    i = b * 4 + ti
                    if i >= 1:
                        tile_S3(i - 1)
                    tile_S1(i, mb, ti)
                    tile_S2(i)
                    if i >= 1:
                        tile_S4(i - 1)
            tile_S3(NT - 1)
            tile_S4(NT - 1)
            if "x2" in dbg:
                S.op("sp", lambda e, inc: inc(e.dma_start(out=dbg_t["lg"], in_=logit[:])), r=[t_logit], dma=True)
            S.barrier()
        with ExitStack() as p5:
            ecap_s = sb("p5ecap", [128, 32], F32, p5)
            xnb = [sb(f"p5cxnb{i}", [128, D], BF16, p5) for i in range(4)]
            t_xnb = [S.tok("cxnb") for _ in range(4)]
            t_w5 = S.tok("w5b")
            S.op("sp", lambda e, inc: inc(e.dma_start(out=ecap_s[:], in_=ecap)), w=[t_w5], dma=True)
            def rb(name, shape, dt=F32):
                return sb("r_" + name, shape, dt, p5)
            t_r = S.tok("router")
            G = logit[:, :, 0:4]
            E4 = logit[:, :, 4:36].rearrange("p n (g j) -> p n g j", g=4)
            gmax = rb("gmax", [128, NT]); goh = rb("goh", [128, NT, 4]); gsh = rb("gsh", [128, NT, 4]); gsum = rb("gsum", [128, NT])
            gw = rb("gw", [128, NT]); sel = rb("sel", [128, NT, 4, 8]); eg = rb("eg", [128, NT, 8]); m1 = rb("m1", [128, NT])
            oh1 = rb("oh1", [128, NT, 8]); eg2 = rb("eg2", [128, NT, 8]); m2 = rb("m2", [128, NT]); oh2 = rb("oh2", [128, NT, 8])
            dd = rb("dd", [128, NT]); ex = rb("ex", [128, NT]); den = rb("den", [128, NT])
            A1 = rb("A1", [128, NT, 4, 8]); A2 = rb("A2", [128, NT, 4, 8]); Ab = rb("Ab", [128, NT, 32], BF16)
            rk = rb("rk", [128, NT, 32]); tmp5 = rb("tmp5", [128, NT, 32]); sl = rb("sl", [128, 2, NT])
            def R_(eng, fn):
                S.op(eng, fn, r=[t_r, t_logit], w=[t_r])
            R_("dve", lambda e: e.tensor_reduce(out=gmax[:], in_=G, axis=AX.X, op=ALU.max))
            R_("dve", lambda e: e.tensor_tensor(out=goh[:], in0=G, in1=gmax[:].unsqueeze(2).to_broadcast([128, NT, 4]), op=ALU.is_equal))
            R_("dve", lambda e: e.tensor_tensor(out=gsh[:], in0=G, in1=gmax[:].unsqueeze(2).to_broadcast([128, NT, 4]), op=ALU.subtract))
            R_("act", lambda e: e.activation(out=gsh[:], in_=gsh[:], func=AF.Exp))
            R_("dve", lambda e: e.tensor_reduce(out=gsum[:], in_=gsh[:], axis=AX.X, op=ALU.add))
            R_("dve", lambda e: e.reciprocal(out=gw[:], in_=gsum[:]))
            R_("dve", lambda e: e.tensor_tensor(out=sel[:], in0=E4, in1=goh[:].unsqueeze(3).to_broadcast([128, NT, 4, 8]), op=ALU.mult))
            R_("dve", lambda e: e.tensor_reduce(out=eg[:], in_=sel[:].rearrange("p n g j -> p n j g"), axis=AX.X, op=ALU.add))
            R_("dve", lambda e: e.tensor_reduce(out=m1[:], in_=eg[:], axis=AX.X, op=ALU.max))
            R_("dve", lambda e: e.tensor_tensor(out=oh1[:], in0=eg[:], in1=m1[:].unsqueeze(2).to_broadcast([128, NT, 8]), op=ALU.is_equal))
            R_("dve", lambda e: e.scalar_tensor_tensor(out=eg2[:], in0=oh1[:], scalar=-1e30, in1=eg[:], op0=ALU.mult, op1=ALU.add))
            R_("dve", lambda e: e.tensor_reduce(out=m2[:], in_=eg2[:], axis=AX.X, op=ALU.max))
            R_("dve", lambda e: e.tensor_tensor(out=oh2[:], in0=eg2[:], in1=m2[:].unsqueeze(2).to_broadcast([128, NT, 8]), op=ALU.is_equal))
            R_("dve", lambda e: e.tensor_sub(out=dd[:], in0=m2[:], in1=m1[:]))
            R_("act", lambda e: e.activation(out=ex[:], in_=dd[:], func=AF.Exp))
            R_("dve", lambda e: e.tensor_scalar_add(out=den[:], in0=ex[:], scalar1=1.0))
            R_("dve", lambda e: e.reciprocal(out=den[:], in_=den[:]))
            S.op("dve", lambda e: e.tensor_mul(out=wgt[:, 0, :], in0=den[:], in1=gw[:]), r=[t_r], w=[t_r, t_route])
            S.op("dve", lambda e: e.tensor_mul(out=wgt[:, 1, :], in0=wgt[:, 0, :], in1=ex[:]), r=[t_r, t_route], w=[t_r, t_route])
            R_("dve", lambda e: e.tensor_tensor(out=A1[:], in0=goh[:].unsqueeze(3).to_broadcast([128, NT, 4, 8]),
                                                in1=oh1[:].unsqueeze(2).to_broadcast([128, NT, 4, 8]), op=ALU.mult))
            R_("dve", lambda e: e.tensor_tensor(out=A2[:], in0=goh[:].unsqueeze(3).to_broadcast([128, NT, 4, 8]),
                                                in1=oh2[:].unsqueeze(2).to_broadcast([128, NT, 4, 8]), op=ALU.mult))
            R_("dve", lambda e: e.tensor_tensor(out=Ab[:], in0=A1[:].rearrange("p n g j -> p n (g j)"), in1=A2[:].rearrange("p n g j -> p n (g j)"), op=ALU.add))
            for i in range(NT):
                bank = i // 16
                oc = slice((i % 16) * 32, (i % 16) * 32 + 32)
                S.op("pe", mm(ps[bank][:, oc], LSTR, Ab[:, i, :], True, i == 0), r=[t_r], w=[pst[bank]])
                for i2 in range(i):
                    S.op("pe", mm(ps[bank][:, oc], ONESB, Ab[:, i2, :], False, i2 == i - 1), r=[t_r], w=[pst[bank]])
            nb_ = (NT + 15) // 16
            for bank in range(nb_):
                n0 = bank * 16
                n1 = min(NT, n0 + 16)
                S.op("dve", lambda e, bank=bank, n0=n0, n1=n1: e.tensor_tensor(
                    out=rk[:, n0:n1, :], in0=ps[bank][:, 0:(n1 - n0) * 32].rearrange("p (n e) -> p n e", e=32),
                    in1=ecap_s[:].unsqueeze(1).to_broadcast([128, n1 - n0, 32]), op=ALU.add), r=[pst[bank], t_r, t_w5], w=[t_r])
            for a_, Aa in ((0, A1), (1, A2)):
                R_("dve", lambda e, Aa=Aa: e.tensor_tensor(out=tmp5[:], in0=rk[:], in1=Aa[:].rearrange("p n g j -> p n (g j)"), op=ALU.mult))
                R_("dve", lambda e, a_=a_: e.tensor_reduce(out=sl[:, a_, :], in_=tmp5[:], axis=AX.X, op=ALU.add))
            S.op("dve", lambda e: e.tensor_copy(out=slot_i[:], in_=sl[:]), r=[t_r], w=[t_route])
            S.barrier()
            if "rt" in dbg:
                S.op("sp", lambda e, inc: (inc(e.dma_start(out=dbg_t["rt"][:, 0:2, :], in_=sl[:])), inc(e.dma_start(out=dbg_t["rt"][:, 2:4, :], in_=wgt[:]))),
                     r=[t_r, t_route], dma=True)
                S.barrier()
            for i in range(NT):
                u = i % 4
                S.op("sp", lambda e, inc, i=i, u=u: inc(e.dma_start(out=xnb[u][:], in_=xn_d[i * 128:(i + 1) * 128, :])), r=[t_xnd[i]], w=[t_xnb[u]], dma=True)
                for a_ in range(2):
                    S.op("pool", lambda e, inc, i=i, u=u, a_=a_: inc(e.indirect_dma_start(
                        out=xg_d[:, :], out_offset=bass.IndirectOffsetOnAxis(ap=slot_i[:, a_, i:i + 1], axis=0),
                        in_=xnb[u][:], in_offset=None, bounds_check=BCREG, oob_is_err=False)),
                        r=[t_xnb[u], t_route], wd=[t_xg], dma=True)
            S.barrier()

        with ExitStack() as p6:
            stg6 = [sb(f"p6stg{i}", [128, 8, 512], F32, p6) for i in range(3)]
            t_stg6 = [S.tok("stg6") for _ in range(3)]
            w1b = [sb(f"p6w1{i}", [128, 8, 512], BF16, p6) for i in range(2)]
            w3b = [sb(f"p6w3{i}", [128, 8, 512], BF16, p6) for i in range(2)]
            w2b = [sb(f"p6w2{i}", [128, 4, D], BF16, p6) for i in range(2)]
            t_w1 = [S.tok("w1") for _ in range(2)]
            t_w3 = [S.tok("w3") for _ in range(2)]
            t_w2 = [S.tok("w2") for _ in range(2)]
            xg_s = [sb(f"p6xg{i}", [128, CT, D], BF16, p6) for i in range(2)]
            t_xgs = [S.tok("xgs") for _ in range(2)]
            xgT = [sb(f"p6xgT{i}", [128, 8, CAP], BF16, p6) for i in range(2)]
            t_xgT = [S.tok("xgT") for _ in range(2)]
            sil = [sb(f"p6sil{i}", [128, CAP], F32, p6) for i in range(2)]
            t_sil = [S.tok("sil") for _ in range(2)]
            hid = [sb(f"p6hid{i}", [128, 4, CAP], BF16, p6) for i in range(2)]
            t_hid = [S.tok("hid") for _ in range(2)]
            ysb = [sb(f"p6y{i}", [128, D], F32, p6) for i in range(2)]
            t_ysb = [S.tok("ysb") for _ in range(2)]
            w1_v = w1.rearrange("e (p k) c -> e p k c", k=8)
            w3_v = w3.rearrange("e (p k) c -> e p k c", k=8)
            w2_v = w2.rearrange("e (p k) c -> e p k c", k=4)
            stg2v = stg6[2][:].rearrange("p k c -> p (k c)").rearrange("p (k c) -> p k c", k=4)

            def load_expert(ex_):
                u = ex_ % 2
                S.op("sp", lambda e, inc: inc(e.dma_start(out=stg6[0][:], in_=w1_v[ex_])), w=[t_stg6[0]], dma=True)
                S.op("sp", lambda e, inc: inc(e.dma_start(out=stg6[1][:], in_=w3_v[ex_])), w=[t_stg6[1]], dma=True)
                S.op("sp", lambda e, inc: inc(e.dma_start(out=stg2v, in_=w2_v[ex_])), w=[t_stg6[2]], dma=True)
                S.op("sp", lambda e, inc: inc(e.dma_start(out=xg_s[u][:], in_=xg_d[ex_ * CAP:(ex_ + 1) * CAP, :].rearrange("(c p) d -> p c d", p=128))),
                     r=[t_xg], w=[t_xgs[u]], dma=True)

            def cast_expert(ex_):
                u = ex_ % 2
                S.op("dve", lambda e: e.tensor_copy(out=w1b[u][:].rearrange("p k (kk m) -> p k kk m", kk=4),
                                                    in_=stg6[0][:].rearrange("p k (m kk) -> p k kk m", kk=4)), r=[t_stg6[0]], w=[t_w1[u]])
                S.op("act", lambda e: e.activation(out=w3b[u][:].rearrange("p k (kk m) -> p k kk m", kk=4),
                                                   in_=stg6[1][:].rearrange("p k (m kk) -> p k kk m", kk=4), func=AF.Copy), r=[t_stg6[1]], w=[t_w3[u]])
                S.op("dve", lambda e: e.tensor_copy(out=w2b[u][:, 0:2, :], in_=stg2v[:, 0:2, :]), r=[t_stg6[2]], wd=[t_w2[u]])
                S.op("act", lambda e: e.activation(out=w2b[u][:, 2:4, :], in_=stg2v[:, 2:4, :], func=AF.Copy), r=[t_stg6[2]], wd=[t_w2[u]])

            load_expert(0)
            cast_expert(0)
            yc = 0
            for ex_ in range(NE):
                u = ex_ % 2
                if ex_ + 1 < NE:
                    load_expert(ex_ + 1)
                for c in range(CT):
                    for k in range(8):
                        bank = 6 + (k // 4) % 2
                        S.op("pe", lambda e, u=u, c=c, k=k, bank=bank: e.transpose(out=psb(bank)[:, (k % 4) * 128:(k % 4 + 1) * 128],
                                                                                   in_=xg_s[u][:, c, k * 128:(k + 1) * 128], identity=IDB),
                             r=[t_xgs[u]], w=[pst[bank]])
                        if k % 4 == 3:
                            kb = k - 3
                            if (k // 4) % 2 == 0:
                                S.op("dve", lambda e, u=u, c=c, kb=kb, bank=bank: e.tensor_copy(
                                    out=xgT[u][:, kb:kb + 4, c * 128:(c + 1) * 128], in_=psb(bank)[:, 0:512].rearrange("p (k t) -> p k t", k=4)),
                                    r=[pst[bank]], wd=[t_xgT[u]])
                            else:
                                S.op("act", lambda e, u=u, c=c, kb=kb, bank=bank: e.activation(
                                    out=xgT[u][:, kb:kb + 4, c * 128:(c + 1) * 128], in_=psb(bank)[:, 0:512].rearrange("p (k t) -> p k t", k=4), func=AF.Copy),
                                    r=[pst[bank]], wd=[t_xgT[u]])
                for fc in range(4):
                    pb0 = 2 * (fc % 2)
                    fs = slice(fc * 128, (fc + 1) * 128)
                    for k in range(8):
                        S.op("pe", mm(ps[pb0][:, 0:CAP], w1b[u][:, k, fs], xgT[u][:, k, :], k == 0, k == 7), r=[t_w1[u], t_xgT[u]], w=[pst[pb0]])
                    for k in range(8):
                        S.op("pe", mm(ps[pb0 + 1][:, 0:CAP], w3b[u][:, k, fs], xgT[u][:, k, :], k == 0, k == 7), r=[t_w3[u], t_xgT[u]], w=[pst[pb0 + 1]])
                    v_ = fc % 2
                    S.op("act", lambda e, v_=v_, pb0=pb0: e.activation(out=sil[v_][:], in_=ps[pb0][:, 0:CAP], func=AF.Silu), r=[pst[pb0]], w=[t_sil[v_]])
                    S.op("dve", lambda e, v_=v_, pb0=pb0, u=u, fc=fc: e.tensor_tensor(out=hid[u][:, fc, :], in0=ps[pb0 + 1][:, 0:CAP], in1=sil[v_][:], op=ALU.mult),
                         r=[pst[pb0 + 1], t_sil[v_]], wd=[t_hid[u]])
                for c in range(CT):
                    yu = yc % 2
                    yc += 1
                    for n in range(2):
                        bank = 4 + n
                        for k in range(4):
                            S.op("pe", mm(ps[bank][:, :], hid[u][:, k, c * 128:(c + 1) * 128], w2b[u][:, k, n * 512:(n + 1) * 512], k == 0, k == 3),
                                 r=[t_hid[u], t_w2[u]], w=[pst[bank]])
                        if n == 0:
                            S.op("act", lambda e, yu=yu, bank=bank: e.activation(out=ysb[yu][:, 0:512], in_=ps[bank][:, :], func=AF.Copy), r=[pst[bank]], wd=[t_ysb[yu]])
                        else:
                            S.op("dve", lambda e, yu=yu, bank=bank: e.tensor_copy(out=ysb[yu][:, 512:1024], in_=ps[bank][:, :]), r=[pst[bank]], wd=[t_ysb[yu]])
                    r0_ = ex_ * CAP + c * 128
                    S.op("sp", lambda e, inc, yu=yu, r0_=r0_: inc(e.dma_start(out=y_d[r0_:r0_ + 128, :], in_=ysb[yu][:])), r=[t_ysb[yu]], wd=[t_yd], dma=True)
                if ex_ + 1 < NE:
                    cast_expert(ex_ + 1)
            S.barrier()

        with ExitStack() as p7:
            NB7 = 4
            x2s = [sb(f"p7x{i}", [128, D], F32, p7) for i in range(NB7)]
            ya = [sb(f"p7ya{i}", [128, D], F32, p7) for i in range(NB7)]
            yb = [sb(f"p7yb{i}", [128, D], F32, p7) for i in range(NB7)]
            t_x2s = [S.tok("x2s") for _ in range(NB7)]
            t_ya = [S.tok("ya") for _ in range(NB7)]
            t_yb = [S.tok("yb") for _ in range(NB7)]
            for i in range(NT):
                u = i % NB7
                S.op("sp", lambda e, inc, i=i, u=u: inc(e.dma_start(out=x2s[u][:], in_=x2_d[i * 128:(i + 1) * 128, :])), r=[t_x2d[i]], w=[t_x2s[u]], dma=True)
                for a_, (yy, t_yy) in enumerate(((ya, t_ya), (yb, t_yb))):
                    S.op("pool", lambda e, inc, i=i, u=u, a_=a_, yy=yy: inc(e.indirect_dma_start(
                        out=yy[u][:], out_offset=None, in_=y_d[:, :],
                        in_offset=bass.IndirectOffsetOnAxis(ap=slot_i[:, a_, i:i + 1], axis=0), bounds_check=BCREG, oob_is_err=False)),
                        r=[t_yd, t_route], w=[t_yy[u]], dma=True)
                S.op("dve", lambda e, i=i, u=u: e.scalar_tensor_tensor(out=x2s[u][:], in0=ya[u][:], scalar=wgt[:, 0, i:i + 1], in1=x2s[u][:], op0=ALU.mult, op1=ALU.add),
                     r=[t_ya[u], t_x2s[u], t_route], w=[t_x2s[u]])
                S.op("dve", lambda e, i=i, u=u: e.scalar_tensor_tensor(out=x2s[u][:], in0=yb[u][:], scalar=wgt[:, 1, i:i + 1], in1=x2s[u][:], op0=ALU.mult, op1=ALU.add),
                     r=[t_yb[u], t_x2s[u], t_route], w=[t_x2s[u]])
                S.op("sp", lambda e, inc, i=i, u=u: inc(e.dma_start(out=out[i * 128:(i + 1) * 128, :], in_=x2s[u][:])), r=[t_x2s[u]], dma=True)
            S.barrier()
        S.barrier()
    return nc


def host_consts(T):
    CAP = cap_for(T)
    p = np.arange(128)
    cm = np.zeros((128, 7, 128), np.float32)
    cm[:, 0, :] = np.eye(128, dtype=np.float32)
    cm[:, 1, :] = ((p[:, None] // 64 == p[None, :] // 64) & (p[:, None] <= p[None, :])).astype(np.float32)
    cm[:, 2, :] = (p[:, None] <= p[None, :]).astype(np.float32)
    cm[:, 3, :] = (p[:, None] < p[None, :]).astype(np.float32)
    cm[:, 4, :] = 1.0
    cm[:, 5, :] = (p[:, None] // 64 == p[None, :] // 64).astype(np.float32)
    m = p
    src = np.where((m % 64) < 32, m + 32, m - 32)
    perm = np.zeros((128, 128), np.float32)
    perm[src, m] = 1.0
    cm[:, 6, :] = perm
    rmask = np.ones((128, 512), np.float32)
    rmask[:, ::64] = 0.0
    ecap = np.broadcast_to((np.arange(32, dtype=np.float32) * CAP)[None, :], (128, 32)).copy()
    half = 32
    inv = (np.float32(10000.0) ** (-np.arange(half, dtype=np.float32) / np.float32(half))).astype(np.float32)
    ang = np.arange(T, dtype=np.float32)[:, None] * inv[None, :]
    cos = np.cos(ang).astype(np.float32).T
    sin = np.sin(ang).astype(np.float32).T
    ropec = np.concatenate([cos, cos, cos, cos], axis=0)
    ropes = np.concatenate([-sin, sin, -sin, sin], axis=0)
    return dict(cmat=cm, rmask=rmask, ecap=ecap, ropec=np.ascontiguousarray(ropec), ropes=np.ascontiguousarray(ropes))


def host_layout(inp, T):
    f = lambda a: np.ascontiguousarray(np.asarray(a, dtype=np.float32))
    d = {}
    d["w_in"] = f(inp["w_in"][0])
    d["gmix"] = f(np.asarray(inp["norm_mix"][0]).reshape(8, 128).T)
    d["hglb"] = f(np.asarray(inp["hg_lb"]).reshape(2, 4, 128).transpose(2, 0, 1).reshape(128, 8))
    d["hgon"] = f(np.asarray(inp["hg_out_norm"][0]).reshape(128, 1))
    d["qkn"] = f(np.stack([np.tile(np.asarray(inp["da_q_norm"][0]), 2), np.tile(np.asarray(inp["da_k_norm"][0]), 2)], axis=1))
    d["lamb"] = f(np.broadcast_to(np.asarray(inp["da_lambda"][0]).reshape(1, 256), (128, 256)))
    d["daon"] = f(np.asarray(inp["da_out_norm"][0]).reshape(128, 1))
    d["wbh"] = f(inp["w_branch_hg"][0])
    d["wbd"] = f(inp["w_branch_da"][0])
    d["w_out"] = f(inp["w_out"][0])
    d["nmoe"] = f(np.broadcast_to(np.asarray(inp["norm_moe"][0]).reshape(1, D), (128, D)))
    d["wr"] = f(np.concatenate([np.asarray(inp["w_router_group"][0]), np.asarray(inp["w_router_expert"][0])], axis=1))
    d["br"] = f(np.broadcast_to(np.concatenate([np.asarray(inp["b_router_group"][0]),
                                                np.asarray(inp["b_router_expert"][0])]).reshape(1, 36), (128, 36)))
    d["w1"] = f(inp["w1"][0])
    d["w3"] = f(inp["w3"][0])
    d["w2"] = f(inp["w2"][0])
    d.update(host_consts(T))
    return d


_NC_CACHE = {}


def kernel(**inputs):
    xfull = np.asarray(inputs["x"], dtype=np.float32)
    B, T, _ = xfull.shape
    shared = host_layout(inputs, T)
    if T not in _NC_CACHE:
        _NC_CACHE[T] = build(T)
    nc = _NC_CACHE[T]
    in_maps = []
    for c in range(B):
        m = dict(shared)
        m["x"] = np.ascontiguousarray(xfull[c])
        in_maps.append(m)
    res = run_bass_kernel_spmd(nc, in_maps, core_ids=list(range(B)))
    return np.stack([np.asarray(r["out"], dtype=np.float32) for r in res.results], axis=0)
```

```python
import math
from contextlib import ExitStack

import numpy as np
import concourse.bass as bass
import concourse.mybir as mybir
from concourse.bass_utils import run_bass_kernel_spmd

F32 = mybir.dt.float32
BF16 = mybir.dt.bfloat16
I32 = mybir.dt.int32
AF = mybir.ActivationFunctionType
ALU = mybir.AluOpType
AX = mybir.AxisListType

D = 1024
IN_COLS = 5632
NE = 32
DFF = 512
EPS = 1e-6
ENGS = ("pe", "act", "dve", "pool", "sp")
SEM_LIMIT = 30000


class Tok:
    __slots__ = ("name", "writers", "rc", "rd")

    def __init__(self, name):
        self.name = name
        self.writers = []
        self.rc = {}
        self.rd = []

    def reset(self):
        self.writers = []
        self.rc = {}
        self.rd = []


class Op:
    __slots__ = ("eng", "fn", "dma", "deps", "signal", "sem", "val")


class Sched:
    def __init__(self, nc, es, n_dma=32, n_eng=5, n_sw=12):
        self.nc = nc
        self.e = {"pe": nc.tensor, "act": nc.scalar, "dve": nc.vector, "pool": nc.gpsimd, "sp": nc.sync}
        self.ops = []
        self.emitted = 0
        self.toks = []
        self.sems = {}
        for k in ("pe", "act", "dve", "pool"):
            for j in range(n_eng):
                self.sems[("e", k, j)] = es.enter_context(nc.semaphore(f"se_{k}{j}"))
        self.dsem_n = n_dma
        for j in range(n_dma):
            self.sems[("d", j)] = es.enter_context(nc.semaphore(f"sd_{j}"))
        self.eidx = {k: 0 for k in ENGS}
        self.ecount = {k: 0 for k in ENGS}
        self.n_eng = n_eng
        self.dtarget = [0] * (n_dma + n_sw)
        self.dnext = 0
        self.n_sw = n_sw
        self.swnext = 0
        for j in range(n_dma, n_dma + n_sw):
            self.sems[("d", j)] = es.enter_context(nc.semaphore(f"sw_{j}"))
        self.waited = {k: {} for k in ENGS}
        self.nwaits = 0

    def tok(self, name="t"):
        t = Tok(name)
        self.toks.append(t)
        return t

    def op(self, eng, fn, r=(), w=(), wd=(), dma=False):
        idx = len(self.ops)
        o = Op()
        o.eng, o.fn, o.dma, o.signal, o.sem, o.val = eng, fn, dma, False, None, 0
        deps = {}
        ops = self.ops

        def add(pidx, raw):
            p = ops[pidx]
            if p.eng == eng and not p.dma and not dma and eng == "pe":
                return
            deps[pidx] = True

        for t in r:
            for pw in t.writers:
                add(pw, True)
        for t in list(w) + list(wd):
            for pw in t.writers:
                add(pw, False)
            for pr in t.rc.values():
                add(pr, False)
            for pr in t.rd:
                add(pr, False)
        for t in r:
            if dma:
                t.rd.append(idx)
            else:
                t.rc[eng] = idx
        for t in w:
            t.writers = [idx]
            t.rc = {}
            t.rd = []
        for t in wd:
            t.writers.append(idx)
        o.deps = sorted(deps)
        ops.append(o)
        return idx

    def _wait(self, eng, key, val):
        if val <= 0 or key is None:
            return
        if self.waited[eng].get(key, 0) >= val:
            return
        self.e[eng].wait_ge(self.sems[key], val)
        self.waited[eng][key] = val
        self.nwaits += 1

    def flush(self):
        ops = self.ops
        for i in range(self.emitted, len(ops)):
            for d in ops[i].deps:
                ops[d].signal = True
        for i in range(self.emitted, len(ops)):
            o = ops[i]
            e = self.e[o.eng]
            for d in o.deps:
                self._wait(o.eng, ops[d].sem, ops[d].val)
            if o.dma:
                if o.eng == "pool":
                    k = self.dsem_n + self.swnext
                    self.swnext = (self.swnext + 1) % self.n_sw
                else:
                    k = self.dnext
                    self.dnext = (k + 1) % self.dsem_n
                key = ("d", k)
                self._wait(o.eng, key, self.dtarget[k])
                s = self.sems[key]
                cnt = [0]

                def inc(ins, s=s, cnt=cnt):
                    ins.then_inc(s, 16)
                    cnt[0] += 1
                    return ins

                o.fn(e, inc)
                self.dtarget[k] += 16 * cnt[0]
                o.sem, o.val = key, self.dtarget[k]
            else:
                ins = o.fn(e)
                if o.signal:
                    c = self.ecount[o.eng] + 1
                    if c > SEM_LIMIT:
                        self.eidx[o.eng] += 1
                        assert self.eidx[o.eng] < self.n_eng, "out of engine semaphores"
                        c = 1
                    self.ecount[o.eng] = c
                    o.sem, o.val = ("e", o.eng, self.eidx[o.eng]), c
                    ins.then_inc(self.sems[o.sem], 1)
            o.fn = None
        self.emitted = len(ops)

    def barrier(self):
        last = {}
        for i in range(self.emitted, len(self.ops)):
            o = self.ops[i]
            if not o.dma:
                last[o.eng] = i
        for i in last.values():
            self.ops[i].signal = True
        self.flush()
        for eng in ENGS:
            for i in last.values():
                self._wait(eng, self.ops[i].sem, self.ops[i].val)
            for k in range(self.dsem_n + self.n_sw):
                self._wait(eng, ("d", k), self.dtarget[k])
        for t in self.toks:
            t.reset()


def cap_for(T):
    return 128 * int(math.ceil((T / 16.0) * 1.5 / 128.0))


def build(T, stop_after=99, dbg=()):
    NT = T // 128
    NB = T // 512
    CAP = cap_for(T)
    CT = CAP // 128
    NSLOT = NE * CAP
    nc = bass.Bass("TRN2", target_bir_lowering=False)

    def din(name, shape, dt=F32):
        return nc.dram_tensor(name, list(shape), dt, kind="ExternalInput").ap()

    x = din("x", [T, D])
    w_in = din("w_in", [D, IN_COLS])
    gmix = din("gmix", [128, 8])
    hglb = din("hglb", [128, 8])
    hgon = din("hgon", [128, 1])
    qkn = din("qkn", [128, 2])
    lamb = din("lamb", [128, 256])
    daon = din("daon", [128, 1])
    wbh = din("wbh", [512, D])
    wbd = din("wbd", [512, D])
    w_out = din("w_out", [D, D])
    nmoe = din("nmoe", [128, D])
    wr = din("wr", [D, 36])
    br = din("br", [128, 36])
    w1 = din("w1", [NE, D, DFF])
    w3 = din("w3", [NE, D, DFF])
    w2 = din("w2", [NE, DFF, D])
    cmat = din("cmat", [128, 7, 128])
    rmask = din("rmask", [128, 512])
    ecap = din("ecap", [128, 32])
    ropec = din("ropec", [128, T])
    ropes = din("ropes", [128, T])
    out = nc.dram_tensor("out", [T, D], F32, kind="ExternalOutput").ap()
    x2_d = nc.dram_tensor("x2_d", [T, D], F32).ap()
    xn_d = nc.dram_tensor("xn_d", [T, D], BF16).ap()
    xg_d = nc.dram_tensor("xg_d", [NSLOT, D], BF16).ap()
    y_d = nc.dram_tensor("y_d", [NSLOT, D], F32).ap()
    hT_d = nc.dram_tensor("hT_d", [128, 8, T], BF16).ap()
    ohg_d = nc.dram_tensor("dbg_ohg" if "ohg" in dbg else "ohg_d", [128, 4, T], BF16, kind="ExternalOutput" if "ohg" in dbg else "Internal").ap()
    oda_d = nc.dram_tensor("dbg_oda" if "oda" in dbg else "oda_d", [128, 4, T], BF16, kind="ExternalOutput" if "oda" in dbg else "Internal").ap()
    dbg_t = {}
    if "ht" in dbg:
        dbg_t["ht"] = nc.dram_tensor("dbg_ht", [128, 8, T], F32, kind="ExternalOutput").ap()
    if "x2" in dbg:
        dbg_t["x2"] = nc.dram_tensor("dbg_x2", [T, D], F32, kind="ExternalOutput").ap()
        dbg_t["lg"] = nc.dram_tensor("dbg_lg", [128, NT, 36], F32, kind="ExternalOutput").ap()
    if "rt" in dbg:
        dbg_t["rt"] = nc.dram_tensor("dbg_rt", [128, 4, NT], F32, kind="ExternalOutput").ap()

    w_in_v = w_in.rearrange("(k p) c -> p k c", p=128)

    with ExitStack() as es:
        S = Sched(nc, es)

        def sb(name, shape, dt, stack=es):
            return stack.enter_context(nc.sbuf_tensor(name, list(shape), dt))

        ps = [es.enter_context(nc.psum_tensor(f"ps{i}", [128, 512], F32)) for i in range(8)]
        pst = [S.tok(f"ps{i}") for i in range(8)]

        def psb(i):
            return ps[i][:].bitcast(BF16)

        cm_f = sb("cm_f", [128, 7, 128], F32)
        cm_b = sb("cm_b", [128, 7, 128], BF16)
        rmask_s = sb("rmask_s", [128, 512], F32)
        gmix_s = sb("gmix_s", [128, 8], F32)
        hglb_s = sb("hglb_s", [128, 8], F32)
        lbv = sb("lbv", [128, 12], F32)
        hgon_s = sb("hgon_s", [128, 1], F32)
        qkn_s = sb("qkn_s", [128, 2], F32)
        lamb_s = sb("lamb_s", [128, 256], F32)
        lam_s = sb("lam_s", [128, 8], F32)
        daon_s = sb("daon_s", [128, 2], F32)
        dmy = sb("dmy", [128, 2], F32)
        t_c = S.tok("consts")

        def ld_consts(e, inc):
            inc(e.dma_start(out=cm_f[:], in_=cmat))
            inc(e.dma_start(out=rmask_s[:], in_=rmask))
            inc(e.dma_start(out=gmix_s[:], in_=gmix))
            inc(e.dma_start(out=hglb_s[:], in_=hglb))
            inc(e.dma_start(out=hgon_s[:], in_=hgon))
            inc(e.dma_start(out=qkn_s[:], in_=qkn))
            inc(e.dma_start(out=lamb_s[:], in_=lamb))
            inc(e.dma_start(out=daon_s[:, 0:1], in_=daon))

        S.op("sp", ld_consts, w=[t_c], dma=True)
        S.op("dve", lambda e: e.tensor_copy(out=cm_b[:], in_=cm_f[:]), r=[t_c], w=[t_c])
        S.op("dve", lambda e: e.tensor_sub(out=lbv[:, 8:12], in0=hglb_s[:, 0:4], in1=hglb_s[:, 4:8]), r=[t_c], w=[t_c])
        S.op("act", lambda e: e.activation(out=lbv[:, 8:12], in_=lbv[:, 8:12], func=AF.Exp), r=[t_c], w=[t_c])
        S.op("dve", lambda e: e.tensor_scalar_add(out=lbv[:, 8:12], in0=lbv[:, 8:12], scalar1=1.0), r=[t_c], w=[t_c])
        S.op("dve", lambda e: e.reciprocal(out=lbv[:, 0:4], in_=lbv[:, 8:12]), r=[t_c], w=[t_c])
        S.op("dve", lambda e: e.tensor_scalar_mul(out=lbv[:, 4:8], in0=lbv[:, 0:4], scalar1=-1.0), r=[t_c], w=[t_c])
        S.op("dve", lambda e: e.tensor_mul(out=lamb_s[:, 0:64], in0=lamb_s[:, 0:64], in1=lamb_s[:, 64:128]), r=[t_c], w=[t_c])
        S.op("dve", lambda e: e.tensor_mul(out=lamb_s[:, 128:192], in0=lamb_s[:, 128:192], in1=lamb_s[:, 192:256]), r=[t_c], w=[t_c])
        S.op("dve", lambda e: e.reduce_sum(out=lam_s[:, 0:1], in_=lamb_s[:, 0:64], axis=AX.X), r=[t_c], w=[t_c])
        S.op("dve", lambda e: e.reduce_sum(out=lam_s[:, 1:2], in_=lamb_s[:, 128:192], axis=AX.X), r=[t_c], w=[t_c])
        S.op("act", lambda e: e.activation(out=lam_s[:, 0:2], in_=lam_s[:, 0:2], func=AF.Exp), r=[t_c], w=[t_c])
        S.op("dve", lambda e: e.tensor_sub(out=lam_s[:, 2:3], in0=lam_s[:, 1:2], in1=lam_s[:, 0:1]), r=[t_c], w=[t_c])
        S.op("dve", lambda e: e.tensor_scalar_add(out=lam_s[:, 2:3], in0=lam_s[:, 2:3], scalar1=-0.2), r=[t_c], w=[t_c])
        S.op("dve", lambda e: e.tensor_scalar_mul(out=daon_s[:, 1:2], in0=daon_s[:, 0:1], scalar1=0.8), r=[t_c], w=[t_c])
        S.op("dve", lambda e: e.memset(lam_s[:, 4:5], EPS), w=[t_c])
        S.op("dve", lambda e: e.memset(lam_s[:, 5:6], 1.0), w=[t_c])
        S.op("dve", lambda e: e.memset(dmy[:], 0.0), w=[t_c])
        S.barrier()

        EPSB = lam_s[:, 4:5]
        ONEB = lam_s[:, 5:6]
        IDF = cm_f[:, 0, :]
        IDB = cm_b[:, 0, :]
        CMASK = cm_b[:, 1, :]
        TRI01 = cm_b[:, 2, :]
        LSTR = cm_b[:, 3, :]
        ONESB = cm_b[:, 4, :]
        BD64 = cm_b[:, 5, :]
        PERM = cm_b[:, 6, :]

        slot_i = sb("slot_i", [128, 2, NT], I32)
        wgt = sb("wgt", [128, 2, NT], F32)
        t_route = S.tok("route")
        t_x2d = [S.tok("x2d") for _ in range(NT)]
        t_xnd = [S.tok("xnd") for _ in range(NT)]
        t_xg = S.tok("xg")
        t_yd = S.tok("yd")
        t_hTd = S.tok("hTd")
        t_ohgd = [[S.tok("ohgd") for _ in range(NB)] for _ in range(4)]
        t_odad = [[S.tok("odad") for _ in range(NB)] for _ in range(4)]
        zt = sb("zt", [128, D], BF16)
        t_zt = S.tok("zt")
        S.op("dve", lambda e: e.memset(zt[:], 0.0), w=[t_zt])
        es_h = ExitStack()
        hT = sb("hT", [128, 8, T], BF16, es_h)
        t_hT = [S.tok(f"hT{i}") for i in range(NB)]

        with ExitStack() as p1:
            xt = [sb(f"p1x{i}", [128, D], F32, p1) for i in range(2)]
            xs = [sb(f"p1xs{i}", [128, D], BF16, p1) for i in range(2)]
            junk = sb("p1junk", [128, D], F32, p1)
            st = [sb(f"p1st{i}", [128, 2], F32, p1) for i in range(2)]
            t_xt = [S.tok("xt") for _ in range(2)]
            t_xs = [S.tok("xs") for _ in range(2)]
            t_st = [S.tok("st") for _ in range(2)]
            t_junk = S.tok("junk")
            for i in range(NT):
                b = i % 2
                S.op("sp", lambda e, inc, i=i, b=b: inc(e.dma_start(out=xt[b][:], in_=x[i * 128:(i + 1) * 128, :])),
                     w=[t_xt[b]], dma=True)
                S.op("act", lambda e, b=b: e.activation(out=junk[:], in_=xt[b][:], func=AF.Square), r=[t_xt[b]], w=[t_junk])
                S.op("dve", lambda e, b=b: e.reduce_sum(out=st[b][:, 0:1], in_=junk[:], axis=AX.X), r=[t_junk], w=[t_st[b]])
                S.op("act", lambda e, b=b: e.activation(out=st[b][:, 1:2], in_=st[b][:, 0:1], func=AF.Ln, scale=1.0 / D, bias=EPSB),
                     r=[t_st[b]], w=[t_st[b]])
                S.op("act", lambda e, b=b: e.activation(out=st[b][:, 1:2], in_=st[b][:, 1:2], func=AF.Exp, scale=-0.5),
                     r=[t_st[b]], w=[t_st[b]])
                S.op("dve", lambda e, b=b: e.tensor_scalar(out=xs[b][:], in0=xt[b][:], scalar1=st[b][:, 1:2], scalar2=None,
                                                           op0=ALU.mult), r=[t_st[b], t_xt[b]], w=[t_xs[b]])
                pb = 0 + (i % 2)
                for k in range(8):
                    S.op("pe", lambda e, k=k, b=b, pb=pb: e.transpose(out=psb(pb)[:, k * 128:(k + 1) * 128],
                                                                       in_=xs[b][:, k * 128:(k + 1) * 128], identity=IDB),
                         r=[t_xs[b]], w=[pst[pb]])
                S.op("dve", lambda e, i=i, pb=pb: e.tensor_tensor(
                    out=hT[:, :, i * 128:(i + 1) * 128],
                    in0=psb(pb).rearrange("p (k t) -> p k t", k=8),
                    in1=gmix_s[:].unsqueeze(2).to_broadcast([128, 8, 128]), op=ALU.mult),
                    r=[pst[pb]], w=[t_hT[i // 4]])
            S.op("sp", lambda e, inc: inc(e.dma_start(out=hT_d, in_=hT[:])), r=t_hT, w=[t_hTd], dma=True)
            S.barrier()
        if "ht" in dbg:
            with ExitStack() as pd:
                tmp = sb("dbgtmp", [128, 8, T], F32, pd)
                tt = S.tok("dbgtmp")
                S.op("dve", lambda e: e.tensor_copy(out=tmp[:], in_=hT[:]), r=t_hT, w=[tt])
                S.op("sp", lambda e, inc: inc(e.dma_start(out=dbg_t["ht"], in_=tmp[:])), r=[tt], dma=True)
                S.barrier()
        if stop_after <= 1:
            S.barrier()
            es_h.close()
            return nc

        def mm(out_, lhsT, rhs, start, stop):
            return lambda e: e.matmul(out_, lhsT, rhs, start=start, stop=stop)

        def act_accum(e, **kw):
            e.activation(**kw)
            return e.activation(out=dmy[:, 0:1], in_=dmy[:, 1:2], func=AF.Copy)

        dump_names = []
        if "dump" in dbg:
            dbg_t["dump"] = nc.dram_tensor("dbg_dump", [24, 128, 512], F32, kind="ExternalOutput").ap()

        def dump(name, ap, toks):
            if "dump" not in dbg or len(dump_names) >= 24:
                return
            k = len(dump_names)
            dump_names.append(name)
            S.op("sp", lambda e, inc, k=k, ap=ap: inc(e.dma_start(out=dbg_t["dump"][k][:, 0:ap.shape[1]], in_=ap)), r=toks, dma=True)

        with ExitStack() as p2:
            wst = sb("p2wst", [128, 8, 512], F32, p2)
            t_wst = S.tok("wst")
            wq = [sb(f"p2wq{i}", [128, 8, 512], BF16, p2) for i in range(2)]
            t_wq = [S.tok("wq") for _ in range(2)]
            state = [sb(f"p2state{i}", [128, 128], F32, p2) for i in range(2)]
            t_state = [S.tok("state") for _ in range(2)]
            stbf = [[sb(f"p2stbf{i}_{j}", [128, 128], BF16, p2) for j in range(2)] for i in range(2)]
            t_stbf = [[S.tok("stbf") for _ in range(2)] for _ in range(2)]
            def mk(name, dt, n=2, shape=(128, 512)):
                return [sb(f"p2{name}{i}", list(shape), dt, p2) for i in range(n)], [S.tok(name) for _ in range(n)]
            v_tm, t_v = mk("v", BF16)
            q_f, t_q = mk("q", F32)
            ef, t_ef = mk("ef", F32)
            e2f, t_e2 = mk("e2", F32)
            kk, t_kk = mk("kk", F32)
            gg, t_g = mk("g", F32)
            bcs, t_b = mk("b", F32)
            bm, t_bm = mk("bm", F32)
            E1, t_E1 = mk("E1", F32)
            E2, t_E2 = mk("E2", F32)
            E3, t_E3 = mk("E3", F32)
            qrel, t_qrel = mk("qrel", BF16)
            qb, t_qb = mk("qb", BF16)
            krel, t_krel = mk("krel", BF16)
            sog, t_sog = mk("sog", F32)
            o_f, t_of = mk("of", F32)
            sqb, t_sqb = mk("sqb", BF16)
            ob2, t_ob2 = mk("ob2", BF16)
            rst, t_rst = mk("rst", F32)
            kt, t_kt = mk("kt", BF16, 4, (128, 128))
            stm, t_stm = mk("stm", BF16, 4, (128, 128))
            sidx = [0, 0]
            RB = [(4, 5, 6, 7), (0, 1, 2, 3)]

            def prep(h, b, u):
                tb = slice(b * 512, (b + 1) * 512)
                for j, bank in ((0, 0), (1, 1), (3, 2)):
                    for k in range(8):
                        S.op("pe", mm(ps[bank][:, :], wq[u][:, k, j * 128:(j + 1) * 128], hT[:, k, tb], k == 0, k == 7),
                             r=[t_wq[u], t_hT[b]], w=[pst[bank]])
                for ti in range(4):
                    for k in range(8):
                        S.op("pe", mm(ps[3][:, ti * 128:(ti + 1) * 128], hT[:, k, b * 512 + ti * 128: b * 512 + (ti + 1) * 128],
                                      wq[u][:, k, 256:384], k == 0, k == 7), r=[t_wq[u], t_hT[b]], w=[pst[3]])
                yield
                S.op("act", lambda e: e.activation(out=e2f[u][:], in_=ps[2][:, :], func=AF.Exp, scale=-1.0), r=[pst[2]], w=[t_e2[u]])
                S.op("act", lambda e: e.activation(out=ef[u][:], in_=ps[1][:, :], func=AF.Exp, scale=-1.0), r=[pst[1]], w=[t_ef[u]])
                S.op("act", lambda e: e.activation(out=q_f[u][:], in_=ps[0][:, :], func=AF.Copy), r=[pst[0]], w=[t_q[u]])
                S.op("act", lambda e: e.activation(out=v_tm[u][:], in_=ps[3][:, :], func=AF.Copy), r=[pst[3]], w=[t_v[u]])
                yield
                S.op("act", lambda e: e.activation(out=e2f[u][:], in_=e2f[u][:], func=AF.Ln, bias=ONEB), r=[t_e2[u]], w=[t_e2[u]])
                S.op("act", lambda e: e.activation(out=ef[u][:], in_=ef[u][:], func=AF.Ln, bias=ONEB), r=[t_ef[u]], w=[t_ef[u]])
                yield
                S.op("act", lambda e: e.activation(out=e2f[u][:], in_=e2f[u][:], func=AF.Exp, scale=-1.0), r=[t_e2[u]], w=[t_e2[u]])
                S.op("act", lambda e: e.activation(out=ef[u][:], in_=ef[u][:], func=AF.Exp, scale=-1.0), r=[t_ef[u]], w=[t_ef[u]])
                yield
                S.op("dve", lambda e: e.tensor_tensor(out=sog[u][:], in0=ps[2][:, :], in1=e2f[u][:], op=ALU.mult),
                     r=[pst[2], t_e2[u]], w=[t_sog[u]])
                S.op("dve", lambda e: e.tensor_scalar(out=kk[u][:], in0=ef[u][:], scalar1=lbv[:, 4 + h:5 + h], scalar2=lbv[:, h:h + 1],
                                                      op0=ALU.mult, op1=ALU.add), r=[t_ef[u]], w=[t_kk[u]])
                yield
                S.op("act", lambda e: e.activation(out=gg[u][:], in_=kk[u][:], func=AF.Ln, scale=-1.0, bias=ONEB), r=[t_kk[u]], w=[t_g[u]])
                yield
                S.op("dve", lambda e: e.tensor_tensor_scan(out=bcs[u][:], data0=rmask_s[:], data1=gg[u][:], initial=0.0,
                                                           op0=ALU.mult, op1=ALU.add), r=[t_g[u]], w=[t_b[u]])
                yield
                S.op("dve", lambda e: e.tensor_tensor(
                    out=bm[u][:].rearrange("p (c t) -> p c t", t=64),
                    in0=bcs[u][:].rearrange("p (c t) -> p c t", t=64),
                    in1=bcs[u][:].rearrange("p (c t) -> p c t", t=64)[:, :, 31:32].to_broadcast([128, 8, 64]),
                    op=ALU.subtract), r=[t_b[u]], w=[t_bm[u]])
                S.op("act", lambda e: e.activation(out=E3[u][:], in_=bcs[u][:], func=AF.Exp), r=[t_b[u]], w=[t_E3[u]])
                yield
                S.op("act", lambda e: e.activation(out=E1[u][:], in_=bm[u][:], func=AF.Exp), r=[t_bm[u]], w=[t_E1[u]])
                S.op("act", lambda e: e.activation(out=E2[u][:], in_=bm[u][:], func=AF.Exp, scale=-1.0), r=[t_bm[u]], w=[t_E2[u]])
                S.op("dve", lambda e: e.tensor_tensor(out=qb[u][:], in0=q_f[u][:], in1=E3[u][:], op=ALU.mult),
                     r=[t_q[u], t_E3[u]], w=[t_qb[u]])
                yield
                S.op("dve", lambda e: e.tensor_tensor(out=qrel[u][:], in0=q_f[u][:], in1=E1[u][:], op=ALU.mult),
                     r=[t_q[u], t_E1[u]], w=[t_qrel[u]])
                S.op("dve", lambda e: e.tensor_tensor(out=krel[u][:], in0=kk[u][:], in1=E2[u][:], op=ALU.mult),
                     r=[t_kk[u], t_E2[u]], w=[t_krel[u]])
                yield

            def tile_pre(u, ti):
                btr, bsT, boT, bdS = RB[u]
                tsl = slice(ti * 128, (ti + 1) * 128)
                w_ = u * 2 + ti % 2
                S.op("pe", lambda e: e.transpose(out=psb(btr)[:, 0:128], in_=krel[u][:, tsl], identity=IDB), r=[t_krel[u]], w=[pst[btr]])
                S.op("act", lambda e: e.activation(out=kt[w_][:], in_=psb(btr)[:, 0:128], func=AF.Copy), r=[pst[btr]], w=[t_kt[w_]])
                S.op("pe", mm(ps[bsT][:, 0:128], krel[u][:, tsl], qrel[u][:, tsl], True, True), r=[t_krel[u], t_qrel[u]], w=[pst[bsT]])
                S.op("dve", lambda e: e.tensor_tensor(out=stm[w_][:], in0=ps[bsT][:, 0:128], in1=CMASK, op=ALU.mult), r=[pst[bsT]], w=[t_stm[w_]])
                S.op("pe", mm(ps[boT][:, 0:128], v_tm[u][:, tsl], stm[w_][:], True, False), r=[t_v[u], t_stm[w_]], w=[pst[boT]])

            def chunk(u, ti, half):
                btr, bsT, boT, bdS = RB[u]
                tsl = slice(ti * 128, (ti + 1) * 128)
                w_ = u * 2 + ti % 2
                c = ti * 2 + half
                csl = slice(ti * 128 + half * 64, ti * 128 + half * 64 + 64)
                pr = slice(half * 64, half * 64 + 64)
                cur = sidx[u] % 2
                S.op("pe", mm(ps[boT][:, half * 64:half * 64 + 64], stbf[u][cur][:], qb[u][:, csl], False, half == 1),
                     r=[t_stbf[u][cur], t_qb[u]], w=[pst[boT]])
                S.op("pe", mm(ps[bdS][:, half * 128:half * 128 + 128], kt[w_][pr, :], v_tm[u][pr, tsl], True, True),
                     r=[t_kt[w_], t_v[u]], w=[pst[bdS]])
                e5 = E3[u][:, c * 64 + 63:c * 64 + 64]
                c1 = E1[u][:, c * 64 + 63:c * 64 + 64]
                S.op("dve", lambda e: e.tensor_scalar(out=state[u][:], in0=state[u][:], scalar1=e5, scalar2=None, op0=ALU.mult),
                     r=[t_state[u], t_E3[u]], w=[t_state[u]])
                S.op("dve", lambda e: e.scalar_tensor_tensor(
                    out=state[u][:], in0=ps[bdS][:, half * 128:half * 128 + 128], scalar=c1, in1=state[u][:], op0=ALU.mult, op1=ALU.add),
                    r=[t_state[u], t_E1[u], pst[bdS]], w=[t_state[u]])
                sidx[u] += 1
                nxt = sidx[u] % 2
                S.op("act", lambda e: e.activation(out=stbf[u][nxt][:], in_=state[u][:], func=AF.Copy), r=[t_state[u]], w=[t_stbf[u][nxt]])

            def tile_post(u, ti):
                btr, bsT, boT, bdS = RB[u]
                tsl = slice(ti * 128, (ti + 1) * 128)
                S.op("dve", lambda e: e.tensor_copy(out=o_f[u][:, tsl], in_=ps[boT][:, 0:128]), r=[pst[boT]], w=[t_of[u]])

            def post(h, b, u):
                btr, bsT, boT, bdS = RB[u]
                tb = slice(b * 512, (b + 1) * 512)
                S.op("act", lambda e: e.activation(out=sqb[u][:], in_=o_f[u][:], func=AF.Square), r=[t_of[u]], w=[t_sqb[u]])
                yield
                S.op("pe", mm(ps[bsT][:, :], ONESB, sqb[u][:], True, True), r=[t_sqb[u]], w=[pst[bsT]])
                yield
                S.op("act", lambda e: e.activation(out=rst[u][:], in_=ps[bsT][:, :], func=AF.Ln, scale=1.0 / 128, bias=EPSB), r=[pst[bsT]], w=[t_rst[u]])
                yield
                S.op("act", lambda e: e.activation(out=rst[u][:], in_=rst[u][:], func=AF.Exp, scale=-0.5), r=[t_rst[u]], w=[t_rst[u]])
                yield
                S.op("dve", lambda e: e.tensor_tensor(out=o_f[u][:], in0=o_f[u][:], in1=rst[u][:], op=ALU.mult), r=[t_of[u], t_rst[u]], w=[t_of[u]])
                yield
                S.op("dve", lambda e: e.scalar_tensor_tensor(out=ob2[u][:], in0=o_f[u][:], scalar=hgon_s[:, 0:1], in1=sog[u][:],
                                                             op0=ALU.mult, op1=ALU.mult), r=[t_of[u], t_sog[u]], w=[t_ob2[u]])
                S.op("sp", lambda e, inc: inc(e.dma_start(out=ohg_d[:, h, tb], in_=ob2[u][:])), r=[t_ob2[u]], w=[t_ohgd[h][b]], dma=True)
                yield

            def rr(gens):
                gens = list(gens)
                while gens:
                    for g in list(gens):
                        try:
                            next(g)
                        except StopIteration:
                            gens.remove(g)

            for hp in range(2):
                for u in range(2):
                    h = 2 * hp + u
                    def ldw(e, inc, h=h):
                        for j in range(4):
                            inc(e.dma_start(out=wst[:, :, j * 128:(j + 1) * 128],
                                            in_=w_in_v[:, :, j * 512 + h * 128: j * 512 + (h + 1) * 128]))
                    S.op("sp", ldw, w=[t_wst], dma=True)
                    if h == 0:
                        for ex_ in range(NE):
                            S.op("sp", lambda e, inc, ex_=ex_: inc(e.dma_start(
                                out=xg_d[ex_ * CAP:(ex_ + 1) * CAP, :].rearrange("(c p) d -> p c d", p=128),
                                in_=zt[:].unsqueeze(1).to_broadcast([128, CT, D]))), r=[t_zt], wd=[t_xg], dma=True)
                    S.op("dve", lambda e, u=u: e.tensor_copy(out=wq[u][:], in_=wst[:]), r=[t_wst], w=[t_wq[u]])
                    S.op("dve", lambda e, u=u: e.memset(state[u][:], 0.0), w=[t_state[u]])
                    S.op("dve", lambda e, u=u, c0=sidx[u] % 2: e.memset(stbf[u][c0][:], 0.0), w=[t_stbf[u][sidx[u] % 2]])
                for b in range(NB):
                    gA, gB = prep(2 * hp, b, 0), prep(2 * hp + 1, b, 1)
                    for _ in range(5):
                        next(gA)
                    rr([gA, gB])
                    for ti in range(4):
                        for u in range(2):
                            tile_pre(u, ti)
                        for half in range(2):
                            for u in range(2):
                                chunk(u, ti, half)
                        for u in range(2):
                            tile_post(u, ti)
                    rr(post(2 * hp + u, b, u) for u in range(2))
            S.barrier()
        if "ht2" in dbg:
            dbg_t["ht2"] = nc.dram_tensor("dbg_ht2", [128, 8, T], F32, kind="ExternalOutput").ap()
            with ExitStack() as pd:
                tmp = sb("dbgtmp3", [128, 8, T], F32, pd)
                tt = S.tok("dbgtmp3")
                S.op("dve", lambda e: e.tensor_copy(out=tmp[:], in_=hT[:]), r=t_hT, w=[tt])
                S.op("sp", lambda e, inc: inc(e.dma_start(out=dbg_t["ht2"], in_=tmp[:])), r=[tt], dma=True)
                S.barrier()
        if stop_after <= 2:
            S.barrier()
            es_h.close()
            return nc

        with ExitStack() as p4:
            ropec_s = sb("p4rc", [128, T], F32, p4)
            ropes_s = sb("p4rs", [128, T], F32, p4)
            t_rope = S.tok("rope")
            S.op("sp", lambda e, inc: (inc(e.dma_start(out=ropec_s[:], in_=ropec)), inc(e.dma_start(out=ropes_s[:], in_=ropes))),
                 w=[t_rope], dma=True)
            wst = sb("p4wst", [128, 8, 384], F32, p4)
            wa = sb("p4wa", [128, 8, 384], BF16, p4)
            t_wst, t_wa = S.tok("wst4"), S.tok("wa4")
            qT_s = sb("p4qT", [128, T], BF16, p4)
            kT_s = sb("p4kT", [128, T], BF16, p4)
            vt = sb("p4v", [128, NT, 128], BF16, p4)
            t_qT = [S.tok("qT") for _ in range(NB)]
            t_kT = [S.tok("kT") for _ in range(NB)]
            t_vt = [S.tok("vt") for _ in range(NB)]
            def mk4(name, dt, n=2, shape=(128, 512)):
                return [sb(f"p4{name}{i}", list(shape), dt, p4) for i in range(n)], [S.tok(name) for _ in range(n)]
            sq4, t_sq4 = mk4("sq", BF16)
            qf4, t_qf4 = mk4("qf", F32)
            rs4, t_rs4 = mk4("rs", F32)
            qn4, t_qn4 = mk4("qn", BF16)
            t14, t_t14 = mk4("t1", F32)
            t24, t_t24 = mk4("t2", F32)
            Pm, t_P = mk4("P", BF16, 4)
            r0, t_r0 = mk4("r0", F32, 1)
            r1, t_r1 = mk4("r1", F32, 1)
            o1, t_o1 = mk4("o1", F32, 1)
            o2, t_o2 = mk4("o2", F32, 1)
            ob4, t_ob4 = mk4("ob4", BF16, 2)
            cnt4 = 0
            for h in range(4):
                def ldw4(e, inc, h=h):
                    for j in range(3):
                        inc(e.dma_start(out=wst[:, :, j * 128:(j + 1) * 128],
                                        in_=w_in_v[:, :, 2048 + j * 512 + h * 128: 2048 + j * 512 + (h + 1) * 128]))
                S.op("sp", ldw4, w=[t_wst], dma=True)
                S.op("dve", lambda e: e.tensor_copy(out=wa[:], in_=wst[:]), r=[t_wst], w=[t_wa])
                units = [(b, j) for b in range(NB) for j in range(2)]

                def stA(n):
                    b, j = units[n]
                    u = n % 2
                    tb = slice(b * 512, (b + 1) * 512)
                    for k in range(8):
                        S.op("pe", mm(ps[u][:, :], wa[:, k, j * 128:(j + 1) * 128], hT[:, k, tb], k == 0, k == 7),
                             r=[t_wa, t_hT[b]], w=[pst[u]])
                    S.op("act", lambda e: e.activation(out=sq4[u][:], in_=ps[u][:, :], func=AF.Square), r=[pst[u]], w=[t_sq4[u]])
                    S.op("act", lambda e: e.activation(out=qf4[u][:], in_=ps[u][:, :], func=AF.Copy), r=[pst[u]], w=[t_qf4[u]])
                    if j == 0:
                        for ti in range(4):
                            for k in range(8):
                                S.op("pe", mm(ps[4][:, ti * 128:(ti + 1) * 128], hT[:, k, b * 512 + ti * 128: b * 512 + (ti + 1) * 128],
                                              wa[:, k, 256:384], k == 0, k == 7), r=[t_wa, t_hT[b]], w=[pst[4]])
                        S.op("act", lambda e: e.activation(out=vt[:, b * 4:(b + 1) * 4, :], in_=ps[4][:, :].rearrange("p (a c) -> p a c", a=4),
                                                           func=AF.Copy), r=[pst[4]], w=[t_vt[b]])

                def stB(n):
                    b, j = units[n]
                    u = n % 2
                    S.op("pe", mm(ps[2][:, :], BD64, sq4[u][:], True, True), r=[t_sq4[u]], w=[pst[2]])
                    S.op("act", lambda e: e.activation(out=rs4[u][:], in_=ps[2][:, :], func=AF.Ln, scale=1.0 / 64, bias=EPSB), r=[pst[2]], w=[t_rs4[u]])
                    S.op("act", lambda e: e.activation(out=rs4[u][:], in_=rs4[u][:], func=AF.Exp, scale=-0.5), r=[t_rs4[u]], w=[t_rs4[u]])
                    S.op("dve", lambda e: e.scalar_tensor_tensor(out=qn4[u][:], in0=qf4[u][:], scalar=qkn_s[:, j:j + 1], in1=rs4[u][:],
                                                                 op0=ALU.mult, op1=ALU.mult), r=[t_qf4[u], t_rs4[u]], w=[t_qn4[u]])

                def stC(n):
                    b, j = units[n]
                    u = n % 2
                    tb = slice(b * 512, (b + 1) * 512)
                    dst, t_dst = (qT_s, t_qT) if j == 0 else (kT_s, t_kT)
                    S.op("pe", mm(ps[3][:, :], PERM, qn4[u][:], True, True), r=[t_qn4[u]], w=[pst[3]])
                    S.op("dve", lambda e: e.tensor_tensor(out=t14[u][:], in0=qn4[u][:], in1=ropec_s[:, tb], op=ALU.mult),
                         r=[t_qn4[u], t_rope], w=[t_t14[u]])
                    S.op("dve", lambda e: e.tensor_tensor(out=t24[u][:], in0=ps[3][:, :], in1=ropes_s[:, tb], op=ALU.mult),
                         r=[pst[3], t_rope], w=[t_t24[u]])
                    S.op("dve", lambda e: e.tensor_tensor(out=dst[:, tb], in0=t14[u][:], in1=t24[u][:], op=ALU.add),
                         r=[t_t14[u], t_t24[u]], w=[t_dst[b]])

                NU = len(units)
                for n in range(NU + 2):
                    if n < NU:
                        stA(n)
                    if 0 <= n - 1 < NU:
                        stB(n - 1)
                    if 0 <= n - 2 < NU:
                        stC(n - 2)
                S.barrier()
                steps = [(j, i) for j in range(NB) for i in range(4 * j + 4)]

                def qk(s):
                    j, i = steps[s]
                    r_ = i - 4 * j
                    col0 = 128 * r_ if r_ > 0 else 0
                    for m in range(2):
                        bank = m * 2 + (s % 2)
                        S.op("pe", mm(ps[bank][:, col0:512], kT_s[m * 64:(m + 1) * 64, i * 128:(i + 1) * 128],
                                      qT_s[m * 64:(m + 1) * 64, j * 512 + col0:(j + 1) * 512], True, True),
                             r=[t_kT[i // 4], t_qT[j]], w=[pst[bank]])

                qk(0)
                for s in range(len(steps)):
                    j, i = steps[s]
                    r_ = i - 4 * j
                    col0 = 128 * r_ if r_ > 0 else 0
                    last = (i == 4 * j + 3)
                    for m in range(2):
                        bank = m * 2 + (s % 2)
                        pb_ = m * 2 + (s % 2)
                        S.op("act", lambda e, bank=bank, pb_=pb_, col0=col0: e.activation(out=Pm[pb_][:, col0:512], in_=ps[bank][:, col0:512],
                                                                                           func=AF.Exp, scale=0.125),
                             r=[pst[bank]], w=[t_P[pb_]])
                    if s + 1 < len(steps):
                        qk(s + 1)
                    for m in range(2):
                        pb_ = m * 2 + (s % 2)
                        if r_ >= 0:
                            S.op("dve", lambda e, pb_=pb_, col0=col0: e.tensor_tensor(out=Pm[pb_][:, col0:col0 + 128], in0=Pm[pb_][:, col0:col0 + 128],
                                                                                       in1=TRI01, op=ALU.mult), r=[t_P[pb_]], w=[t_P[pb_]])
                        S.op("pe", mm(ps[4 + m][:, col0:512], vt[:, i, :], Pm[pb_][:, col0:512], i == 0, last), r=[t_vt[i // 4], t_P[pb_]], w=[pst[4 + m]])
                        S.op("pe", mm(ps[6 + m][:, col0:512], ONESB, Pm[pb_][:, col0:512], i == 0, last), r=[t_P[pb_]], w=[pst[6 + m]])
                    if last:
                        jb = slice(j * 512, (j + 1) * 512)
                        S.op("dve", lambda e: e.tensor_copy(out=o1[0][:], in_=ps[4][:, :]), r=[pst[4]], w=[t_o1[0]])
                        S.op("dve", lambda e: e.tensor_copy(out=o2[0][:], in_=ps[5][:, :]), r=[pst[5]], w=[t_o2[0]])
                        S.op("act", lambda e: e.activation(out=r0[0][:], in_=ps[6][:, :], func=AF.Ln), r=[pst[6]], w=[t_r0[0]])
                        S.op("act", lambda e: e.activation(out=r1[0][:], in_=ps[7][:, :], func=AF.Ln), r=[pst[7]], w=[t_r1[0]])
                        S.op("act", lambda e: e.activation(out=r0[0][:], in_=r0[0][:], func=AF.Exp, scale=-1.0), r=[t_r0[0]], w=[t_r0[0]])
                        S.op("act", lambda e: e.activation(out=r1[0][:], in_=r1[0][:], func=AF.Exp, scale=-1.0), r=[t_r1[0]], w=[t_r1[0]])
                        S.op("dve", lambda e: e.tensor_tensor(out=o1[0][:], in0=o1[0][:], in1=r0[0][:], op=ALU.mult), r=[t_o1[0], t_r0[0]], w=[t_o1[0]])
                        S.op("dve", lambda e: e.tensor_tensor(out=o2[0][:], in0=o2[0][:], in1=r1[0][:], op=ALU.mult), r=[t_o2[0], t_r1[0]], w=[t_o2[0]])
                        S.op("dve", lambda e: e.scalar_tensor_tensor(out=o1[0][:], in0=o2[0][:], scalar=lam_s[:, 2:3], in1=o1[0][:], op0=ALU.mult, op1=ALU.add),
                             r=[t_o1[0], t_o2[0]], w=[t_o1[0]])
                        S.op("act", lambda e: e.activation(out=sq4[0][:], in_=o1[0][:], func=AF.Square), r=[t_o1[0]], w=[t_sq4[0]])
                        S.op("pe", mm(ps[6][:, :], ONESB, sq4[0][:], True, True), r=[t_sq4[0]], w=[pst[6]])
                        S.op("act", lambda e: e.activation(out=rs4[0][:], in_=ps[6][:, :], func=AF.Ln, scale=1.0 / 128, bias=EPSB), r=[pst[6]], w=[t_rs4[0]])
                        S.op("act", lambda e: e.activation(out=rs4[0][:], in_=rs4[0][:], func=AF.Exp, scale=-0.5), r=[t_rs4[0]], w=[t_rs4[0]])
                        ou = j % 2
                        S.op("dve", lambda e, ou=ou: e.scalar_tensor_tensor(out=ob4[ou][:], in0=o1[0][:], scalar=daon_s[:, 1:2], in1=rs4[0][:],
                                                                            op0=ALU.mult, op1=ALU.mult), r=[t_o1[0], t_rs4[0]], w=[t_ob4[ou]])
                        S.op("sp", lambda e, inc, ou=ou, h=h, jb=jb: inc(e.dma_start(out=oda_d[:, h, jb], in_=ob4[ou][:])), r=[t_ob4[ou]], w=[t_odad[h][j]], dma=True)
                S.barrier()
        es_h.close()
        if stop_after <= 4:
            S.barrier()
            return nc

        BCREG = nc.gpsimd.to_reg(NSLOT - 1)
        logit = sb("logit", [128, NT, 36], F32)
        t_logit = S.tok("logit")
        with ExitStack() as p5:
            wbh_s = sb("p5wbh", [128, 4, D], BF16, p5)
            wbd_s = sb("p5wbd", [128, 4, D], BF16, p5)
            wg_s = sb("p5wg", [128, 8, 2048], BF16, p5)
            wo_s = sb("p5wo", [128, 8, D], BF16, p5)
            nm_s = sb("p5nm", [128, D], F32, p5)
            wr_s = sb("p5wr", [128, 8, 36], F32, p5)
            br_s = sb("p5br", [128, 36], F32, p5)
            t_w5 = S.tok("w5")
            wbh_v = wbh.rearrange("(k p) c -> p k c", p=128)
            wbd_v = wbd.rearrange("(k p) c -> p k c", p=128)
            wo_v = w_out.rearrange("(k p) c -> p k c", p=128)
            stg = [sb("p5stg0", [128, 8, 512], F32, p5)] * 2
            t_stg = [S.tok("stg")] * 2
            t_wbh = [S.tok("wbh") for _ in range(2)]
            t_wbd = [S.tok("wbd") for _ in range(2)]
            t_wo = [S.tok("wo") for _ in range(2)]
            t_wg = [S.tok("wg") for _ in range(4)]
            sgc = [0]
            def ldcast(src_ap, dst_ap, kk_, tk):
                g = sgc[0] % 2
                sgc[0] += 1
                S.op("sp", lambda e, inc, g=g: inc(e.dma_start(out=stg[g][:, 0:kk_, :], in_=src_ap)), w=[t_stg[g]], dma=True)
                if g == 0:
                    S.op("dve", lambda e, g=g: e.tensor_copy(out=dst_ap, in_=stg[g][:, 0:kk_, :]), r=[t_stg[g]], w=[tk])
                else:
                    S.op("act", lambda e, g=g: e.activation(out=dst_ap, in_=stg[g][:, 0:kk_, :], func=AF.Copy), r=[t_stg[g]], w=[tk])
            def ld_wg(n):
                ldcast(w_in_v[:, :, 3584 + n * 512: 3584 + (n + 1) * 512], wg_s[:, :, n * 512:(n + 1) * 512], 8, t_wg[n])
            for n in range(2):
                ldcast(wbh_v[:, :, n * 512:(n + 1) * 512], wbh_s[:, :, n * 512:(n + 1) * 512], 4, t_wbh[n])
                ldcast(wbd_v[:, :, n * 512:(n + 1) * 512], wbd_s[:, :, n * 512:(n + 1) * 512], 4, t_wbd[n])
                ld_wg(n)
                ld_wg(2 + n)
            for n in range(2):
                ldcast(wo_v[:, :, n * 512:(n + 1) * 512], wo_s[:, :, n * 512:(n + 1) * 512], 8, t_wo[n])
            S.op("sp", lambda e, inc: (inc(e.dma_start(out=nm_s[:], in_=nmoe)), inc(e.dma_start(out=wr_s[:], in_=wr.rearrange("(k p) c -> p k c", p=128))),
                                       inc(e.dma_start(out=br_s[:], in_=br))), w=[t_w5], dma=True)
            hTb = [sb(f"p5hT{i}", [128, 8, 512], BF16, p5) for i in range(2)]
            ohb = [sb(f"p5oh{i}", [128, 4, 512], BF16, p5) for i in range(2)]
            odb = [sb(f"p5od{i}", [128, 4, 512], BF16, p5) for i in range(2)]
            t_hTb = [S.tok("hTb") for _ in range(2)]
            t_ohb = [S.tok("ohb") for _ in range(2)]
            t_odb = [S.tok("odb") for _ in range(2)]
            mixT = [sb(f"p5mix{i}", [128, 8, 512], BF16, p5) for i in range(2)]
            t_mix = [S.tok("mix") for _ in range(2)]
            def mk5(name, dt, n=2, shape=(128, 512)):
                return [sb(f"p5{name}{i}", list(shape), dt, p5) for i in range(n)], [S.tok(name) for _ in range(n)]
            s1b, t_s1 = mk5("s1", F32)
            s2b, t_s2 = mk5("s2", F32)
            m1b, t_m1 = mk5("m1", F32, 1)
            m2b, t_m2 = mk5("m2", F32, 1)
            xt5, t_xt5 = mk5("xt", F32, 2, (128, D))
            x2b, t_x2 = mk5("x2", F32, 2, (128, D))
            xnf, t_xnf = mk5("xnf", F32, 2, (128, D))
            xnb, t_xnb = mk5("xnb", BF16, 2, (128, D))
            xnT, t_xnT = mk5("xnT", F32, 2, (128, D))
            st5, t_st5 = mk5("st", F32, 2, (128, 2))
            def tile_S1(i, mb, ti):
                u = i % 2
                if i == 0:
                    S.op("sp", lambda e, inc: inc(e.dma_start(out=xt5[0][:], in_=x[0:128, :])), w=[t_xt5[0]], dma=True)
                if i + 1 < NT:
                    S.op("sp", lambda e, inc: inc(e.dma_start(out=xt5[(i + 1) % 2][:], in_=x[(i + 1) * 128:(i + 2) * 128, :])), w=[t_xt5[(i + 1) % 2]], dma=True)
                for n in range(2):
                    for k in range(8):
                        S.op("pe", mm(ps[n][:, :], mixT[mb][:, k, ti * 128:(ti + 1) * 128], wo_s[:, k, n * 512:(n + 1) * 512], k == 0, k == 7),
                             r=[t_mix[mb], t_wo[n]], w=[pst[n]])
                    S.op("dve", lambda e, n=n: e.tensor_tensor(out=x2b[u][:, n * 512:(n + 1) * 512], in0=ps[n][:, :], in1=xt5[u][:, n * 512:(n + 1) * 512],
                                                              op=ALU.add), r=[pst[n], t_xt5[u]], wd=[t_x2[u]])

            def tile_S2(i):
                u = i % 2
                S.op("sp", lambda e, inc: inc(e.dma_start(out=x2_d[i * 128:(i + 1) * 128, :], in_=x2b[u][:])), r=[t_x2[u]], w=[t_x2d[i]], dma=True)
                if "x2" in dbg:
                    S.op("sp", lambda e, inc: inc(e.dma_start(out=dbg_t["x2"][i * 128:(i + 1) * 128, :], in_=x2b[u][:])), r=[t_x2[u]], dma=True)
                S.op("act", lambda e: e.activation(out=xnT[u][:], in_=x2b[u][:], func=AF.Square), r=[t_x2[u]], w=[t_xnT[u]])
                S.op("dve", lambda e: e.reduce_sum(out=st5[u][:, 0:1], in_=xnT[u][:], axis=AX.X), r=[t_xnT[u]], w=[t_st5[u]])
                S.op("act", lambda e: e.activation(out=st5[u][:, 1:2], in_=st5[u][:, 0:1], func=AF.Ln, scale=1.0 / D, bias=EPSB), r=[t_st5[u]], w=[t_st5[u]])
                S.op("act", lambda e: e.activation(out=st5[u][:, 1:2], in_=st5[u][:, 1:2], func=AF.Exp, scale=-0.5), r=[t_st5[u]], w=[t_st5[u]])
                S.op("dve", lambda e: e.scalar_tensor_tensor(out=xnf[u][:], in0=x2b[u][:], scalar=st5[u][:, 1:2], in1=nm_s[:], op0=ALU.mult, op1=ALU.mult),
                     r=[t_x2[u], t_st5[u], t_w5], w=[t_xnf[u]])
                S.op("act", lambda e: e.activation(out=xnb[u][:].rearrange("p (k j) -> p k j", k=8), in_=xnf[u][:].rearrange("p (j k) -> p k j", k=8),
                                                   func=AF.Copy), r=[t_xnf[u]], w=[t_xnb[u]])
                S.op("sp", lambda e, inc: inc(e.dma_start(out=xn_d[i * 128:(i + 1) * 128, :], in_=xnb[u][:])), r=[t_xnb[u]], w=[t_xnd[i]], dma=True)

            def tile_S3(i):
                u = i % 2
                for k in range(8):
                    bank = 2 + k // 4
                    S.op("pe", lambda e, k=k, bank=bank: e.transpose(out=ps[bank][:, (k % 4) * 128:(k % 4 + 1) * 128],
                                                                   in_=xnf[u][:, k * 128:(k + 1) * 128], identity=IDF),
                         r=[t_xnf[u]], w=[pst[bank]])
                S.op("act", lambda e: e.activation(out=xnT[u][:, 0:512], in_=ps[2][:, :], func=AF.Copy), r=[pst[2]], wd=[t_xnT[u]])
                S.op("dve", lambda e: e.tensor_copy(out=xnT[u][:, 512:1024], in_=ps[3][:, :]), r=[pst[3]], wd=[t_xnT[u]])

            def tile_S4(i):
                u = i % 2
                for k in range(8):
                    S.op("pe", mm(ps[2][:, 0:36], xnT[u][:, k * 128:(k + 1) * 128], wr_s[:, k, :], k == 0, k == 7), r=[t_xnT[u], t_w5], w=[pst[2]])
                S.op("dve", lambda e: e.tensor_tensor(out=logit[:, i, :], in0=ps[2][:, 0:36], in1=br_s[:], op=ALU.add), r=[pst[2], t_w5], wd=[t_logit])

            cc = 0
            for b in range(NB):
                tb = slice(b * 512, (b + 1) * 512)
                mb = b % 2
                S.op("sp", lambda e, inc, mb=mb, tb=tb: inc(e.dma_start(out=hTb[mb][:], in_=hT_d[:, :, tb])), r=[t_hTd], w=[t_hTb[mb]], dma=True)
                S.op("sp", lambda e, inc, mb=mb, tb=tb: inc(e.dma_start(out=ohb[mb][:], in_=ohg_d[:, :, tb])), r=[t_ohgd[k][b] for k in range(4)], w=[t_ohb[mb]], dma=True)
                S.op("sp", lambda e, inc, mb=mb, tb=tb: inc(e.dma_start(out=odb[mb][:], in_=oda_d[:, :, tb])), r=[t_odad[k][b] for k in range(4)], w=[t_odb[mb]], dma=True)
                for c in range(8):
                    u = cc % 2
                    cc += 1
                    cs = slice(c * 128, (c + 1) * 128)
                    pb0 = 4 * u
                    for k in range(4):
                        S.op("pe", mm(ps[pb0][:, :], wbh_s[:, k, cs], ohb[mb][:, k, :], k == 0, k == 3), r=[t_wbh[c // 4], t_ohb[mb]], w=[pst[pb0]])
                    for k in range(4):
                        S.op("pe", mm(ps[pb0 + 1][:, :], wbd_s[:, k, cs], odb[mb][:, k, :], k == 0, k == 3), r=[t_wbd[c // 4], t_odb[mb]], w=[pst[pb0 + 1]])
                    for k in range(8):
                        S.op("pe", mm(ps[pb0 + 2][:, :], wg_s[:, k, c * 128:(c + 1) * 128], hTb[mb][:, k, :], k == 0, k == 7), r=[t_wg[c // 4], t_hTb[mb]], w=[pst[pb0 + 2]])
                    for k in range(8):
                        S.op("pe", mm(ps[pb0 + 3][:, :], wg_s[:, k, 1024 + c * 128:1024 + (c + 1) * 128], hTb[mb][:, k, :], k == 0, k == 7),
                             r=[t_wg[2 + c // 4], t_hTb[mb]], w=[pst[pb0 + 3]])
                    S.op("act", lambda e, u=u, pb0=pb0: e.activation(out=s1b[u][:], in_=ps[pb0 + 2][:, :], func=AF.Sigmoid), r=[pst[pb0 + 2]], w=[t_s1[u]])
                    S.op("act", lambda e, u=u, pb0=pb0: e.activation(out=s2b[u][:], in_=ps[pb0 + 3][:, :], func=AF.Sigmoid), r=[pst[pb0 + 3]], w=[t_s2[u]])
                    S.op("dve", lambda e, u=u, pb0=pb0: e.tensor_tensor(out=m1b[0][:], in0=ps[pb0][:, :], in1=s1b[u][:], op=ALU.mult), r=[pst[pb0], t_s1[u]], w=[t_m1[0]])
                    S.op("dve", lambda e, u=u, pb0=pb0: e.tensor_tensor(out=m2b[0][:], in0=ps[pb0 + 1][:, :], in1=s2b[u][:], op=ALU.mult), r=[pst[pb0 + 1], t_s2[u]], w=[t_m2[0]])
                    S.op("dve", lambda e, mb=mb, c=c: e.tensor_tensor(out=mixT[mb][:, c, :], in0=m1b[0][:], in1=m2b[0][:], op=ALU.add),
                         r=[t_m1[0], t_m2[0]], wd=[t_mix[mb]])
                for ti in range(4):
                    i = b * 4 + ti
                    if i >= 1:
                        tile_S3(i - 1)
                    tile_S1(i, mb, ti)
                    tile_S2(i)
                    if i >= 1:
                        tile_S4(i - 1)
            tile_S3(NT - 1)
            tile_S4(NT - 1)
            if "x2" in dbg:
                S.op("sp", lambda e, inc: inc(e.dma_start(out=dbg_t["lg"], in_=logit[:])), r=[t_logit], dma=True)
            S.barrier()
        with ExitStack() as p5:
            ecap_s = sb("p5ecap", [128, 32], F32, p5)
            xnb = [sb(f"p5cxnb{i}", [128, D], BF16, p5) for i in range(4)]
            t_xnb = [S.tok("cxnb") for _ in range(4)]
            t_w5 = S.tok("w5b")
            S.op("sp", lambda e, inc: inc(e.dma_start(out=ecap_s[:], in_=ecap)), w=[t_w5], dma=True)
            def rb(name, shape, dt=F32):
                return sb("r_" + name, shape, dt, p5)
            t_r = S.tok("router")
            G = logit[:, :, 0:4]
            E4 = logit[:, :, 4:36].rearrange("p n (g j) -> p n g j", g=4)
            gmax = rb("gmax", [128, NT]); goh = rb("goh", [128, NT, 4]); gsh = rb("gsh", [128, NT, 4]); gsum = rb("gsum", [128, NT])
            gw = rb("gw", [128, NT]); sel = rb("sel", [128, NT, 4, 8]); eg = rb("eg", [128, NT, 8]); m1 = rb("m1", [128, NT])
            oh1 = rb("oh1", [128, NT, 8]); eg2 = rb("eg2", [128, NT, 8]); m2 = rb("m2", [128, NT]); oh2 = rb("oh2", [128, NT, 8])
            dd = rb("dd", [128, NT]); ex = rb("ex", [128, NT]); den = rb("den", [128, NT])
            A1 = rb("A1", [128, NT, 4, 8]); A2 = rb("A2", [128, NT, 4, 8]); Ab = rb("Ab", [128, NT, 32], BF16)
            rk = rb("rk", [128, NT, 32]); tmp5 = rb("tmp5", [128, NT, 32]); sl = rb("sl", [128, 2, NT])
            def R_(eng, fn):
                S.op(eng, fn, r=[t_r, t_logit], w=[t_r])
            R_("dve", lambda e: e.tensor_reduce(out=gmax[:], in_=G, axis=AX.X, op=ALU.max))
            R_("dve", lambda e: e.tensor_tensor(out=goh[:], in0=G, in1=gmax[:].unsqueeze(2).to_broadcast([128, NT, 4]), op=ALU.is_equal))
            R_("dve", lambda e: e.tensor_tensor(out=gsh[:], in0=G, in1=gmax[:].unsqueeze(2).to_broadcast([128, NT, 4]), op=ALU.subtract))
            R_("act", lambda e: e.activation(out=gsh[:], in_=gsh[:], func=AF.Exp))
            R_("dve", lambda e: e.tensor_reduce(out=gsum[:], in_=gsh[:], axis=AX.X, op=ALU.add))
            R_("dve", lambda e: e.reciprocal(out=gw[:], in_=gsum[:]))
            R_("dve", lambda e: e.tensor_tensor(out=sel[:], in0=E4, in1=goh[:].unsqueeze(3).to_broadcast([128, NT, 4, 8]), op=ALU.mult))
            R_("dve", lambda e: e.tensor_reduce(out=eg[:], in_=sel[:].rearrange("p n g j -> p n j g"), axis=AX.X, op=ALU.add))
            R_("dve", lambda e: e.tensor_reduce(out=m1[:], in_=eg[:], axis=AX.X, op=ALU.max))
            R_("dve", lambda e: e.tensor_tensor(out=oh1[:], in0=eg[:], in1=m1[:].unsqueeze(2).to_broadcast([128, NT, 8]), op=ALU.is_equal))
            R_("dve", lambda e: e.scalar_tensor_tensor(out=eg2[:], in0=oh1[:], scalar=-1e30, in1=eg[:], op0=ALU.mult, op1=ALU.add))
            R_("dve", lambda e: e.tensor_reduce(out=m2[:], in_=eg2[:], axis=AX.X, op=ALU.max))
            R_("dve", lambda e: e.tensor_tensor(out=oh2[:], in0=eg2[:], in1=m2[:].unsqueeze(2).to_broadcast([128, NT, 8]), op=ALU.is_equal))
            R_("dve", lambda e: e.tensor_sub(out=dd[:], in0=m2[:], in1=m1[:]))
            R_("act", lambda e: e.activation(out=ex[:], in_=dd[:], func=AF.Exp))
            R_("dve", lambda e: e.tensor_scalar_add(out=den[:], in0=ex[:], scalar1=1.0))
            R_("dve", lambda e: e.reciprocal(out=den[:], in_=den[:]))
            S.op("dve", lambda e: e.tensor_mul(out=wgt[:, 0, :], in0=den[:], in1=gw[:]), r=[t_r], w=[t_r, t_route])
            S.op("dve", lambda e: e.tensor_mul(out=wgt[:, 1, :], in0=wgt[:, 0, :], in1=ex[:]), r=[t_r, t_route], w=[t_r, t_route])
            R_("dve", lambda e: e.tensor_tensor(out=A1[:], in0=goh[:].unsqueeze(3).to_broadcast([128, NT, 4, 8]),
                                                in1=oh1[:].unsqueeze(2).to_broadcast([128, NT, 4, 8]), op=ALU.mult))
            R_("dve", lambda e: e.tensor_tensor(out=A2[:], in0=goh[:].unsqueeze(3).to_broadcast([128, NT, 4, 8]),
                                                in1=oh2[:].unsqueeze(2).to_broadcast([128, NT, 4, 8]), op=ALU.mult))
            R_("dve", lambda e: e.tensor_tensor(out=Ab[:], in0=A1[:].rearrange("p n g j -> p n (g j)"), in1=A2[:].rearrange("p n g j -> p n (g j)"), op=ALU.add))
            for i in range(NT):
                bank = i // 16
                oc = slice((i % 16) * 32, (i % 16) * 32 + 32)
                S.op("pe", mm(ps[bank][:, oc], LSTR, Ab[:, i, :], True, i == 0), r=[t_r], w=[pst[bank]])
                for i2 in range(i):
                    S.op("pe", mm(ps[bank][:, oc], ONESB, Ab[:, i2, :], False, i2 == i - 1), r=[t_r], w=[pst[bank]])
            nb_ = (NT + 15) // 16
            for bank in range(nb_):
                n0 = bank * 16
                n1 = min(NT, n0 + 16)
                S.op("dve", lambda e, bank=bank, n0=n0, n1=n1: e.tensor_tensor(
                    out=rk[:, n0:n1, :], in0=ps[bank][:, 0:(n1 - n0) * 32].rearrange("p (n e) -> p n e", e=32),
                    in1=ecap_s[:].unsqueeze(1).to_broadcast([128, n1 - n0, 32]), op=ALU.add), r=[pst[bank], t_r, t_w5], w=[t_r])
            for a_, Aa in ((0, A1), (1, A2)):
                R_("dve", lambda e, Aa=Aa: e.tensor_tensor(out=tmp5[:], in0=rk[:], in1=Aa[:].rearrange("p n g j -> p n (g j)"), op=ALU.mult))
                R_("dve", lambda e, a_=a_: e.tensor_reduce(out=sl[:, a_, :], in_=tmp5[:], axis=AX.X, op=ALU.add))
            S.op("dve", lambda e: e.tensor_copy(out=slot_i[:], in_=sl[:]), r=[t_r], w=[t_route])
            S.barrier()
            if "rt" in dbg:
                S.op("sp", lambda e, inc: (inc(e.dma_start(out=dbg_t["rt"][:, 0:2, :], in_=sl[:])), inc(e.dma_start(out=dbg_t["rt"][:, 2:4, :], in_=wgt[:]))),
                     r=[t_r, t_route], dma=True)
                S.barrier()
            for i in range(NT):
                u = i % 4
                S.op("sp", lambda e, inc, i=i, u=u: inc(e.dma_start(out=xnb[u][:], in_=xn_d[i * 128:(i + 1) * 128, :])), r=[t_xnd[i]], w=[t_xnb[u]], dma=True)
                for a_ in range(2):
                    S.op("pool", lambda e, inc, i=i, u=u, a_=a_: inc(e.indirect_dma_start(
                        out=xg_d[:, :], out_offset=bass.IndirectOffsetOnAxis(ap=slot_i[:, a_, i:i + 1], axis=0),
                        in_=xnb[u][:], in_offset=None, bounds_check=BCREG, oob_is_err=False)),
                        r=[t_xnb[u], t_route], wd=[t_xg], dma=True)
            S.barrier()

        with ExitStack() as p6:
            stg6 = [sb(f"p6stg{i}", [128, 8, 512], F32, p6) for i in range(3)]
            t_stg6 = [S.tok("stg6") for _ in range(3)]
            w1b = [sb(f"p6w1{i}", [128, 8, 512], BF16, p6) for i in range(2)]
            w3b = [sb(f"p6w3{i}", [128, 8, 512], BF16, p6) for i in range(2)]
            w2b = [sb(f"p6w2{i}", [128, 4, D], BF16, p6) for i in range(2)]
            t_w1 = [S.tok("w1") for _ in range(2)]
            t_w3 = [S.tok("w3") for _ in range(2)]
            t_w2 = [S.tok("w2") for _ in range(2)]
            xg_s = [sb(f"p6xg{i}", [128, CT, D], BF16, p6) for i in range(2)]
            t_xgs = [S.tok("xgs") for _ in range(2)]
            xgT = [sb(f"p6xgT{i}", [128, 8, CAP], BF16, p6) for i in range(2)]
            t_xgT = [S.tok("xgT") for _ in range(2)]
            sil = [sb(f"p6sil{i}", [128, CAP], F32, p6) for i in range(2)]
            t_sil = [S.tok("sil") for _ in range(2)]
            hid = [sb(f"p6hid{i}", [128, 4, CAP], BF16, p6) for i in range(2)]
            t_hid = [S.tok("hid") for _ in range(2)]
            ysb = [sb(f"p6y{i}", [128, D], F32, p6) for i in range(2)]
            t_ysb = [S.tok("ysb") for _ in range(2)]
            w1_v = w1.rearrange("e (p k) c -> e p k c", k=8)
            w3_v = w3.rearrange("e (p k) c -> e p k c", k=8)
            w2_v = w2.rearrange("e (p k) c -> e p k c", k=4)
            stg2v = stg6[2][:].rearrange("p k c -> p (k c)").rearrange("p (k c) -> p k c", k=4)

            def load_expert(ex_):
                u = ex_ % 2
                S.op("sp", lambda e, inc: inc(e.dma_start(out=stg6[0][:], in_=w1_v[ex_])), w=[t_stg6[0]], dma=True)
                S.op("sp", lambda e, inc: inc(e.dma_start(out=stg6[1][:], in_=w3_v[ex_])), w=[t_stg6[1]], dma=True)
                S.op("sp", lambda e, inc: inc(e.dma_start(out=stg2v, in_=w2_v[ex_])), w=[t_stg6[2]], dma=True)
                S.op("sp", lambda e, inc: inc(e.dma_start(out=xg_s[u][:], in_=xg_d[ex_ * CAP:(ex_ + 1) * CAP, :].rearrange("(c p) d -> p c d", p=128))),
                     r=[t_xg], w=[t_xgs[u]], dma=True)

            def cast_expert(ex_):
                u = ex_ % 2
                S.op("dve", lambda e: e.tensor_copy(out=w1b[u][:].rearrange("p k (kk m) -> p k kk m", kk=4),
                                                    in_=stg6[0][:].rearrange("p k (m kk) -> p k kk m", kk=4)), r=[t_stg6[0]], w=[t_w1[u]])
                S.op("act", lambda e: e.activation(out=w3b[u][:].rearrange("p k (kk m) -> p k kk m", kk=4),
                                                   in_=stg6[1][:].rearrange("p k (m kk) -> p k kk m", kk=4), func=AF.Copy), r=[t_stg6[1]], w=[t_w3[u]])
                S.op("dve", lambda e: e.tensor_copy(out=w2b[u][:, 0:2, :], in_=stg2v[:, 0:2, :]), r=[t_stg6[2]], wd=[t_w2[u]])
                S.op("act", lambda e: e.activation(out=w2b[u][:, 2:4, :], in_=stg2v[:, 2:4, :], func=AF.Copy), r=[t_stg6[2]], wd=[t_w2[u]])

            load_expert(0)
            cast_expert(0)
            yc = 0
            for ex_ in range(NE):
                u = ex_ % 2
                if ex_ + 1 < NE:
                    load_expert(ex_ + 1)
                for c in range(CT):
                    for k in range(8):
                        bank = 6 + (k // 4) % 2
                        S.op("pe", lambda e, u=u, c=c, k=k, bank=bank: e.transpose(out=psb(bank)[:, (k % 4) * 128:(k % 4 + 1) * 128],
                                                                                   in_=xg_s[u][:, c, k * 128:(k + 1) * 128], identity=IDB),
                             r=[t_xgs[u]], w=[pst[bank]])
                        if k % 4 == 3:
                            kb = k - 3
                            if (k // 4) % 2 == 0:
                                S.op("dve", lambda e, u=u, c=c, kb=kb, bank=bank: e.tensor_copy(
                                    out=xgT[u][:, kb:kb + 4, c * 128:(c + 1) * 128], in_=psb(bank)[:, 0:512].rearrange("p (k t) -> p k t", k=4)),
                                    r=[pst[bank]], wd=[t_xgT[u]])
                            else:
                                S.op("act", lambda e, u=u, c=c, kb=kb, bank=bank: e.activation(
                                    out=xgT[u][:, kb:kb + 4, c * 128:(c + 1) * 128], in_=psb(bank)[:, 0:512].rearrange("p (k t) -> p k t", k=4), func=AF.Copy),
                                    r=[pst[bank]], wd=[t_xgT[u]])
                for fc in range(4):
                    pb0 = 2 * (fc % 2)
                    fs = slice(fc * 128, (fc + 1) * 128)
                    for k in range(8):
                        S.op("pe", mm(ps[pb0][:, 0:CAP], w1b[u][:, k, fs], xgT[u][:, k, :], k == 0, k == 7), r=[t_w1[u], t_xgT[u]], w=[pst[pb0]])
                    for k in range(8):
                        S.op("pe", mm(ps[pb0 + 1][:, 0:CAP], w3b[u][:, k, fs], xgT[u][:, k, :], k == 0, k == 7), r=[t_w3[u], t_xgT[u]], w=[pst[pb0 + 1]])
                    v_ = fc % 2
                    S.op("act", lambda e, v_=v_, pb0=pb0: e.activation(out=sil[v_][:], in_=ps[pb0][:, 0:CAP], func=AF.Silu), r=[pst[pb0]], w=[t_sil[v_]])
                    S.op("dve", lambda e, v_=v_, pb0=pb0, u=u, fc=fc: e.tensor_tensor(out=hid[u][:, fc, :], in0=ps[pb0 + 1][:, 0:CAP], in1=sil[v_][:], op=ALU.mult),
                         r=[pst[pb0 + 1], t_sil[v_]], wd=[t_hid[u]])
                for c in range(CT):
                    yu = yc % 2
                    yc += 1
                    for n in range(2):
                        bank = 4 + n
                        for k in range(4):
                            S.op("pe", mm(ps[bank][:, :], hid[u][:, k, c * 128:(c + 1) * 128], w2b[u][:, k, n * 512:(n + 1) * 512], k == 0, k == 3),
                                 r=[t_hid[u], t_w2[u]], w=[pst[bank]])
                        if n == 0:
                            S.op("act", lambda e, yu=yu, bank=bank: e.activation(out=ysb[yu][:, 0:512], in_=ps[bank][:, :], func=AF.Copy), r=[pst[bank]], wd=[t_ysb[yu]])
                        else:
                            S.op("dve", lambda e, yu=yu, bank=bank: e.tensor_copy(out=ysb[yu][:, 512:1024], in_=ps[bank][:, :]), r=[pst[bank]], wd=[t_ysb[yu]])
                    r0_ = ex_ * CAP + c * 128
                    S.op("sp", lambda e, inc, yu=yu, r0_=r0_: inc(e.dma_start(out=y_d[r0_:r0_ + 128, :], in_=ysb[yu][:])), r=[t_ysb[yu]], wd=[t_yd], dma=True)
                if ex_ + 1 < NE:
                    cast_expert(ex_ + 1)
            S.barrier()

        with ExitStack() as p7:
            NB7 = 4
            x2s = [sb(f"p7x{i}", [128, D], F32, p7) for i in range(NB7)]
            ya = [sb(f"p7ya{i}", [128, D], F32, p7) for i in range(NB7)]
            yb = [sb(f"p7yb{i}", [128, D], F32, p7) for i in range(NB7)]
            t_x2s = [S.tok("x2s") for _ in range(NB7)]
            t_ya = [S.tok("ya") for _ in range(NB7)]
            t_yb = [S.tok("yb") for _ in range(NB7)]
            for i in range(NT):
                u = i % NB7
                S.op("sp", lambda e, inc, i=i, u=u: inc(e.dma_start(out=x2s[u][:], in_=x2_d[i * 128:(i + 1) * 128, :])), r=[t_x2d[i]], w=[t_x2s[u]], dma=True)
                for a_, (yy, t_yy) in enumerate(((ya, t_ya), (yb, t_yb))):
                    S.op("pool", lambda e, inc, i=i, u=u, a_=a_, yy=yy: inc(e.indirect_dma_start(
                        out=yy[u][:], out_offset=None, in_=y_d[:, :],
                        in_offset=bass.IndirectOffsetOnAxis(ap=slot_i[:, a_, i:i + 1], axis=0), bounds_check=BCREG, oob_is_err=False)),
                        r=[t_yd, t_route], w=[t_yy[u]], dma=True)
                S.op("dve", lambda e, i=i, u=u: e.scalar_tensor_tensor(out=x2s[u][:], in0=ya[u][:], scalar=wgt[:, 0, i:i + 1], in1=x2s[u][:], op0=ALU.mult, op1=ALU.add),
                     r=[t_ya[u], t_x2s[u], t_route], w=[t_x2s[u]])
                S.op("dve", lambda e, i=i, u=u: e.scalar_tensor_tensor(out=x2s[u][:], in0=yb[u][:], scalar=wgt[:, 1, i:i + 1], in1=x2s[u][:], op0=ALU.mult, op1=ALU.add),
                     r=[t_yb[u], t_x2s[u], t_route], w=[t_x2s[u]])
                S.op("sp", lambda e, inc, i=i, u=u: inc(e.dma_start(out=out[i * 128:(i + 1) * 128, :], in_=x2s[u][:])), r=[t_x2s[u]], dma=True)
            S.barrier()
        S.barrier()
    return nc


def host_consts(T):
    CAP = cap_for(T)
    p = np.arange(128)
    cm = np.zeros((128, 7, 128), np.float32)
    cm[:, 0, :] = np.eye(128, dtype=np.float32)
    cm[:, 1, :] = ((p[:, None] // 64 == p[None, :] // 64) & (p[:, None] <= p[None, :])).astype(np.float32)
    cm[:, 2, :] = (p[:, None] <= p[None, :]).astype(np.float32)
    cm[:, 3, :] = (p[:, None] < p[None, :]).astype(np.float32)
    cm[:, 4, :] = 1.0
    cm[:, 5, :] = (p[:, None] // 64 == p[None, :] // 64).astype(np.float32)
    m = p
    src = np.where((m % 64) < 32, m + 32, m - 32)
    perm = np.zeros((128, 128), np.float32)
    perm[src, m] = 1.0
    cm[:, 6, :] = perm
    rmask = np.ones((128, 512), np.float32)
    rmask[:, ::64] = 0.0
    ecap = np.broadcast_to((np.arange(32, dtype=np.float32) * CAP)[None, :], (128, 32)).copy()
    half = 32
    inv = (np.float32(10000.0) ** (-np.arange(half, dtype=np.float32) / np.float32(half))).astype(np.float32)
    ang = np.arange(T, dtype=np.float32)[:, None] * inv[None, :]
    cos = np.cos(ang).astype(np.float32).T
    sin = np.sin(ang).astype(np.float32).T
    ropec = np.concatenate([cos, cos, cos, cos], axis=0)
    ropes = np.concatenate([-sin, sin, -sin, sin], axis=0)
    return dict(cmat=cm, rmask=rmask, ecap=ecap, ropec=np.ascontiguousarray(ropec), ropes=np.ascontiguousarray(ropes))


def host_layout(inp, T):
    f = lambda a: np.ascontiguousarray(np.asarray(a, dtype=np.float32))
    d = {}
    d["w_in"] = f(inp["w_in"][0])
    d["gmix"] = f(np.asarray(inp["norm_mix"][0]).reshape(8, 128).T)
    d["hglb"] = f(np.asarray(inp["hg_lb"]).reshape(2, 4, 128).transpose(2, 0, 1).reshape(128, 8))
    d["hgon"] = f(np.asarray(inp["hg_out_norm"][0]).reshape(128, 1))
    d["qkn"] = f(np.stack([np.tile(np.asarray(inp["da_q_norm"][0]), 2), np.tile(np.asarray(inp["da_k_norm"][0]), 2)], axis=1))
    d["lamb"] = f(np.broadcast_to(np.asarray(inp["da_lambda"][0]).reshape(1, 256), (128, 256)))
    d["daon"] = f(np.asarray(inp["da_out_norm"][0]).reshape(128, 1))
    d["wbh"] = f(inp["w_branch_hg"][0])
    d["wbd"] = f(inp["w_branch_da"][0])
    d["w_out"] = f(inp["w_out"][0])
    d["nmoe"] = f(np.broadcast_to(np.asarray(inp["norm_moe"][0]).reshape(1, D), (128, D)))
    d["wr"] = f(np.concatenate([np.asarray(inp["w_router_group"][0]), np.asarray(inp["w_router_expert"][0])], axis=1))
    d["br"] = f(np.broadcast_to(np.concatenate([np.asarray(inp["b_router_group"][0]),
                                                np.asarray(inp["b_router_expert"][0])]).reshape(1, 36), (128, 36)))
    d["w1"] = f(inp["w1"][0])
    d["w3"] = f(inp["w3"][0])
    d["w2"] = f(inp["w2"][0])
    d.update(host_consts(T))
    return d


_NC_CACHE = {}


def kernel(**inputs):
    xfull = np.asarray(inputs["x"], dtype=np.float32)
    B, T, _ = xfull.shape
    shared = host_layout(inputs, T)
    if T not in _NC_CACHE:
        _NC_CACHE[T] = build(T)
    nc = _NC_CACHE[T]
    in_maps = []
    for c in range(B):
        m = dict(shared)
        m["x"] = np.ascontiguousarray(xfull[c])
        in_maps.append(m)
    res = run_bass_kernel_spmd(nc, in_maps, core_ids=list(range(B)))
    return np.stack([np.asarray(r["out"], dtype=np.float32) for r in res.results], axis=0)
```
